# Optimizing a Trainium2 kernel written in Bass

```python
import math
import jax
import jax.numpy as jnp
from jax import lax
import numpy as np

D_MODEL = 1024
BATCH = 32
SEQ = 2048
DEPTH = 2

CTX_LEN = 256
GRID_W = 64
NORM_EPS = 1e-6

RW_HEADS = 8
RW_HEAD = 64
RW_DIM = 512
RW_DECAY_LORA = 64
RW_ICL_LORA = 64
RW_GATE_LORA = 128
RW_GN_EPS = 64e-5
RW_SPLITS = (512, 512, 512, 64, 64, 64, 64, 128)
RW_COLS = 1920

SSM_HEADS = 16
SSM_HEAD = 64
SSM_DIM = 1024
SSM_GROUPS = 2
SSM_STATE = 128
SSM_CONV = 5
SSM_CHUNK = 64
XBC_DIM = 1536
EVEN_COLS = 4512
MIX_DIM = 1536

MLA_HEADS = 16
MLA_Q_LORA = 384
MLA_KV_LORA = 256
MLA_NOPE = 64
MLA_ROPE = 32
MLA_V = 64
ODD_COLS = 672
ROPE_THETA = 10000.0
Q_BLOCK = 128

N_EXPERTS = 64
TOP_K = 6
N_EXPERT_GROUPS = 8
TOPK_GROUPS = 4
EXPERT_FF = 256
SHARED_FF = 256
ROUTED_SCALE = 2.5

kernel_name = "hybrid_rwkv7_mamba2_mla_moe_prefix_dit"


def split_sizes(u, sizes):
    return jnp.split(u, np.cumsum(sizes)[:-1].tolist(), axis=-1)


def rms_normalize(u, eps=NORM_EPS):
    u32 = u.astype(jnp.float32)
    return (u32 * lax.rsqrt(jnp.mean(u32 * u32, axis=-1, keepdims=True) + eps)).astype(u.dtype)


def rmsnorm(u, g):
    return rms_normalize(u) * g


def modulate(h, shift, scale):
    return h * (1 + scale) + shift


def axial_rope_tables(n_tokens, dtype):
    rows = n_tokens // GRID_W
    r_idx, c_idx = jnp.meshgrid(jnp.arange(rows), jnp.arange(GRID_W), indexing='ij')
    r_idx = r_idx.reshape(-1).astype(jnp.float32)
    c_idx = c_idx.reshape(-1).astype(jnp.float32)
    axis_dim = MLA_ROPE // 2
    inv_freq = ROPE_THETA ** (-jnp.arange(0, axis_dim, 2, dtype=jnp.float32) / axis_dim)
    ang = jnp.concatenate([r_idx[:, None] * inv_freq, c_idx[:, None] * inv_freq], axis=-1)
    return jnp.cos(ang).astype(dtype), jnp.sin(ang).astype(dtype)


def apply_rope(u, cos, sin):
    u1, u2 = jnp.split(u, 2, axis=-1)
    return jnp.concatenate([u1 * cos - u2 * sin, u1 * sin + u2 * cos], axis=-1)


def bidir_shift_mix(p, mu):
    zero = jnp.zeros_like(p[:, :1])
    prev = jnp.concatenate([zero, p[:, :-1]], axis=1)
    nxt = jnp.concatenate([p[:, 1:], zero], axis=1)
    return p + mu * (0.5 * (prev + nxt) - p)


def depthwise_conv_centred(u, w, b):
    k = w.shape[0]
    out = lax.conv_general_dilated(u, w[:, None, :], window_strides=(1,), padding=[(k // 2, k // 2)],
                                   dimension_numbers=('NWC', 'WIO', 'NWC'), feature_group_count=u.shape[-1])
    return out + b


def rwkv7_scan(r, w, k, v, kk, b, s0, reverse):
    def step(s, inp):
        r_t, w_t, k_t, v_t, kk_t, b_t = inp
        sa = jnp.einsum('bhvk,bhk->bhv', s, kk_t)
        s = s * w_t[:, :, None, :] - sa[..., None] * b_t[:, :, None, :] + v_t[..., None] * k_t[:, :, None, :]
        return s, jnp.einsum('bhvk,bhk->bhv', s, r_t)
    xs = tuple(jnp.moveaxis(u, 1, 0) for u in (r, w, k, v, kk, b))
    s_fin, ys = lax.scan(step, s0, xs, reverse=reverse)
    return jnp.moveaxis(ys, 0, 1), s_fin


def rwkv7_branch(cols, s_fwd, s_bwd, mu, w0, w2, a0, a2, g2, k_k, k_a, r_k, gn_w, gn_b):
    bsz, t = cols.shape[:2]

    def hd(u):
        return u.reshape(*u.shape[:-1], RW_HEADS, RW_HEAD)

    cols = bidir_shift_mix(cols, mu)
    r, k, v, xw_f, xw_b, xa_f, xa_b, xg = split_sizes(cols, RW_SPLITS)
    kk = hd(k * k_k).astype(jnp.float32)
    kk = (kk * lax.rsqrt(jnp.sum(kk * kk, axis=-1, keepdims=True) + 1e-12)).astype(k.dtype)
    ys, ks, finals = [], [], []
    for d, (xw, xa, s0, rev) in enumerate(((xw_f, xa_f, s_fwd, False), (xw_b, xa_b, s_bwd, True))):
        w_log = -jax.nn.softplus(-(w0[d] + jnp.tanh(xw) @ w2[d])) - 0.5
        decay = jnp.exp(-jnp.exp(w_log))
        a = jax.nn.sigmoid(a0[d] + xa @ a2[d])
        k_d = k * (1 + (a - 1) * k_a)
        y, s_fin = rwkv7_scan(hd(r), hd(decay), hd(k_d), hd(v), kk, kk * hd(a), s0, rev)
        ys.append(y)
        ks.append(k_d)
        finals.append(s_fin)
    y = ys[0] + ys[1]
    mean = jnp.mean(y, axis=-1, keepdims=True)
    var = jnp.mean(jnp.square(y - mean), axis=-1, keepdims=True)
    y = ((y - mean) * lax.rsqrt(var + RW_GN_EPS)).astype(r.dtype) * hd(gn_w) + hd(gn_b)
    bonus = jnp.sum(hd(r) * hd(ks[0] + ks[1]) * hd(r_k), axis=-1, keepdims=True) * hd(v)
    g = jax.nn.sigmoid(xg) @ g2
    out = (y + bonus).reshape(bsz, t, RW_DIM) * g
    return out, finals[0], finals[1]


def ssd_scan(xs, dt, a_neg, bm, cm, s0):
    b, t, h, p = xs.shape
    g, n = bm.shape[2], bm.shape[3]
    j = h // g
    nc = t // SSM_CHUNK
    cl = SSM_CHUNK
    da = (dt.astype(jnp.float32) * a_neg.astype(jnp.float32)).reshape(b, nc, cl, h)
    cs = jnp.cumsum(da, axis=2)
    xdt = (xs * dt[..., None]).reshape(b, nc, cl, g, j, p)
    bc = bm.reshape(b, nc, cl, g, n)
    cc = cm.reshape(b, nc, cl, g, n)
    causal = jnp.tril(jnp.ones((cl, cl), bool))
    seg = cs[:, :, :, None, :] - cs[:, :, None, :, :]
    lmat = jnp.exp(jnp.where(causal[:, :, None], seg, -jnp.inf)).reshape(b, nc, cl, cl, g, j).astype(xs.dtype)
    cb = jnp.einsum('bclgn,bcsgn->bclsg', cc, bc)
    y_diag = jnp.einsum('bclsg,bclsgj,bcsgjp->bclgjp', cb, lmat, xdt)
    decay_to_end = jnp.exp(cs[:, :, -1:, :] - cs).reshape(b, nc, cl, g, j).astype(xs.dtype)
    states = jnp.einsum('bclgn,bclgj,bclgjp->bcgjpn', bc, decay_to_end, xdt).reshape(b, nc, h, p, n)
    chunk_decay = jnp.exp(cs[:, :, -1, :])

    def step(s, inp):
        dec, st = inp
        return s * dec[:, :, None, None] + st, s

    s_fin, s_in = lax.scan(step, s0, (jnp.moveaxis(chunk_decay, 1, 0),
                                      jnp.moveaxis(states.astype(jnp.float32), 1, 0)))
    s_in = jnp.moveaxis(s_in, 0, 1).reshape(b, nc, g, j, p, n).astype(xs.dtype)
    decay_from_start = jnp.exp(cs).reshape(b, nc, cl, g, j).astype(xs.dtype)
    y_off = jnp.einsum('bclgn,bcgjpn,bclgj->bclgjp', cc, s_in, decay_from_start)
    return (y_diag + y_off).reshape(b, t, h, p), s_fin


def mamba2_branch(z, xbc, dt_raw, s_fwd, s_bwd, conv_w, conv_b, dt_bias, a_log, d_skip, norm_w):
    bsz, t = z.shape[:2]
    xbc = jax.nn.silu(depthwise_conv_centred(xbc, conv_w, conv_b))
    xs, bm, cm = split_sizes(xbc, (SSM_DIM, SSM_GROUPS * SSM_STATE, SSM_GROUPS * SSM_STATE))
    xs = xs.reshape(bsz, t, SSM_HEADS, SSM_HEAD)
    bm = bm.reshape(bsz, t, SSM_GROUPS, SSM_STATE)
    cm = cm.reshape(bsz, t, SSM_GROUPS, SSM_STATE)
    dt = jax.nn.softplus(dt_raw.reshape(bsz, t, 2, SSM_HEADS) + dt_bias)
    a_neg = -jnp.exp(a_log)
    y_f, s_f = ssd_scan(xs, dt[:, :, 0], a_neg[0], bm, cm, s_fwd)
    flip = lambda u: jnp.flip(u, axis=1)
    y_b, s_b = ssd_scan(flip(xs), flip(dt[:, :, 1]), a_neg[1], flip(bm), flip(cm), s_bwd)
    y = y_f + flip(y_b) + xs * d_skip[:, None]
    y = (y.reshape(bsz, t, SSM_DIM) * jax.nn.silu(z)).reshape(bsz, t, SSM_GROUPS, SSM_DIM // SSM_GROUPS)
    y = rms_normalize(y).reshape(bsz, t, SSM_DIM) * norm_w
    return y, s_f, s_b


def even_mixer(h_lat, h_ctx, w_in, w_out, rw_mu, rw_w0, rw_w2, rw_a0, rw_a2, rw_g2, rw_kk, rw_ka, rw_rk,
               rw_gn_w, rw_gn_b, conv_w, conv_b, dt_bias, a_log, d_skip, ssm_norm_w):
    bsz = h_lat.shape[0]
    rw_f = rw_b = jnp.zeros((bsz, RW_HEADS, RW_HEAD, RW_HEAD), jnp.float32)
    ss_f = ss_b = jnp.zeros((bsz, SSM_HEADS, SSM_HEAD, SSM_STATE), jnp.float32)
    outs = []
    for h in (h_ctx, h_lat):
        rw_cols, z, xbc, dt_raw = split_sizes(h @ w_in, (RW_COLS, SSM_DIM, XBC_DIM, 2 * SSM_HEADS))
        y_rw, rw_f, rw_b = rwkv7_branch(rw_cols, rw_f, rw_b, rw_mu, rw_w0, rw_w2, rw_a0, rw_a2, rw_g2,
                                        rw_kk, rw_ka, rw_rk, rw_gn_w, rw_gn_b)
        y_ss, ss_f, ss_b = mamba2_branch(z, xbc, dt_raw, ss_f, ss_b, conv_w, conv_b, dt_bias, a_log,
                                         d_skip, ssm_norm_w)
        outs.append(jnp.concatenate([y_rw, y_ss], axis=-1) @ w_out)
    return outs[1], outs[0]


def block_attention(qn, qr, kn, kr, v, scale):
    b, t, h, _ = qn.shape
    nb = t // Q_BLOCK

    def to_blocks(u):
        return jnp.moveaxis(u.reshape(b, nb, Q_BLOCK, *u.shape[2:]), 1, 0)

    def one_block(q_pair):
        qn_i, qr_i = q_pair
        s = jnp.einsum('bqhd,bkhd->bhqk', qn_i, kn) + jnp.einsum('bqhr,bkr->bhqk', qr_i, kr)
        pr = jax.nn.softmax(s.astype(jnp.float32) * scale, axis=-1).astype(v.dtype)
        return jnp.einsum('bhqk,bkhd->bqhd', pr, v)

    out = lax.map(one_block, (to_blocks(qn), to_blocks(qr)))
    return jnp.moveaxis(out, 0, 1).reshape(b, t, h, v.shape[-1])


def mla_mixer(h_lat, h_ctx, w_in, q_norm, q_up, kv_norm, kv_up, w_out, cos, sin, need_ctx):
    scale = (MLA_NOPE + MLA_ROPE) ** -0.5

    def split_heads(u, width):
        return u.reshape(u.shape[0], u.shape[1], MLA_HEADS, width)

    def queries(cq):
        q = split_heads(rmsnorm(cq, q_norm) @ q_up, MLA_NOPE + MLA_ROPE)
        return q[..., :MLA_NOPE], q[..., MLA_NOPE:]

    def keys_values(ckv):
        kv = split_heads(rmsnorm(ckv, kv_norm) @ kv_up, MLA_NOPE + MLA_V)
        return kv[..., :MLA_NOPE], kv[..., MLA_NOPE:]

    bsz, t = h_lat.shape[:2]
    cq_l, ckv_l, kr_l = split_sizes(h_lat @ w_in, (MLA_Q_LORA, MLA_KV_LORA, MLA_ROPE))
    cq_c, ckv_c, kr_c = split_sizes(h_ctx @ w_in, (MLA_Q_LORA, MLA_KV_LORA, MLA_ROPE))
    qn_l, qr_l = queries(cq_l)
    qr_l = apply_rope(qr_l, cos[:, None, :], sin[:, None, :])
    kr_l = apply_rope(kr_l, cos, sin)
    kn_l, v_l = keys_values(ckv_l)
    kn_c, v_c = keys_values(ckv_c)
    kn = jnp.concatenate([kn_c, kn_l], axis=1)
    kr = jnp.concatenate([kr_c, kr_l], axis=1)
    v = jnp.concatenate([v_c, v_l], axis=1)
    out_l = block_attention(qn_l, qr_l, kn, kr, v, scale).reshape(bsz, t, MLA_HEADS * MLA_V) @ w_out
    out_c = None
    if need_ctx:
        qn_c, qr_c = queries(cq_c)
        out_c = block_attention(qn_c, qr_c, kn_c, kr_c, v_c, scale).reshape(
            bsz, h_ctx.shape[1], MLA_HEADS * MLA_V) @ w_out
    return out_l, out_c


def moe_ffn(tok, router_w, router_bias, w1, w3, w2, sw1, sw3, sw2):
    logits = (tok @ router_w).astype(jnp.float32)
    scores = jax.nn.sigmoid(logits)
    sel = scores + router_bias
    grp = sel.reshape(-1, N_EXPERT_GROUPS, N_EXPERTS // N_EXPERT_GROUPS)
    grp_score = jnp.sum(lax.top_k(grp, 2)[0], axis=-1)
    _, gidx = lax.top_k(grp_score, TOPK_GROUPS)
    gmask = jnp.sum(jax.nn.one_hot(gidx, N_EXPERT_GROUPS, dtype=jnp.float32), axis=-2)
    emask = jnp.repeat(gmask, N_EXPERTS // N_EXPERT_GROUPS, axis=-1)
    sel = jnp.where(emask > 0, sel, -jnp.inf)
    _, eidx = lax.top_k(sel, TOP_K)
    wts = jnp.take_along_axis(scores, eidx, axis=-1)
    wts = wts / jnp.sum(wts, axis=-1, keepdims=True) * ROUTED_SCALE
    gates = jnp.sum(jax.nn.one_hot(eidx, N_EXPERTS, dtype=jnp.float32) * wts[..., None], axis=-2).astype(tok.dtype)

    def expert_step(acc, ew):
        w1e, w3e, w2e, ge = ew
        hid = jax.nn.silu(tok @ w1e) * (tok @ w3e)
        return acc + ge[:, None] * (hid @ w2e), None

    routed, _ = lax.scan(expert_step, jnp.zeros_like(tok), (w1, w3, w2, gates.T))
    shared = (jax.nn.silu(tok @ sw1) * (tok @ sw3)) @ sw2
    return routed + shared


def setup_inputs(seed: int = 0) -> dict:
    key = jax.random.key(seed)
    keys = iter(jax.random.split(key, 64))
    f32 = jnp.float32
    d = D_MODEL
    ne, no = (DEPTH + 1) // 2, DEPTH // 2

    def nrm(shape, scale):
        return jax.random.normal(next(keys), shape, f32) * scale

    def gain(shape):
        return 1.0 + nrm(shape, 0.02)

    def unif(shape, lo, hi):
        return jax.random.uniform(next(keys), shape, f32, lo, hi)

    dt0 = jnp.exp(unif((ne, 2, SSM_HEADS), math.log(1e-3), math.log(1e-1)))
    return {
        "x": nrm((BATCH, SEQ, d), 1.0),
        "c": nrm((BATCH, d), 1.0),
        "ctx": nrm((BATCH, CTX_LEN, d), 1.0),
        "c_ctx": nrm((d,), 1.0),
        "ada_w": nrm((DEPTH, d, 6 * d), 0.5 * d ** -0.5),
        "ada_b": nrm((DEPTH, 6 * d), 0.01),
        "norm_mix": gain((DEPTH, d)),
        "norm_ffn": gain((DEPTH, d)),
        "ev_w_in": nrm((ne, d, EVEN_COLS), d ** -0.5),
        "ev_w_out": nrm((ne, MIX_DIM, d), MIX_DIM ** -0.5),
        "rw_mu": unif((ne, RW_COLS), 0.0, 1.0),
        "rw_w0": unif((ne, 2, RW_DIM), -6.0, -1.0),
        "rw_w2": nrm((ne, 2, RW_DECAY_LORA, RW_DIM), 0.5 * RW_DECAY_LORA ** -0.5),
        "rw_a0": nrm((ne, 2, RW_DIM), 0.1),
        "rw_a2": nrm((ne, 2, RW_ICL_LORA, RW_DIM), RW_ICL_LORA ** -0.5),
        "rw_g2": nrm((ne, RW_GATE_LORA, RW_DIM), RW_GATE_LORA ** -0.5),
        "rw_kk": 0.85 + nrm((ne, RW_DIM), 0.02),
        "rw_ka": 1.0 + nrm((ne, RW_DIM), 0.02),
        "rw_rk": nrm((ne, RW_DIM), 0.1),
        "rw_gn_w": gain((ne, RW_DIM)),
        "rw_gn_b": nrm((ne, RW_DIM), 0.01),
        "ssm_conv_w": nrm((ne, SSM_CONV, XBC_DIM), SSM_CONV ** -0.5),
        "ssm_conv_b": nrm((ne, XBC_DIM), 0.01),
        "ssm_dt_bias": dt0 + jnp.log(-jnp.expm1(-dt0)),
        "ssm_a_log": jnp.log(unif((ne, 2, SSM_HEADS), 1.0, 16.0)),
        "ssm_d": gain((ne, SSM_HEADS)),
        "ssm_norm_w": gain((ne, SSM_DIM)),
        "mla_w_in": nrm((no, d, ODD_COLS), d ** -0.5),
        "mla_q_norm": gain((no, MLA_Q_LORA)),
        "mla_q_up": nrm((no, MLA_Q_LORA, MLA_HEADS * (MLA_NOPE + MLA_ROPE)), MLA_Q_LORA ** -0.5),
        "mla_kv_norm": gain((no, MLA_KV_LORA)),
        "mla_kv_up": nrm((no, MLA_KV_LORA, MLA_HEADS * (MLA_NOPE + MLA_V)), MLA_KV_LORA ** -0.5),
        "mla_w_out": nrm((no, MLA_HEADS * MLA_V, d), (MLA_HEADS * MLA_V) ** -0.5),
        "router_w": nrm((DEPTH, d, N_EXPERTS), d ** -0.5),
        "router_bias": nrm((DEPTH, N_EXPERTS), 0.01),
        "exp_w1": nrm((DEPTH, N_EXPERTS, d, EXPERT_FF), d ** -0.5),
        "exp_w3": nrm((DEPTH, N_EXPERTS, d, EXPERT_FF), d ** -0.5),
        "exp_w2": nrm((DEPTH, N_EXPERTS, EXPERT_FF, d), 0.5 * EXPERT_FF ** -0.5),
        "sh_w1": nrm((DEPTH, d, SHARED_FF), d ** -0.5),
        "sh_w3": nrm((DEPTH, d, SHARED_FF), d ** -0.5),
        "sh_w2": nrm((DEPTH, SHARED_FF, d), 0.5 * SHARED_FF ** -0.5),
        "final_norm": gain((d,)),
    }


def reference(x, c, ctx, c_ctx, ada_w, ada_b, norm_mix, norm_ffn,
              ev_w_in, ev_w_out, rw_mu, rw_w0, rw_w2, rw_a0, rw_a2, rw_g2, rw_kk, rw_ka, rw_rk,
              rw_gn_w, rw_gn_b, ssm_conv_w, ssm_conv_b, ssm_dt_bias, ssm_a_log, ssm_d, ssm_norm_w,
              mla_w_in, mla_q_norm, mla_q_up, mla_kv_norm, mla_kv_up, mla_w_out,
              router_w, router_bias, exp_w1, exp_w3, exp_w2, sh_w1, sh_w3, sh_w2, final_norm):
    cos, sin = axial_rope_tables(x.shape[1], x.dtype)
    xl, xc = x, ctx
    silu_c, silu_cc = jax.nn.silu(c), jax.nn.silu(c_ctx)
    for i in range(DEPTH):
        last = i == DEPTH - 1
        mod_l = jnp.split((silu_c @ ada_w[i] + ada_b[i])[:, None, :], 6, axis=-1)
        mod_c = jnp.split(silu_cc @ ada_w[i] + ada_b[i], 6, axis=-1)
        hl = modulate(rmsnorm(xl, norm_mix[i]), mod_l[0], mod_l[1])
        hc = modulate(rmsnorm(xc, norm_mix[i]), mod_c[0], mod_c[1])
        if i % 2 == 0:
            e = i // 2
            ol, oc = even_mixer(hl, hc, ev_w_in[e], ev_w_out[e], rw_mu[e], rw_w0[e], rw_w2[e], rw_a0[e],
                                rw_a2[e], rw_g2[e], rw_kk[e], rw_ka[e], rw_rk[e], rw_gn_w[e], rw_gn_b[e],
                                ssm_conv_w[e], ssm_conv_b[e], ssm_dt_bias[e], ssm_a_log[e], ssm_d[e],
                                ssm_norm_w[e])
        else:
            o = i // 2
            ol, oc = mla_mixer(hl, hc, mla_w_in[o], mla_q_norm[o], mla_q_up[o], mla_kv_norm[o],
                               mla_kv_up[o], mla_w_out[o], cos, sin, not last)
        xl = xl + mod_l[2] * ol
        hl = modulate(rmsnorm(xl, norm_ffn[i]), mod_l[3], mod_l[4])
        bsz, t, d = hl.shape
        if last:
            f_l = moe_ffn(hl.reshape(-1, d), router_w[i], router_bias[i], exp_w1[i], exp_w3[i], exp_w2[i],
                          sh_w1[i], sh_w3[i], sh_w2[i])
            xl = xl + mod_l[5] * f_l.reshape(bsz, t, d)
        else:
            xc = xc + mod_c[2] * oc
            hc = modulate(rmsnorm(xc, norm_ffn[i]), mod_c[3], mod_c[4])
            tok = jnp.concatenate([hl.reshape(-1, d), hc.reshape(-1, d)], axis=0)
            f = moe_ffn(tok, router_w[i], router_bias[i], exp_w1[i], exp_w3[i], exp_w2[i],
                        sh_w1[i], sh_w3[i], sh_w2[i])
            xl = xl + mod_l[5] * f[:bsz * t].reshape(bsz, t, d)
            xc = xc + mod_c[5] * f[bsz * t:].reshape(xc.shape)
    return rmsnorm(xl, final_norm)
```

```python
import numpy as np
import concourse.bass as bass
import concourse.mybir as mybir
from concourse.bass_utils import run_bass_kernel_spmd
from contextlib import ExitStack

F32 = mybir.dt.float32
BF16 = mybir.dt.bfloat16
I32 = mybir.dt.int32
U32 = mybir.dt.uint32
ALU = mybir.AluOpType
AF = mybir.ActivationFunctionType
AX = mybir.AxisListType

ENGS = ("pe", "act", "dve", "pool", "sp")


class Buf:
    def __init__(self, S, name, t, space):
        self.S = S
        self.name = name
        self.t = t
        self.space = space
        self.w = []
        self.r = []
        self.dsem = None
        self.dcnt = 0

    def __getitem__(self, k):
        return self.t[k]

    def ap(self):
        return self.t.ap() if hasattr(self.t, "ap") else self.t[:]


class View:
    def __init__(self, buf, ap):
        self.buf, self.t = buf, ap

    def __getitem__(self, k):
        return self.t[k]


def _b(x):
    return x.buf if isinstance(x, View) else x


class _Phase:
    def __init__(self, S):
        self.S = S

    def __enter__(self):
        S = self.S
        self.prev = S.cur
        self.nb = len(S.bufs)
        self.prev_ds = S.phase_dsems
        S.phase_dsems = []
        self.es = ExitStack()
        self.es.__enter__()
        S.cur = self.es
        return self

    def __exit__(self, *a):
        S = self.S
        S.marks.append({e: sum(1 for o in S.ops[e] if o[1] is not None) for e in ENGS})
        S.barrier()
        S.free_dsems.extend(S.phase_dsems)
        S.phase_dsems = self.prev_ds
        S.bufs = S.bufs[:self.nb] + [b for b in S.bufs[self.nb:] if b.space == "dram"]
        S.cur = self.prev
        self.es.__exit__(*a)
        return False


class Sched:
    def __init__(self, nc, es):
        self.nc = nc
        self.es = es
        self.ops = {e: [] for e in ENGS}
        self.sems = {}
        self.eng_sem = {}
        self.eng_cnt = {e: 0 for e in ENGS}
        self.seen = {e: {} for e in ENGS}
        self.nsem = 0
        self.nins = 0
        self.cur = es
        self.bufs = []
        self.sem_val = {}
        self.free_dsems = []
        self.phase_dsems = []
        self.pending = {e: False for e in ENGS}
        self.marks = []
        for e in ("pe", "act", "dve", "pool"):
            self.eng_sem[e] = self.new_sem("prog_" + e)

    def new_sem(self, name):
        h = self.es.enter_context(self.nc.semaphore(name))
        sid = self.nsem
        self.nsem += 1
        self.sems[sid] = h
        return sid

    def sb(self, name, shape, dt=F32):
        self.uid = getattr(self, "uid", 0) + 1
        name = "%s_u%d" % (name, self.uid)
        t = self.cur.enter_context(self.nc.sbuf_tensor(name, list(shape), dt))
        b = Buf(self, name, t, "sb")
        self.bufs.append(b)
        return b

    def ps(self, name, shape, dt=F32):
        self.uid = getattr(self, "uid", 0) + 1
        name = "%s_u%d" % (name, self.uid)
        t = self.cur.enter_context(self.nc.psum_tensor(name, list(shape), dt))
        b = Buf(self, name, t, "ps")
        self.bufs.append(b)
        return b

    def dram(self, name, shape, dt=F32, kind="Internal"):
        t = self.nc.dram_tensor(name, list(shape), dt, kind=kind).ap()
        b = Buf(self, name, t, "dram")
        self.bufs.append(b)
        return b

    def alloc_dsem(self, name):
        if self.free_dsems:
            sid = self.free_dsems.pop()
        else:
            sid = self.new_sem("dma%d" % self.nsem)
            self.sem_val[sid] = 0
        self.phase_dsems.append(sid)
        return sid

    def barrier(self):
        assert not any(self.pending.values()), "un-signalled PE group at barrier"
        targets = []
        for e in ("pe", "act", "dve", "pool"):
            if self.eng_cnt[e]:
                targets.append((self.eng_sem[e], self.eng_cnt[e]))
        for sid, v in self.sem_val.items():
            if v:
                targets.append((sid, v))
        for e in ENGS:
            waits = []
            for (sid, v) in targets:
                if self.seen[e].get(sid, 0) < v:
                    self.seen[e][sid] = v
                    waits.append((sid, v))
            if waits:
                self.ops[e].append((waits, None, None, 0))
        for b in self.bufs:
            b.w = []
            b.r = []

    def phase(self):
        return _Phase(self)

    def _waits(self, eng, reads, writes):
        need = {}
        for b in reads:
            for (s, v) in b.w:
                need[s] = max(need.get(s, 0), v)
            if b.space == "ps":
                for (s, v) in b.r:
                    need[s] = max(need.get(s, 0), v)
        for b in writes:
            if not (b.space == "dram" and eng in ("sp", "pool_dma", "act_dma")):
                for (s, v) in b.w:
                    need[s] = max(need.get(s, 0), v)
            for (s, v) in b.r:
                need[s] = max(need.get(s, 0), v)
        out = []
        seen = self.seen[eng]
        for s, v in need.items():
            if seen.get(s, 0) >= v:
                continue
            seen[s] = v
            out.append((s, v))
        return out

    def _record(self, ev, reads, writes):
        for b in writes:
            b.w = [ev]
            b.r = []
        for b in reads:
            if b in writes:
                continue
            b.r = [(s, v) for (s, v) in b.r if s != ev[0]] + [ev]

    def op(self, eng, fn, reads=(), writes=(), inc=True):
        reads = [_b(x) for x in reads]
        writes = [_b(x) for x in writes]
        waits = self._waits(eng, reads, writes)
        if eng == "pe":
            waits = [(s_, v_) for (s_, v_) in waits if s_ != self.eng_sem["pe"]]
        if inc:
            self.eng_cnt[eng] += 1
            ev = (self.eng_sem[eng], self.eng_cnt[eng])
            self.pending[eng] = False
        else:
            assert eng == "pe"
            ev = (self.eng_sem[eng], self.eng_cnt[eng] + 1)
            self.pending[eng] = True
        self.ops[eng].append((waits, fn, ev, 1 if inc else 0))
        self._record(ev, reads, writes)
        self.nins += 1

    def dma(self, out_buf, out_ap, in_buf, in_ap, eng="sp", **kw):
        out_buf, in_buf = _b(out_buf), _b(in_buf)
        own = out_buf if out_buf.space != "dram" else in_buf
        if own.dsem is None:
            own.dsem = self.alloc_dsem(own.name)
            own.dcnt = self.sem_val[own.dsem]
        waits = self._waits(eng, [in_buf], [out_buf])
        if own.dcnt > 0 and self.seen[eng].get(own.dsem, 0) < own.dcnt:
            self.seen[eng][own.dsem] = own.dcnt
            waits.append((own.dsem, own.dcnt))
        own.dcnt += 16
        self.sem_val[own.dsem] = own.dcnt
        ev = (own.dsem, own.dcnt)

        def fn(e, out_ap=out_ap, in_ap=in_ap, kw=kw):
            return e.dma_start(out=out_ap, in_=in_ap, **kw)
        self.ops[eng].append((waits, fn, ev, 16))
        if out_buf.space == "dram":
            out_buf.w = [(s, v) for (s, v) in out_buf.w if s != ev[0]] + [ev]
            out_buf.r = []
            in_buf.r = [(s, v) for (s, v) in in_buf.r if s != ev[0]] + [ev]
        else:
            self._record(ev, [in_buf], [out_buf])
        self.nins += 1

    def reset_dram(self, b):
        b.w = []
        b.r = []

    def emit(self, final_bufs=()):
        nc = self.nc
        fw = {}
        for b in final_bufs:
            for (s, v) in b.w:
                fw[s] = max(fw.get(s, 0), v)
        for e in ("pe", "act", "dve", "pool"):
            if self.eng_cnt[e]:
                fw[self.eng_sem[e]] = self.eng_cnt[e]
        sems = self.sems
        ops = self.ops
        with nc.Block() as block:
            def run(engobj, lst):
                for (waits, fn, ev, inc) in lst:
                    for (s, v) in waits:
                        engobj.wait_ge(sems[s], v)
                    if fn is not None:
                        ins = fn(engobj)
                        if inc:
                            ins.then_inc(sems[ev[0]], inc)

            @block.sync
            def _(e):
                run(e, ops["sp"])
                for s, v in fw.items():
                    e.wait_ge(sems[s], v)

            @block.tensor
            def _(e):
                run(e, ops["pe"])

            @block.scalar
            def _(e):
                run(e, ops["act"])

            @block.vector
            def _(e):
                run(e, ops["dve"])

            @block.gpsimd
            def _(e):
                run(e, ops["pool"])


class Cfg:
    def __init__(self, NB=4, TL=2048, TC=256):
        self.NB, self.TL, self.TC = NB, TL, TC
        self.T = TL + TC
        self.NTOK = NB * self.T
        self.D = 1024


class Ctx:
    def __init__(self, S, cfg, io):
        self.S, self.cfg, self.io = S, cfg, io
        self.rr = 0

    def D(self, name, shape, dt=F32):
        kind = self.io.get(name, "Internal")
        return self.S.dram(name, shape, dt, kind=kind)


def alt(i):
    return "act" if i % 2 == 0 else "dve"


def evac(S, i, out_ap, in_ap, reads, writes):
    if i % 2 == 0:
        S.op("act", lambda e: e.copy(out=out_ap, in_=in_ap), reads=reads, writes=writes)
    else:
        S.op("dve", lambda e: e.tensor_copy(out=out_ap, in_=in_ap), reads=reads, writes=writes)


def phase_mod(C, c_in, cctx_in, ada_w, ada_b, modv):
    S, cfg = C.S, C.cfg
    NB = cfg.NB
    R = NB + 1
    cT = S.sb("mod_cT", [128, 8, R])
    for b in range(NB):
        S.dma(cT, cT[:, :, b:b + 1], c_in, c_in[b, :].rearrange("(k p o) -> p k o", p=128, o=1), allow_slow_non_contiguous=True)
    S.dma(cT, cT[:, :, NB:R], cctx_in, cctx_in.t.rearrange("(k p o) -> p k o", p=128, o=1), allow_slow_non_contiguous=True)
    sT = S.sb("mod_sT", [128, 8, R])
    S.op("act", lambda e: e.activation(out=sT[:], in_=cT[:], func=AF.Silu), reads=[cT], writes=[sT])
    wbufs = [S.sb("mod_w%d" % i, [128, 8, 512]) for i in range(2)]
    pss = [S.ps("mod_ps%d" % i, [R, 512]) for i in range(2)]
    bias = S.sb("mod_bias", [R, 6144])
    orow = S.sb("mod_orow", [R, 6144])
    for l in range(2):
        S.dma(bias, bias[:], ada_b, ada_b[l, :].partition_broadcast(R))
        wv = ada_w[l].rearrange("(k p) c -> p k c", p=128)
        for j in range(12):
            wb = wbufs[j % 2]
            ps = pss[j % 2]
            S.dma(wb, wb[:], ada_w, wv[:, :, j * 512:(j + 1) * 512])
            for k in range(8):
                S.op("pe", lambda e, ps=ps, wb=wb, k=k: e.matmul(ps[:], sT[:, k, :], wb[:, k, :], start=(k == 0), stop=(k == 7)),
                     reads=[sT, wb], writes=[ps], inc=(k == 7))
            S.op("dve", lambda e, ps=ps, j=j: e.tensor_tensor(out=orow[:, j * 512:(j + 1) * 512], in0=ps[:], in1=bias[:, j * 512:(j + 1) * 512], op=ALU.add),
                 reads=[ps, bias], writes=[orow])
        S.dma(modv, modv[l], orow, orow[:])


class ModTiles:
    def __init__(self, C, name, modv, layer, shift_idx, scale_idx, gvec_ap, gvec_buf):
        S, cfg = C.S, C.cfg
        R = cfg.NB + 1
        D = cfg.D
        self.G = [S.sb("%s_G%d" % (name, r), [128, D]) for r in range(R)]
        self.Sh = [S.sb("%s_S%d" % (name, r), [128, D]) for r in range(R)]
        gb = S.sb("%s_g" % name, [128, D])
        S.dma(gb, gb[:], gvec_buf, gvec_ap.partition_broadcast(128))
        for r in range(R):
            G, Sh = self.G[r], self.Sh[r]
            S.dma(G, G[:], modv, modv[layer, r, scale_idx * D:(scale_idx + 1) * D].partition_broadcast(128))
            S.dma(Sh, Sh[:], modv, modv[layer, r, shift_idx * D:(shift_idx + 1) * D].partition_broadcast(128))
            S.op("dve", lambda e, G=G: e.scalar_tensor_tensor(out=G[:], in0=G[:], scalar=1.0, in1=gb[:], op0=ALU.add, op1=ALU.mult),
                 reads=[G, gb], writes=[G])

    def row(self, cfg, tile_idx):
        tpb = cfg.T // 128
        b, tt = divmod(tile_idx, tpb)
        return cfg.NB if tt < cfg.TC // 128 else b


class NormT:
    def __init__(self, C, name, ident, want32=False):
        S = C.S
        self.C, self.name, self.ident = C, name, ident
        D = C.cfg.D
        self.xt = [S.sb("%s_xt%d" % (name, i), [128, D]) for i in range(2)]
        self.junk = S.sb("%s_junk" % name, [128, D], BF16)
        self.ss = [S.sb("%s_ss%d" % (name, i), [128, 1]) for i in range(2)]
        self.h = [S.sb("%s_h%d" % (name, i), [128, D]) for i in range(2)]
        self.pt = [S.ps("%s_pt%d" % (name, i), [128, 4, 128]) for i in range(2)]
        self.n = 0

    def tile(self, xsrc, row0, mt, r, outs):
        S = self.C.S
        D = self.C.cfg.D
        i = self.n % 2
        self.n += 1
        xt, ss, h = self.xt[i], self.ss[i], self.h[i]
        S.dma(xt, xt[:], xsrc, xsrc[row0:row0 + 128, :])
        junk = self.junk
        S.op("act", lambda e: e.activation(out=junk[:], in_=xt[:], func=AF.Square, accum_out=ss[:]), reads=[xt], writes=[junk, ss])
        S.op("act", lambda e: e.activation(out=ss[:], in_=ss[:], func=AF.Sqrt, bias=NORM_EPS, scale=1.0 / D), reads=[ss], writes=[ss])
        S.op("dve", lambda e: e.reciprocal(out=ss[:], in_=ss[:]), reads=[ss], writes=[ss])
        G, Sh = mt.G[r], mt.Sh[r]
        S.op("dve", lambda e: e.scalar_tensor_tensor(out=h[:], in0=xt[:], scalar=ss[:, 0:1], in1=G[:], op0=ALU.mult, op1=ALU.mult),
             reads=[xt, ss, G], writes=[h])
        S.op("pool", lambda e: e.tensor_tensor(out=h[:], in0=h[:], in1=Sh[:], op=ALU.add), reads=[h, Sh], writes=[h])
        ident = self.ident
        for half in range(2):
            pt = self.pt[half]
            for kk in range(4):
                k = half * 4 + kk
                S.op("pe", lambda e, pt=pt, kk=kk, k=k: e.transpose(pt[:, kk, :], h[:, k * 128:(k + 1) * 128], ident[:]),
                     reads=[h, ident], writes=[pt], inc=(kk == 3))
            for oi, (ob, ofn) in enumerate(outs):
                evac(S, half + oi, ofn(half * 4, half * 4 + 4), pt[:], [pt], [ob])


NORM_EPS = 1e-6


def load_w_bf16(C, name, wsrc_buf, wv, ncols, kch):
    S = C.S
    wb = S.sb(name, [128, kch, ncols], BF16)
    CH = max(128, min(512, 2048 // kch))
    stg = [S.sb("%s_stg%d" % (name, i), [128, kch, CH]) for i in range(2)]
    nchunk = (ncols + CH - 1) // CH
    for j in range(nchunk):
        c0 = j * CH
        c1 = min(ncols, c0 + CH)
        st = stg[j % 2]
        S.dma(st, st[:, :, 0:c1 - c0], wsrc_buf, wv[:, :, c0:c1])
        S.op("pool", lambda e, st=st, c0=c0, c1=c1: e.tensor_copy(out=wb[:, :, c0:c1], in_=st[:, :, 0:c1 - c0]), reads=[st], writes=[wb])
    return wb


def phase_normproj(C, name, xsrc, gvec_buf, gvec_ap, modv, layer, W_buf, W_ap, ncols, outT, ident):
    S, cfg = C.S, C.cfg
    mt = ModTiles(C, name + "_mt", modv, layer, 0, 1, gvec_ap, gvec_buf)
    wb = load_w_bf16(C, name + "_w", W_buf, W_ap.rearrange("(k p) c -> p k c", p=128), ncols, 8)
    nt = NormT(C, name + "_nt", ident)
    hT = [S.sb("%s_hT%d" % (name, i), [128, 8, 512], BF16) for i in range(2)]
    pss = [S.ps("%s_ps%d" % (name, i), [128, 512]) for i in range(3)]
    ost = [S.sb("%s_ost%d" % (name, i), [128, 512]) for i in range(3)]
    ntl = cfg.NTOK // 128
    nsb = (ntl + 3) // 4
    ncj = (ncols + 127) // 128
    cnt = 0
    for sb in range(nsb):
        ht = hT[sb % 2]
        nti = min(4, ntl - sb * 4)
        n = nti * 128
        for ti in range(nti):
            tile_idx = sb * 4 + ti
            r = mt.row(cfg, tile_idx)
            nt.tile(xsrc, tile_idx * 128, mt, r, [(ht, lambda k0, k1, ti=ti, ht=ht: ht[:, k0:k1, ti * 128:(ti + 1) * 128])])
        for j in range(ncj):
            c0 = j * 128
            cw = min(128, ncols - c0)
            ps = pss[cnt % 3]
            ob = ost[cnt % 3]
            for k in range(8):
                S.op("pe", lambda e, ps=ps, k=k, c0=c0, cw=cw, ht=ht, n=n: e.matmul(ps[0:cw, 0:n], wb[:, k, c0:c0 + cw], ht[:, k, 0:n], start=(k == 0), stop=(k == 7)),
                     reads=[wb, ht], writes=[ps], inc=(k == 7))
            evac(S, cnt, ob[0:cw, 0:n], ps[0:cw, 0:n], [ps], [ob])
            S.dma(outT, outT[c0:c0 + cw, sb * 512:sb * 512 + n], ob, ob[0:cw, 0:n])
            cnt += 1


def seg_blocks(cfg, blk=512):
    out = []
    for b in range(cfg.NB):
        for (s0, s1) in ((0, cfg.TC), (cfg.TC, cfg.T)):
            t = s0
            while t < s1:
                n = min(blk, s1 - t)
                out.append((b, t, n, t > s0, t + n < s1))
                t += n
    return out


def vec_cols(C, name, src_buf, src_ap_1d, nch):
    S = C.S
    t = S.sb(name, [128, nch])
    S.dma(t, t[:], src_buf, src_ap_1d.rearrange("(c p) -> p c", p=128), allow_slow_non_contiguous=True)
    return t


def phase_rwprep(C, colsT, P, A, onesblk, ident, vtok):
    S, cfg = C.S, C.cfg
    T = cfg.T
    mu = vec_cols(C, "rp_mu", P["rw_mu"], P["rw_mu"][0, :], 15)
    w0 = [vec_cols(C, "rp_w0%d" % d, P["rw_w0"], P["rw_w0"][0, d, :], 4) for d in range(2)]
    a0 = [vec_cols(C, "rp_a0%d" % d, P["rw_a0"], P["rw_a0"][0, d, :], 4) for d in range(2)]
    kkv = vec_cols(C, "rp_kkv", P["rw_kk"], P["rw_kk"][0, :], 4)
    kav = vec_cols(C, "rp_kav", P["rw_ka"], P["rw_ka"][0, :], 4)
    rkv = vec_cols(C, "rp_rkv", P["rw_rk"], P["rw_rk"][0, :], 4)
    omka = S.sb("rp_omka", [128, 4])
    S.op("dve", lambda e: e.tensor_scalar(out=omka[:], in0=kav[:], scalar1=-1.0, scalar2=1.0, op0=ALU.mult, op1=ALU.add), reads=[kav], writes=[omka])
    W2 = S.sb("rp_W2", [128, 512])
    A2 = S.sb("rp_A2", [128, 512])
    G2 = S.sb("rp_G2", [128, 512])
    for d in range(2):
        S.dma(W2, W2[d * 64:(d + 1) * 64, :], P["rw_w2"], P["rw_w2"][0, d, :, :])
        S.dma(A2, A2[d * 64:(d + 1) * 64, :], P["rw_a2"], P["rw_a2"][0, d, :, :])
    S.dma(G2, G2[:], P["rw_g2"], P["rw_g2"][0, :, :])
    NB_ = 512
    raw = [S.sb("rp_raw%d" % i, [128, NB_ + 2]) for i in range(3)]
    MX = [S.sb("rp_mx%d" % c, [128, NB_]) for c in range(15)]
    tmp = [S.sb("rp_tmp%d" % i, [128, NB_]) for i in range(2)]
    TH = S.sb("rp_th", [128, NB_])
    SG = S.sb("rp_sg", [128, NB_])
    Aa = [[S.sb("rp_a%d_%d" % (d, cc), [128, NB_]) for cc in range(4)] for d in range(2)]
    KD = [[S.sb("rp_kd%d_%d" % (d, cc), [128, NB_]) for cc in range(4)] for d in range(2)]
    KK = [S.sb("rp_kk%d" % cc, [128, NB_]) for cc in range(4)]
    ost = [S.sb("rp_ost%d" % i, [128, NB_]) for i in range(4)]
    pss = [S.ps("rp_ps%d" % i, [128, NB_]) for i in range(4)]
    vtb = [S.sb("rp_vtb%d" % i, [128, 512], BF16) for i in range(2)]
    oc = [0]
    pc = [0]

    def nps():
        pc[0] += 1
        return pss[pc[0] % 4]

    def nost():
        oc[0] += 1
        return ost[oc[0] % 4]

    def store(arr, cc, b, t0, n, buf, ap):
        S.dma(arr, arr[cc * 128:(cc + 1) * 128, b * T + t0:b * T + t0 + n], buf, ap)

    for bi, (b, t0, n, hl, hr) in enumerate(seg_blocks(cfg, NB_)):
        g0 = b * T + t0
        for c in range(15):
            rw = raw[c % 3]
            if not hl:
                S.op("pool", lambda e, rw=rw: e.memset(rw[:, 0:1], 0.0), writes=[rw])
            if not hr:
                S.op("pool", lambda e, rw=rw, n=n: e.memset(rw[:, n + 1:n + 2], 0.0), writes=[rw])
            lo = g0 - (1 if hl else 0)
            hi = g0 + n + (1 if hr else 0)
            S.dma(rw, rw[:, (0 if hl else 1):(0 if hl else 1) + hi - lo], colsT, colsT[c * 128:(c + 1) * 128, lo:hi])
            tp = tmp[c % 2]
            mx = MX[c]
            S.op("dve", lambda e, rw=rw, tp=tp, n=n: e.tensor_tensor(out=tp[:, 0:n], in0=rw[:, 0:n], in1=rw[:, 2:n + 2], op=ALU.add), reads=[rw], writes=[tp])
            S.op("dve", lambda e, rw=rw, tp=tp, n=n: e.scalar_tensor_tensor(out=tp[:, 0:n], in0=tp[:, 0:n], scalar=0.5, in1=rw[:, 1:n + 1], op0=ALU.mult, op1=ALU.subtract),
                 reads=[rw, tp], writes=[tp])
            S.op("dve", lambda e, rw=rw, tp=tp, n=n, c=c, mx=mx: e.scalar_tensor_tensor(out=mx[:, 0:n], in0=tp[:, 0:n], scalar=mu[:, c:c + 1], in1=rw[:, 1:n + 1], op0=ALU.mult, op1=ALU.add),
                 reads=[rw, tp, mu], writes=[mx])
        for cc in range(4):
            store(A["r"], cc, b, t0, n, MX[cc], MX[cc][:, 0:n])
        for j in range(n // 128):
            ps = nps()
            for cc in range(4):
                S.op("pe", lambda e, ps=ps, cc=cc, j=j: e.transpose(ps[:, cc * 128:(cc + 1) * 128], MX[8 + cc][:, j * 128:(j + 1) * 128], ident[:]),
                     reads=[MX[8 + cc], ident], writes=[ps], inc=(cc == 3))
            vb = vtb[j % 2]
            evac(S, j, vb[:], ps[:, 0:512], [ps], [vb])
            S.dma(vtok, vtok[g0 + j * 128:g0 + (j + 1) * 128, :], vb, vb[:])
        for cc in range(4):
            kk = KK[cc]
            tp = tmp[cc % 2]
            S.op("dve", lambda e, cc=cc, kk=kk, n=n: e.tensor_scalar(out=kk[:, 0:n], in0=MX[4 + cc][:, 0:n], scalar1=kkv[:, cc:cc + 1], scalar2=None, op0=ALU.mult),
                 reads=[MX[4 + cc], kkv], writes=[kk])
            S.op("pool", lambda e, kk=kk, tp=tp, n=n: e.tensor_tensor(out=tp[:, 0:n], in0=kk[:, 0:n], in1=kk[:, 0:n], op=ALU.mult), reads=[kk], writes=[tp])
            ps = nps()
            S.op("pe", lambda e, ps=ps, tp=tp, n=n: e.matmul(ps[:, 0:n], onesblk[:], tp[:, 0:n], start=True, stop=True), reads=[onesblk, tp], writes=[ps])
            S.op("act", lambda e, ps=ps, tp=tp, n=n: e.activation(out=tp[:, 0:n], in_=ps[:, 0:n], func=AF.Sqrt, bias=1e-12, scale=1.0), reads=[ps], writes=[tp])
            S.op("dve", lambda e, tp=tp, n=n: e.reciprocal(out=tp[:, 0:n], in_=tp[:, 0:n]), reads=[tp], writes=[tp])
            S.op("dve", lambda e, kk=kk, tp=tp, n=n: e.tensor_tensor(out=kk[:, 0:n], in0=kk[:, 0:n], in1=tp[:, 0:n], op=ALU.mult), reads=[kk, tp], writes=[kk])
            store(A["kk"], cc, b, t0, n, kk, kk[:, 0:n])
        S.op("act", lambda e, n=n: e.activation(out=TH[:, 0:n], in_=MX[12][:, 0:n], func=AF.Tanh), reads=[MX[12]], writes=[TH])
        for d in range(2):
            for cc in range(4):
                ps = nps()
                S.op("pe", lambda e, ps=ps, d=d, cc=cc, n=n: e.matmul(ps[:, 0:n], W2[d * 64:(d + 1) * 64, cc * 128:(cc + 1) * 128], TH[d * 64:(d + 1) * 64, 0:n], start=True, stop=True),
                     reads=[W2, TH], writes=[ps])
                ob = nost()
                S.op("act", lambda e, ps=ps, ob=ob, d=d, cc=cc, n=n: e.activation(out=ob[:, 0:n], in_=ps[:, 0:n], func=AF.Sigmoid, bias=w0[d][:, cc:cc + 1], scale=1.0),
                     reads=[ps, w0[d]], writes=[ob])
                S.op("act", lambda e, ob=ob, n=n: e.activation(out=ob[:, 0:n], in_=ob[:, 0:n], func=AF.Exp, scale=-0.6065306597126334), reads=[ob], writes=[ob])
                store(A["w%d" % d], cc, b, t0, n, ob, ob[:, 0:n])
        for d in range(2):
            for cc in range(4):
                ps = nps()
                S.op("pe", lambda e, ps=ps, d=d, cc=cc, n=n: e.matmul(ps[:, 0:n], A2[d * 64:(d + 1) * 64, cc * 128:(cc + 1) * 128], MX[13][d * 64:(d + 1) * 64, 0:n], start=True, stop=True),
                     reads=[A2, MX[13]], writes=[ps])
                aa = Aa[d][cc]
                S.op("act", lambda e, ps=ps, aa=aa, d=d, cc=cc, n=n: e.activation(out=aa[:, 0:n], in_=ps[:, 0:n], func=AF.Sigmoid, bias=a0[d][:, cc:cc + 1], scale=1.0),
                     reads=[ps, a0[d]], writes=[aa])
                ob = nost()
                S.op("pool", lambda e, ob=ob, aa=aa, cc=cc, n=n: e.tensor_tensor(out=ob[:, 0:n], in0=KK[cc][:, 0:n], in1=aa[:, 0:n], op=ALU.mult), reads=[KK[cc], aa], writes=[ob])
                store(A["b%d" % d], cc, b, t0, n, ob, ob[:, 0:n])
                kd = KD[d][cc]
                S.op("dve", lambda e, kd=kd, aa=aa, cc=cc, n=n: e.tensor_scalar(out=kd[:, 0:n], in0=aa[:, 0:n], scalar1=kav[:, cc:cc + 1], scalar2=omka[:, cc:cc + 1], op0=ALU.mult, op1=ALU.add),
                     reads=[aa, kav, omka], writes=[kd])
                S.op("dve", lambda e, kd=kd, cc=cc, n=n: e.tensor_tensor(out=kd[:, 0:n], in0=kd[:, 0:n], in1=MX[4 + cc][:, 0:n], op=ALU.mult), reads=[kd, MX[4 + cc]], writes=[kd])
                store(A["kd%d" % d], cc, b, t0, n, kd, kd[:, 0:n])
        for cc in range(4):
            tp = tmp[cc % 2]
            S.op("pool", lambda e, tp=tp, cc=cc, n=n: e.tensor_tensor(out=tp[:, 0:n], in0=KD[0][cc][:, 0:n], in1=KD[1][cc][:, 0:n], op=ALU.add), reads=[KD[0][cc], KD[1][cc]], writes=[tp])
            S.op("dve", lambda e, tp=tp, cc=cc, n=n: e.scalar_tensor_tensor(out=tp[:, 0:n], in0=tp[:, 0:n], scalar=rkv[:, cc:cc + 1], in1=MX[cc][:, 0:n], op0=ALU.mult, op1=ALU.mult),
                 reads=[tp, rkv, MX[cc]], writes=[tp])
            ps = nps()
            S.op("pe", lambda e, ps=ps, tp=tp, n=n: e.matmul(ps[:, 0:n], onesblk[:], tp[:, 0:n], start=True, stop=True), reads=[onesblk, tp], writes=[ps])
            ob = nost()
            S.op("dve", lambda e, ps=ps, ob=ob, cc=cc, n=n: e.tensor_tensor(out=ob[:, 0:n], in0=ps[:, 0:n], in1=MX[8 + cc][:, 0:n], op=ALU.mult), reads=[ps, MX[8 + cc]], writes=[ob])
            store(A["bonus"], cc, b, t0, n, ob, ob[:, 0:n])
        S.op("act", lambda e, n=n: e.activation(out=SG[:, 0:n], in_=MX[14][:, 0:n], func=AF.Sigmoid), reads=[MX[14]], writes=[SG])
        for cc in range(4):
            ps = nps()
            S.op("pe", lambda e, ps=ps, cc=cc, n=n: e.matmul(ps[:, 0:n], G2[:, cc * 128:(cc + 1) * 128], SG[:, 0:n], start=True, stop=True), reads=[G2, SG], writes=[ps])
            ob = nost()
            evac(S, cc, ob[:, 0:n], ps[:, 0:n], [ps], [ob])
            store(A["g"], cc, b, t0, n, ob, ob[:, 0:n])


def cust_ap(base_ap, dims):
    pa = base_ap.ap[0]
    return bass.AP(base_ap.tensor, base_ap.offset, [[pa[0], pa[1]]] + [[st, ct] for (st, ct) in dims])


def phase_rwscan(C, A, vtok, YD, onesblk, identb, Eq):
    S, cfg = C.S, C.cfg
    NB, T, TC = cfg.NB, cfg.T, cfg.TC
    NCB = 4 * NB
    Fh = NCB * 64
    NQh = Fh // 512
    NQ = 2 * NQh
    TB = 64
    YB = 4

    def st(name):
        return [S.sb("%s%d" % (name, d), [128, Fh]) for d in range(2)]
    M, MW, TMP, TMP2, TMP3, T4 = st("sc_M"), st("sc_MW"), st("sc_TMP"), st("sc_TMP2"), st("sc_TMP3"), st("sc_T4")
    saPS = [S.ps("sc_saPS%d" % d, [128, Fh]) for d in range(2)]
    vPS = [S.ps("sc_vPS%d" % d, [128, Fh]) for d in range(2)]
    OPS = [S.sb("sc_OPS%d" % i, [128, 5, 2, NCB, TB]) for i in range(2)]
    VT = [S.sb("sc_VT%d" % i, [TB, 2, 2, NCB, 64], BF16) for i in range(2)]
    YS = [[S.sb("sc_YS%d_%d" % (d, i), [2 * NQh, YB, 512]) for i in range(2)] for d in range(2)]
    for d in range(2):
        S.op("pool", lambda e, d=d: e.memset(M[d][:], 0.0), writes=[M[d]])

    def v3(buf):
        return buf[:].rearrange("p (c v) -> p c v", c=NCB)

    def tb_of(s):
        return TC - 1 - s if s < TC else T + TC - 1 - s

    names = [("kk", "kk"), ("w0", "w1"), ("b0", "b1"), ("kd0", "kd1"), ("r", "r")]
    for blk in range(T // TB):
        s0 = blk * TB
        ops = OPS[blk % 2]
        vt = VT[blk % 2]
        lo = tb_of(s0 + TB - 1)
        tok0 = (s0, lo)
        for a_, nm in enumerate(names):
            for d in range(2):
                arr = A[nm[d]]
                av = arr.t.rearrange("(c p) (b t) -> c p b t", p=128, b=NB)
                for c in range(4):
                    S.dma(ops, ops[:, a_, d, c * NB:(c + 1) * NB, :], arr, av[c, :, :, tok0[d]:tok0[d] + TB])
        vv = vtok.t.rearrange("(b t) (c h v) -> b t c h v", b=NB, c=4, h=2)
        for d in range(2):
            for c in range(4):
                for b in range(NB):
                    S.dma(vt, vt[:, :, d, c * NB + b, :], vtok, vv[b, tok0[d]:tok0[d] + TB, c, :, :])
        for j in range(TB):
            s = s0 + j
            col = (j, tb_of(s) - lo)

            def opnd(a_, d, ops=ops, col=col):
                base = ops[:, a_, d, 0, col[d]:col[d] + 1]
                return cust_ap(base, [(TB, NCB), (0, 64)])
            for d in range(2):
                KKo = opnd(0, d)
                S.op("dve", lambda e, d=d, KKo=KKo: e.tensor_tensor(out=v3(TMP[d]), in0=v3(M[d]), in1=KKo, op=ALU.mult), reads=[M[d], ops], writes=[TMP[d]])
                for q in range(NQh):
                    S.op("pe", lambda e, d=d, q=q: e.matmul(saPS[d][:, q * 512:(q + 1) * 512], onesblk[:], TMP[d][:, q * 512:(q + 1) * 512], start=True, stop=True),
                         reads=[onesblk, TMP[d]], writes=[saPS[d]], inc=(q == NQh - 1))
            for d in range(2):
                Wo = opnd(1, d)
                S.op("pool", lambda e, d=d, Wo=Wo: e.tensor_tensor(out=v3(MW[d]), in0=v3(M[d]), in1=Wo, op=ALU.mult), reads=[M[d], ops], writes=[MW[d]])
                sel = identb[0:TB, col[d]:col[d] + 1].to_broadcast([TB, 64])
                for h2 in range(2):
                    vsrc = vt[:, h2, d].rearrange("t c v -> t (c v)")
                    for q in range(NQh):
                        S.op("pe", lambda e, d=d, h2=h2, q=q, sel=sel, vsrc=vsrc: e.matmul(vPS[d][h2 * 64:(h2 + 1) * 64, q * 512:(q + 1) * 512], sel, vsrc[:, q * 512:(q + 1) * 512], start=True, stop=True),
                             reads=[identb, vt], writes=[vPS[d]], inc=(h2 == 1 and q == NQh - 1))
            for d in range(2):
                KDo = opnd(3, d)
                S.op("dve", lambda e, d=d, KDo=KDo: e.tensor_tensor(out=v3(TMP3[d]), in0=v3(vPS[d]), in1=KDo, op=ALU.mult), reads=[vPS[d], ops], writes=[TMP3[d]])
                S.op("pool", lambda e, d=d: e.tensor_tensor(out=MW[d][:], in0=MW[d][:], in1=TMP3[d][:], op=ALU.add), reads=[MW[d], TMP3[d]], writes=[MW[d]])
            for d in range(2):
                Bo = opnd(2, d)
                S.op("dve", lambda e, d=d, Bo=Bo: e.tensor_tensor(out=v3(TMP2[d]), in0=v3(saPS[d]), in1=Bo, op=ALU.mult), reads=[saPS[d], ops], writes=[TMP2[d]])
                S.op("dve", lambda e, d=d: e.tensor_tensor(out=M[d][:], in0=MW[d][:], in1=TMP2[d][:], op=ALU.subtract), reads=[MW[d], TMP2[d]], writes=[M[d]])
            for d, eng in ((0, "dve"), (1, "pool")):
                Ro = opnd(4, d)
                S.op(eng, lambda e, d=d, Ro=Ro: e.tensor_tensor(out=v3(T4[d]), in0=v3(M[d]), in1=Ro, op=ALU.mult), reads=[M[d], ops], writes=[T4[d]])
                for q in range(NQh):
                    S.op("pe", lambda e, d=d, q=q: e.matmul(saPS[d][0:2 * NQh, 0:512], Eq[:, q, 0:2 * NQh], T4[d][:, q * 512:(q + 1) * 512], start=(q == 0), stop=(q == NQh - 1)),
                         reads=[Eq, T4[d]], writes=[saPS[d]], inc=(q == NQh - 1))
                ys = YS[d][(s // YB) % 2]
                S.op("act", lambda e, d=d, ys=ys, s=s: e.copy(out=ys[:, s % YB, :], in_=saPS[d][0:2 * NQh, 0:512]), reads=[saPS[d]], writes=[ys])
                if s % YB == YB - 1:
                    if d == 0:
                        sa_ = s - YB + 1
                        S.dma(YD, YD[0:2 * NQh, sa_:sa_ + YB, :], ys, ys[:])
                    else:
                        tlo = tb_of(s)
                        S.dma(YD, YD[2 * NQh:4 * NQh, tlo:tlo + YB, :][:, ::-1, :], ys, ys[:])


def phase_rwpost(C, YD, A, P, mixT, ident):
    S, cfg = C.S, C.cfg
    NB, T = cfg.NB, cfg.T
    gnw = vec_cols(C, "rq_gnw", P["rw_gn_w"], P["rw_gn_w"][0, :], 4)
    gnb = vec_cols(C, "rq_gnb", P["rw_gn_b"], P["rw_gn_b"][0, :], 4)
    Y = [S.sb("rq_Y%d" % i, [128, 2, 4, 2, 64]) for i in range(2)]
    ysum = S.sb("rq_ysum", [128, 8, 64])
    sq = S.sb("rq_sq", [128, 8, 64])
    st1 = S.sb("rq_st1", [128, 8])
    st2 = S.sb("rq_st2", [128, 8])
    BG = [S.sb("rq_BG%d" % i, [128, 2, 4, 128]) for i in range(2)]
    pt = S.ps("rq_pt", [128, 4, 128])
    o32 = S.sb("rq_o32", [128, 4, 128])
    ob = [S.sb("rq_ob%d" % i, [128, 4, 128], BF16) for i in range(2)]
    bv = A["bonus"].t.rearrange("(c p) n -> p c n", p=128)
    gv = A["g"].t.rearrange("(c p) n -> p c n", p=128)
    mv = mixT.t[0:512, :].rearrange("(c p) n -> p c n", p=128)
    for ti in range(cfg.NTOK // 128):
        b, tt = divmod(ti, T // 128)
        t0 = tt * 128
        g0 = ti * 128
        y = Y[ti % 2]
        bg = BG[ti % 2]
        for d in range(2):
            for c in range(4):
                n0 = ((d * 4 + c) * NB + b) * 64
                q, col0 = n0 // 512, n0 % 512
                for h2 in range(2):
                    S.dma(y, y[:, d, c, h2, :], YD, YD[2 * q + h2, t0:t0 + 128, col0:col0 + 64])
        S.dma(bg, bg[:, 0], A["bonus"], bv[:, :, g0:g0 + 128])
        S.dma(bg, bg[:, 1], A["g"], gv[:, :, g0:g0 + 128])
        yf = y[:, 0].rearrange("p c h v -> p (c h) v")
        yb = y[:, 1].rearrange("p c h v -> p (c h) v")
        S.op("dve", lambda e, yf=yf, yb=yb: e.tensor_tensor(out=ysum[:], in0=yf, in1=yb, op=ALU.add), reads=[y], writes=[ysum])
        S.op("dve", lambda e: e.reduce_sum(out=st1[:], in_=ysum[:], axis=AX.X), reads=[ysum], writes=[st1])
        S.op("dve", lambda e: e.tensor_scalar(out=st1[:], in0=st1[:], scalar1=1.0 / 64, scalar2=None, op0=ALU.mult), reads=[st1], writes=[st1])
        S.op("dve", lambda e: e.tensor_tensor(out=ysum[:], in0=ysum[:], in1=st1[:].unsqueeze(2).to_broadcast([128, 8, 64]), op=ALU.subtract), reads=[ysum, st1], writes=[ysum])
        S.op("pool", lambda e: e.tensor_tensor(out=sq[:], in0=ysum[:], in1=ysum[:], op=ALU.mult), reads=[ysum], writes=[sq])
        S.op("dve", lambda e: e.reduce_sum(out=st2[:], in_=sq[:], axis=AX.X), reads=[sq], writes=[st2])
        S.op("act", lambda e: e.activation(out=st2[:], in_=st2[:], func=AF.Sqrt, bias=RW_GN_EPS, scale=1.0 / 64), reads=[st2], writes=[st2])
        S.op("dve", lambda e: e.reciprocal(out=st2[:], in_=st2[:]), reads=[st2], writes=[st2])
        S.op("dve", lambda e: e.tensor_tensor(out=ysum[:], in0=ysum[:], in1=st2[:].unsqueeze(2).to_broadcast([128, 8, 64]), op=ALU.mult), reads=[ysum, st2], writes=[ysum])
        for c in range(4):
            S.op("pe", lambda e, c=c: e.transpose(pt[:, c, :], ysum[:, 2 * c:2 * c + 2, :].rearrange("p h v -> p (h v)"), ident[:]), reads=[ysum, ident], writes=[pt], inc=(c == 3))
        for c in range(4):
            S.op("dve", lambda e, c=c: e.tensor_scalar(out=o32[:, c, :], in0=pt[:, c, :], scalar1=gnw[:, c:c + 1], scalar2=gnb[:, c:c + 1], op0=ALU.mult, op1=ALU.add),
                 reads=[pt, gnw, gnb], writes=[o32])
        S.op("pool", lambda e, bg=bg: e.tensor_tensor(out=o32[:], in0=o32[:], in1=bg[:, 0], op=ALU.add), reads=[o32, bg], writes=[o32])
        o = ob[ti % 2]
        S.op("dve", lambda e, bg=bg, o=o: e.tensor_tensor(out=o[:], in0=o32[:], in1=bg[:, 1], op=ALU.mult), reads=[o32, bg], writes=[o])
        S.dma(mixT, mv[:, :, g0:g0 + 128], o, o[:])


RW_GN_EPS = 64e-5


def phase_ssprep(C, colsT, P, xbc_tok, BCT, dtda_tok, ident):
    S, cfg = C.S, C.cfg
    T = cfg.T
    cw = S.sb("sp_cw", [128, 12, 5])
    for j in range(5):
        S.dma(cw, cw[:, :, j:j + 1], P["ssm_conv_w"], P["ssm_conv_w"][0, j, :].rearrange("(c p o) -> p c o", p=128, o=1), allow_slow_non_contiguous=True)
    cb = vec_cols(C, "sp_cb", P["ssm_conv_b"], P["ssm_conv_b"][0, :], 12)
    dtb = S.sb("sp_dtb", [64, 1])
    aneg = S.sb("sp_aneg", [64, 1])
    dbv = P["ssm_dt_bias"][0].rearrange("d (h o) -> (d h) o", o=1)
    alv = P["ssm_a_log"][0].rearrange("d (h o) -> (d h) o", o=1)
    S.dma(dtb, dtb[0:32, :], P["ssm_dt_bias"], dbv, allow_slow_non_contiguous=True)
    S.dma(dtb, dtb[32:64, :], P["ssm_dt_bias"], dbv, allow_slow_non_contiguous=True)
    S.dma(aneg, aneg[32:64, :], P["ssm_a_log"], alv, allow_slow_non_contiguous=True)
    S.op("act", lambda e: e.activation(out=aneg[32:64, :], in_=aneg[32:64, :], func=AF.Exp), reads=[aneg], writes=[aneg])
    S.op("dve", lambda e: e.tensor_scalar(out=aneg[32:64, :], in0=aneg[32:64, :], scalar1=-1.0, scalar2=None, op0=ALU.mult), reads=[aneg], writes=[aneg])
    NB_ = 512
    raw = [S.sb("sp_raw%d" % i, [128, NB_ + 4]) for i in range(3)]
    acc = [S.sb("sp_acc%d" % i, [128, NB_]) for i in range(2)]
    XC = [S.sb("sp_xc%d" % c, [128, NB_]) for c in range(12)]
    bcb = [S.sb("sp_bcb%d" % i, [128, NB_], BF16) for i in range(2)]
    DD = S.sb("sp_dd", [64, NB_])
    pss = [S.ps("sp_ps%d" % i, [128, 512]) for i in range(3)]
    pdd = S.ps("sp_pdd", [128, 64])
    tk = [S.sb("sp_tk%d" % i, [128, 1536]) for i in range(2)]
    dk = [S.sb("sp_dk%d" % i, [128, 64]) for i in range(2)]
    pc = [0]
    for (b, t0, n, hl, hr) in seg_blocks(cfg, NB_):
        g0 = b * T + t0
        for c in range(12):
            rw = raw[c % 3]
            nl = 2 if hl else 0
            nr = 2 if hr else 0
            if not hl:
                S.op("pool", lambda e, rw=rw: e.memset(rw[:, 0:2], 0.0), writes=[rw])
            if not hr:
                S.op("pool", lambda e, rw=rw, n=n: e.memset(rw[:, n + 2:n + 4], 0.0), writes=[rw])
            S.dma(rw, rw[:, 2 - nl:2 + n + nr], colsT, colsT[2944 + c * 128:2944 + (c + 1) * 128, g0 - nl:g0 + n + nr])
            ac = acc[c % 2]
            S.op("dve", lambda e, rw=rw, ac=ac, c=c, n=n: e.tensor_scalar(out=ac[:, 0:n], in0=rw[:, 0:n], scalar1=cw[:, c, 0:1], scalar2=None, op0=ALU.mult), reads=[rw, cw], writes=[ac])
            for j in range(1, 5):
                eng = "dve"
                S.op(eng, lambda e, rw=rw, ac=ac, c=c, n=n, j=j: e.scalar_tensor_tensor(out=ac[:, 0:n], in0=rw[:, j:j + n], scalar=cw[:, c, j:j + 1], in1=ac[:, 0:n], op0=ALU.mult, op1=ALU.add),
                     reads=[rw, cw, ac], writes=[ac])
            xc = XC[c]
            S.op("act", lambda e, ac=ac, xc=xc, c=c, n=n: e.activation(out=xc[:, 0:n], in_=ac[:, 0:n], func=AF.Silu, bias=cb[:, c:c + 1], scale=1.0), reads=[ac, cb], writes=[xc])
            if c >= 8:
                bb = bcb[c % 2]
                S.op("pool", lambda e, bb=bb, xc=xc, n=n: e.tensor_copy(out=bb[:, 0:n], in_=xc[:, 0:n]), reads=[xc], writes=[bb])
                S.dma(BCT, BCT[(c - 8) * 128:(c - 7) * 128, g0:g0 + n], bb, bb[:, 0:n])
        S.dma(DD, DD[0:32, 0:n], colsT, colsT[4480:4512, g0:g0 + n])
        S.dma(DD, DD[32:64, 0:n], colsT, colsT[4480:4512, g0:g0 + n])
        S.op("act", lambda e, n=n: e.activation(out=DD[:, 0:n], in_=DD[:, 0:n], func=AF.Exp, bias=dtb[:, 0:1], scale=1.0), reads=[DD, dtb], writes=[DD])
        S.op("act", lambda e, n=n: e.activation(out=DD[:, 0:n], in_=DD[:, 0:n], func=AF.Ln, bias=1.0, scale=1.0), reads=[DD], writes=[DD])
        S.op("dve", lambda e, n=n: e.tensor_scalar(out=DD[32:64, 0:n], in0=DD[32:64, 0:n], scalar1=aneg[32:64, 0:1], scalar2=None, op0=ALU.mult), reads=[DD, aneg], writes=[DD])
        for j in range(n // 128):
            tkb = tk[j % 2]
            for q in range(3):
                ps = pss[pc[0] % 3]
                pc[0] += 1
                for cc in range(4):
                    c = q * 4 + cc
                    S.op("pe", lambda e, ps=ps, cc=cc, c=c, j=j: e.transpose(ps[:, cc * 128:(cc + 1) * 128], XC[c][:, j * 128:(j + 1) * 128], ident[:]), reads=[XC[c], ident], writes=[ps], inc=(cc == 3))
                evac(S, q, tkb[:, q * 512:(q + 1) * 512], ps[:], [ps], [tkb])
            S.dma(xbc_tok, xbc_tok[g0 + j * 128:g0 + (j + 1) * 128, :], tkb, tkb[:])
            S.op("pe", lambda e, j=j: e.transpose(pdd[:, :], DD[:, j * 128:(j + 1) * 128], ident[0:64, 0:64]), reads=[DD, ident], writes=[pdd])
            dkb = dk[j % 2]
            S.op("act", lambda e, dkb=dkb: e.copy(out=dkb[:], in_=pdd[:]), reads=[pdd], writes=[dkb])
            S.dma(dtda_tok, dtda_tok[g0 + j * 128:g0 + (j + 1) * 128, :], dkb, dkb[:])


def phase_ssscan(C, xbc_tok, BCT, dtda_tok, ytmp, K):
    S, cfg = C.S, C.cfg
    NB, T, TC = cfg.NB, cfg.T, cfg.TC
    Tt, TCt = T // 128, TC // 128
    XT = [S.sb("ss_xt%d" % i, [128, 1536]) for i in range(2)]
    DT = [S.sb("ss_dt%d" % i, [128, 64]) for i in range(2)]
    BC = [S.sb("ss_bc%d" % i, [128, 4, 128], BF16) for i in range(2)]
    miscPS = S.ps("ss_miscPS", [128, 512])
    cPS = View(miscPS, miscPS[:, 0:32])
    csTPS = View(miscPS, miscPS[0:8, 32:288].rearrange("p (g l) -> p g l", g=2))
    cbPS = View(miscPS, miscPS[:, 288:416])
    segPS = [S.ps("ss_segPS%d" % i, [128, 4, 128]) for i in range(2)]
    ydPS = S.ps("ss_ydPS", [128, 1024])
    yoPS = S.ps("ss_yoPS", [128, 512])
    dsPS = S.ps("ss_dsPS", [128, 512])
    c_sb = S.sb("ss_c", [128, 16])
    dfs = S.sb("ss_dfs", [128, 16])
    cdb = S.sb("ss_cdb", [128, 16])
    dte = S.sb("ss_dte", [128, 16])
    dtdte = S.sb("ss_dtdte", [128, 16])
    negcsT = S.sb("ss_negcsT", [8, 2, 128])
    csdiag = S.sb("ss_csdiag", [8, 2, 1024])
    xdt = S.sb("ss_xdt", [128, 16, 64], BF16)
    xdd = S.sb("ss_xdd", [128, 16, 64], BF16)
    btok = S.sb("ss_btok", [128, 2, 128], BF16)
    cbt = [S.sb("ss_cbt%d" % i, [128, 128], BF16) for i in range(2)]
    Eb = [S.sb("ss_E%d" % i, [128, 4, 128], BF16) for i in range(2)]
    Gb = [S.sb("ss_G%d" % i, [128, 4, 128], BF16) for i in range(2)]
    yacc = [S.sb("ss_yacc%d" % i, [128, 1024]) for i in range(2)]
    S32 = [S.sb("ss_S32_%d" % g, [128, 512]) for g in range(2)]
    Sbf = [S.sb("ss_Sbf_%d" % g, [128, 512], BF16) for g in range(2)]
    bcv = BCT.t.rearrange("(j p) n -> p j n", p=128)
    it = 0
    for d in range(2):
        tri = K["tri%d" % d]
        mneg = K["mneg%d" % d]
        for b in range(NB):
            for g in range(2):
                S.op("pool", lambda e, g=g: e.memset(S32[g][:], 0.0), writes=[S32[g]])
                S.op("pool", lambda e, g=g: e.memset(Sbf[g][:], 0.0), writes=[Sbf[g]])
            order = list(range(Tt)) if d == 0 else (list(range(TCt - 1, -1, -1)) + list(range(Tt - 1, TCt - 1, -1)))
            for tt in order:
                g0 = b * T + tt * 128
                xt, dtt, bc = XT[it % 2], DT[it % 2], BC[it % 2]
                ya = yacc[it % 2]
                it += 1
                S.dma(xt, xt[:], xbc_tok, xbc_tok[g0:g0 + 128, :])
                S.dma(dtt, dtt[:], dtda_tok, dtda_tok[g0:g0 + 128, :])
                S.dma(bc, bc[:], BCT, bcv[:, :, g0:g0 + 128])
                da = dtt[:, 32 + d * 16:32 + d * 16 + 16]
                dtd = dtt[:, d * 16:d * 16 + 16]
                S.op("pe", lambda e, da=da, tri=tri: e.matmul(cPS[:, 0:16], tri[:], da, start=True, stop=True), reads=[tri, dtt], writes=[cPS])
                S.op("pe", lambda e, da=da: e.matmul(cPS[:, 16:32], K["ones"][:], da, start=True, stop=True), reads=[K["ones"], dtt], writes=[cPS])
                for g in range(2):
                    S.op("pe", lambda e, g=g, dtt=dtt, d=d, tri=tri: e.matmul(csTPS[:, g, :], dtt[:, 32 + d * 16 + g * 8:32 + d * 16 + g * 8 + 8], tri[:], start=True, stop=True),
                         reads=[tri, dtt], writes=[csTPS])
                S.op("act", lambda e: e.copy(out=c_sb[:], in_=cPS[:, 0:16]), reads=[cPS], writes=[c_sb])
                S.op("act", lambda e: e.activation(out=dfs[:], in_=cPS[:, 0:16], func=AF.Exp), reads=[cPS], writes=[dfs])
                S.op("act", lambda e: e.activation(out=cdb[:], in_=cPS[:, 16:32], func=AF.Exp), reads=[cPS], writes=[cdb])
                S.op("dve", lambda e: e.tensor_tensor(out=dte[:], in0=cPS[:, 16:32], in1=c_sb[:], op=ALU.subtract), reads=[cPS, c_sb], writes=[dte])
                S.op("act", lambda e: e.activation(out=dte[:], in_=dte[:], func=AF.Exp), reads=[dte], writes=[dte])
                S.op("dve", lambda e, dtd=dtd: e.tensor_tensor(out=dtdte[:], in0=dte[:], in1=dtd, op=ALU.mult), reads=[dte, dtt], writes=[dtdte])
                S.op("act", lambda e: e.mul(out=negcsT[:], in_=csTPS[:], mul=-1.0), reads=[csTPS], writes=[negcsT])
                for g in range(2):
                    S.op("dve", lambda e, g=g: e.tensor_tensor(out=csdiag[:, g, :].rearrange("p (h l) -> p h l", h=8), in0=K["blk"][:].rearrange("p (h l) -> p h l", h=8),
                                                              in1=csTPS[:, g, :].unsqueeze(1).to_broadcast([8, 8, 128]), op=ALU.mult), reads=[K["blk"], csTPS], writes=[csdiag])
                xs3 = xt[:, 0:1024].rearrange("p (h v) -> p h v", h=16)
                S.op("dve", lambda e, xs3=xs3, dtd=dtd: e.tensor_tensor(out=xdt[:], in0=xs3, in1=dtd.unsqueeze(2).to_broadcast([128, 16, 64]), op=ALU.mult), reads=[xt, dtt], writes=[xdt])
                S.op("pool", lambda e, xs3=xs3: e.tensor_tensor(out=xdd[:], in0=xs3, in1=dtdte[:].unsqueeze(2).to_broadcast([128, 16, 64]), op=ALU.mult), reads=[xt, dtdte], writes=[xdd])
                S.op("pool", lambda e, xt=xt: e.tensor_copy(out=btok[:], in_=xt[:, 1024:1280].rearrange("p (g n) -> p g n", g=2)), reads=[xt], writes=[btok])
                si = 0
                for g in range(2):
                    S.op("pe", lambda e, g=g, bc=bc: e.matmul(cbPS[:], bc[:, g, :], bc[:, 2 + g, :], start=True, stop=True), reads=[bc], writes=[cbPS])
                    cb_ = cbt[g]
                    S.op("act", lambda e, cb_=cb_: e.copy(out=cb_[:], in_=cbPS[:]), reads=[cbPS], writes=[cb_])
                    for half in range(2):
                        sp = segPS[si % 2]
                        E, G = Eb[si % 2], Gb[si % 2]
                        si += 1
                        S.op("pe", lambda e, sp=sp, g=g, half=half: e.matmul(sp[:].rearrange("p h l -> p (h l)"), K["ones8"][:], csdiag[:, g, half * 512:(half + 1) * 512], start=True, stop=False),
                             reads=[K["ones8"], csdiag], writes=[sp], inc=False)
                        S.op("pe", lambda e, sp=sp, g=g, half=half: e.matmul(sp[:].rearrange("p h l -> p (h l)"), negcsT[:, g, :], K["blk"][:, half * 512:(half + 1) * 512], start=False, stop=False),
                             reads=[negcsT, K["blk"]], writes=[sp], inc=False)
                        S.op("pe", lambda e, sp=sp, mneg=mneg: e.matmul(sp[:].rearrange("p h l -> p (h l)"), K["identb"][:], mneg[:], start=False, stop=True),
                             reads=[K["identb"], mneg], writes=[sp])
                        S.op("act", lambda e, sp=sp, E=E: e.activation(out=E[:], in_=sp[:], func=AF.Exp), reads=[sp], writes=[E])
                        S.op("dve", lambda e, E=E, G=G, cb_=cb_: e.tensor_tensor(out=G[:], in0=E[:], in1=cb_[:].unsqueeze(1).to_broadcast([128, 4, 128]), op=ALU.mult), reads=[E, cb_], writes=[G])
                        for hh in range(4):
                            h = g * 8 + half * 4 + hh
                            S.op("pe", lambda e, G=G, hh=hh, h=h: e.matmul(ydPS[:, h * 64:(h + 1) * 64], G[:, hh, :], xdt[:, h, :], start=True, stop=True), reads=[G, xdt], writes=[ydPS], inc=(hh == 3))
                    S.op("pe", lambda e, g=g, bc=bc: e.matmul(yoPS[:], bc[:, 2 + g, :], Sbf[g][:], start=True, stop=True), reads=[bc, Sbf[g]], writes=[yoPS])
                    S.op("dve", lambda e, g=g, ya=ya: e.tensor_tensor(out=ya[:, g * 512:(g + 1) * 512].rearrange("p (h v) -> p h v", h=8), in0=yoPS[:].rearrange("p (h v) -> p h v", h=8),
                                                                   in1=dfs[:, g * 8:(g + 1) * 8].unsqueeze(2).to_broadcast([128, 8, 64]), op=ALU.mult), reads=[yoPS, dfs], writes=[ya])
                    S.op("pe", lambda e, g=g: e.matmul(dsPS[:], btok[:, g, :], xdd[:, g * 8:(g + 1) * 8, :].rearrange("p h v -> p (h v)"), start=True, stop=True), reads=[btok, xdd], writes=[dsPS])
                    S.op("pool", lambda e, g=g: e.tensor_tensor(out=S32[g][:].rearrange("p (h v) -> p h v", h=8), in0=S32[g][:].rearrange("p (h v) -> p h v", h=8),
                                                               in1=cdb[:, g * 8:(g + 1) * 8].unsqueeze(2).to_broadcast([128, 8, 64]), op=ALU.mult), reads=[S32[g], cdb], writes=[S32[g]])
                    S.op("dve", lambda e, g=g: e.tensor_tensor(out=S32[g][:], in0=S32[g][:], in1=dsPS[:], op=ALU.add), reads=[S32[g], dsPS], writes=[S32[g]])
                    S.op("act", lambda e, g=g: e.copy(out=Sbf[g][:], in_=S32[g][:]), reads=[S32[g]], writes=[Sbf[g]])
                S.op("dve", lambda e, ya=ya: e.tensor_tensor(out=ya[:], in0=ya[:], in1=ydPS[:], op=ALU.add), reads=[ya, ydPS], writes=[ya])
                S.dma(ytmp[d], ytmp[d][g0:g0 + 128, :], ya, ya[:])


def phase_sspost(C, ytmp, xbc_tok, colsT, P, mixT, ident):
    S, cfg = C.S, C.cfg
    dsk = S.sb("so_dsk", [128, 16])
    S.dma(dsk, dsk[:], P["ssm_d"], P["ssm_d"][0, :].partition_broadcast(128))
    nw = S.sb("so_nw", [128, 1024])
    S.dma(nw, nw[:], P["ssm_norm_w"], P["ssm_norm_w"][0, :].partition_broadcast(128))
    Y1 = [S.sb("so_y1_%d" % i, [128, 1024]) for i in range(2)]
    Y2 = [S.sb("so_y2_%d" % i, [128, 1024]) for i in range(2)]
    XS = [S.sb("so_xs_%d" % i, [128, 1024]) for i in range(2)]
    ZT = [S.sb("so_zt_%d" % i, [128, 8, 128]) for i in range(2)]
    zs = S.sb("so_zs", [128, 1024])
    junk = S.sb("so_junk", [128, 512], BF16)
    ss = S.sb("so_ss", [128, 2])
    pz = S.ps("so_pz", [128, 1024])
    po = S.ps("so_po", [128, 8, 128])
    ob = [S.sb("so_ob%d" % i, [128, 8, 128], BF16) for i in range(2)]
    zv = colsT.t[1920:2944, :].rearrange("(c p) n -> p c n", p=128)
    mv = mixT.t[512:1536, :].rearrange("(c p) n -> p c n", p=128)
    for ti in range(cfg.NTOK // 128):
        g0 = ti * 128
        y1, y2, xs, zt = Y1[ti % 2], Y2[ti % 2], XS[ti % 2], ZT[ti % 2]
        S.dma(y1, y1[:], ytmp[0], ytmp[0][g0:g0 + 128, :])
        S.dma(y2, y2[:], ytmp[1], ytmp[1][g0:g0 + 128, :])
        S.dma(xs, xs[:], xbc_tok, xbc_tok[g0:g0 + 128, 0:1024])
        S.dma(zt, zt[:], colsT, zv[:, :, g0:g0 + 128])
        for c in range(8):
            S.op("pe", lambda e, c=c, zt=zt: e.transpose(pz[:, c * 128:(c + 1) * 128], zt[:, c, :], ident[:]), reads=[zt, ident], writes=[pz], inc=(c == 7))
        S.op("act", lambda e: e.activation(out=zs[:], in_=pz[:], func=AF.Silu), reads=[pz], writes=[zs])
        S.op("dve", lambda e, y1=y1, y2=y2: e.tensor_tensor(out=y1[:], in0=y1[:], in1=y2[:], op=ALU.add), reads=[y1, y2], writes=[y1])
        S.op("pool", lambda e, xs=xs: e.tensor_tensor(out=xs[:].rearrange("p (h v) -> p h v", h=16), in0=xs[:].rearrange("p (h v) -> p h v", h=16),
                                                    in1=dsk[:].unsqueeze(2).to_broadcast([128, 16, 64]), op=ALU.mult), reads=[xs, dsk], writes=[xs])
        S.op("dve", lambda e, y1=y1, xs=xs: e.tensor_tensor(out=y1[:], in0=y1[:], in1=xs[:], op=ALU.add), reads=[y1, xs], writes=[y1])
        S.op("dve", lambda e, y1=y1: e.tensor_tensor(out=y1[:], in0=y1[:], in1=zs[:], op=ALU.mult), reads=[y1, zs], writes=[y1])
        for gI in range(2):
            S.op("act", lambda e, y1=y1, gI=gI: e.activation(out=junk[:], in_=y1[:, gI * 512:(gI + 1) * 512], func=AF.Square, accum_out=ss[:, gI:gI + 1]), reads=[y1], writes=[junk, ss])
        S.op("act", lambda e: e.activation(out=ss[:], in_=ss[:], func=AF.Sqrt, bias=NORM_EPS, scale=1.0 / 512), reads=[ss], writes=[ss])
        S.op("dve", lambda e: e.reciprocal(out=ss[:], in_=ss[:]), reads=[ss], writes=[ss])
        S.op("dve", lambda e, y1=y1: e.tensor_tensor(out=y1[:].rearrange("p (g v) -> p g v", g=2), in0=y1[:].rearrange("p (g v) -> p g v", g=2),
                                                    in1=ss[:].unsqueeze(2).to_broadcast([128, 2, 512]), op=ALU.mult), reads=[y1, ss], writes=[y1])
        S.op("pool", lambda e, y1=y1: e.tensor_tensor(out=y1[:], in0=y1[:], in1=nw[:], op=ALU.mult), reads=[y1, nw], writes=[y1])
        for c in range(8):
            S.op("pe", lambda e, c=c, y1=y1: e.transpose(po[:, c, :], y1[:, c * 128:(c + 1) * 128], ident[:]), reads=[y1, ident], writes=[po], inc=(c == 7))
        o = ob[ti % 2]
        S.op("act", lambda e, o=o: e.copy(out=o[:, 0:4, :], in_=po[:, 0:4, :]), reads=[po], writes=[o])
        S.op("dve", lambda e, o=o: e.tensor_copy(out=o[:, 4:8, :], in_=po[:, 4:8, :]), reads=[po], writes=[o])
        S.dma(mixT, mv[:, :, g0:g0 + 128], o, o[:])


def gate_tiles(C, name, modv, layer, idx):
    S, cfg = C.S, C.cfg
    D = cfg.D
    out = []
    for r in range(cfg.NB + 1):
        t = S.sb("%s_%d" % (name, r), [128, D])
        S.dma(t, t[:], modv, modv[layer, r, idx * D:(idx + 1) * D].partition_broadcast(128))
        out.append(t)
    return out


def tile_row(cfg, tile_idx):
    tpb = cfg.T // 128
    b, tt = divmod(tile_idx, tpb)
    return cfg.NB if tt < cfg.TC // 128 else b


def lat_tiles(cfg):
    tpb = cfg.T // 128
    return [b * tpb + tt for b in range(cfg.NB) for tt in range(cfg.TC // 128, tpb)]


def phase_outproj(C, name, src, src_tokmajor, kdim, W_buf, W_ap, modv, layer, xin, xout, tiles, ident_bf):
    S, cfg = C.S, C.cfg
    kch = kdim // 128
    wb = load_w_bf16(C, name + "_w", W_buf, W_ap.rearrange("(k p) c -> p k c", p=128), 1024, kch)
    gt = gate_tiles(C, name + "_gt", modv, layer, 2)
    mt_ = [S.sb("%s_m%d" % (name, i), [128, kch, 128], BF16) for i in range(2)]
    if src_tokmajor:
        tk_ = [S.sb("%s_tk%d" % (name, i), [128, kdim], BF16) for i in range(2)]
        ptb = S.ps(name + "_ptb", [128, kch, 128], BF16)
    xt_ = [S.sb("%s_x%d" % (name, i), [128, 1024]) for i in range(2)]
    ot_ = [S.sb("%s_o%d" % (name, i), [128, 1024]) for i in range(2)]
    ps_ = [S.ps("%s_ps%d" % (name, i), [128, 1024]) for i in range(2)]
    if not src_tokmajor:
        sv = src.t.rearrange("(k p) n -> p k n", p=128)
    for i, ti in enumerate(tiles):
        g0 = ti * 128
        m, xt, ot, ps = mt_[i % 2], xt_[i % 2], ot_[i % 2], ps_[i % 2]
        if src_tokmajor:
            tk = tk_[i % 2]
            S.dma(tk, tk[:], src, src[g0:g0 + 128, :])
            for k in range(kch):
                S.op("pe", lambda e, k=k, tk=tk: e.transpose(ptb[:, k, :], tk[:, k * 128:(k + 1) * 128], ident_bf[:]), reads=[tk, ident_bf], writes=[ptb], inc=(k == kch - 1))
            evac(S, i, m[:], ptb[:], [ptb], [m])
        else:
            S.dma(m, m[:], src, sv[:, :, g0:g0 + 128])
        S.dma(xt, xt[:], xin, xin[g0:g0 + 128, :])
        for half in range(2):
            for k in range(kch):
                S.op("pe", lambda e, k=k, half=half, m=m, ps=ps: e.matmul(ps[:, half * 512:(half + 1) * 512], m[:, k, :], wb[:, k, half * 512:(half + 1) * 512], start=(k == 0), stop=(k == kch - 1)),
                     reads=[m, wb], writes=[ps], inc=(k == kch - 1))
        g = gt[tile_row(cfg, ti)]
        S.op("dve", lambda e, ps=ps, ot=ot, g=g: e.tensor_tensor(out=ot[:], in0=ps[:], in1=g[:], op=ALU.mult), reads=[ps, g], writes=[ot])
        S.op("pool", lambda e, ot=ot, xt=xt: e.tensor_tensor(out=ot[:], in0=ot[:], in1=xt[:], op=ALU.add), reads=[ot, xt], writes=[ot])
        S.dma(xout, xout[g0:g0 + 128, :], ot, ot[:])


def phase_final(C, xin, fn_buf, out):
    S, cfg = C.S, C.cfg
    D = cfg.D
    g = S.sb("fn_g", [128, D])
    S.dma(g, g[:], fn_buf, fn_buf.t.partition_broadcast(128))
    xt_ = [S.sb("fn_x%d" % i, [128, D]) for i in range(2)]
    ot_ = [S.sb("fn_o%d" % i, [128, D]) for i in range(2)]
    junk = S.sb("fn_junk", [128, D], BF16)
    ss_ = [S.sb("fn_ss%d" % i, [128, 1]) for i in range(2)]
    tpl = cfg.TL // 128
    for i, ti in enumerate(lat_tiles(cfg)):
        xt, ot, ss = xt_[i % 2], ot_[i % 2], ss_[i % 2]
        S.dma(xt, xt[:], xin, xin[ti * 128:(ti + 1) * 128, :])
        S.op("act", lambda e, xt=xt, ss=ss: e.activation(out=junk[:], in_=xt[:], func=AF.Square, accum_out=ss[:]), reads=[xt], writes=[junk, ss])
        S.op("act", lambda e, ss=ss: e.activation(out=ss[:], in_=ss[:], func=AF.Sqrt, bias=NORM_EPS, scale=1.0 / D), reads=[ss], writes=[ss])
        S.op("dve", lambda e, ss=ss: e.reciprocal(out=ss[:], in_=ss[:]), reads=[ss], writes=[ss])
        S.op("dve", lambda e, xt=xt, ot=ot, ss=ss: e.scalar_tensor_tensor(out=ot[:], in0=xt[:], scalar=ss[:, 0:1], in1=g[:], op0=ALU.mult, op1=ALU.mult), reads=[xt, ss, g], writes=[ot])
        S.dma(out, out[i * 128:(i + 1) * 128, :], ot, ot[:])


def phase_moe_cast(C, layer, EW, wbf):
    S = C.S
    st = [S.sb("mc_st%d" % i, [128, 6144]) for i in range(2)]
    ob = [S.sb("mc_ob%d" % i, [128, 6144], BF16) for i in range(2)]
    for e in range(65):
        s_, o_ = st[e % 2], ob[e % 2]
        if e < 64:
            w1, w3, w2 = EW["exp_w1"][layer, e], EW["exp_w3"][layer, e], EW["exp_w2"][layer, e]
            b1, b3, b2 = EW["exp_w1"], EW["exp_w3"], EW["exp_w2"]
        else:
            w1, w3, w2 = EW["sh_w1"][layer], EW["sh_w3"][layer], EW["sh_w2"][layer]
            b1, b3, b2 = EW["sh_w1"], EW["sh_w3"], EW["sh_w2"]
        S.dma(s_, s_[:, 0:2048].rearrange("p (k f) -> p k f", k=8), b1, w1.rearrange("(k p) f -> p k f", p=128))
        S.dma(s_, s_[:, 2048:4096].rearrange("p (k f) -> p k f", k=8), b3, w3.rearrange("(k p) f -> p k f", p=128))
        S.dma(s_, s_[:, 4096:6144].rearrange("p (j f) -> p j f", j=2), b2, w2.rearrange("(j p) f -> p j f", p=128))
        S.op("act", lambda e_, s_=s_, o_=o_: e_.copy(out=o_[:, 0:2048], in_=s_[:, 0:2048]), reads=[s_], writes=[o_])
        S.op("dve", lambda e_, s_=s_, o_=o_: e_.tensor_copy(out=o_[:, 2048:4096], in_=s_[:, 2048:4096]), reads=[s_], writes=[o_])
        S.op("pool", lambda e_, s_=s_, o_=o_: e_.tensor_copy(out=o_[:, 4096:6144], in_=s_[:, 4096:6144]), reads=[s_], writes=[o_])
        S.dma(wbf, wbf[e], o_, o_[:])


def phase_moe_route(C, layer, xin, gvec_buf, gvec_ap, modv, rw_buf, rb_buf, hT, gates, tiles, ident):
    S, cfg = C.S, C.cfg
    mt = ModTiles(C, "mr_mt", modv, layer, 3, 4, gvec_ap, gvec_buf)
    nt = NormT(C, "mr_nt", ident)
    rw = S.sb("mr_rw", [128, 8, 64])
    S.dma(rw, rw[:], rw_buf, rw_buf[layer].rearrange("(k p) e -> p k e", p=128))
    rb = S.sb("mr_rb", [128, 64])
    S.dma(rb, rb[:], rb_buf, rb_buf[layer, :].partition_broadcast(128))
    hb_ = [S.sb("mr_hb%d" % i, [128, 8, 128], BF16) for i in range(2)]
    h32_ = [S.sb("mr_h32%d" % i, [128, 8, 128]) for i in range(2)]
    lg = S.ps("mr_lg", [128, 64])
    sc = S.sb("mr_sc", [128, 64])
    sel = S.sb("mr_sel", [128, 64])
    eq = S.sb("mr_eq", [128, 64])
    m1 = S.sb("mr_m1", [128, 8])
    m2 = S.sb("mr_m2", [128, 8])
    t8 = S.sb("mr_t8", [128, 8])
    pen = S.sb("mr_pen", [128, 8])
    ws = S.sb("mr_ws", [128, 1])
    gt_ = [S.sb("mr_gt%d" % i, [128, 65]) for i in range(2)]
    for g in gt_:
        S.op("pool", lambda e, g=g: e.memset(g[:, 64:65], 1.0), writes=[g])
    hv = hT.t.rearrange("(k p) n -> p k n", p=128)

    def v3(b):
        return b[:].rearrange("p (g i) -> p g i", g=8)
    for i, ti in enumerate(tiles):
        g0 = ti * 128
        hb, h32, gt = hb_[i % 2], h32_[i % 2], gt_[i % 2]
        nt.tile(xin, g0, mt, tile_row(cfg, ti), [(hb, lambda k0, k1, hb=hb: hb[:, k0:k1, :]), (h32, lambda k0, k1, h32=h32: h32[:, k0:k1, :])])
        S.dma(hT, hv[:, :, g0:g0 + 128], hb, hb[:])
        for k in range(8):
            S.op("pe", lambda e, k=k, h32=h32: e.matmul(lg[:], h32[:, k, :], rw[:, k, :], start=(k == 0), stop=(k == 7)), reads=[h32, rw], writes=[lg], inc=(k == 7))
        S.op("act", lambda e: e.activation(out=sc[:], in_=lg[:], func=AF.Sigmoid), reads=[lg], writes=[sc])
        S.op("dve", lambda e: e.tensor_tensor(out=sel[:], in0=sc[:], in1=rb[:], op=ALU.add), reads=[sc, rb], writes=[sel])
        S.op("dve", lambda e: e.reduce_max(out=m1[:], in_=v3(sel), axis=AX.X), reads=[sel], writes=[m1])
        S.op("dve", lambda e: e.tensor_tensor(out=v3(eq), in0=v3(sel), in1=m1[:].unsqueeze(2).to_broadcast([128, 8, 8]), op=ALU.is_equal), reads=[sel, m1], writes=[eq])
        S.op("dve", lambda e: e.scalar_tensor_tensor(out=eq[:], in0=eq[:], scalar=-1e30, in1=sel[:], op0=ALU.mult, op1=ALU.add), reads=[eq, sel], writes=[eq])
        S.op("dve", lambda e: e.reduce_max(out=m2[:], in_=v3(eq), axis=AX.X), reads=[eq], writes=[m2])
        S.op("dve", lambda e: e.tensor_tensor(out=m1[:], in0=m1[:], in1=m2[:], op=ALU.add), reads=[m1, m2], writes=[m1])
        S.op("dve", lambda e: e.max(out=t8[:], in_=m1[:]), reads=[m1], writes=[t8])
        S.op("dve", lambda e: e.tensor_scalar(out=pen[:], in0=m1[:], scalar1=t8[:, 3:4], scalar2=None, op0=ALU.is_ge), reads=[m1, t8], writes=[pen])
        S.op("dve", lambda e: e.tensor_scalar(out=pen[:], in0=pen[:], scalar1=1e30, scalar2=-1e30, op0=ALU.mult, op1=ALU.add), reads=[pen], writes=[pen])
        S.op("dve", lambda e: e.tensor_tensor(out=v3(sel), in0=v3(sel), in1=pen[:].unsqueeze(2).to_broadcast([128, 8, 8]), op=ALU.add), reads=[sel, pen], writes=[sel])
        S.op("dve", lambda e: e.max(out=t8[:], in_=sel[:]), reads=[sel], writes=[t8])
        S.op("dve", lambda e: e.tensor_scalar(out=eq[:], in0=sel[:], scalar1=t8[:, 5:6], scalar2=None, op0=ALU.is_ge), reads=[sel, t8], writes=[eq])
        S.op("dve", lambda e: e.tensor_tensor(out=eq[:], in0=eq[:], in1=sc[:], op=ALU.mult), reads=[eq, sc], writes=[eq])
        S.op("dve", lambda e: e.reduce_sum(out=ws[:], in_=eq[:], axis=AX.X), reads=[eq], writes=[ws])
        S.op("dve", lambda e: e.reciprocal(out=ws[:], in_=ws[:]), reads=[ws], writes=[ws])
        S.op("dve", lambda e, gt=gt: e.tensor_scalar(out=gt[:, 0:64], in0=eq[:], scalar1=ws[:, 0:1], scalar2=ROUTED_SCALE, op0=ALU.mult, op1=ALU.mult), reads=[eq, ws], writes=[gt])
        S.dma(gates, gates[g0:g0 + 128, :], gt, gt[:])


ROUTED_SCALE = 2.5


def phase_moe_experts(C, layer, hT, gates, wbf, modv, xin, xout, tiles):
    S, cfg = C.S, C.cfg
    TSU = 8
    gtl = gate_tiles(C, "me_gt", modv, layer, 5)
    hs = S.sb("me_hs", [128, 8, TSU * 128], BF16)
    gs = S.sb("me_gs", [128, TSU, 65])
    acc = S.sb("me_acc", [128, TSU, 1024])
    wb_ = [S.sb("me_w%d" % i, [128, 6144], BF16) for i in range(3)]
    h1_ = [S.ps("me_h1_%d" % i, [128, 512]) for i in range(2)]
    h3_ = [S.ps("me_h3_%d" % i, [128, 512]) for i in range(2)]
    op_ = [S.ps("me_o%d" % i, [128, 512]) for i in range(3)]
    s1_ = [S.sb("me_s1_%d" % i, [128, 512]) for i in range(2)]
    hid_ = [S.sb("me_hid%d" % i, [128, 2, 512], BF16) for i in range(2)]
    xt_ = [S.sb("me_x%d" % i, [128, 1024]) for i in range(2)]
    hv = hT.t.rearrange("(k p) n -> p k n", p=128)
    nst = (len(tiles) + TSU - 1) // TSU
    cnt = 0
    oc = 0
    for st in range(nst):
        tl = tiles[st * TSU:(st + 1) * TSU]
        nt_ = len(tl)
        for j, ti in enumerate(tl):
            S.dma(hs, hs[:, :, j * 128:(j + 1) * 128], hT, hv[:, :, ti * 128:(ti + 1) * 128])
            S.dma(gs, gs[:, j, :], gates, gates[ti * 128:(ti + 1) * 128, :])
        S.op("pool", lambda e: e.memset(acc[:], 0.0), writes=[acc])
        for e_ in range(65):
            wb = wb_[cnt % 3]
            S.dma(wb, wb[:], wbf, wbf[e_])
            for tb in range((nt_ + 3) // 4):
                ntok = min(4, nt_ - tb * 4) * 128
                hid = hid_[cnt % 2]
                for j in range(2):
                    h1, h3, s1 = h1_[j], h3_[j], s1_[j]
                    for k in range(8):
                        S.op("pe", lambda e, k=k, j=j, wb=wb, h1=h1, tb=tb, ntok=ntok: e.matmul(h1[:, 0:ntok], wb[:, k * 256 + j * 128:k * 256 + (j + 1) * 128], hs[:, k, tb * 512:tb * 512 + ntok], start=(k == 0), stop=(k == 7)),
                             reads=[wb, hs], writes=[h1], inc=(k == 7))
                    for k in range(8):
                        S.op("pe", lambda e, k=k, j=j, wb=wb, h3=h3, tb=tb, ntok=ntok: e.matmul(h3[:, 0:ntok], wb[:, 2048 + k * 256 + j * 128:2048 + k * 256 + (j + 1) * 128], hs[:, k, tb * 512:tb * 512 + ntok], start=(k == 0), stop=(k == 7)),
                             reads=[wb, hs], writes=[h3], inc=(k == 7))
                    S.op("act", lambda e, h1=h1, s1=s1, ntok=ntok: e.activation(out=s1[:, 0:ntok], in_=h1[:, 0:ntok], func=AF.Silu), reads=[h1], writes=[s1])
                    S.op("dve", lambda e, h3=h3, s1=s1, hid=hid, j=j, ntok=ntok: e.tensor_tensor(out=hid[:, j, 0:ntok], in0=h3[:, 0:ntok], in1=s1[:, 0:ntok], op=ALU.mult), reads=[h3, s1], writes=[hid])
                for q in range(ntok // 128):
                    tj = tb * 4 + q
                    for half in range(2):
                        o = op_[oc % 3]
                        oc += 1
                        for j in range(2):
                            S.op("pe", lambda e, o=o, hid=hid, j=j, q=q, half=half, wb=wb: e.matmul(o[:], hid[:, j, q * 128:(q + 1) * 128], wb[:, 4096 + j * 1024 + half * 512:4096 + j * 1024 + (half + 1) * 512], start=(j == 0), stop=(j == 1)),
                                 reads=[hid, wb], writes=[o], inc=(j == 1))
                        S.op("dve", lambda e, o=o, tj=tj, half=half, e_=e_: e.scalar_tensor_tensor(out=acc[:, tj, half * 512:(half + 1) * 512], in0=o[:], scalar=gs[:, tj, e_:e_ + 1], in1=acc[:, tj, half * 512:(half + 1) * 512], op0=ALU.mult, op1=ALU.add),
                             reads=[o, gs, acc], writes=[acc])
                cnt += 1
        for j, ti in enumerate(tl):
            xt = xt_[j % 2]
            S.dma(xt, xt[:], xin, xin[ti * 128:(ti + 1) * 128, :])
            g = gtl[tile_row(cfg, ti)]
            S.op("pool", lambda e, j=j, g=g: e.tensor_tensor(out=acc[:, j, :], in0=acc[:, j, :], in1=g[:], op=ALU.mult), reads=[acc, g], writes=[acc])
            S.op("pool", lambda e, j=j, xt=xt: e.tensor_tensor(out=xt[:], in0=acc[:, j, :], in1=xt[:], op=ALU.add), reads=[acc, xt], writes=[xt])
            S.dma(xout, xout[ti * 128:(ti + 1) * 128, :], xt, xt[:])


def phase_mla_prep(C, layer, xin, gvec_buf, gvec_ap, modv, P, rope_cs, KT, QT, Vtok, ident, identb):
    S, cfg = C.S, C.cfg
    TCt = cfg.TC // 128
    tpb = cfg.T // 128
    mt = ModTiles(C, "mp_mt", modv, layer, 0, 1, gvec_ap, gvec_buf)
    nt = NormT(C, "mp_nt", ident)
    win = load_w_bf16(C, "mp_win", P["mla_w_in"], P["mla_w_in"][0].rearrange("(k p) c -> p k c", p=128), 672, 8)
    qup = load_w_bf16(C, "mp_qup", P["mla_q_up"], P["mla_q_up"][0].rearrange("(k p) c -> p k c", p=128), 1536, 3)
    kvup = load_w_bf16(C, "mp_kvup", P["mla_kv_up"], P["mla_kv_up"][0].rearrange("(k p) c -> p k c", p=128), 2048, 2)
    nb = S.sb("mp_nb", [128, 640])
    S.dma(nb, nb[:, 0:384], P["mla_q_norm"], P["mla_q_norm"][0, :].partition_broadcast(128))
    S.dma(nb, nb[:, 384:640], P["mla_kv_norm"], P["mla_kv_norm"][0, :].partition_broadcast(128))
    hb_ = [S.sb("mp_hb%d" % i, [128, 8, 128], BF16) for i in range(2)]
    A_ = S.ps("mp_A", [128, 1024])
    B_ = S.ps("mp_B", [128, 1024])
    Cp = S.ps("mp_C", [96, 16, 128], BF16)
    c_sb = S.sb("mp_c", [128, 672])
    junk = S.sb("mp_junk", [128, 384], BF16)
    ss = S.sb("mp_ss", [128, 2])
    cn = S.sb("mp_cn", [128, 640])
    cnT = S.sb("mp_cnT", [128, 5, 128], BF16)
    vt_ = [S.sb("mp_vt%d" % i, [128, 16, 64], BF16) for i in range(2)]
    Kf = S.sb("mp_Kf", [128, 16, 96], BF16)
    Qf = S.sb("mp_Qf", [128, 16, 96], BF16)
    q32 = S.sb("mp_q32", [128, 16, 96])
    cs_ = [S.sb("mp_cs%d" % i, [128, 32]) for i in range(2)]
    krr = S.sb("mp_krr", [128, 32])
    t1 = S.sb("mp_t1", [128, 16, 16])
    t2 = S.sb("mp_t2", [128, 16, 16])
    kT_ = [S.sb("mp_kT%d" % i, [96, 16, 128], BF16) for i in range(2)]
    qT_ = [S.sb("mp_qT%d" % i, [96, 16, 128], BF16) for i in range(2)]
    ktv = KT.t.rearrange("h d n -> d h n")
    qtv = QT.t.rearrange("h d n -> d h n")
    for ti in range(cfg.NTOK // 128):
        b, tt = divmod(ti, tpb)
        lat = tt >= TCt
        g0 = ti * 128
        hb, vt, kT, qT, cs = hb_[ti % 2], vt_[ti % 2], kT_[ti % 2], qT_[ti % 2], cs_[ti % 2]
        nt.tile(xin, g0, mt, tile_row(cfg, ti), [(hb, lambda k0, k1, hb=hb: hb[:, k0:k1, :])])
        for (c0, c1) in ((0, 512), (512, 672)):
            for k in range(8):
                S.op("pe", lambda e, k=k, c0=c0, c1=c1, hb=hb: e.matmul(B_[:, c0:c1], hb[:, k, :], win[:, k, c0:c1], start=(k == 0), stop=(k == 7)), reads=[hb, win], writes=[B_], inc=(k == 7))
        S.op("act", lambda e: e.copy(out=c_sb[:, 0:512], in_=B_[:, 0:512]), reads=[B_], writes=[c_sb])
        S.op("dve", lambda e: e.tensor_copy(out=c_sb[:, 512:672], in_=B_[:, 512:672]), reads=[B_], writes=[c_sb])
        S.op("act", lambda e: e.activation(out=junk[:, 0:384], in_=c_sb[:, 0:384], func=AF.Square, accum_out=ss[:, 0:1]), reads=[c_sb], writes=[junk, ss])
        S.op("act", lambda e: e.activation(out=junk[:, 0:256], in_=c_sb[:, 384:640], func=AF.Square, accum_out=ss[:, 1:2]), reads=[c_sb], writes=[junk, ss])
        S.op("act", lambda e: e.activation(out=ss[:, 0:1], in_=ss[:, 0:1], func=AF.Sqrt, bias=NORM_EPS, scale=1.0 / 384), reads=[ss], writes=[ss])
        S.op("act", lambda e: e.activation(out=ss[:, 1:2], in_=ss[:, 1:2], func=AF.Sqrt, bias=NORM_EPS, scale=1.0 / 256), reads=[ss], writes=[ss])
        S.op("dve", lambda e: e.reciprocal(out=ss[:], in_=ss[:]), reads=[ss], writes=[ss])
        S.op("dve", lambda e: e.scalar_tensor_tensor(out=cn[:, 0:384], in0=c_sb[:, 0:384], scalar=ss[:, 0:1], in1=nb[:, 0:384], op0=ALU.mult, op1=ALU.mult), reads=[c_sb, ss, nb], writes=[cn])
        S.op("dve", lambda e: e.scalar_tensor_tensor(out=cn[:, 384:640], in0=c_sb[:, 384:640], scalar=ss[:, 1:2], in1=nb[:, 384:640], op0=ALU.mult, op1=ALU.mult), reads=[c_sb, ss, nb], writes=[cn])
        Bv = B_[:, 0:640].rearrange("p (k n) -> p k n", k=5)
        for k in range(5):
            S.op("pe", lambda e, k=k, Bv=Bv: e.transpose(Bv[:, k, :], cn[:, k * 128:(k + 1) * 128], ident[:]), reads=[cn, ident], writes=[B_], inc=(k == 4))
        S.op("act", lambda e, Bv=Bv: e.copy(out=cnT[:], in_=Bv), reads=[B_], writes=[cnT])
        for ps_ in range(2):
            for blk in range(2):
                for kc in range(2):
                    S.op("pe", lambda e, ps_=ps_, blk=blk, kc=kc: e.matmul(A_[:, blk * 512:(blk + 1) * 512], cnT[:, 3 + kc, :], kvup[:, kc, ps_ * 1024 + blk * 512:ps_ * 1024 + (blk + 1) * 512], start=(kc == 0), stop=(kc == 1)),
                         reads=[cnT, kvup], writes=[A_], inc=(kc == 1))
            Av = A_[:].rearrange("p (h x) -> p h x", h=8)
            S.op("act", lambda e, ps_=ps_, Av=Av, vt=vt: e.copy(out=vt[:, ps_ * 8:(ps_ + 1) * 8, :], in_=Av[:, :, 64:128]), reads=[A_], writes=[vt])
            S.op("dve", lambda e, ps_=ps_, Av=Av: e.tensor_copy(out=Kf[:, ps_ * 8:(ps_ + 1) * 8, 0:64], in_=Av[:, :, 0:64]), reads=[A_], writes=[Kf])
        S.dma(Vtok, Vtok[g0:g0 + 128, :], vt, vt[:].rearrange("p h v -> p (h v)"))
        if lat:
            p0 = (tt - TCt) * 128
            S.dma(cs, cs[:], rope_cs, rope_cs[p0:p0 + 128, :])
            u1, u2 = c_sb[:, 640:656], c_sb[:, 656:672]
            S.op("dve", lambda e, cs=cs, u1=u1: e.tensor_tensor(out=t1[:, 0, :], in0=u1, in1=cs[:, 0:16], op=ALU.mult), reads=[c_sb, cs], writes=[t1])
            S.op("dve", lambda e, cs=cs, u2=u2: e.tensor_tensor(out=t2[:, 0, :], in0=u2, in1=cs[:, 16:32], op=ALU.mult), reads=[c_sb, cs], writes=[t2])
            S.op("dve", lambda e: e.tensor_tensor(out=krr[:, 0:16], in0=t1[:, 0, :], in1=t2[:, 0, :], op=ALU.subtract), reads=[t1, t2], writes=[krr])
            S.op("dve", lambda e, cs=cs, u1=u1: e.tensor_tensor(out=t1[:, 0, :], in0=u1, in1=cs[:, 16:32], op=ALU.mult), reads=[c_sb, cs], writes=[t1])
            S.op("dve", lambda e, cs=cs, u2=u2: e.tensor_tensor(out=t2[:, 0, :], in0=u2, in1=cs[:, 0:16], op=ALU.mult), reads=[c_sb, cs], writes=[t2])
            S.op("dve", lambda e: e.tensor_tensor(out=krr[:, 16:32], in0=t1[:, 0, :], in1=t2[:, 0, :], op=ALU.add), reads=[t1, t2], writes=[krr])
        else:
            S.op("dve", lambda e: e.tensor_copy(out=krr[:], in_=c_sb[:, 640:672]), reads=[c_sb], writes=[krr])
        S.op("dve", lambda e: e.tensor_copy(out=Kf[:, :, 64:96], in_=krr[:].unsqueeze(1).to_broadcast([128, 16, 32])), reads=[krr], writes=[Kf])
        for h in range(16):
            S.op("pe", lambda e, h=h: e.transpose(Cp[:, h, :], Kf[:, h, :], identb[:]), reads=[Kf, identb], writes=[Cp], inc=(h == 15))
        S.op("act", lambda e, kT=kT: e.copy(out=kT[:], in_=Cp[:]), reads=[Cp], writes=[kT])
        S.dma(KT, ktv[:, :, g0:g0 + 128], kT, kT[:])
        if lat:
            for ps_ in range(2):
                for (c0, c1) in ((0, 512), (512, 768)):
                    for kc in range(3):
                        S.op("pe", lambda e, ps_=ps_, c0=c0, c1=c1, kc=kc: e.matmul(A_[:, c0:c1], cnT[:, kc, :], qup[:, kc, ps_ * 768 + c0:ps_ * 768 + c1], start=(kc == 0), stop=(kc == 2)),
                             reads=[cnT, qup], writes=[A_], inc=(kc == 2))
                S.op("act", lambda e, ps_=ps_: e.copy(out=q32[:, ps_ * 8:(ps_ + 1) * 8, :], in_=A_[:, 0:768].rearrange("p (h x) -> p h x", h=8)), reads=[A_], writes=[q32])
            S.op("pool", lambda e: e.tensor_copy(out=Qf[:, :, 0:64], in_=q32[:, :, 0:64]), reads=[q32], writes=[Qf])
            U1, U2 = q32[:, :, 64:80], q32[:, :, 80:96]
            cosb = cs[:, 0:16].unsqueeze(1).to_broadcast([128, 16, 16])
            sinb = cs[:, 16:32].unsqueeze(1).to_broadcast([128, 16, 16])
            S.op("dve", lambda e, U1=U1, cosb=cosb: e.tensor_tensor(out=t1[:], in0=U1, in1=cosb, op=ALU.mult), reads=[q32, cs], writes=[t1])
            S.op("dve", lambda e, U2=U2, sinb=sinb: e.tensor_tensor(out=t2[:], in0=U2, in1=sinb, op=ALU.mult), reads=[q32, cs], writes=[t2])
            S.op("dve", lambda e: e.tensor_tensor(out=Qf[:, :, 64:80], in0=t1[:], in1=t2[:], op=ALU.subtract), reads=[t1, t2], writes=[Qf])
            S.op("dve", lambda e, U1=U1, sinb=sinb: e.tensor_tensor(out=t1[:], in0=U1, in1=sinb, op=ALU.mult), reads=[q32, cs], writes=[t1])
            S.op("dve", lambda e, U2=U2, cosb=cosb: e.tensor_tensor(out=t2[:], in0=U2, in1=cosb, op=ALU.mult), reads=[q32, cs], writes=[t2])
            S.op("dve", lambda e: e.tensor_tensor(out=Qf[:, :, 80:96], in0=t1[:], in1=t2[:], op=ALU.add), reads=[t1, t2], writes=[Qf])
            for h in range(16):
                S.op("pe", lambda e, h=h: e.transpose(Cp[:, h, :], Qf[:, h, :], identb[:]), reads=[Qf, identb], writes=[Cp], inc=(h == 15))
            S.op("act", lambda e, qT=qT: e.copy(out=qT[:], in_=Cp[:]), reads=[Cp], writes=[qT])
            S.dma(QT, qtv[:, :, g0:g0 + 128], qT, qT[:])


def phase_mla_attn(C, KT, QT, Vtok, AO):
    S, cfg = C.S, C.cfg
    NB, T, TC, TL = cfg.NB, cfg.T, cfg.TC, cfg.TL
    Tt = T // 128
    scale = (64 + 32) ** -0.5
    Ks_ = [S.sb("at_K%d" % i, [96, T], BF16) for i in range(2)]
    Qs_ = [S.sb("at_Q%d" % i, [96, TL], BF16) for i in range(2)]
    Vs_ = [S.sb("at_V%d" % i, [128, Tt, 65], BF16) for i in range(2)]
    for v in Vs_:
        S.op("pool", lambda e, v=v: e.memset(v[:, :, 64:65], 1.0), writes=[v])
    sp_ = [S.ps("at_s%d" % i, [128, 512]) for i in range(3)]
    E_ = [S.sb("at_E%d" % i, [128, 512], BF16) for i in range(3)]
    acc_ = [S.ps("at_acc%d" % i, [128, 4, 128]) for i in range(2)]
    rc = S.sb("at_rc", [128, 4, 1])
    ob_ = [S.sb("at_o%d" % i, [128, 4, 64], BF16) for i in range(2)]
    n = 0
    m = 0
    for b in range(NB):
        for h in range(16):
            Ks, Qs, Vs = Ks_[n % 2], Qs_[n % 2], Vs_[n % 2]
            n += 1
            S.dma(Ks, Ks[:], KT, KT[h, :, b * T:(b + 1) * T])
            S.dma(Qs, Qs[:], QT, QT[h, :, b * T + TC:(b + 1) * T])
            S.dma(Vs, Vs[:, :, 0:64], Vtok, Vtok[b * T:(b + 1) * T, h * 64:(h + 1) * 64].rearrange("(k p) d -> p k d", p=128))
            for qb in range(TL // 512):
                acc = acc_[qb % 2]
                for kt in range(Tt):
                    sp, E = sp_[m % 3], E_[m % 3]
                    m += 1
                    S.op("pe", lambda e, sp=sp, Ks=Ks, Qs=Qs, kt=kt, qb=qb: e.matmul(sp[:], Ks[:, kt * 128:(kt + 1) * 128], Qs[:, qb * 512:(qb + 1) * 512], start=True, stop=True), reads=[Ks, Qs], writes=[sp])
                    S.op("act", lambda e, sp=sp, E=E: e.activation(out=E[:], in_=sp[:], func=AF.Exp, scale=scale), reads=[sp], writes=[E])
                    for i in range(4):
                        S.op("pe", lambda e, acc=acc, E=E, Vs=Vs, kt=kt, i=i: e.matmul(acc[:, i, 0:65], E[:, i * 128:(i + 1) * 128], Vs[:, kt, :], start=(kt == 0 and i == 0), stop=(kt == Tt - 1), skip_group_check=True),
                             reads=[E, Vs], writes=[acc], inc=(i == 3))
                S.op("dve", lambda e, acc=acc: e.reciprocal(out=rc[:], in_=acc[:, :, 64:65]), reads=[acc], writes=[rc])
                ob = ob_[qb % 2]
                S.op("dve", lambda e, acc=acc, ob=ob: e.tensor_tensor(out=ob[:], in0=acc[:, :, 0:64], in1=rc[:].to_broadcast([128, 4, 64]), op=ALU.mult), reads=[acc, rc], writes=[ob])
                r0 = b * T + TC + qb * 512
                S.dma(AO, AO[r0:r0 + 512, h * 64:(h + 1) * 64].rearrange("(i p) d -> p i d", p=128), ob, ob[:])


import ml_dtypes

N_CORES = 8
PARAM_SHAPES = dict(
    c=None, c_ctx=[1024], ada_w=[2, 1024, 6144], ada_b=[2, 6144], norm_mix=[2, 1024], norm_ffn=[2, 1024],
    ev_w_in=[1, 1024, 4512], ev_w_out=[1, 1536, 1024], rw_mu=[1, 1920], rw_w0=[1, 2, 512], rw_w2=[1, 2, 64, 512],
    rw_a0=[1, 2, 512], rw_a2=[1, 2, 64, 512], rw_g2=[1, 128, 512], rw_kk=[1, 512], rw_ka=[1, 512], rw_rk=[1, 512],
    rw_gn_w=[1, 512], rw_gn_b=[1, 512], ssm_conv_w=[1, 5, 1536], ssm_conv_b=[1, 1536], ssm_dt_bias=[1, 2, 16],
    ssm_a_log=[1, 2, 16], ssm_d=[1, 16], ssm_norm_w=[1, 1024], mla_w_in=[1, 1024, 672], mla_q_norm=[1, 384],
    mla_q_up=[1, 384, 1536], mla_kv_norm=[1, 256], mla_kv_up=[1, 256, 2048], mla_w_out=[1, 1024, 1024],
    router_w=[2, 1024, 64], router_bias=[2, 64], exp_w1=[2, 64, 1024, 256], exp_w3=[2, 64, 1024, 256],
    exp_w2=[2, 64, 256, 1024], sh_w1=[2, 1024, 256], sh_w3=[2, 1024, 256], sh_w2=[2, 256, 1024], final_norm=[1024])


def host_consts(cfg):
    K = {}
    bf = ml_dtypes.bfloat16
    K["ident"] = np.eye(128, dtype=np.float32)
    K["identb"] = np.eye(128).astype(bf)
    K["onesblk"] = np.kron(np.eye(2), np.ones((64, 64))).astype(np.float32)
    NQ = 8 * cfg.NB * 64 // 512
    Eq = np.zeros((128, NQ, 2 * NQ), np.float32)
    for p in range(128):
        for q in range(NQ):
            Eq[p, q, 2 * q + p // 64] = 1
    K["Eq"] = Eq
    l = np.arange(128)
    K["tri0"] = (l[:, None] <= l[None, :]).astype(np.float32)
    K["tri1"] = (l[:, None] >= l[None, :]).astype(np.float32)
    K["ones"] = np.ones((128, 128), np.float32)
    blk = np.zeros((8, 8, 128), np.float32)
    for h in range(8):
        blk[h, h, :] = 1
    K["blk"] = blk.reshape(8, 1024)
    K["ones8"] = np.ones((8, 128), np.float32)
    m0 = np.where(l[None, :] >= l[:, None], 0.0, -30000.0)
    m1 = np.where(l[None, :] <= l[:, None], 0.0, -30000.0)
    K["mneg0"] = np.tile(m0[:, None, :], (1, 4, 1)).reshape(128, 512).astype(bf)
    K["mneg1"] = np.tile(m1[:, None, :], (1, 4, 1)).reshape(128, 512).astype(bf)
    rows = cfg.TL // 64
    r_idx, c_idx = np.meshgrid(np.arange(rows), np.arange(64), indexing='ij')
    r_idx = r_idx.reshape(-1).astype(np.float32)
    c_idx = c_idx.reshape(-1).astype(np.float32)
    inv_freq = (10000.0 ** (-np.arange(0, 16, 2, dtype=np.float32) / 16)).astype(np.float32)
    ang = np.concatenate([r_idx[:, None] * inv_freq, c_idx[:, None] * inv_freq], -1).astype(np.float32)
    K["rope_cs"] = np.concatenate([np.cos(ang), np.sin(ang)], 1).astype(np.float32)
    return K


def build_program(cfg, kconst, dbg=()):
    nc = bass.Bass("TRN2", target_bir_lowering=False)
    NB, NTOK, T = cfg.NB, cfg.NTOK, cfg.T
    with ExitStack() as es:
        S = Sched(nc, es)
        io = {k: "ExternalInput" for k in PARAM_SHAPES}
        io.update(xin="ExternalInput", out="ExternalOutput")
        io.update({"K_" + k: "ExternalInput" for k in kconst})
        io.update({k: "ExternalOutput" for k in dbg})
        C = Ctx(S, cfg, io)
        P = {}
        for k, shp in PARAM_SHAPES.items():
            P[k] = C.D(k, [NB, 1024] if k == "c" else shp)
        xin = C.D("xin", [NTOK, 1024])
        out = C.D("out", [NB * cfg.TL, 1024])
        K = {}
        for k, v in kconst.items():
            dt_ = BF16 if v.dtype == ml_dtypes.bfloat16 else F32
            dd = C.D("K_" + k, list(v.shape), dt_)
            if k == "rope_cs":
                K[k] = dd
                continue
            sbt = S.sb("Ksb_" + k, list(v.shape), dt_)
            S.dma(sbt, sbt[:], dd, dd.t)
            K[k] = sbt
        ident, identb, onesblk = K["ident"], K["identb"], K["onesblk"]
        NQ = 8 * NB * 64 // 512
        modv = C.D("modv", [2, NB + 1, 6144])
        colsT = C.D("colsT", [4512, NTOK])
        A = {k: C.D("A_" + k, [512, NTOK]) for k in ["kk", "r", "bonus", "g", "w0", "w1", "b0", "b1", "kd0", "kd1"]}
        vtok = C.D("vtok", [NTOK, 512], BF16)
        YD = C.D("YD", [2 * NQ, T, 512])
        mixT = C.D("mixT", [1536, NTOK], BF16)
        xbc_tok = C.D("xbc_tok", [NTOK, 1536])
        BCT = C.D("BCT", [512, NTOK], BF16)
        dtda = C.D("dtda_tok", [NTOK, 64])
        ytmp = [C.D("ytmp%d" % d, [NTOK, 1024]) for d in range(2)]
        xr = [C.D("xr%d" % i, [NTOK, 1024]) for i in range(4)]
        hT = C.D("hT", [1024, NTOK], BF16)
        gates = C.D("gates", [NTOK, 65])
        wbf = C.D("wbf", [65, 128, 6144], BF16)
        KT = C.D("KT", [16, 96, NTOK], BF16)
        QT = C.D("QT", [16, 96, NTOK], BF16)
        Vtok = C.D("Vtok", [NTOK, 1024], BF16)
        AO = C.D("AO", [NTOK, 1024], BF16)
        EW = {k: P[k] for k in ("exp_w1", "exp_w3", "exp_w2", "sh_w1", "sh_w3", "sh_w2")}
        all_tiles = list(range(NTOK // 128))
        lt = lat_tiles(cfg)
        with S.phase():
            phase_mod(C, P["c"], P["c_ctx"], P["ada_w"], P["ada_b"], modv)
        with S.phase():
            phase_normproj(C, "np0", xin, P["norm_mix"], P["norm_mix"][0, :], modv, 0, P["ev_w_in"], P["ev_w_in"][0], 4512, colsT, ident)
        with S.phase():
            phase_rwprep(C, colsT, P, A, onesblk, ident, vtok)
        with S.phase():
            phase_rwscan(C, A, vtok, YD, onesblk, identb, K["Eq"])
        with S.phase():
            phase_rwpost(C, YD, A, P, mixT, ident)
        with S.phase():
            phase_ssprep(C, colsT, P, xbc_tok, BCT, dtda, ident)
        with S.phase():
            phase_ssscan(C, xbc_tok, BCT, dtda, ytmp, K)
        with S.phase():
            phase_sspost(C, ytmp, xbc_tok, colsT, P, mixT, ident)
        with S.phase():
            phase_outproj(C, "op0", mixT, False, 1536, P["ev_w_out"], P["ev_w_out"][0], modv, 0, xin, xr[0], all_tiles, identb)
        with S.phase():
            phase_moe_cast(C, 0, EW, wbf)
        with S.phase():
            phase_moe_route(C, 0, xr[0], P["norm_ffn"], P["norm_ffn"][0, :], modv, P["router_w"], P["router_bias"], hT, gates, all_tiles, ident)
        with S.phase():
            phase_moe_experts(C, 0, hT, gates, wbf, modv, xr[0], xr[1], all_tiles)
        with S.phase():
            phase_mla_prep(C, 1, xr[1], P["norm_mix"], P["norm_mix"][1, :], modv, P, K["rope_cs"], KT, QT, Vtok, ident, identb)
        with S.phase():
            phase_mla_attn(C, KT, QT, Vtok, AO)
        with S.phase():
            phase_outproj(C, "op1", AO, True, 1024, P["mla_w_out"], P["mla_w_out"][0], modv, 1, xr[1], xr[2], lt, identb)
        with S.phase():
            phase_moe_cast(C, 1, EW, wbf)
        with S.phase():
            phase_moe_route(C, 1, xr[2], P["norm_ffn"], P["norm_ffn"][1, :], modv, P["router_w"], P["router_bias"], hT, gates, lt, ident)
        with S.phase():
            phase_moe_experts(C, 1, hT, gates, wbf, modv, xr[2], xr[3], lt)
        with S.phase():
            phase_final(C, xr[3], P["final_norm"], out)
        S.emit(final_bufs=[out] + [b for b in S.bufs if b.space == "dram" and b.name in dbg])
        build_program.stats = (S.nins, S.nsem)
        build_program.marks = S.marks
    return nc


def kernel(**inputs):
    cfg = Cfg(4, 2048, 256)
    kconst = host_consts(cfg)
    nc = build_program(cfg, kconst)
    x = np.asarray(inputs["x"], np.float32)
    ctx = np.asarray(inputs["ctx"], np.float32)
    c = np.asarray(inputs["c"], np.float32)
    in_maps = []
    shared = {k: np.ascontiguousarray(np.asarray(inputs[k], np.float32)) for k in PARAM_SHAPES if k != "c"}
    shared.update({"K_" + k: v for k, v in kconst.items()})
    for i in range(N_CORES):
        sl = slice(i * cfg.NB, (i + 1) * cfg.NB)
        m = dict(shared)
        m["xin"] = np.ascontiguousarray(np.concatenate([ctx[sl], x[sl]], axis=1).reshape(cfg.NTOK, 1024))
        m["c"] = np.ascontiguousarray(c[sl])
        in_maps.append(m)
    res = run_bass_kernel_spmd(nc, in_maps, core_ids=list(range(N_CORES)))
    outs = [np.asarray(r["out"]).reshape(cfg.NB, cfg.TL, 1024) for r in res.results]
    return np.concatenate(outs, axis=0).astype(np.float32)
```

```python
import numpy as np
import concourse.bass as bass
import concourse.mybir as mybir
from concourse.bass_utils import run_bass_kernel_spmd
from contextlib import ExitStack

F32 = mybir.dt.float32
BF16 = mybir.dt.bfloat16
I32 = mybir.dt.int32
U32 = mybir.dt.uint32
ALU = mybir.AluOpType
AF = mybir.ActivationFunctionType
AX = mybir.AxisListType

ENGS = ("pe", "act", "dve", "pool", "sp")


class Buf:
    def __init__(self, S, name, t, space):
        self.S = S
        self.name = name
        self.t = t
        self.space = space
        self.w = []
        self.r = []
        self.dsem = None
        self.dcnt = 0

    def __getitem__(self, k):
        return self.t[k]

    def ap(self):
        return self.t.ap() if hasattr(self.t, "ap") else self.t[:]


class View:
    def __init__(self, buf, ap):
        self.buf, self.t = buf, ap

    def __getitem__(self, k):
        return self.t[k]


def _b(x):
    return x.buf if isinstance(x, View) else x


class _Phase:
    def __init__(self, S):
        self.S = S

    def __enter__(self):
        S = self.S
        self.prev = S.cur
        self.nb = len(S.bufs)
        self.prev_ds = S.phase_dsems
        S.phase_dsems = []
        self.es = ExitStack()
        self.es.__enter__()
        S.cur = self.es
        return self

    def __exit__(self, *a):
        S = self.S
        S.marks.append({e: sum(1 for o in S.ops[e] if o[1] is not None) for e in ENGS})
        S.barrier()
        S.free_dsems.extend(S.phase_dsems)
        S.phase_dsems = self.prev_ds
        S.bufs = S.bufs[:self.nb] + [b for b in S.bufs[self.nb:] if b.space == "dram"]
        S.cur = self.prev
        self.es.__exit__(*a)
        return False


class Sched:
    def __init__(self, nc, es):
        self.nc = nc
        self.es = es
        self.ops = {e: [] for e in ENGS}
        self.sems = {}
        self.eng_sem = {}
        self.eng_cnt = {e: 0 for e in ENGS}
        self.seen = {e: {} for e in ENGS}
        self.nsem = 0
        self.nins = 0
        self.cur = es
        self.bufs = []
        self.sem_val = {}
        self.free_dsems = []
        self.phase_dsems = []
        self.pending = {e: False for e in ENGS}
        self.marks = []
        for e in ("pe", "act", "dve", "pool"):
            self.eng_sem[e] = self.new_sem("prog_" + e)

    def new_sem(self, name):
        h = self.es.enter_context(self.nc.semaphore(name))
        sid = self.nsem
        self.nsem += 1
        self.sems[sid] = h
        return sid

    def sb(self, name, shape, dt=F32):
        self.uid = getattr(self, "uid", 0) + 1
        name = "%s_u%d" % (name, self.uid)
        t = self.cur.enter_context(self.nc.sbuf_tensor(name, list(shape), dt))
        b = Buf(self, name, t, "sb")
        self.bufs.append(b)
        return b

    def ps(self, name, shape, dt=F32):
        self.uid = getattr(self, "uid", 0) + 1
        name = "%s_u%d" % (name, self.uid)
        t = self.cur.enter_context(self.nc.psum_tensor(name, list(shape), dt))
        b = Buf(self, name, t, "ps")
        self.bufs.append(b)
        return b

    def dram(self, name, shape, dt=F32, kind="Internal"):
        t = self.nc.dram_tensor(name, list(shape), dt, kind=kind).ap()
        b = Buf(self, name, t, "dram")
        self.bufs.append(b)
        return b

    def alloc_dsem(self, name):
        if self.free_dsems:
            sid = self.free_dsems.pop()
        else:
            sid = self.new_sem("dma%d" % self.nsem)
            self.sem_val[sid] = 0
        self.phase_dsems.append(sid)
        return sid

    def barrier(self):
        assert not any(self.pending.values()), "un-signalled PE group at barrier"
        targets = []
        for e in ("pe", "act", "dve", "pool"):
            if self.eng_cnt[e]:
                targets.append((self.eng_sem[e], self.eng_cnt[e]))
        for sid, v in self.sem_val.items():
            if v:
                targets.append((sid, v))
        for e in ENGS:
            waits = []
            for (sid, v) in targets:
                if self.seen[e].get(sid, 0) < v:
                    self.seen[e][sid] = v
                    waits.append((sid, v))
            if waits:
                self.ops[e].append((waits, None, None, 0))
        for b in self.bufs:
            b.w = []
            b.r = []

    def phase(self):
        return _Phase(self)

    def _waits(self, eng, reads, writes):
        need = {}
        for b in reads:
            for (s, v) in b.w:
                need[s] = max(need.get(s, 0), v)
            if b.space == "ps":
                for (s, v) in b.r:
                    need[s] = max(need.get(s, 0), v)
        for b in writes:
            if not (b.space == "dram" and eng in ("sp", "pool_dma", "act_dma")):
                for (s, v) in b.w:
                    need[s] = max(need.get(s, 0), v)
            for (s, v) in b.r:
                need[s] = max(need.get(s, 0), v)
        out = []
        seen = self.seen[eng]
        for s, v in need.items():
            if seen.get(s, 0) >= v:
                continue
            seen[s] = v
            out.append((s, v))
        return out

    def _record(self, ev, reads, writes):
        for b in writes:
            b.w = [ev]
            b.r = []
        for b in reads:
            if b in writes:
                continue
            b.r = [(s, v) for (s, v) in b.r if s != ev[0]] + [ev]

    def op(self, eng, fn, reads=(), writes=(), inc=True, skip_self=False):
        reads = [_b(x) for x in reads]
        writes = [_b(x) for x in writes]
        waits = self._waits(eng, reads, writes)
        if eng == "pe" or skip_self:
            waits = [(s_, v_) for (s_, v_) in waits if s_ != self.eng_sem[eng]]
        if inc:
            self.eng_cnt[eng] += 1
            ev = (self.eng_sem[eng], self.eng_cnt[eng])
            self.pending[eng] = False
        else:
            assert eng == "pe"
            ev = (self.eng_sem[eng], self.eng_cnt[eng] + 1)
            self.pending[eng] = True
        self.ops[eng].append((waits, fn, ev, 1 if inc else 0))
        self._record(ev, reads, writes)
        self.nins += 1

    def dma(self, out_buf, out_ap, in_buf, in_ap, eng="sp", **kw):
        out_buf, in_buf = _b(out_buf), _b(in_buf)
        own = out_buf if out_buf.space != "dram" else in_buf
        if own.dsem is None:
            own.dsem = self.alloc_dsem(own.name)
            own.dcnt = self.sem_val[own.dsem]
        waits = self._waits(eng, [in_buf], [out_buf])
        same_gen = (out_buf.space != "dram" and not out_buf.r and out_buf.w and all(s_ == own.dsem for (s_, _) in out_buf.w))
        if (not same_gen) and own.dcnt > 0 and self.seen[eng].get(own.dsem, 0) < own.dcnt:
            self.seen[eng][own.dsem] = own.dcnt
            waits.append((own.dsem, own.dcnt))
        own.dcnt += 16
        self.sem_val[own.dsem] = own.dcnt
        ev = (own.dsem, own.dcnt)

        def fn(e, out_ap=out_ap, in_ap=in_ap, kw=kw):
            return e.dma_start(out=out_ap, in_=in_ap, **kw)
        self.ops[eng].append((waits, fn, ev, 16))
        if out_buf.space == "dram":
            out_buf.w = [(s, v) for (s, v) in out_buf.w if s != ev[0]] + [ev]
            out_buf.r = []
            in_buf.r = [(s, v) for (s, v) in in_buf.r if s != ev[0]] + [ev]
        else:
            self._record(ev, [in_buf], [out_buf])
        self.nins += 1

    def reset_dram(self, b):
        b.w = []
        b.r = []

    def emit(self, final_bufs=()):
        nc = self.nc
        fw = {}
        for b in final_bufs:
            for (s, v) in b.w:
                fw[s] = max(fw.get(s, 0), v)
        for e in ("pe", "act", "dve", "pool"):
            if self.eng_cnt[e]:
                fw[self.eng_sem[e]] = self.eng_cnt[e]
        sems = self.sems
        ops = self.ops
        with nc.Block() as block:
            def run(engobj, lst):
                for (waits, fn, ev, inc) in lst:
                    for (s, v) in waits:
                        engobj.wait_ge(sems[s], v)
                    if fn is not None:
                        ins = fn(engobj)
                        if inc:
                            ins.then_inc(sems[ev[0]], inc)

            @block.sync
            def _(e):
                run(e, ops["sp"])
                for s, v in fw.items():
                    e.wait_ge(sems[s], v)

            @block.tensor
            def _(e):
                run(e, ops["pe"])

            @block.scalar
            def _(e):
                run(e, ops["act"])

            @block.vector
            def _(e):
                run(e, ops["dve"])

            @block.gpsimd
            def _(e):
                run(e, ops["pool"])


class Cfg:
    def __init__(self, NB=4, TL=2048, TC=256):
        self.NB, self.TL, self.TC = NB, TL, TC
        self.T = TL + TC
        self.NTOK = NB * self.T
        self.D = 1024


class Ctx:
    def __init__(self, S, cfg, io):
        self.S, self.cfg, self.io = S, cfg, io
        self.rr = 0

    def D(self, name, shape, dt=F32):
        kind = self.io.get(name, "Internal")
        return self.S.dram(name, shape, dt, kind=kind)


def alt(i):
    return "act" if i % 2 == 0 else "dve"


def evac(S, i, out_ap, in_ap, reads, writes):
    if i % 2 == 0:
        S.op("act", lambda e: e.copy(out=out_ap, in_=in_ap), reads=reads, writes=writes)
    else:
        S.op("dve", lambda e: e.tensor_copy(out=out_ap, in_=in_ap), reads=reads, writes=writes)


def phase_mod(C, c_in, cctx_in, ada_w, ada_b, modv):
    S, cfg = C.S, C.cfg
    NB = cfg.NB
    R = NB + 1
    cT = S.sb("mod_cT", [128, 8, R])
    for b in range(NB):
        S.dma(cT, cT[:, :, b:b + 1], c_in, c_in[b, :].rearrange("(k p o) -> p k o", p=128, o=1), allow_slow_non_contiguous=True)
    S.dma(cT, cT[:, :, NB:R], cctx_in, cctx_in.t.rearrange("(k p o) -> p k o", p=128, o=1), allow_slow_non_contiguous=True)
    sT = S.sb("mod_sT", [128, 8, R])
    S.op("act", lambda e: e.activation(out=sT[:], in_=cT[:], func=AF.Silu), reads=[cT], writes=[sT])
    wbufs = [S.sb("mod_w%d" % i, [128, 8, 512]) for i in range(2)]
    pss = [S.ps("mod_ps%d" % i, [R, 512]) for i in range(2)]
    bias = S.sb("mod_bias", [R, 6144])
    orow = S.sb("mod_orow", [R, 6144])
    for l in range(2):
        S.dma(bias, bias[:], ada_b, ada_b[l, :].partition_broadcast(R))
        wv = ada_w[l].rearrange("(k p) c -> p k c", p=128)
        for j in range(12):
            wb = wbufs[j % 2]
            ps = pss[j % 2]
            S.dma(wb, wb[:], ada_w, wv[:, :, j * 512:(j + 1) * 512])
            for k in range(8):
                S.op("pe", lambda e, ps=ps, wb=wb, k=k: e.matmul(ps[:], sT[:, k, :], wb[:, k, :], start=(k == 0), stop=(k == 7)),
                     reads=[sT, wb], writes=[ps], inc=(k == 7))
            S.op("dve", lambda e, ps=ps, j=j: e.tensor_tensor(out=orow[:, j * 512:(j + 1) * 512], in0=ps[:], in1=bias[:, j * 512:(j + 1) * 512], op=ALU.add),
                 reads=[ps, bias], writes=[orow])
        S.dma(modv, modv[l], orow, orow[:])


class ModTiles:
    def __init__(self, C, name, modv, layer, shift_idx, scale_idx, gvec_ap, gvec_buf):
        S, cfg = C.S, C.cfg
        R = cfg.NB + 1
        D = cfg.D
        self.G = [S.sb("%s_G%d" % (name, r), [128, D]) for r in range(R)]
        self.Sh = [S.sb("%s_S%d" % (name, r), [128, D]) for r in range(R)]
        gb = S.sb("%s_g" % name, [128, D])
        S.dma(gb, gb[:], gvec_buf, gvec_ap.partition_broadcast(128))
        for r in range(R):
            G, Sh = self.G[r], self.Sh[r]
            S.dma(G, G[:], modv, modv[layer, r, scale_idx * D:(scale_idx + 1) * D].partition_broadcast(128))
            S.dma(Sh, Sh[:], modv, modv[layer, r, shift_idx * D:(shift_idx + 1) * D].partition_broadcast(128))
            S.op("dve", lambda e, G=G: e.scalar_tensor_tensor(out=G[:], in0=G[:], scalar=1.0, in1=gb[:], op0=ALU.add, op1=ALU.mult),
                 reads=[G, gb], writes=[G])

    def row(self, cfg, tile_idx):
        tpb = cfg.T // 128
        b, tt = divmod(tile_idx, tpb)
        return cfg.NB if tt < cfg.TC // 128 else b


class NormT:
    def __init__(self, C, name, ident, want32=False):
        S = C.S
        self.C, self.name, self.ident = C, name, ident
        D = C.cfg.D
        self.xt = [S.sb("%s_xt%d" % (name, i), [128, D]) for i in range(2)]
        self.junk = S.sb("%s_junk" % name, [128, D], BF16)
        self.ss = [S.sb("%s_ss%d" % (name, i), [128, 1]) for i in range(2)]
        self.h = [S.sb("%s_h%d" % (name, i), [128, D]) for i in range(2)]
        self.pt = [S.ps("%s_pt%d" % (name, i), [128, 4, 128]) for i in range(2)]
        self.n = 0

    def tile(self, xsrc, row0, mt, r, outs):
        S = self.C.S
        D = self.C.cfg.D
        i = self.n % 2
        self.n += 1
        xt, ss, h = self.xt[i], self.ss[i], self.h[i]
        S.dma(xt, xt[:], xsrc, xsrc[row0:row0 + 128, :])
        junk = self.junk
        S.op("act", lambda e: e.activation(out=junk[:], in_=xt[:], func=AF.Square, accum_out=ss[:]), reads=[xt], writes=[junk, ss])
        S.op("act", lambda e: e.activation(out=ss[:], in_=ss[:], func=AF.Sqrt, bias=NORM_EPS, scale=1.0 / D), reads=[ss], writes=[ss])
        S.op("dve", lambda e: e.reciprocal(out=ss[:], in_=ss[:]), reads=[ss], writes=[ss])
        G, Sh = mt.G[r], mt.Sh[r]
        S.op("dve", lambda e: e.scalar_tensor_tensor(out=h[:], in0=xt[:], scalar=ss[:, 0:1], in1=G[:], op0=ALU.mult, op1=ALU.mult),
             reads=[xt, ss, G], writes=[h])
        S.op("pool", lambda e: e.tensor_tensor(out=h[:], in0=h[:], in1=Sh[:], op=ALU.add), reads=[h, Sh], writes=[h])
        ident = self.ident
        for half in range(2):
            pt = self.pt[half]
            for kk in range(4):
                k = half * 4 + kk
                S.op("pe", lambda e, pt=pt, kk=kk, k=k: e.transpose(pt[:, kk, :], h[:, k * 128:(k + 1) * 128], ident[:]),
                     reads=[h, ident], writes=[pt], inc=(kk == 3))
            for oi, (ob, ofn) in enumerate(outs):
                evac(S, half + oi, ofn(half * 4, half * 4 + 4), pt[:], [pt], [ob])


NORM_EPS = 1e-6


def load_w_bf16(C, name, wsrc_buf, wv, ncols, kch):
    S = C.S
    wb = S.sb(name, [128, kch, ncols], BF16)
    CH = max(128, min(512, 2048 // kch))
    stg = [S.sb("%s_stg%d" % (name, i), [128, kch, CH]) for i in range(2)]
    nchunk = (ncols + CH - 1) // CH
    for j in range(nchunk):
        c0 = j * CH
        c1 = min(ncols, c0 + CH)
        st = stg[j % 2]
        S.dma(st, st[:, :, 0:c1 - c0], wsrc_buf, wv[:, :, c0:c1])
        S.op("pool", lambda e, st=st, c0=c0, c1=c1: e.tensor_copy(out=wb[:, :, c0:c1], in_=st[:, :, 0:c1 - c0]), reads=[st], writes=[wb])
    return wb


def phase_normproj(C, name, xsrc, gvec_buf, gvec_ap, modv, layer, W_buf, W_ap, ncols, outT, ident):
    S, cfg = C.S, C.cfg
    mt = ModTiles(C, name + "_mt", modv, layer, 0, 1, gvec_ap, gvec_buf)
    wb = load_w_bf16(C, name + "_w", W_buf, W_ap.rearrange("(k p) c -> p k c", p=128), ncols, 8)
    nt = NormT(C, name + "_nt", ident)
    hT = [S.sb("%s_hT%d" % (name, i), [128, 8, 512], BF16) for i in range(2)]
    pss = [S.ps("%s_ps%d" % (name, i), [128, 512]) for i in range(3)]
    ost = [S.sb("%s_ost%d" % (name, i), [128, 512]) for i in range(3)]
    ntl = cfg.NTOK // 128
    nsb = (ntl + 3) // 4
    ncj = (ncols + 127) // 128
    cnt = 0
    for sb in range(nsb):
        ht = hT[sb % 2]
        nti = min(4, ntl - sb * 4)
        n = nti * 128
        for ti in range(nti):
            tile_idx = sb * 4 + ti
            r = mt.row(cfg, tile_idx)
            nt.tile(xsrc, tile_idx * 128, mt, r, [(ht, lambda k0, k1, ti=ti, ht=ht: ht[:, k0:k1, ti * 128:(ti + 1) * 128])])
        for j in range(ncj):
            c0 = j * 128
            cw = min(128, ncols - c0)
            ps = pss[cnt % 3]
            ob = ost[cnt % 3]
            for k in range(8):
                S.op("pe", lambda e, ps=ps, k=k, c0=c0, cw=cw, ht=ht, n=n: e.matmul(ps[0:cw, 0:n], wb[:, k, c0:c0 + cw], ht[:, k, 0:n], start=(k == 0), stop=(k == 7)),
                     reads=[wb, ht], writes=[ps], inc=(k == 7))
            evac(S, cnt, ob[0:cw, 0:n], ps[0:cw, 0:n], [ps], [ob])
            S.dma(outT, outT[c0:c0 + cw, sb * 512:sb * 512 + n], ob, ob[0:cw, 0:n])
            cnt += 1


def seg_blocks(cfg, blk=512):
    out = []
    for b in range(cfg.NB):
        for (s0, s1) in ((0, cfg.TC), (cfg.TC, cfg.T)):
            t = s0
            while t < s1:
                n = min(blk, s1 - t)
                out.append((b, t, n, t > s0, t + n < s1))
                t += n
    return out


def vec_cols(C, name, src_buf, src_ap_1d, nch):
    S = C.S
    t = S.sb(name, [128, nch])
    S.dma(t, t[:], src_buf, src_ap_1d.rearrange("(c p) -> p c", p=128), allow_slow_non_contiguous=True)
    return t


def phase_rwprep(C, colsT, P, A, onesblk, ident, vtok):
    S, cfg = C.S, C.cfg
    T = cfg.T
    mu = vec_cols(C, "rp_mu", P["rw_mu"], P["rw_mu"][0, :], 15)
    w0 = [vec_cols(C, "rp_w0%d" % d, P["rw_w0"], P["rw_w0"][0, d, :], 4) for d in range(2)]
    a0 = [vec_cols(C, "rp_a0%d" % d, P["rw_a0"], P["rw_a0"][0, d, :], 4) for d in range(2)]
    kkv = vec_cols(C, "rp_kkv", P["rw_kk"], P["rw_kk"][0, :], 4)
    kav = vec_cols(C, "rp_kav", P["rw_ka"], P["rw_ka"][0, :], 4)
    rkv = vec_cols(C, "rp_rkv", P["rw_rk"], P["rw_rk"][0, :], 4)
    omka = S.sb("rp_omka", [128, 4])
    S.op("dve", lambda e: e.tensor_scalar(out=omka[:], in0=kav[:], scalar1=-1.0, scalar2=1.0, op0=ALU.mult, op1=ALU.add), reads=[kav], writes=[omka])
    W2 = S.sb("rp_W2", [128, 512])
    A2 = S.sb("rp_A2", [128, 512])
    G2 = S.sb("rp_G2", [128, 512])
    for d in range(2):
        S.dma(W2, W2[d * 64:(d + 1) * 64, :], P["rw_w2"], P["rw_w2"][0, d, :, :])
        S.dma(A2, A2[d * 64:(d + 1) * 64, :], P["rw_a2"], P["rw_a2"][0, d, :, :])
    S.dma(G2, G2[:], P["rw_g2"], P["rw_g2"][0, :, :])
    NB_ = 512
    raw = [S.sb("rp_raw%d" % i, [128, NB_ + 2]) for i in range(3)]
    MX = [S.sb("rp_mx%d" % c, [128, NB_]) for c in range(15)]
    tmp = [S.sb("rp_tmp%d" % i, [128, NB_]) for i in range(2)]
    TH = S.sb("rp_th", [128, NB_])
    SG = S.sb("rp_sg", [128, NB_])
    Aa = [[S.sb("rp_a%d_%d" % (d, cc), [128, NB_]) for cc in range(4)] for d in range(2)]
    KD = [[S.sb("rp_kd%d_%d" % (d, cc), [128, NB_]) for cc in range(4)] for d in range(2)]
    KK = [S.sb("rp_kk%d" % cc, [128, NB_]) for cc in range(4)]
    ost = [S.sb("rp_ost%d" % i, [128, NB_]) for i in range(4)]
    pss = [S.ps("rp_ps%d" % i, [128, NB_]) for i in range(4)]
    vtb = [S.sb("rp_vtb%d" % i, [128, 512], BF16) for i in range(2)]
    oc = [0]
    pc = [0]

    def nps():
        pc[0] += 1
        return pss[pc[0] % 4]

    def nost():
        oc[0] += 1
        return ost[oc[0] % 4]

    def store(arr, cc, b, t0, n, buf, ap):
        S.dma(arr, arr[cc * 128:(cc + 1) * 128, b * T + t0:b * T + t0 + n], buf, ap)

    for bi, (b, t0, n, hl, hr) in enumerate(seg_blocks(cfg, NB_)):
        g0 = b * T + t0
        for c in range(15):
            rw = raw[c % 3]
            if not hl:
                S.op("pool", lambda e, rw=rw: e.memset(rw[:, 0:1], 0.0), writes=[rw])
            if not hr:
                S.op("pool", lambda e, rw=rw, n=n: e.memset(rw[:, n + 1:n + 2], 0.0), writes=[rw])
            lo = g0 - (1 if hl else 0)
            hi = g0 + n + (1 if hr else 0)
            S.dma(rw, rw[:, (0 if hl else 1):(0 if hl else 1) + hi - lo], colsT, colsT[c * 128:(c + 1) * 128, lo:hi])
            tp = tmp[c % 2]
            mx = MX[c]
            S.op("dve", lambda e, rw=rw, tp=tp, n=n: e.tensor_tensor(out=tp[:, 0:n], in0=rw[:, 0:n], in1=rw[:, 2:n + 2], op=ALU.add), reads=[rw], writes=[tp])
            S.op("dve", lambda e, rw=rw, tp=tp, n=n: e.scalar_tensor_tensor(out=tp[:, 0:n], in0=tp[:, 0:n], scalar=0.5, in1=rw[:, 1:n + 1], op0=ALU.mult, op1=ALU.subtract),
                 reads=[rw, tp], writes=[tp])
            S.op("dve", lambda e, rw=rw, tp=tp, n=n, c=c, mx=mx: e.scalar_tensor_tensor(out=mx[:, 0:n], in0=tp[:, 0:n], scalar=mu[:, c:c + 1], in1=rw[:, 1:n + 1], op0=ALU.mult, op1=ALU.add),
                 reads=[rw, tp, mu], writes=[mx])
        for cc in range(4):
            store(A["r"], cc, b, t0, n, MX[cc], MX[cc][:, 0:n])
        for j in range(n // 128):
            ps = nps()
            for cc in range(4):
                S.op("pe", lambda e, ps=ps, cc=cc, j=j: e.transpose(ps[:, cc * 128:(cc + 1) * 128], MX[8 + cc][:, j * 128:(j + 1) * 128], ident[:]),
                     reads=[MX[8 + cc], ident], writes=[ps], inc=(cc == 3))
            vb = vtb[j % 2]
            evac(S, j, vb[:], ps[:, 0:512], [ps], [vb])
            S.dma(vtok, vtok[g0 + j * 128:g0 + (j + 1) * 128, :], vb, vb[:])
        for cc in range(4):
            kk = KK[cc]
            tp = tmp[cc % 2]
            S.op("dve", lambda e, cc=cc, kk=kk, n=n: e.tensor_scalar(out=kk[:, 0:n], in0=MX[4 + cc][:, 0:n], scalar1=kkv[:, cc:cc + 1], scalar2=None, op0=ALU.mult),
                 reads=[MX[4 + cc], kkv], writes=[kk])
            S.op("pool", lambda e, kk=kk, tp=tp, n=n: e.tensor_tensor(out=tp[:, 0:n], in0=kk[:, 0:n], in1=kk[:, 0:n], op=ALU.mult), reads=[kk], writes=[tp])
            ps = nps()
            S.op("pe", lambda e, ps=ps, tp=tp, n=n: e.matmul(ps[:, 0:n], onesblk[:], tp[:, 0:n], start=True, stop=True), reads=[onesblk, tp], writes=[ps])
            S.op("act", lambda e, ps=ps, tp=tp, n=n: e.activation(out=tp[:, 0:n], in_=ps[:, 0:n], func=AF.Sqrt, bias=1e-12, scale=1.0), reads=[ps], writes=[tp])
            S.op("dve", lambda e, tp=tp, n=n: e.reciprocal(out=tp[:, 0:n], in_=tp[:, 0:n]), reads=[tp], writes=[tp])
            S.op("dve", lambda e, kk=kk, tp=tp, n=n: e.tensor_tensor(out=kk[:, 0:n], in0=kk[:, 0:n], in1=tp[:, 0:n], op=ALU.mult), reads=[kk, tp], writes=[kk])
            store(A["kk"], cc, b, t0, n, kk, kk[:, 0:n])
        S.op("act", lambda e, n=n: e.activation(out=TH[:, 0:n], in_=MX[12][:, 0:n], func=AF.Tanh), reads=[MX[12]], writes=[TH])
        for d in range(2):
            for cc in range(4):
                ps = nps()
                S.op("pe", lambda e, ps=ps, d=d, cc=cc, n=n: e.matmul(ps[:, 0:n], W2[d * 64:(d + 1) * 64, cc * 128:(cc + 1) * 128], TH[d * 64:(d + 1) * 64, 0:n], start=True, stop=True),
                     reads=[W2, TH], writes=[ps])
                ob = nost()
                S.op("act", lambda e, ps=ps, ob=ob, d=d, cc=cc, n=n: e.activation(out=ob[:, 0:n], in_=ps[:, 0:n], func=AF.Sigmoid, bias=w0[d][:, cc:cc + 1], scale=1.0),
                     reads=[ps, w0[d]], writes=[ob])
                S.op("act", lambda e, ob=ob, n=n: e.activation(out=ob[:, 0:n], in_=ob[:, 0:n], func=AF.Exp, scale=-0.6065306597126334), reads=[ob], writes=[ob])
                store(A["w%d" % d], cc, b, t0, n, ob, ob[:, 0:n])
        for d in range(2):
            for cc in range(4):
                ps = nps()
                S.op("pe", lambda e, ps=ps, d=d, cc=cc, n=n: e.matmul(ps[:, 0:n], A2[d * 64:(d + 1) * 64, cc * 128:(cc + 1) * 128], MX[13][d * 64:(d + 1) * 64, 0:n], start=True, stop=True),
                     reads=[A2, MX[13]], writes=[ps])
                aa = Aa[d][cc]
                S.op("act", lambda e, ps=ps, aa=aa, d=d, cc=cc, n=n: e.activation(out=aa[:, 0:n], in_=ps[:, 0:n], func=AF.Sigmoid, bias=a0[d][:, cc:cc + 1], scale=1.0),
                     reads=[ps, a0[d]], writes=[aa])
                ob = nost()
                S.op("pool", lambda e, ob=ob, aa=aa, cc=cc, n=n: e.tensor_tensor(out=ob[:, 0:n], in0=KK[cc][:, 0:n], in1=aa[:, 0:n], op=ALU.mult), reads=[KK[cc], aa], writes=[ob])
                store(A["b%d" % d], cc, b, t0, n, ob, ob[:, 0:n])
                kd = KD[d][cc]
                S.op("dve", lambda e, kd=kd, aa=aa, cc=cc, n=n: e.tensor_scalar(out=kd[:, 0:n], in0=aa[:, 0:n], scalar1=kav[:, cc:cc + 1], scalar2=omka[:, cc:cc + 1], op0=ALU.mult, op1=ALU.add),
                     reads=[aa, kav, omka], writes=[kd])
                S.op("dve", lambda e, kd=kd, cc=cc, n=n: e.tensor_tensor(out=kd[:, 0:n], in0=kd[:, 0:n], in1=MX[4 + cc][:, 0:n], op=ALU.mult), reads=[kd, MX[4 + cc]], writes=[kd])
                store(A["kd%d" % d], cc, b, t0, n, kd, kd[:, 0:n])
        for cc in range(4):
            tp = tmp[cc % 2]
            S.op("pool", lambda e, tp=tp, cc=cc, n=n: e.tensor_tensor(out=tp[:, 0:n], in0=KD[0][cc][:, 0:n], in1=KD[1][cc][:, 0:n], op=ALU.add), reads=[KD[0][cc], KD[1][cc]], writes=[tp])
            S.op("dve", lambda e, tp=tp, cc=cc, n=n: e.scalar_tensor_tensor(out=tp[:, 0:n], in0=tp[:, 0:n], scalar=rkv[:, cc:cc + 1], in1=MX[cc][:, 0:n], op0=ALU.mult, op1=ALU.mult),
                 reads=[tp, rkv, MX[cc]], writes=[tp])
            ps = nps()
            S.op("pe", lambda e, ps=ps, tp=tp, n=n: e.matmul(ps[:, 0:n], onesblk[:], tp[:, 0:n], start=True, stop=True), reads=[onesblk, tp], writes=[ps])
            ob = nost()
            S.op("dve", lambda e, ps=ps, ob=ob, cc=cc, n=n: e.tensor_tensor(out=ob[:, 0:n], in0=ps[:, 0:n], in1=MX[8 + cc][:, 0:n], op=ALU.mult), reads=[ps, MX[8 + cc]], writes=[ob])
            store(A["bonus"], cc, b, t0, n, ob, ob[:, 0:n])
        S.op("act", lambda e, n=n: e.activation(out=SG[:, 0:n], in_=MX[14][:, 0:n], func=AF.Sigmoid), reads=[MX[14]], writes=[SG])
        for cc in range(4):
            ps = nps()
            S.op("pe", lambda e, ps=ps, cc=cc, n=n: e.matmul(ps[:, 0:n], G2[:, cc * 128:(cc + 1) * 128], SG[:, 0:n], start=True, stop=True), reads=[G2, SG], writes=[ps])
            ob = nost()
            evac(S, cc, ob[:, 0:n], ps[:, 0:n], [ps], [ob])
            store(A["g"], cc, b, t0, n, ob, ob[:, 0:n])


SCAN_ACT = False


def cust_ap(base_ap, dims):
    pa = base_ap.ap[0]
    return bass.AP(base_ap.tensor, base_ap.offset, [[pa[0], pa[1]]] + [[st, ct] for (st, ct) in dims])


def phase_rwscan(C, A, vtok, YD, onesblk, identb, Eq):
    S, cfg = C.S, C.cfg
    NB, T, TC = cfg.NB, cfg.T, cfg.TC
    NCB = 4 * NB
    Fh = NCB * 64
    NQh = Fh // 512
    NQ = 2 * NQh
    TB = 64
    YB = 4

    def st(name):
        return [S.sb("%s%d" % (name, d), [128, Fh]) for d in range(2)]
    M, MW, TMP, TMP2, TMP3, T4 = st("sc_M"), st("sc_MW"), st("sc_TMP"), st("sc_TMP2"), st("sc_TMP3"), st("sc_T4")
    saPS = [S.ps("sc_saPS%d" % d, [128, Fh]) for d in range(2)]
    vPS = [S.ps("sc_vPS%d" % d, [128, Fh]) for d in range(2)]
    OPS = [S.sb("sc_OPS%d" % i, [128, 5, 2, NCB, TB]) for i in range(2)]
    VT = [S.sb("sc_VT%d" % i, [TB, 2, 2, NCB, 64], BF16) for i in range(2)]
    YS = [[S.sb("sc_YS%d_%d" % (d, i), [2 * NQh, YB, 512]) for i in range(2)] for d in range(2)]
    for d in range(2):
        S.op("pool", lambda e, d=d: e.memset(M[d][:], 0.0), writes=[M[d]])

    def v3(buf):
        return buf[:].rearrange("p (c v) -> p c v", c=NCB)

    def tb_of(s):
        return TC - 1 - s if s < TC else T + TC - 1 - s

    names = [("kk", "kk"), ("w0", "w1"), ("b0", "b1"), ("kd0", "kd1"), ("r", "r")]
    for blk in range(T // TB):
        s0 = blk * TB
        ops = OPS[blk % 2]
        vt = VT[blk % 2]
        lo = tb_of(s0 + TB - 1)
        tok0 = (s0, lo)
        for a_, nm in enumerate(names):
            for d in range(2):
                arr = A[nm[d]]
                av = arr.t.rearrange("(c p) (b t) -> c p b t", p=128, b=NB)
                for c in range(4):
                    S.dma(ops, ops[:, a_, d, c * NB:(c + 1) * NB, :], arr, av[c, :, :, tok0[d]:tok0[d] + TB])
        vv = vtok.t.rearrange("(b t) (c h v) -> b t c h v", b=NB, c=4, h=2)
        for d in range(2):
            for c in range(4):
                for b in range(NB):
                    S.dma(vt, vt[:, :, d, c * NB + b, :], vtok, vv[b, tok0[d]:tok0[d] + TB, c, :, :])
        for j in range(TB):
            s = s0 + j
            col = (j, tb_of(s) - lo)

            def opnd(a_, d, ops=ops, col=col):
                base = ops[:, a_, d, 0, col[d]:col[d] + 1]
                return cust_ap(base, [(TB, NCB), (0, 64)])
            for d in range(2):
                KKo = opnd(0, d)
                S.op("dve", lambda e, d=d, KKo=KKo: e.tensor_tensor(out=v3(TMP[d]), in0=v3(M[d]), in1=KKo, op=ALU.mult), reads=[M[d], ops], writes=[TMP[d]])
                for q in range(NQh):
                    S.op("pe", lambda e, d=d, q=q: e.matmul(saPS[d][:, q * 512:(q + 1) * 512], onesblk[:], TMP[d][:, q * 512:(q + 1) * 512], start=True, stop=True),
                         reads=[onesblk, TMP[d]], writes=[saPS[d]], inc=(q == NQh - 1))
            for d in range(2):
                if SCAN_ACT:
                    for cb in range(NCB):
                        S.op("act", lambda e, d=d, cb=cb, ops=ops, col=col: e.activation(out=MW[d][:, cb * 64:(cb + 1) * 64], in_=M[d][:, cb * 64:(cb + 1) * 64], func=AF.Copy,
                                                                                      scale=ops[:, 1, d, cb, col[d]:col[d] + 1]),
                             reads=[M[d], ops], writes=[MW[d]], skip_self=(cb > 0))
                else:
                    Wo = opnd(1, d)
                    S.op("pool", lambda e, d=d, Wo=Wo: e.tensor_tensor(out=v3(MW[d]), in0=v3(M[d]), in1=Wo, op=ALU.mult), reads=[M[d], ops], writes=[MW[d]])
                sel = identb[0:TB, col[d]:col[d] + 1].to_broadcast([TB, 64])
                for h2 in range(2):
                    vsrc = vt[:, h2, d].rearrange("t c v -> t (c v)")
                    for q in range(NQh):
                        S.op("pe", lambda e, d=d, h2=h2, q=q, sel=sel, vsrc=vsrc: e.matmul(vPS[d][h2 * 64:(h2 + 1) * 64, q * 512:(q + 1) * 512], sel, vsrc[:, q * 512:(q + 1) * 512], start=True, stop=True),
                             reads=[identb, vt], writes=[vPS[d]], inc=(h2 == 1 and q == NQh - 1))
            for d in range(2):
                KDo = opnd(3, d)
                S.op("dve", lambda e, d=d, KDo=KDo: e.tensor_tensor(out=v3(TMP3[d]), in0=v3(vPS[d]), in1=KDo, op=ALU.mult), reads=[vPS[d], ops], writes=[TMP3[d]])
                S.op("pool", lambda e, d=d: e.tensor_tensor(out=MW[d][:], in0=MW[d][:], in1=TMP3[d][:], op=ALU.add), reads=[MW[d], TMP3[d]], writes=[MW[d]])
            for d in range(2):
                Bo = opnd(2, d)
                S.op("dve", lambda e, d=d, Bo=Bo: e.tensor_tensor(out=v3(TMP2[d]), in0=v3(saPS[d]), in1=Bo, op=ALU.mult), reads=[saPS[d], ops], writes=[TMP2[d]])
                S.op("pool" if SCAN_ACT else "dve", lambda e, d=d: e.tensor_tensor(out=M[d][:], in0=MW[d][:], in1=TMP2[d][:], op=ALU.subtract), reads=[MW[d], TMP2[d]], writes=[M[d]])
            for d, eng in ((0, "dve"), (1, "pool")):
                Ro = opnd(4, d)
                S.op(eng, lambda e, d=d, Ro=Ro: e.tensor_tensor(out=v3(T4[d]), in0=v3(M[d]), in1=Ro, op=ALU.mult), reads=[M[d], ops], writes=[T4[d]])
                for q in range(NQh):
                    S.op("pe", lambda e, d=d, q=q: e.matmul(saPS[d][0:2 * NQh, 0:512], Eq[:, q, 0:2 * NQh], T4[d][:, q * 512:(q + 1) * 512], start=(q == 0), stop=(q == NQh - 1)),
                         reads=[Eq, T4[d]], writes=[saPS[d]], inc=(q == NQh - 1))
                ys = YS[d][(s // YB) % 2]
                S.op("act", lambda e, d=d, ys=ys, s=s: e.copy(out=ys[:, s % YB, :], in_=saPS[d][0:2 * NQh, 0:512]), reads=[saPS[d]], writes=[ys])
                if s % YB == YB - 1:
                    if d == 0:
                        sa_ = s - YB + 1
                        S.dma(YD, YD[0:2 * NQh, sa_:sa_ + YB, :], ys, ys[:])
                    else:
                        tlo = tb_of(s)
                        S.dma(YD, YD[2 * NQh:4 * NQh, tlo:tlo + YB, :][:, ::-1, :], ys, ys[:])


def phase_rwpost(C, YD, A, P, mixT, ident):
    S, cfg = C.S, C.cfg
    NB, T = cfg.NB, cfg.T
    gnw = vec_cols(C, "rq_gnw", P["rw_gn_w"], P["rw_gn_w"][0, :], 4)
    gnb = vec_cols(C, "rq_gnb", P["rw_gn_b"], P["rw_gn_b"][0, :], 4)
    Y = [S.sb("rq_Y%d" % i, [128, 2, 4, 2, 64]) for i in range(2)]
    ysum = S.sb("rq_ysum", [128, 8, 64])
    sq = S.sb("rq_sq", [128, 8, 64])
    st1 = S.sb("rq_st1", [128, 8])
    st2 = S.sb("rq_st2", [128, 8])
    BG = [S.sb("rq_BG%d" % i, [128, 2, 4, 128]) for i in range(2)]
    pt = S.ps("rq_pt", [128, 4, 128])
    o32 = S.sb("rq_o32", [128, 4, 128])
    ob = [S.sb("rq_ob%d" % i, [128, 4, 128], BF16) for i in range(2)]
    bv = A["bonus"].t.rearrange("(c p) n -> p c n", p=128)
    gv = A["g"].t.rearrange("(c p) n -> p c n", p=128)
    mv = mixT.t[0:512, :].rearrange("(c p) n -> p c n", p=128)
    for ti in range(cfg.NTOK // 128):
        b, tt = divmod(ti, T // 128)
        t0 = tt * 128
        g0 = ti * 128
        y = Y[ti % 2]
        bg = BG[ti % 2]
        for d in range(2):
            for c in range(4):
                n0 = ((d * 4 + c) * NB + b) * 64
                q, col0 = n0 // 512, n0 % 512
                for h2 in range(2):
                    S.dma(y, y[:, d, c, h2, :], YD, YD[2 * q + h2, t0:t0 + 128, col0:col0 + 64])
        S.dma(bg, bg[:, 0], A["bonus"], bv[:, :, g0:g0 + 128])
        S.dma(bg, bg[:, 1], A["g"], gv[:, :, g0:g0 + 128])
        yf = y[:, 0].rearrange("p c h v -> p (c h) v")
        yb = y[:, 1].rearrange("p c h v -> p (c h) v")
        S.op("dve", lambda e, yf=yf, yb=yb: e.tensor_tensor(out=ysum[:], in0=yf, in1=yb, op=ALU.add), reads=[y], writes=[ysum])
        S.op("dve", lambda e: e.reduce_sum(out=st1[:], in_=ysum[:], axis=AX.X), reads=[ysum], writes=[st1])
        S.op("dve", lambda e: e.tensor_scalar(out=st1[:], in0=st1[:], scalar1=1.0 / 64, scalar2=None, op0=ALU.mult), reads=[st1], writes=[st1])
        S.op("dve", lambda e: e.tensor_tensor(out=ysum[:], in0=ysum[:], in1=st1[:].unsqueeze(2).to_broadcast([128, 8, 64]), op=ALU.subtract), reads=[ysum, st1], writes=[ysum])
        S.op("pool", lambda e: e.tensor_tensor(out=sq[:], in0=ysum[:], in1=ysum[:], op=ALU.mult), reads=[ysum], writes=[sq])
        S.op("dve", lambda e: e.reduce_sum(out=st2[:], in_=sq[:], axis=AX.X), reads=[sq], writes=[st2])
        S.op("act", lambda e: e.activation(out=st2[:], in_=st2[:], func=AF.Sqrt, bias=RW_GN_EPS, scale=1.0 / 64), reads=[st2], writes=[st2])
        S.op("dve", lambda e: e.reciprocal(out=st2[:], in_=st2[:]), reads=[st2], writes=[st2])
        S.op("dve", lambda e: e.tensor_tensor(out=ysum[:], in0=ysum[:], in1=st2[:].unsqueeze(2).to_broadcast([128, 8, 64]), op=ALU.mult), reads=[ysum, st2], writes=[ysum])
        for c in range(4):
            S.op("pe", lambda e, c=c: e.transpose(pt[:, c, :], ysum[:, 2 * c:2 * c + 2, :].rearrange("p h v -> p (h v)"), ident[:]), reads=[ysum, ident], writes=[pt], inc=(c == 3))
        for c in range(4):
            S.op("dve", lambda e, c=c: e.tensor_scalar(out=o32[:, c, :], in0=pt[:, c, :], scalar1=gnw[:, c:c + 1], scalar2=gnb[:, c:c + 1], op0=ALU.mult, op1=ALU.add),
                 reads=[pt, gnw, gnb], writes=[o32])
        S.op("pool", lambda e, bg=bg: e.tensor_tensor(out=o32[:], in0=o32[:], in1=bg[:, 0], op=ALU.add), reads=[o32, bg], writes=[o32])
        o = ob[ti % 2]
        S.op("dve", lambda e, bg=bg, o=o: e.tensor_tensor(out=o[:], in0=o32[:], in1=bg[:, 1], op=ALU.mult), reads=[o32, bg], writes=[o])
        S.dma(mixT, mv[:, :, g0:g0 + 128], o, o[:])


RW_GN_EPS = 64e-5


def phase_ssprep(C, colsT, P, xbc_tok, BCT, dtda_tok, ident):
    S, cfg = C.S, C.cfg
    T = cfg.T
    cw = S.sb("sp_cw", [128, 12, 5])
    for j in range(5):
        S.dma(cw, cw[:, :, j:j + 1], P["ssm_conv_w"], P["ssm_conv_w"][0, j, :].rearrange("(c p o) -> p c o", p=128, o=1), allow_slow_non_contiguous=True)
    cb = vec_cols(C, "sp_cb", P["ssm_conv_b"], P["ssm_conv_b"][0, :], 12)
    dtb = S.sb("sp_dtb", [64, 1])
    aneg = S.sb("sp_aneg", [64, 1])
    dbv = P["ssm_dt_bias"][0].rearrange("d (h o) -> (d h) o", o=1)
    alv = P["ssm_a_log"][0].rearrange("d (h o) -> (d h) o", o=1)
    S.dma(dtb, dtb[0:32, :], P["ssm_dt_bias"], dbv, allow_slow_non_contiguous=True)
    S.dma(dtb, dtb[32:64, :], P["ssm_dt_bias"], dbv, allow_slow_non_contiguous=True)
    S.dma(aneg, aneg[32:64, :], P["ssm_a_log"], alv, allow_slow_non_contiguous=True)
    S.op("act", lambda e: e.activation(out=aneg[32:64, :], in_=aneg[32:64, :], func=AF.Exp), reads=[aneg], writes=[aneg])
    S.op("dve", lambda e: e.tensor_scalar(out=aneg[32:64, :], in0=aneg[32:64, :], scalar1=-1.0, scalar2=None, op0=ALU.mult), reads=[aneg], writes=[aneg])
    NB_ = 512
    raw = [S.sb("sp_raw%d" % i, [128, NB_ + 4]) for i in range(3)]
    acc = [S.sb("sp_acc%d" % i, [128, NB_]) for i in range(2)]
    XC = [S.sb("sp_xc%d" % c, [128, NB_]) for c in range(12)]
    bcb = [S.sb("sp_bcb%d" % i, [128, NB_], BF16) for i in range(2)]
    DD = S.sb("sp_dd", [64, NB_])
    pss = [S.ps("sp_ps%d" % i, [128, 512]) for i in range(3)]
    pdd = S.ps("sp_pdd", [128, 64])
    tk = [S.sb("sp_tk%d" % i, [128, 1536]) for i in range(2)]
    dk = [S.sb("sp_dk%d" % i, [128, 64]) for i in range(2)]
    pc = [0]
    for (b, t0, n, hl, hr) in seg_blocks(cfg, NB_):
        g0 = b * T + t0
        for c in range(12):
            rw = raw[c % 3]
            nl = 2 if hl else 0
            nr = 2 if hr else 0
            if not hl:
                S.op("pool", lambda e, rw=rw: e.memset(rw[:, 0:2], 0.0), writes=[rw])
            if not hr:
                S.op("pool", lambda e, rw=rw, n=n: e.memset(rw[:, n + 2:n + 4], 0.0), writes=[rw])
            S.dma(rw, rw[:, 2 - nl:2 + n + nr], colsT, colsT[2944 + c * 128:2944 + (c + 1) * 128, g0 - nl:g0 + n + nr])
            ac = acc[c % 2]
            S.op("dve", lambda e, rw=rw, ac=ac, c=c, n=n: e.tensor_scalar(out=ac[:, 0:n], in0=rw[:, 0:n], scalar1=cw[:, c, 0:1], scalar2=None, op0=ALU.mult), reads=[rw, cw], writes=[ac])
            for j in range(1, 5):
                eng = "dve"
                S.op(eng, lambda e, rw=rw, ac=ac, c=c, n=n, j=j: e.scalar_tensor_tensor(out=ac[:, 0:n], in0=rw[:, j:j + n], scalar=cw[:, c, j:j + 1], in1=ac[:, 0:n], op0=ALU.mult, op1=ALU.add),
                     reads=[rw, cw, ac], writes=[ac])
            xc = XC[c]
            S.op("act", lambda e, ac=ac, xc=xc, c=c, n=n: e.activation(out=xc[:, 0:n], in_=ac[:, 0:n], func=AF.Silu, bias=cb[:, c:c + 1], scale=1.0), reads=[ac, cb], writes=[xc])
            if c >= 8:
                bb = bcb[c % 2]
                S.op("pool", lambda e, bb=bb, xc=xc, n=n: e.tensor_copy(out=bb[:, 0:n], in_=xc[:, 0:n]), reads=[xc], writes=[bb])
                S.dma(BCT, BCT[(c - 8) * 128:(c - 7) * 128, g0:g0 + n], bb, bb[:, 0:n])
        S.dma(DD, DD[0:32, 0:n], colsT, colsT[4480:4512, g0:g0 + n])
        S.dma(DD, DD[32:64, 0:n], colsT, colsT[4480:4512, g0:g0 + n])
        S.op("act", lambda e, n=n: e.activation(out=DD[:, 0:n], in_=DD[:, 0:n], func=AF.Exp, bias=dtb[:, 0:1], scale=1.0), reads=[DD, dtb], writes=[DD])
        S.op("act", lambda e, n=n: e.activation(out=DD[:, 0:n], in_=DD[:, 0:n], func=AF.Ln, bias=1.0, scale=1.0), reads=[DD], writes=[DD])
        S.op("dve", lambda e, n=n: e.tensor_scalar(out=DD[32:64, 0:n], in0=DD[32:64, 0:n], scalar1=aneg[32:64, 0:1], scalar2=None, op0=ALU.mult), reads=[DD, aneg], writes=[DD])
        for j in range(n // 128):
            tkb = tk[j % 2]
            for q in range(3):
                ps = pss[pc[0] % 3]
                pc[0] += 1
                for cc in range(4):
                    c = q * 4 + cc
                    S.op("pe", lambda e, ps=ps, cc=cc, c=c, j=j: e.transpose(ps[:, cc * 128:(cc + 1) * 128], XC[c][:, j * 128:(j + 1) * 128], ident[:]), reads=[XC[c], ident], writes=[ps], inc=(cc == 3))
                evac(S, q, tkb[:, q * 512:(q + 1) * 512], ps[:], [ps], [tkb])
            S.dma(xbc_tok, xbc_tok[g0 + j * 128:g0 + (j + 1) * 128, :], tkb, tkb[:])
            S.op("pe", lambda e, j=j: e.transpose(pdd[:, :], DD[:, j * 128:(j + 1) * 128], ident[0:64, 0:64]), reads=[DD, ident], writes=[pdd])
            dkb = dk[j % 2]
            S.op("act", lambda e, dkb=dkb: e.copy(out=dkb[:], in_=pdd[:]), reads=[pdd], writes=[dkb])
            S.dma(dtda_tok, dtda_tok[g0 + j * 128:g0 + (j + 1) * 128, :], dkb, dkb[:])


def phase_ssscan(C, xbc_tok, BCT, dtda_tok, ytmp, K):
    S, cfg = C.S, C.cfg
    NB, T, TC = cfg.NB, cfg.T, cfg.TC
    Tt, TCt = T // 128, TC // 128
    XT = [S.sb("ss_xt%d" % i, [128, 1536]) for i in range(2)]
    DT = [S.sb("ss_dt%d" % i, [128, 64]) for i in range(2)]
    BC = [S.sb("ss_bc%d" % i, [128, 4, 128], BF16) for i in range(2)]
    miscPS = S.ps("ss_miscPS", [128, 512])
    cPS = View(miscPS, miscPS[:, 0:32])
    csTPS = View(miscPS, miscPS[0:8, 32:288].rearrange("p (g l) -> p g l", g=2))
    cbPS = View(miscPS, miscPS[:, 288:416])
    segPS = [S.ps("ss_segPS%d" % i, [128, 4, 128]) for i in range(2)]
    ydPS = S.ps("ss_ydPS", [128, 1024])
    yoPS = S.ps("ss_yoPS", [128, 512])
    dsPS = S.ps("ss_dsPS", [128, 512])
    c_sb = S.sb("ss_c", [128, 16])
    dfs = S.sb("ss_dfs", [128, 16])
    cdb = S.sb("ss_cdb", [128, 16])
    dte = S.sb("ss_dte", [128, 16])
    dtdte = S.sb("ss_dtdte", [128, 16])
    negcsT = S.sb("ss_negcsT", [8, 2, 128])
    csdiag = S.sb("ss_csdiag", [8, 2, 1024])
    xdt = S.sb("ss_xdt", [128, 16, 64], BF16)
    xdd = S.sb("ss_xdd", [128, 16, 64], BF16)
    btok = S.sb("ss_btok", [128, 2, 128], BF16)
    cbt = [S.sb("ss_cbt%d" % i, [128, 128], BF16) for i in range(2)]
    Eb = [S.sb("ss_E%d" % i, [128, 4, 128], BF16) for i in range(2)]
    Gb = [S.sb("ss_G%d" % i, [128, 4, 128], BF16) for i in range(2)]
    yacc = [S.sb("ss_yacc%d" % i, [128, 1024]) for i in range(2)]
    S32 = [S.sb("ss_S32_%d" % g, [128, 512]) for g in range(2)]
    Sbf = [S.sb("ss_Sbf_%d" % g, [128, 512], BF16) for g in range(2)]
    bcv = BCT.t.rearrange("(j p) n -> p j n", p=128)
    it = 0
    for d in range(2):
        tri = K["tri%d" % d]
        mneg = K["mneg%d" % d]
        for b in range(NB):
            for g in range(2):
                S.op("pool", lambda e, g=g: e.memset(S32[g][:], 0.0), writes=[S32[g]])
                S.op("pool", lambda e, g=g: e.memset(Sbf[g][:], 0.0), writes=[Sbf[g]])
            order = list(range(Tt)) if d == 0 else (list(range(TCt - 1, -1, -1)) + list(range(Tt - 1, TCt - 1, -1)))
            for tt in order:
                g0 = b * T + tt * 128
                xt, dtt, bc = XT[it % 2], DT[it % 2], BC[it % 2]
                ya = yacc[it % 2]
                it += 1
                S.dma(xt, xt[:], xbc_tok, xbc_tok[g0:g0 + 128, :])
                S.dma(dtt, dtt[:], dtda_tok, dtda_tok[g0:g0 + 128, :])
                S.dma(bc, bc[:], BCT, bcv[:, :, g0:g0 + 128])
                da = dtt[:, 32 + d * 16:32 + d * 16 + 16]
                dtd = dtt[:, d * 16:d * 16 + 16]
                S.op("pe", lambda e, da=da, tri=tri: e.matmul(cPS[:, 0:16], tri[:], da, start=True, stop=True), reads=[tri, dtt], writes=[cPS])
                S.op("pe", lambda e, da=da: e.matmul(cPS[:, 16:32], K["ones"][:], da, start=True, stop=True), reads=[K["ones"], dtt], writes=[cPS])
                for g in range(2):
                    S.op("pe", lambda e, g=g, dtt=dtt, d=d, tri=tri: e.matmul(csTPS[:, g, :], dtt[:, 32 + d * 16 + g * 8:32 + d * 16 + g * 8 + 8], tri[:], start=True, stop=True),
                         reads=[tri, dtt], writes=[csTPS])
                S.op("act", lambda e: e.copy(out=c_sb[:], in_=cPS[:, 0:16]), reads=[cPS], writes=[c_sb])
                S.op("act", lambda e: e.activation(out=dfs[:], in_=cPS[:, 0:16], func=AF.Exp), reads=[cPS], writes=[dfs])
                S.op("act", lambda e: e.activation(out=cdb[:], in_=cPS[:, 16:32], func=AF.Exp), reads=[cPS], writes=[cdb])
                S.op("dve", lambda e: e.tensor_tensor(out=dte[:], in0=cPS[:, 16:32], in1=c_sb[:], op=ALU.subtract), reads=[cPS, c_sb], writes=[dte])
                S.op("act", lambda e: e.activation(out=dte[:], in_=dte[:], func=AF.Exp), reads=[dte], writes=[dte])
                S.op("dve", lambda e, dtd=dtd: e.tensor_tensor(out=dtdte[:], in0=dte[:], in1=dtd, op=ALU.mult), reads=[dte, dtt], writes=[dtdte])
                S.op("act", lambda e: e.mul(out=negcsT[:], in_=csTPS[:], mul=-1.0), reads=[csTPS], writes=[negcsT])
                for g in range(2):
                    S.op("dve", lambda e, g=g: e.tensor_tensor(out=csdiag[:, g, :].rearrange("p (h l) -> p h l", h=8), in0=K["blk"][:].rearrange("p (h l) -> p h l", h=8),
                                                              in1=csTPS[:, g, :].unsqueeze(1).to_broadcast([8, 8, 128]), op=ALU.mult), reads=[K["blk"], csTPS], writes=[csdiag])
                xs3 = xt[:, 0:1024].rearrange("p (h v) -> p h v", h=16)
                S.op("dve", lambda e, xs3=xs3, dtd=dtd: e.tensor_tensor(out=xdt[:], in0=xs3, in1=dtd.unsqueeze(2).to_broadcast([128, 16, 64]), op=ALU.mult), reads=[xt, dtt], writes=[xdt])
                S.op("pool", lambda e, xs3=xs3: e.tensor_tensor(out=xdd[:], in0=xs3, in1=dtdte[:].unsqueeze(2).to_broadcast([128, 16, 64]), op=ALU.mult), reads=[xt, dtdte], writes=[xdd])
                S.op("pool", lambda e, xt=xt: e.tensor_copy(out=btok[:], in_=xt[:, 1024:1280].rearrange("p (g n) -> p g n", g=2)), reads=[xt], writes=[btok])
                si = 0
                for g in range(2):
                    S.op("pe", lambda e, g=g, bc=bc: e.matmul(cbPS[:], bc[:, g, :], bc[:, 2 + g, :], start=True, stop=True), reads=[bc], writes=[cbPS])
                    cb_ = cbt[g]
                    S.op("act", lambda e, cb_=cb_: e.copy(out=cb_[:], in_=cbPS[:]), reads=[cbPS], writes=[cb_])
                    for half in range(2):
                        sp = segPS[si % 2]
                        E, G = Eb[si % 2], Gb[si % 2]
                        si += 1
                        S.op("pe", lambda e, sp=sp, g=g, half=half: e.matmul(sp[:].rearrange("p h l -> p (h l)"), K["ones8"][:], csdiag[:, g, half * 512:(half + 1) * 512], start=True, stop=False),
                             reads=[K["ones8"], csdiag], writes=[sp], inc=False)
                        S.op("pe", lambda e, sp=sp, g=g, half=half: e.matmul(sp[:].rearrange("p h l -> p (h l)"), negcsT[:, g, :], K["blk"][:, half * 512:(half + 1) * 512], start=False, stop=False),
                             reads=[negcsT, K["blk"]], writes=[sp], inc=False)
                        S.op("pe", lambda e, sp=sp, mneg=mneg: e.matmul(sp[:].rearrange("p h l -> p (h l)"), K["identb"][:], mneg[:], start=False, stop=True),
                             reads=[K["identb"], mneg], writes=[sp])
                        S.op("act", lambda e, sp=sp, E=E: e.activation(out=E[:], in_=sp[:], func=AF.Exp), reads=[sp], writes=[E])
                        S.op("dve", lambda e, E=E, G=G, cb_=cb_: e.tensor_tensor(out=G[:], in0=E[:], in1=cb_[:].unsqueeze(1).to_broadcast([128, 4, 128]), op=ALU.mult), reads=[E, cb_], writes=[G])
                        for hh in range(4):
                            h = g * 8 + half * 4 + hh
                            S.op("pe", lambda e, G=G, hh=hh, h=h: e.matmul(ydPS[:, h * 64:(h + 1) * 64], G[:, hh, :], xdt[:, h, :], start=True, stop=True), reads=[G, xdt], writes=[ydPS], inc=(hh == 3))
                    S.op("pe", lambda e, g=g, bc=bc: e.matmul(yoPS[:], bc[:, 2 + g, :], Sbf[g][:], start=True, stop=True), reads=[bc, Sbf[g]], writes=[yoPS])
                    S.op("dve", lambda e, g=g, ya=ya: e.tensor_tensor(out=ya[:, g * 512:(g + 1) * 512].rearrange("p (h v) -> p h v", h=8), in0=yoPS[:].rearrange("p (h v) -> p h v", h=8),
                                                                   in1=dfs[:, g * 8:(g + 1) * 8].unsqueeze(2).to_broadcast([128, 8, 64]), op=ALU.mult), reads=[yoPS, dfs], writes=[ya])
                    S.op("pe", lambda e, g=g: e.matmul(dsPS[:], btok[:, g, :], xdd[:, g * 8:(g + 1) * 8, :].rearrange("p h v -> p (h v)"), start=True, stop=True), reads=[btok, xdd], writes=[dsPS])
                    S.op("pool", lambda e, g=g: e.tensor_tensor(out=S32[g][:].rearrange("p (h v) -> p h v", h=8), in0=S32[g][:].rearrange("p (h v) -> p h v", h=8),
                                                               in1=cdb[:, g * 8:(g + 1) * 8].unsqueeze(2).to_broadcast([128, 8, 64]), op=ALU.mult), reads=[S32[g], cdb], writes=[S32[g]])
                    S.op("dve", lambda e, g=g: e.tensor_tensor(out=S32[g][:], in0=S32[g][:], in1=dsPS[:], op=ALU.add), reads=[S32[g], dsPS], writes=[S32[g]])
                    S.op("act", lambda e, g=g: e.copy(out=Sbf[g][:], in_=S32[g][:]), reads=[S32[g]], writes=[Sbf[g]])
                S.op("dve", lambda e, ya=ya: e.tensor_tensor(out=ya[:], in0=ya[:], in1=ydPS[:], op=ALU.add), reads=[ya, ydPS], writes=[ya])
                S.dma(ytmp[d], ytmp[d][g0:g0 + 128, :], ya, ya[:])


def phase_sspost(C, ytmp, xbc_tok, colsT, P, mixT, ident):
    S, cfg = C.S, C.cfg
    dsk = S.sb("so_dsk", [128, 16])
    S.dma(dsk, dsk[:], P["ssm_d"], P["ssm_d"][0, :].partition_broadcast(128))
    nw = S.sb("so_nw", [128, 1024])
    S.dma(nw, nw[:], P["ssm_norm_w"], P["ssm_norm_w"][0, :].partition_broadcast(128))
    Y1 = [S.sb("so_y1_%d" % i, [128, 1024]) for i in range(2)]
    Y2 = [S.sb("so_y2_%d" % i, [128, 1024]) for i in range(2)]
    XS = [S.sb("so_xs_%d" % i, [128, 1024]) for i in range(2)]
    ZT = [S.sb("so_zt_%d" % i, [128, 8, 128]) for i in range(2)]
    zs = S.sb("so_zs", [128, 1024])
    junk = S.sb("so_junk", [128, 512], BF16)
    ss = S.sb("so_ss", [128, 2])
    pz = S.ps("so_pz", [128, 1024])
    po = S.ps("so_po", [128, 8, 128])
    ob = [S.sb("so_ob%d" % i, [128, 8, 128], BF16) for i in range(2)]
    zv = colsT.t[1920:2944, :].rearrange("(c p) n -> p c n", p=128)
    mv = mixT.t[512:1536, :].rearrange("(c p) n -> p c n", p=128)
    for ti in range(cfg.NTOK // 128):
        g0 = ti * 128
        y1, y2, xs, zt = Y1[ti % 2], Y2[ti % 2], XS[ti % 2], ZT[ti % 2]
        S.dma(y1, y1[:], ytmp[0], ytmp[0][g0:g0 + 128, :])
        S.dma(y2, y2[:], ytmp[1], ytmp[1][g0:g0 + 128, :])
        S.dma(xs, xs[:], xbc_tok, xbc_tok[g0:g0 + 128, 0:1024])
        S.dma(zt, zt[:], colsT, zv[:, :, g0:g0 + 128])
        for c in range(8):
            S.op("pe", lambda e, c=c, zt=zt: e.transpose(pz[:, c * 128:(c + 1) * 128], zt[:, c, :], ident[:]), reads=[zt, ident], writes=[pz], inc=(c == 7))
        S.op("act", lambda e: e.activation(out=zs[:], in_=pz[:], func=AF.Silu), reads=[pz], writes=[zs])
        S.op("dve", lambda e, y1=y1, y2=y2: e.tensor_tensor(out=y1[:], in0=y1[:], in1=y2[:], op=ALU.add), reads=[y1, y2], writes=[y1])
        S.op("pool", lambda e, xs=xs: e.tensor_tensor(out=xs[:].rearrange("p (h v) -> p h v", h=16), in0=xs[:].rearrange("p (h v) -> p h v", h=16),
                                                    in1=dsk[:].unsqueeze(2).to_broadcast([128, 16, 64]), op=ALU.mult), reads=[xs, dsk], writes=[xs])
        S.op("dve", lambda e, y1=y1, xs=xs: e.tensor_tensor(out=y1[:], in0=y1[:], in1=xs[:], op=ALU.add), reads=[y1, xs], writes=[y1])
        S.op("dve", lambda e, y1=y1: e.tensor_tensor(out=y1[:], in0=y1[:], in1=zs[:], op=ALU.mult), reads=[y1, zs], writes=[y1])
        for gI in range(2):
            S.op("act", lambda e, y1=y1, gI=gI: e.activation(out=junk[:], in_=y1[:, gI * 512:(gI + 1) * 512], func=AF.Square, accum_out=ss[:, gI:gI + 1]), reads=[y1], writes=[junk, ss])
        S.op("act", lambda e: e.activation(out=ss[:], in_=ss[:], func=AF.Sqrt, bias=NORM_EPS, scale=1.0 / 512), reads=[ss], writes=[ss])
        S.op("dve", lambda e: e.reciprocal(out=ss[:], in_=ss[:]), reads=[ss], writes=[ss])
        S.op("dve", lambda e, y1=y1: e.tensor_tensor(out=y1[:].rearrange("p (g v) -> p g v", g=2), in0=y1[:].rearrange("p (g v) -> p g v", g=2),
                                                    in1=ss[:].unsqueeze(2).to_broadcast([128, 2, 512]), op=ALU.mult), reads=[y1, ss], writes=[y1])
        S.op("pool", lambda e, y1=y1: e.tensor_tensor(out=y1[:], in0=y1[:], in1=nw[:], op=ALU.mult), reads=[y1, nw], writes=[y1])
        for c in range(8):
            S.op("pe", lambda e, c=c, y1=y1: e.transpose(po[:, c, :], y1[:, c * 128:(c + 1) * 128], ident[:]), reads=[y1, ident], writes=[po], inc=(c == 7))
        o = ob[ti % 2]
        S.op("act", lambda e, o=o: e.copy(out=o[:, 0:4, :], in_=po[:, 0:4, :]), reads=[po], writes=[o])
        S.op("dve", lambda e, o=o: e.tensor_copy(out=o[:, 4:8, :], in_=po[:, 4:8, :]), reads=[po], writes=[o])
        S.dma(mixT, mv[:, :, g0:g0 + 128], o, o[:])


def gate_tiles(C, name, modv, layer, idx):
    S, cfg = C.S, C.cfg
    D = cfg.D
    out = []
    for r in range(cfg.NB + 1):
        t = S.sb("%s_%d" % (name, r), [128, D])
        S.dma(t, t[:], modv, modv[layer, r, idx * D:(idx + 1) * D].partition_broadcast(128))
        out.append(t)
    return out


def tile_row(cfg, tile_idx):
    tpb = cfg.T // 128
    b, tt = divmod(tile_idx, tpb)
    return cfg.NB if tt < cfg.TC // 128 else b


def lat_tiles(cfg):
    tpb = cfg.T // 128
    return [b * tpb + tt for b in range(cfg.NB) for tt in range(cfg.TC // 128, tpb)]


def phase_outproj(C, name, src, src_tokmajor, kdim, W_buf, W_ap, modv, layer, xin, xout, tiles, ident_bf):
    S, cfg = C.S, C.cfg
    kch = kdim // 128
    wb = load_w_bf16(C, name + "_w", W_buf, W_ap.rearrange("(k p) c -> p k c", p=128), 1024, kch)
    gt = gate_tiles(C, name + "_gt", modv, layer, 2)
    mt_ = [S.sb("%s_m%d" % (name, i), [128, kch, 128], BF16) for i in range(2)]
    if src_tokmajor:
        tk_ = [S.sb("%s_tk%d" % (name, i), [128, kdim], BF16) for i in range(2)]
        ptb = S.ps(name + "_ptb", [128, kch, 128], BF16)
    xt_ = [S.sb("%s_x%d" % (name, i), [128, 1024]) for i in range(2)]
    ot_ = [S.sb("%s_o%d" % (name, i), [128, 1024]) for i in range(2)]
    ps_ = [S.ps("%s_ps%d" % (name, i), [128, 1024]) for i in range(2)]
    if not src_tokmajor:
        sv = src.t.rearrange("(k p) n -> p k n", p=128)
    for i, ti in enumerate(tiles):
        g0 = ti * 128
        m, xt, ot, ps = mt_[i % 2], xt_[i % 2], ot_[i % 2], ps_[i % 2]
        if src_tokmajor:
            tk = tk_[i % 2]
            S.dma(tk, tk[:], src, src[g0:g0 + 128, :])
            for k in range(kch):
                S.op("pe", lambda e, k=k, tk=tk: e.transpose(ptb[:, k, :], tk[:, k * 128:(k + 1) * 128], ident_bf[:]), reads=[tk, ident_bf], writes=[ptb], inc=(k == kch - 1))
            evac(S, i, m[:], ptb[:], [ptb], [m])
        else:
            S.dma(m, m[:], src, sv[:, :, g0:g0 + 128])
        S.dma(xt, xt[:], xin, xin[g0:g0 + 128, :])
        for half in range(2):
            for k in range(kch):
                S.op("pe", lambda e, k=k, half=half, m=m, ps=ps: e.matmul(ps[:, half * 512:(half + 1) * 512], m[:, k, :], wb[:, k, half * 512:(half + 1) * 512], start=(k == 0), stop=(k == kch - 1)),
                     reads=[m, wb], writes=[ps], inc=(k == kch - 1))
        g = gt[tile_row(cfg, ti)]
        S.op("dve", lambda e, ps=ps, ot=ot, g=g: e.tensor_tensor(out=ot[:], in0=ps[:], in1=g[:], op=ALU.mult), reads=[ps, g], writes=[ot])
        S.op("pool", lambda e, ot=ot, xt=xt: e.tensor_tensor(out=ot[:], in0=ot[:], in1=xt[:], op=ALU.add), reads=[ot, xt], writes=[ot])
        S.dma(xout, xout[g0:g0 + 128, :], ot, ot[:])


def phase_final(C, xin, fn_buf, out):
    S, cfg = C.S, C.cfg
    D = cfg.D
    g = S.sb("fn_g", [128, D])
    S.dma(g, g[:], fn_buf, fn_buf.t.partition_broadcast(128))
    xt_ = [S.sb("fn_x%d" % i, [128, D]) for i in range(2)]
    ot_ = [S.sb("fn_o%d" % i, [128, D]) for i in range(2)]
    junk = S.sb("fn_junk", [128, D], BF16)
    ss_ = [S.sb("fn_ss%d" % i, [128, 1]) for i in range(2)]
    tpl = cfg.TL // 128
    for i, ti in enumerate(lat_tiles(cfg)):
        xt, ot, ss = xt_[i % 2], ot_[i % 2], ss_[i % 2]
        S.dma(xt, xt[:], xin, xin[ti * 128:(ti + 1) * 128, :])
        S.op("act", lambda e, xt=xt, ss=ss: e.activation(out=junk[:], in_=xt[:], func=AF.Square, accum_out=ss[:]), reads=[xt], writes=[junk, ss])
        S.op("act", lambda e, ss=ss: e.activation(out=ss[:], in_=ss[:], func=AF.Sqrt, bias=NORM_EPS, scale=1.0 / D), reads=[ss], writes=[ss])
        S.op("dve", lambda e, ss=ss: e.reciprocal(out=ss[:], in_=ss[:]), reads=[ss], writes=[ss])
        S.op("dve", lambda e, xt=xt, ot=ot, ss=ss: e.scalar_tensor_tensor(out=ot[:], in0=xt[:], scalar=ss[:, 0:1], in1=g[:], op0=ALU.mult, op1=ALU.mult), reads=[xt, ss, g], writes=[ot])
        S.dma(out, out[i * 128:(i + 1) * 128, :], ot, ot[:])


def phase_moe_cast(C, layer, EW, wbf):
    S = C.S
    st = [S.sb("mc_st%d" % i, [128, 6144]) for i in range(2)]
    ob = [S.sb("mc_ob%d" % i, [128, 6144], BF16) for i in range(2)]
    for e in range(65):
        s_, o_ = st[e % 2], ob[e % 2]
        if e < 64:
            w1, w3, w2 = EW["exp_w1"][layer, e], EW["exp_w3"][layer, e], EW["exp_w2"][layer, e]
            b1, b3, b2 = EW["exp_w1"], EW["exp_w3"], EW["exp_w2"]
        else:
            w1, w3, w2 = EW["sh_w1"][layer], EW["sh_w3"][layer], EW["sh_w2"][layer]
            b1, b3, b2 = EW["sh_w1"], EW["sh_w3"], EW["sh_w2"]
        S.dma(s_, s_[:, 0:2048].rearrange("p (k f) -> p k f", k=8), b1, w1.rearrange("(k p) f -> p k f", p=128))
        S.dma(s_, s_[:, 2048:4096].rearrange("p (k f) -> p k f", k=8), b3, w3.rearrange("(k p) f -> p k f", p=128))
        S.dma(s_, s_[:, 4096:6144].rearrange("p (j f) -> p j f", j=2), b2, w2.rearrange("(j p) f -> p j f", p=128))
        S.op("act", lambda e_, s_=s_, o_=o_: e_.copy(out=o_[:, 0:2048], in_=s_[:, 0:2048]), reads=[s_], writes=[o_])
        S.op("dve", lambda e_, s_=s_, o_=o_: e_.tensor_copy(out=o_[:, 2048:4096], in_=s_[:, 2048:4096]), reads=[s_], writes=[o_])
        S.op("pool", lambda e_, s_=s_, o_=o_: e_.tensor_copy(out=o_[:, 4096:6144], in_=s_[:, 4096:6144]), reads=[s_], writes=[o_])
        S.dma(wbf, wbf[e], o_, o_[:])


def phase_moe_route(C, layer, xin, gvec_buf, gvec_ap, modv, rw_buf, rb_buf, hT, gates, tiles, ident):
    S, cfg = C.S, C.cfg
    mt = ModTiles(C, "mr_mt", modv, layer, 3, 4, gvec_ap, gvec_buf)
    nt = NormT(C, "mr_nt", ident)
    rw = S.sb("mr_rw", [128, 8, 64])
    S.dma(rw, rw[:], rw_buf, rw_buf[layer].rearrange("(k p) e -> p k e", p=128))
    rb = S.sb("mr_rb", [128, 64])
    S.dma(rb, rb[:], rb_buf, rb_buf[layer, :].partition_broadcast(128))
    hb_ = [S.sb("mr_hb%d" % i, [128, 8, 128], BF16) for i in range(2)]
    h32_ = [S.sb("mr_h32%d" % i, [128, 8, 128]) for i in range(2)]
    lg = S.ps("mr_lg", [128, 64])
    sc = S.sb("mr_sc", [128, 64])
    sel = S.sb("mr_sel", [128, 64])
    eq = S.sb("mr_eq", [128, 64])
    m1 = S.sb("mr_m1", [128, 8])
    m2 = S.sb("mr_m2", [128, 8])
    t8 = S.sb("mr_t8", [128, 8])
    pen = S.sb("mr_pen", [128, 8])
    ws = S.sb("mr_ws", [128, 1])
    gt_ = [S.sb("mr_gt%d" % i, [128, 65]) for i in range(2)]
    for g in gt_:
        S.op("pool", lambda e, g=g: e.memset(g[:, 64:65], 1.0), writes=[g])
    hv = hT.t.rearrange("(k p) n -> p k n", p=128)

    def v3(b):
        return b[:].rearrange("p (g i) -> p g i", g=8)
    for i, ti in enumerate(tiles):
        g0 = ti * 128
        hb, h32, gt = hb_[i % 2], h32_[i % 2], gt_[i % 2]
        nt.tile(xin, g0, mt, tile_row(cfg, ti), [(hb, lambda k0, k1, hb=hb: hb[:, k0:k1, :]), (h32, lambda k0, k1, h32=h32: h32[:, k0:k1, :])])
        S.dma(hT, hv[:, :, g0:g0 + 128], hb, hb[:])
        for k in range(8):
            S.op("pe", lambda e, k=k, h32=h32: e.matmul(lg[:], h32[:, k, :], rw[:, k, :], start=(k == 0), stop=(k == 7)), reads=[h32, rw], writes=[lg], inc=(k == 7))
        S.op("act", lambda e: e.activation(out=sc[:], in_=lg[:], func=AF.Sigmoid), reads=[lg], writes=[sc])
        S.op("dve", lambda e: e.tensor_tensor(out=sel[:], in0=sc[:], in1=rb[:], op=ALU.add), reads=[sc, rb], writes=[sel])
        S.op("dve", lambda e: e.reduce_max(out=m1[:], in_=v3(sel), axis=AX.X), reads=[sel], writes=[m1])
        S.op("dve", lambda e: e.tensor_tensor(out=v3(eq), in0=v3(sel), in1=m1[:].unsqueeze(2).to_broadcast([128, 8, 8]), op=ALU.is_equal), reads=[sel, m1], writes=[eq])
        S.op("dve", lambda e: e.scalar_tensor_tensor(out=eq[:], in0=eq[:], scalar=-1e30, in1=sel[:], op0=ALU.mult, op1=ALU.add), reads=[eq, sel], writes=[eq])
        S.op("dve", lambda e: e.reduce_max(out=m2[:], in_=v3(eq), axis=AX.X), reads=[eq], writes=[m2])
        S.op("dve", lambda e: e.tensor_tensor(out=m1[:], in0=m1[:], in1=m2[:], op=ALU.add), reads=[m1, m2], writes=[m1])
        S.op("dve", lambda e: e.max(out=t8[:], in_=m1[:]), reads=[m1], writes=[t8])
        S.op("dve", lambda e: e.tensor_scalar(out=pen[:], in0=m1[:], scalar1=t8[:, 3:4], scalar2=None, op0=ALU.is_ge), reads=[m1, t8], writes=[pen])
        S.op("dve", lambda e: e.tensor_scalar(out=pen[:], in0=pen[:], scalar1=1e30, scalar2=-1e30, op0=ALU.mult, op1=ALU.add), reads=[pen], writes=[pen])
        S.op("dve", lambda e: e.tensor_tensor(out=v3(sel), in0=v3(sel), in1=pen[:].unsqueeze(2).to_broadcast([128, 8, 8]), op=ALU.add), reads=[sel, pen], writes=[sel])
        S.op("dve", lambda e: e.max(out=t8[:], in_=sel[:]), reads=[sel], writes=[t8])
        S.op("dve", lambda e: e.tensor_scalar(out=eq[:], in0=sel[:], scalar1=t8[:, 5:6], scalar2=None, op0=ALU.is_ge), reads=[sel, t8], writes=[eq])
        S.op("dve", lambda e: e.tensor_tensor(out=eq[:], in0=eq[:], in1=sc[:], op=ALU.mult), reads=[eq, sc], writes=[eq])
        S.op("dve", lambda e: e.reduce_sum(out=ws[:], in_=eq[:], axis=AX.X), reads=[eq], writes=[ws])
        S.op("dve", lambda e: e.reciprocal(out=ws[:], in_=ws[:]), reads=[ws], writes=[ws])
        S.op("dve", lambda e, gt=gt: e.tensor_scalar(out=gt[:, 0:64], in0=eq[:], scalar1=ws[:, 0:1], scalar2=ROUTED_SCALE, op0=ALU.mult, op1=ALU.mult), reads=[eq, ws], writes=[gt])
        S.dma(gates, gates[g0:g0 + 128, :], gt, gt[:])


ROUTED_SCALE = 2.5


def phase_moe_experts(C, layer, hT, gates, wbf, modv, xin, xout, tiles):
    S, cfg = C.S, C.cfg
    TSU = 8
    gtl = gate_tiles(C, "me_gt", modv, layer, 5)
    hs = S.sb("me_hs", [128, 8, TSU * 128], BF16)
    gs = S.sb("me_gs", [128, TSU, 65])
    acc = S.sb("me_acc", [128, TSU, 1024])
    wb_ = [S.sb("me_w%d" % i, [128, 6144], BF16) for i in range(3)]
    h1_ = [S.ps("me_h1_%d" % i, [128, 512]) for i in range(2)]
    h3_ = [S.ps("me_h3_%d" % i, [128, 512]) for i in range(2)]
    op_ = [S.ps("me_o%d" % i, [128, 512]) for i in range(3)]
    s1_ = [S.sb("me_s1_%d" % i, [128, 512]) for i in range(2)]
    hid_ = [S.sb("me_hid%d" % i, [128, 2, 512], BF16) for i in range(2)]
    xt_ = [S.sb("me_x%d" % i, [128, 1024]) for i in range(2)]
    hv = hT.t.rearrange("(k p) n -> p k n", p=128)
    nst = (len(tiles) + TSU - 1) // TSU
    cnt = 0
    oc = 0
    for st in range(nst):
        tl = tiles[st * TSU:(st + 1) * TSU]
        nt_ = len(tl)
        for j, ti in enumerate(tl):
            S.dma(hs, hs[:, :, j * 128:(j + 1) * 128], hT, hv[:, :, ti * 128:(ti + 1) * 128])
            S.dma(gs, gs[:, j, :], gates, gates[ti * 128:(ti + 1) * 128, :])
        S.op("pool", lambda e: e.memset(acc[:], 0.0), writes=[acc])
        items = []
        for e_ in range(65):
            wb = wb_[e_ % 3]
            for tb in range((nt_ + 3) // 4):
                items.append((e_, tb, wb))

        def H_groups(it, idx):
            e_, tb, wb = it
            ntok = min(4, nt_ - tb * 4) * 128
            hid = hid_[idx % 2]
            gl = []
            for j in range(2):
                h1, h3, s1 = h1_[j], h3_[j], s1_[j]

                def g1(j=j, h1=h1):
                    if tb == 0 and j == 0:
                        S.dma(wb, wb[:], wbf, wbf[e_])
                    for k in range(8):
                        S.op("pe", lambda e, k=k: e.matmul(h1[:, 0:ntok], wb[:, k * 256 + j * 128:k * 256 + (j + 1) * 128], hs[:, k, tb * 512:tb * 512 + ntok], start=(k == 0), stop=(k == 7)),
                             reads=[wb, hs], writes=[h1], inc=(k == 7))

                def g3(j=j, h1=h1, h3=h3, s1=s1):
                    for k in range(8):
                        S.op("pe", lambda e, k=k: e.matmul(h3[:, 0:ntok], wb[:, 2048 + k * 256 + j * 128:2048 + k * 256 + (j + 1) * 128], hs[:, k, tb * 512:tb * 512 + ntok], start=(k == 0), stop=(k == 7)),
                             reads=[wb, hs], writes=[h3], inc=(k == 7))
                    S.op("act", lambda e: e.activation(out=s1[:, 0:ntok], in_=h1[:, 0:ntok], func=AF.Silu), reads=[h1], writes=[s1])
                    S.op("dve", lambda e: e.tensor_tensor(out=hid[:, j, 0:ntok], in0=h3[:, 0:ntok], in1=s1[:, 0:ntok], op=ALU.mult), reads=[h3, s1], writes=[hid])
                gl += [g1, g3]
            return gl

        def O_groups(it, idx):
            nonlocal oc
            e_, tb, wb = it
            ntok = min(4, nt_ - tb * 4) * 128
            hid = hid_[idx % 2]
            gl = []
            for q in range(ntok // 128):
                tj = tb * 4 + q

                def go(q=q, tj=tj):
                    nonlocal oc
                    for half in range(2):
                        o = op_[oc % 3]
                        oc += 1
                        for j in range(2):
                            S.op("pe", lambda e, o=o, j=j, half=half: e.matmul(o[:], hid[:, j, q * 128:(q + 1) * 128], wb[:, 4096 + j * 1024 + half * 512:4096 + j * 1024 + (half + 1) * 512], start=(j == 0), stop=(j == 1)),
                                 reads=[hid, wb], writes=[o], inc=(j == 1))
                        S.op("dve", lambda e, o=o, half=half: e.scalar_tensor_tensor(out=acc[:, tj, half * 512:(half + 1) * 512], in0=o[:], scalar=gs[:, tj, e_:e_ + 1], in1=acc[:, tj, half * 512:(half + 1) * 512], op0=ALU.mult, op1=ALU.add),
                             reads=[o, gs, acc], writes=[acc])
                gl.append(go)
            return gl
        for g in H_groups(items[0], 0):
            g()
        for idx, it in enumerate(items):
            og = O_groups(it, idx)
            hg = H_groups(items[idx + 1], idx + 1) if idx + 1 < len(items) else []
            n = max(len(og), len(hg))
            for t in range(n):
                if t < len(hg):
                    hg[t]()
                if t < len(og):
                    og[t]()
        for j, ti in enumerate(tl):
            xt = xt_[j % 2]
            S.dma(xt, xt[:], xin, xin[ti * 128:(ti + 1) * 128, :])
            g = gtl[tile_row(cfg, ti)]
            S.op("pool", lambda e, j=j, g=g: e.tensor_tensor(out=acc[:, j, :], in0=acc[:, j, :], in1=g[:], op=ALU.mult), reads=[acc, g], writes=[acc])
            S.op("pool", lambda e, j=j, xt=xt: e.tensor_tensor(out=xt[:], in0=acc[:, j, :], in1=xt[:], op=ALU.add), reads=[acc, xt], writes=[xt])
            S.dma(xout, xout[ti * 128:(ti + 1) * 128, :], xt, xt[:])


def phase_mla_prep(C, layer, xin, gvec_buf, gvec_ap, modv, P, rope_cs, KT, QT, Vtok, ident, identb):
    S, cfg = C.S, C.cfg
    TCt = cfg.TC // 128
    tpb = cfg.T // 128
    mt = ModTiles(C, "mp_mt", modv, layer, 0, 1, gvec_ap, gvec_buf)
    nt = NormT(C, "mp_nt", ident)
    win = load_w_bf16(C, "mp_win", P["mla_w_in"], P["mla_w_in"][0].rearrange("(k p) c -> p k c", p=128), 672, 8)
    qup = load_w_bf16(C, "mp_qup", P["mla_q_up"], P["mla_q_up"][0].rearrange("(k p) c -> p k c", p=128), 1536, 3)
    kvup = load_w_bf16(C, "mp_kvup", P["mla_kv_up"], P["mla_kv_up"][0].rearrange("(k p) c -> p k c", p=128), 2048, 2)
    nb = S.sb("mp_nb", [128, 640])
    S.dma(nb, nb[:, 0:384], P["mla_q_norm"], P["mla_q_norm"][0, :].partition_broadcast(128))
    S.dma(nb, nb[:, 384:640], P["mla_kv_norm"], P["mla_kv_norm"][0, :].partition_broadcast(128))
    hb_ = [S.sb("mp_hb%d" % i, [128, 8, 128], BF16) for i in range(2)]
    A_ = S.ps("mp_A", [128, 1024])
    B_ = S.ps("mp_B", [128, 1024])
    Cp = S.ps("mp_C", [96, 16, 128], BF16)
    c_sb = S.sb("mp_c", [128, 672])
    junk = S.sb("mp_junk", [128, 384], BF16)
    ss = S.sb("mp_ss", [128, 2])
    cn = S.sb("mp_cn", [128, 640])
    cnT = S.sb("mp_cnT", [128, 5, 128], BF16)
    vt_ = [S.sb("mp_vt%d" % i, [128, 16, 64], BF16) for i in range(2)]
    Kf = S.sb("mp_Kf", [128, 16, 96], BF16)
    Qf = S.sb("mp_Qf", [128, 16, 96], BF16)
    q32 = S.sb("mp_q32", [128, 16, 96])
    cs_ = [S.sb("mp_cs%d" % i, [128, 32]) for i in range(2)]
    krr = S.sb("mp_krr", [128, 32])
    t1 = S.sb("mp_t1", [128, 16, 16])
    t2 = S.sb("mp_t2", [128, 16, 16])
    kT_ = [S.sb("mp_kT%d" % i, [96, 16, 128], BF16) for i in range(2)]
    qT_ = [S.sb("mp_qT%d" % i, [96, 16, 128], BF16) for i in range(2)]
    ktv = KT.t.rearrange("h d n -> d h n")
    qtv = QT.t.rearrange("h d n -> d h n")
    for ti in range(cfg.NTOK // 128):
        b, tt = divmod(ti, tpb)
        lat = tt >= TCt
        g0 = ti * 128
        hb, vt, kT, qT, cs = hb_[ti % 2], vt_[ti % 2], kT_[ti % 2], qT_[ti % 2], cs_[ti % 2]
        nt.tile(xin, g0, mt, tile_row(cfg, ti), [(hb, lambda k0, k1, hb=hb: hb[:, k0:k1, :])])
        for (c0, c1) in ((0, 512), (512, 672)):
            for k in range(8):
                S.op("pe", lambda e, k=k, c0=c0, c1=c1, hb=hb: e.matmul(B_[:, c0:c1], hb[:, k, :], win[:, k, c0:c1], start=(k == 0), stop=(k == 7)), reads=[hb, win], writes=[B_], inc=(k == 7))
        S.op("act", lambda e: e.copy(out=c_sb[:, 0:512], in_=B_[:, 0:512]), reads=[B_], writes=[c_sb])
        S.op("dve", lambda e: e.tensor_copy(out=c_sb[:, 512:672], in_=B_[:, 512:672]), reads=[B_], writes=[c_sb])
        S.op("act", lambda e: e.activation(out=junk[:, 0:384], in_=c_sb[:, 0:384], func=AF.Square, accum_out=ss[:, 0:1]), reads=[c_sb], writes=[junk, ss])
        S.op("act", lambda e: e.activation(out=junk[:, 0:256], in_=c_sb[:, 384:640], func=AF.Square, accum_out=ss[:, 1:2]), reads=[c_sb], writes=[junk, ss])
        S.op("act", lambda e: e.activation(out=ss[:, 0:1], in_=ss[:, 0:1], func=AF.Sqrt, bias=NORM_EPS, scale=1.0 / 384), reads=[ss], writes=[ss])
        S.op("act", lambda e: e.activation(out=ss[:, 1:2], in_=ss[:, 1:2], func=AF.Sqrt, bias=NORM_EPS, scale=1.0 / 256), reads=[ss], writes=[ss])
        S.op("dve", lambda e: e.reciprocal(out=ss[:], in_=ss[:]), reads=[ss], writes=[ss])
        S.op("dve", lambda e: e.scalar_tensor_tensor(out=cn[:, 0:384], in0=c_sb[:, 0:384], scalar=ss[:, 0:1], in1=nb[:, 0:384], op0=ALU.mult, op1=ALU.mult), reads=[c_sb, ss, nb], writes=[cn])
        S.op("dve", lambda e: e.scalar_tensor_tensor(out=cn[:, 384:640], in0=c_sb[:, 384:640], scalar=ss[:, 1:2], in1=nb[:, 384:640], op0=ALU.mult, op1=ALU.mult), reads=[c_sb, ss, nb], writes=[cn])
        Bv = B_[:, 0:640].rearrange("p (k n) -> p k n", k=5)
        for k in range(5):
            S.op("pe", lambda e, k=k, Bv=Bv: e.transpose(Bv[:, k, :], cn[:, k * 128:(k + 1) * 128], ident[:]), reads=[cn, ident], writes=[B_], inc=(k == 4))
        S.op("act", lambda e, Bv=Bv: e.copy(out=cnT[:], in_=Bv), reads=[B_], writes=[cnT])
        for ps_ in range(2):
            for blk in range(2):
                for kc in range(2):
                    S.op("pe", lambda e, ps_=ps_, blk=blk, kc=kc: e.matmul(A_[:, blk * 512:(blk + 1) * 512], cnT[:, 3 + kc, :], kvup[:, kc, ps_ * 1024 + blk * 512:ps_ * 1024 + (blk + 1) * 512], start=(kc == 0), stop=(kc == 1)),
                         reads=[cnT, kvup], writes=[A_], inc=(kc == 1))
            Av = A_[:].rearrange("p (h x) -> p h x", h=8)
            S.op("act", lambda e, ps_=ps_, Av=Av, vt=vt: e.copy(out=vt[:, ps_ * 8:(ps_ + 1) * 8, :], in_=Av[:, :, 64:128]), reads=[A_], writes=[vt])
            S.op("dve", lambda e, ps_=ps_, Av=Av: e.tensor_copy(out=Kf[:, ps_ * 8:(ps_ + 1) * 8, 0:64], in_=Av[:, :, 0:64]), reads=[A_], writes=[Kf])
        S.dma(Vtok, Vtok[g0:g0 + 128, :], vt, vt[:].rearrange("p h v -> p (h v)"))
        if lat:
            p0 = (tt - TCt) * 128
            S.dma(cs, cs[:], rope_cs, rope_cs[p0:p0 + 128, :])
            u1, u2 = c_sb[:, 640:656], c_sb[:, 656:672]
            S.op("dve", lambda e, cs=cs, u1=u1: e.tensor_tensor(out=t1[:, 0, :], in0=u1, in1=cs[:, 0:16], op=ALU.mult), reads=[c_sb, cs], writes=[t1])
            S.op("dve", lambda e, cs=cs, u2=u2: e.tensor_tensor(out=t2[:, 0, :], in0=u2, in1=cs[:, 16:32], op=ALU.mult), reads=[c_sb, cs], writes=[t2])
            S.op("dve", lambda e: e.tensor_tensor(out=krr[:, 0:16], in0=t1[:, 0, :], in1=t2[:, 0, :], op=ALU.subtract), reads=[t1, t2], writes=[krr])
            S.op("dve", lambda e, cs=cs, u1=u1: e.tensor_tensor(out=t1[:, 0, :], in0=u1, in1=cs[:, 16:32], op=ALU.mult), reads=[c_sb, cs], writes=[t1])
            S.op("dve", lambda e, cs=cs, u2=u2: e.tensor_tensor(out=t2[:, 0, :], in0=u2, in1=cs[:, 0:16], op=ALU.mult), reads=[c_sb, cs], writes=[t2])
            S.op("dve", lambda e: e.tensor_tensor(out=krr[:, 16:32], in0=t1[:, 0, :], in1=t2[:, 0, :], op=ALU.add), reads=[t1, t2], writes=[krr])
        else:
            S.op("dve", lambda e: e.tensor_copy(out=krr[:], in_=c_sb[:, 640:672]), reads=[c_sb], writes=[krr])
        S.op("dve", lambda e: e.tensor_copy(out=Kf[:, :, 64:96], in_=krr[:].unsqueeze(1).to_broadcast([128, 16, 32])), reads=[krr], writes=[Kf])
        for h in range(16):
            S.op("pe", lambda e, h=h: e.transpose(Cp[:, h, :], Kf[:, h, :], identb[:]), reads=[Kf, identb], writes=[Cp], inc=(h == 15))
        S.op("act", lambda e, kT=kT: e.copy(out=kT[:], in_=Cp[:]), reads=[Cp], writes=[kT])
        S.dma(KT, ktv[:, :, g0:g0 + 128], kT, kT[:])
        if lat:
            for ps_ in range(2):
                for (c0, c1) in ((0, 512), (512, 768)):
                    for kc in range(3):
                        S.op("pe", lambda e, ps_=ps_, c0=c0, c1=c1, kc=kc: e.matmul(A_[:, c0:c1], cnT[:, kc, :], qup[:, kc, ps_ * 768 + c0:ps_ * 768 + c1], start=(kc == 0), stop=(kc == 2)),
                             reads=[cnT, qup], writes=[A_], inc=(kc == 2))
                S.op("act", lambda e, ps_=ps_: e.copy(out=q32[:, ps_ * 8:(ps_ + 1) * 8, :], in_=A_[:, 0:768].rearrange("p (h x) -> p h x", h=8)), reads=[A_], writes=[q32])
            S.op("pool", lambda e: e.tensor_copy(out=Qf[:, :, 0:64], in_=q32[:, :, 0:64]), reads=[q32], writes=[Qf])
            U1, U2 = q32[:, :, 64:80], q32[:, :, 80:96]
            cosb = cs[:, 0:16].unsqueeze(1).to_broadcast([128, 16, 16])
            sinb = cs[:, 16:32].unsqueeze(1).to_broadcast([128, 16, 16])
            S.op("dve", lambda e, U1=U1, cosb=cosb: e.tensor_tensor(out=t1[:], in0=U1, in1=cosb, op=ALU.mult), reads=[q32, cs], writes=[t1])
            S.op("dve", lambda e, U2=U2, sinb=sinb: e.tensor_tensor(out=t2[:], in0=U2, in1=sinb, op=ALU.mult), reads=[q32, cs], writes=[t2])
            S.op("dve", lambda e: e.tensor_tensor(out=Qf[:, :, 64:80], in0=t1[:], in1=t2[:], op=ALU.subtract), reads=[t1, t2], writes=[Qf])
            S.op("dve", lambda e, U1=U1, sinb=sinb: e.tensor_tensor(out=t1[:], in0=U1, in1=sinb, op=ALU.mult), reads=[q32, cs], writes=[t1])
            S.op("dve", lambda e, U2=U2, cosb=cosb: e.tensor_tensor(out=t2[:], in0=U2, in1=cosb, op=ALU.mult), reads=[q32, cs], writes=[t2])
            S.op("dve", lambda e: e.tensor_tensor(out=Qf[:, :, 80:96], in0=t1[:], in1=t2[:], op=ALU.add), reads=[t1, t2], writes=[Qf])
            for h in range(16):
                S.op("pe", lambda e, h=h: e.transpose(Cp[:, h, :], Qf[:, h, :], identb[:]), reads=[Qf, identb], writes=[Cp], inc=(h == 15))
            S.op("act", lambda e, qT=qT: e.copy(out=qT[:], in_=Cp[:]), reads=[Cp], writes=[qT])
            S.dma(QT, qtv[:, :, g0:g0 + 128], qT, qT[:])


def phase_mla_attn(C, KT, QT, Vtok, AO):
    S, cfg = C.S, C.cfg
    NB, T, TC, TL = cfg.NB, cfg.T, cfg.TC, cfg.TL
    Tt = T // 128
    scale = (64 + 32) ** -0.5
    Ks_ = [S.sb("at_K%d" % i, [96, T], BF16) for i in range(2)]
    Qs_ = [S.sb("at_Q%d" % i, [96, TL], BF16) for i in range(2)]
    Vs_ = [S.sb("at_V%d" % i, [128, Tt, 65], BF16) for i in range(2)]
    for v in Vs_:
        S.op("pool", lambda e, v=v: e.memset(v[:, :, 64:65], 1.0), writes=[v])
    sp_ = [S.ps("at_s%d" % i, [128, 512]) for i in range(3)]
    E_ = [S.sb("at_E%d" % i, [128, 512], BF16) for i in range(3)]
    acc_ = [S.ps("at_acc%d" % i, [128, 4, 128]) for i in range(2)]
    rc = S.sb("at_rc", [128, 4, 1])
    ob_ = [S.sb("at_o%d" % i, [128, 4, 64], BF16) for i in range(2)]
    n = 0
    m = 0
    for b in range(NB):
        for h in range(16):
            Ks, Qs, Vs = Ks_[n % 2], Qs_[n % 2], Vs_[n % 2]
            n += 1
            S.dma(Ks, Ks[:], KT, KT[h, :, b * T:(b + 1) * T])
            S.dma(Qs, Qs[:], QT, QT[h, :, b * T + TC:(b + 1) * T])
            S.dma(Vs, Vs[:, :, 0:64], Vtok, Vtok[b * T:(b + 1) * T, h * 64:(h + 1) * 64].rearrange("(k p) d -> p k d", p=128))
            for qb in range(TL // 512):
                acc = acc_[qb % 2]

                def score(kt, Ks=Ks, Qs=Qs, qb=qb):
                    nonlocal m
                    sp, E = sp_[m % 3], E_[m % 3]
                    m += 1
                    S.op("pe", lambda e, sp=sp, kt=kt: e.matmul(sp[:], Ks[:, kt * 128:(kt + 1) * 128], Qs[:, qb * 512:(qb + 1) * 512], start=True, stop=True), reads=[Ks, Qs], writes=[sp])
                    return sp, E
                cur = score(0)
                for kt in range(Tt):
                    nxt = score(kt + 1) if kt + 1 < Tt else None
                    sp, E = cur
                    S.op("act", lambda e, sp=sp, E=E: e.activation(out=E[:], in_=sp[:], func=AF.Exp, scale=scale), reads=[sp], writes=[E])
                    for i in range(4):
                        S.op("pe", lambda e, acc=acc, E=E, Vs=Vs, kt=kt, i=i: e.matmul(acc[:, i, 0:65], E[:, i * 128:(i + 1) * 128], Vs[:, kt, :], start=(kt == 0 and i == 0), stop=(kt == Tt - 1), skip_group_check=True),
                             reads=[E, Vs], writes=[acc], inc=(i == 3))
                    cur = nxt
                S.op("dve", lambda e, acc=acc: e.reciprocal(out=rc[:], in_=acc[:, :, 64:65]), reads=[acc], writes=[rc])
                ob = ob_[qb % 2]
                S.op("dve", lambda e, acc=acc, ob=ob: e.tensor_tensor(out=ob[:], in0=acc[:, :, 0:64], in1=rc[:].to_broadcast([128, 4, 64]), op=ALU.mult), reads=[acc, rc], writes=[ob])
                r0 = b * T + TC + qb * 512
                S.dma(AO, AO[r0:r0 + 512, h * 64:(h + 1) * 64].rearrange("(i p) d -> p i d", p=128), ob, ob[:])


import ml_dtypes

N_CORES = 8
PARAM_SHAPES = dict(
    c=None, c_ctx=[1024], ada_w=[2, 1024, 6144], ada_b=[2, 6144], norm_mix=[2, 1024], norm_ffn=[2, 1024],
    ev_w_in=[1, 1024, 4512], ev_w_out=[1, 1536, 1024], rw_mu=[1, 1920], rw_w0=[1, 2, 512], rw_w2=[1, 2, 64, 512],
    rw_a0=[1, 2, 512], rw_a2=[1, 2, 64, 512], rw_g2=[1, 128, 512], rw_kk=[1, 512], rw_ka=[1, 512], rw_rk=[1, 512],
    rw_gn_w=[1, 512], rw_gn_b=[1, 512], ssm_conv_w=[1, 5, 1536], ssm_conv_b=[1, 1536], ssm_dt_bias=[1, 2, 16],
    ssm_a_log=[1, 2, 16], ssm_d=[1, 16], ssm_norm_w=[1, 1024], mla_w_in=[1, 1024, 672], mla_q_norm=[1, 384],
    mla_q_up=[1, 384, 1536], mla_kv_norm=[1, 256], mla_kv_up=[1, 256, 2048], mla_w_out=[1, 1024, 1024],
    router_w=[2, 1024, 64], router_bias=[2, 64], exp_w1=[2, 64, 1024, 256], exp_w3=[2, 64, 1024, 256],
    exp_w2=[2, 64, 256, 1024], sh_w1=[2, 1024, 256], sh_w3=[2, 1024, 256], sh_w2=[2, 256, 1024], final_norm=[1024])


def host_consts(cfg):
    K = {}
    bf = ml_dtypes.bfloat16
    K["ident"] = np.eye(128, dtype=np.float32)
    K["identb"] = np.eye(128).astype(bf)
    K["onesblk"] = np.kron(np.eye(2), np.ones((64, 64))).astype(np.float32)
    NQ = 8 * cfg.NB * 64 // 512
    Eq = np.zeros((128, NQ, 2 * NQ), np.float32)
    for p in range(128):
        for q in range(NQ):
            Eq[p, q, 2 * q + p // 64] = 1
    K["Eq"] = Eq
    l = np.arange(128)
    K["tri0"] = (l[:, None] <= l[None, :]).astype(np.float32)
    K["tri1"] = (l[:, None] >= l[None, :]).astype(np.float32)
    K["ones"] = np.ones((128, 128), np.float32)
    blk = np.zeros((8, 8, 128), np.float32)
    for h in range(8):
        blk[h, h, :] = 1
    K["blk"] = blk.reshape(8, 1024)
    K["ones8"] = np.ones((8, 128), np.float32)
    m0 = np.where(l[None, :] >= l[:, None], 0.0, -30000.0)
    m1 = np.where(l[None, :] <= l[:, None], 0.0, -30000.0)
    K["mneg0"] = np.tile(m0[:, None, :], (1, 4, 1)).reshape(128, 512).astype(bf)
    K["mneg1"] = np.tile(m1[:, None, :], (1, 4, 1)).reshape(128, 512).astype(bf)
    rows = cfg.TL // 64
    r_idx, c_idx = np.meshgrid(np.arange(rows), np.arange(64), indexing='ij')
    r_idx = r_idx.reshape(-1).astype(np.float32)
    c_idx = c_idx.reshape(-1).astype(np.float32)
    inv_freq = (10000.0 ** (-np.arange(0, 16, 2, dtype=np.float32) / 16)).astype(np.float32)
    ang = np.concatenate([r_idx[:, None] * inv_freq, c_idx[:, None] * inv_freq], -1).astype(np.float32)
    K["rope_cs"] = np.concatenate([np.cos(ang), np.sin(ang)], 1).astype(np.float32)
    return K


def build_program(cfg, kconst, dbg=()):
    nc = bass.Bass("TRN2", target_bir_lowering=False)
    NB, NTOK, T = cfg.NB, cfg.NTOK, cfg.T
    with ExitStack() as es:
        S = Sched(nc, es)
        io = {k: "ExternalInput" for k in PARAM_SHAPES}
        io.update(xin="ExternalInput", out="ExternalOutput")
        io.update({"K_" + k: "ExternalInput" for k in kconst})
        io.update({k: "ExternalOutput" for k in dbg})
        C = Ctx(S, cfg, io)
        P = {}
        for k, shp in PARAM_SHAPES.items():
            P[k] = C.D(k, [NB, 1024] if k == "c" else shp)
        xin = C.D("xin", [NTOK, 1024])
        out = C.D("out", [NB * cfg.TL, 1024])
        K = {}
        for k, v in kconst.items():
            dt_ = BF16 if v.dtype == ml_dtypes.bfloat16 else F32
            dd = C.D("K_" + k, list(v.shape), dt_)
            if k == "rope_cs":
                K[k] = dd
                continue
            sbt = S.sb("Ksb_" + k, list(v.shape), dt_)
            S.dma(sbt, sbt[:], dd, dd.t)
            K[k] = sbt
        ident, identb, onesblk = K["ident"], K["identb"], K["onesblk"]
        NQ = 8 * NB * 64 // 512
        modv = C.D("modv", [2, NB + 1, 6144])
        colsT = C.D("colsT", [4512, NTOK])
        A = {k: C.D("A_" + k, [512, NTOK]) for k in ["kk", "r", "bonus", "g", "w0", "w1", "b0", "b1", "kd0", "kd1"]}
        vtok = C.D("vtok", [NTOK, 512], BF16)
        YD = C.D("YD", [2 * NQ, T, 512])
        mixT = C.D("mixT", [1536, NTOK], BF16)
        xbc_tok = C.D("xbc_tok", [NTOK, 1536])
        BCT = C.D("BCT", [512, NTOK], BF16)
        dtda = C.D("dtda_tok", [NTOK, 64])
        ytmp = [C.D("ytmp%d" % d, [NTOK, 1024]) for d in range(2)]
        xr = [C.D("xr%d" % i, [NTOK, 1024]) for i in range(4)]
        hT = C.D("hT", [1024, NTOK], BF16)
        gates = C.D("gates", [NTOK, 65])
        wbf = C.D("wbf", [65, 128, 6144], BF16)
        KT = C.D("KT", [16, 96, NTOK], BF16)
        QT = C.D("QT", [16, 96, NTOK], BF16)
        Vtok = C.D("Vtok", [NTOK, 1024], BF16)
        AO = C.D("AO", [NTOK, 1024], BF16)
        EW = {k: P[k] for k in ("exp_w1", "exp_w3", "exp_w2", "sh_w1", "sh_w3", "sh_w2")}
        all_tiles = list(range(NTOK // 128))
        lt = lat_tiles(cfg)
        with S.phase():
            phase_mod(C, P["c"], P["c_ctx"], P["ada_w"], P["ada_b"], modv)
        with S.phase():
            phase_normproj(C, "np0", xin, P["norm_mix"], P["norm_mix"][0, :], modv, 0, P["ev_w_in"], P["ev_w_in"][0], 4512, colsT, ident)
        with S.phase():
            phase_rwprep(C, colsT, P, A, onesblk, ident, vtok)
        with S.phase():
            phase_rwscan(C, A, vtok, YD, onesblk, identb, K["Eq"])
        with S.phase():
            phase_rwpost(C, YD, A, P, mixT, ident)
        with S.phase():
            phase_ssprep(C, colsT, P, xbc_tok, BCT, dtda, ident)
        with S.phase():
            phase_ssscan(C, xbc_tok, BCT, dtda, ytmp, K)
        with S.phase():
            phase_sspost(C, ytmp, xbc_tok, colsT, P, mixT, ident)
        with S.phase():
            phase_outproj(C, "op0", mixT, False, 1536, P["ev_w_out"], P["ev_w_out"][0], modv, 0, xin, xr[0], all_tiles, identb)
        with S.phase():
            phase_moe_cast(C, 0, EW, wbf)
        with S.phase():
            phase_moe_route(C, 0, xr[0], P["norm_ffn"], P["norm_ffn"][0, :], modv, P["router_w"], P["router_bias"], hT, gates, all_tiles, ident)
        with S.phase():
            phase_moe_experts(C, 0, hT, gates, wbf, modv, xr[0], xr[1], all_tiles)
        with S.phase():
            phase_mla_prep(C, 1, xr[1], P["norm_mix"], P["norm_mix"][1, :], modv, P, K["rope_cs"], KT, QT, Vtok, ident, identb)
        with S.phase():
            phase_mla_attn(C, KT, QT, Vtok, AO)
        with S.phase():
            phase_outproj(C, "op1", AO, True, 1024, P["mla_w_out"], P["mla_w_out"][0], modv, 1, xr[1], xr[2], lt, identb)
        with S.phase():
            phase_moe_cast(C, 1, EW, wbf)
        with S.phase():
            phase_moe_route(C, 1, xr[2], P["norm_ffn"], P["norm_ffn"][1, :], modv, P["router_w"], P["router_bias"], hT, gates, lt, ident)
        with S.phase():
            phase_moe_experts(C, 1, hT, gates, wbf, modv, xr[2], xr[3], lt)
        with S.phase():
            phase_final(C, xr[3], P["final_norm"], out)
        S.emit(final_bufs=[out] + [b for b in S.bufs if b.space == "dram" and b.name in dbg])
        build_program.stats = (S.nins, S.nsem)
        build_program.marks = S.marks
    return nc


def kernel(**inputs):
    cfg = Cfg(4, 2048, 256)
    kconst = host_consts(cfg)
    nc = build_program(cfg, kconst)
    x = np.asarray(inputs["x"], np.float32)
    ctx = np.asarray(inputs["ctx"], np.float32)
    c = np.asarray(inputs["c"], np.float32)
    in_maps = []
    shared = {k: np.ascontiguousarray(np.asarray(inputs[k], np.float32)) for k in PARAM_SHAPES if k != "c"}
    shared.update({"K_" + k: v for k, v in kconst.items()})
    for i in range(N_CORES):
        sl = slice(i * cfg.NB, (i + 1) * cfg.NB)
        m = dict(shared)
        m["xin"] = np.ascontiguousarray(np.concatenate([ctx[sl], x[sl]], axis=1).reshape(cfg.NTOK, 1024))
        m["c"] = np.ascontiguousarray(c[sl])
        in_maps.append(m)
    res = run_bass_kernel_spmd(nc, in_maps, core_ids=list(range(N_CORES)))
    outs = [np.asarray(r["out"]).reshape(cfg.NB, cfg.TL, 1024) for r in res.results]
    return np.concatenate(outs, axis=0).astype(np.float32)
```

```python
import numpy as np
import concourse.bass as bass
import concourse.mybir as mybir
from concourse.bass_utils import run_bass_kernel_spmd
from contextlib import ExitStack

F32 = mybir.dt.float32
BF16 = mybir.dt.bfloat16
I32 = mybir.dt.int32
U32 = mybir.dt.uint32
ALU = mybir.AluOpType
AF = mybir.ActivationFunctionType
AX = mybir.AxisListType

ENGS = ("pe", "act", "dve", "pool", "sp")


class Buf:
    def __init__(self, S, name, t, space):
        self.S = S
        self.name = name
        self.t = t
        self.space = space
        self.w = []
        self.r = []
        self.dsem = None
        self.dcnt = 0

    def __getitem__(self, k):
        return self.t[k]

    def ap(self):
        return self.t.ap() if hasattr(self.t, "ap") else self.t[:]


class View:
    def __init__(self, buf, ap):
        self.buf, self.t = buf, ap

    def __getitem__(self, k):
        return self.t[k]


def _b(x):
    return x.buf if isinstance(x, View) else x


class _Phase:
    def __init__(self, S):
        self.S = S

    def __enter__(self):
        S = self.S
        self.prev = S.cur
        self.nb = len(S.bufs)
        self.prev_ds = S.phase_dsems
        S.phase_dsems = []
        self.es = ExitStack()
        self.es.__enter__()
        S.cur = self.es
        return self

    def __exit__(self, *a):
        S = self.S
        S.marks.append({e: sum(1 for o in S.ops[e] if o[1] is not None) for e in ENGS})
        S.barrier()
        S.free_dsems.extend(S.phase_dsems)
        S.phase_dsems = self.prev_ds
        S.bufs = S.bufs[:self.nb] + [b for b in S.bufs[self.nb:] if b.space == "dram"]
        S.cur = self.prev
        self.es.__exit__(*a)
        return False


class Sched:
    def __init__(self, nc, es):
        self.nc = nc
        self.es = es
        self.ops = {e: [] for e in ENGS}
        self.sems = {}
        self.eng_sem = {}
        self.eng_cnt = {e: 0 for e in ENGS}
        self.seen = {e: {} for e in ENGS}
        self.nsem = 0
        self.nins = 0
        self.cur = es
        self.bufs = []
        self.sem_val = {}
        self.free_dsems = []
        self.phase_dsems = []
        self.pending = {e: False for e in ENGS}
        self.marks = []
        for e in ("pe", "act", "dve", "pool"):
            self.eng_sem[e] = self.new_sem("prog_" + e)

    def new_sem(self, name):
        h = self.es.enter_context(self.nc.semaphore(name))
        sid = self.nsem
        self.nsem += 1
        self.sems[sid] = h
        return sid

    def sb(self, name, shape, dt=F32):
        self.uid = getattr(self, "uid", 0) + 1
        name = "%s_u%d" % (name, self.uid)
        t = self.cur.enter_context(self.nc.sbuf_tensor(name, list(shape), dt))
        b = Buf(self, name, t, "sb")
        self.bufs.append(b)
        return b

    def ps(self, name, shape, dt=F32):
        self.uid = getattr(self, "uid", 0) + 1
        name = "%s_u%d" % (name, self.uid)
        t = self.cur.enter_context(self.nc.psum_tensor(name, list(shape), dt))
        b = Buf(self, name, t, "ps")
        self.bufs.append(b)
        return b

    def dram(self, name, shape, dt=F32, kind="Internal"):
        t = self.nc.dram_tensor(name, list(shape), dt, kind=kind).ap()
        b = Buf(self, name, t, "dram")
        self.bufs.append(b)
        return b

    def alloc_dsem(self, name):
        if self.free_dsems:
            sid = self.free_dsems.pop()
        else:
            sid = self.new_sem("dma%d" % self.nsem)
            self.sem_val[sid] = 0
        self.phase_dsems.append(sid)
        return sid

    def barrier(self):
        assert not any(self.pending.values()), "un-signalled PE group at barrier"
        targets = []
        for e in ("pe", "act", "dve", "pool"):
            if self.eng_cnt[e]:
                targets.append((self.eng_sem[e], self.eng_cnt[e]))
        for sid, v in self.sem_val.items():
            if v:
                targets.append((sid, v))
        for e in ENGS:
            waits = []
            for (sid, v) in targets:
                if self.seen[e].get(sid, 0) < v:
                    self.seen[e][sid] = v
                    waits.append((sid, v))
            if waits:
                self.ops[e].append((waits, None, None, 0))
        for b in self.bufs:
            b.w = []
            b.r = []

    def phase(self):
        return _Phase(self)

    def _waits(self, eng, reads, writes):
        need = {}
        for b in reads:
            for (s, v) in b.w:
                need[s] = max(need.get(s, 0), v)
            if b.space == "ps":
                for (s, v) in b.r:
                    need[s] = max(need.get(s, 0), v)
        for b in writes:
            if not (b.space == "dram" and eng in ("sp", "pool_dma", "act_dma")):
                for (s, v) in b.w:
                    need[s] = max(need.get(s, 0), v)
            for (s, v) in b.r:
                need[s] = max(need.get(s, 0), v)
        out = []
        seen = self.seen[eng]
        for s, v in need.items():
            if seen.get(s, 0) >= v:
                continue
            seen[s] = v
            out.append((s, v))
        return out

    def _record(self, ev, reads, writes):
        for b in writes:
            b.w = [ev]
            b.r = []
        for b in reads:
            if b in writes:
                continue
            b.r = [(s, v) for (s, v) in b.r if s != ev[0]] + [ev]

    def op(self, eng, fn, reads=(), writes=(), inc=True, skip_self=False):
        reads = [_b(x) for x in reads]
        writes = [_b(x) for x in writes]
        waits = self._waits(eng, reads, writes)
        if eng == "pe" or skip_self:
            waits = [(s_, v_) for (s_, v_) in waits if s_ != self.eng_sem[eng]]
        if inc:
            self.eng_cnt[eng] += 1
            ev = (self.eng_sem[eng], self.eng_cnt[eng])
            self.pending[eng] = False
        else:
            assert eng == "pe"
            ev = (self.eng_sem[eng], self.eng_cnt[eng] + 1)
            self.pending[eng] = True
        self.ops[eng].append((waits, fn, ev, 1 if inc else 0))
        self._record(ev, reads, writes)
        self.nins += 1

    def dma(self, out_buf, out_ap, in_buf, in_ap, eng="sp", **kw):
        out_buf, in_buf = _b(out_buf), _b(in_buf)
        own = out_buf if out_buf.space != "dram" else in_buf
        if own.dsem is None:
            own.dsem = self.alloc_dsem(own.name)
            own.dcnt = self.sem_val[own.dsem]
        waits = self._waits(eng, [in_buf], [out_buf])
        same_gen = (out_buf.space != "dram" and not out_buf.r and out_buf.w and all(s_ == own.dsem for (s_, _) in out_buf.w))
        if (not same_gen) and own.dcnt > 0 and self.seen[eng].get(own.dsem, 0) < own.dcnt:
            self.seen[eng][own.dsem] = own.dcnt
            waits.append((own.dsem, own.dcnt))
        own.dcnt += 16
        self.sem_val[own.dsem] = own.dcnt
        ev = (own.dsem, own.dcnt)

        def fn(e, out_ap=out_ap, in_ap=in_ap, kw=kw):
            return e.dma_start(out=out_ap, in_=in_ap, **kw)
        self.ops[eng].append((waits, fn, ev, 16))
        if out_buf.space == "dram":
            out_buf.w = [(s, v) for (s, v) in out_buf.w if s != ev[0]] + [ev]
            out_buf.r = []
            in_buf.r = [(s, v) for (s, v) in in_buf.r if s != ev[0]] + [ev]
        else:
            self._record(ev, [in_buf], [out_buf])
        self.nins += 1

    def reset_dram(self, b):
        b.w = []
        b.r = []

    def emit(self, final_bufs=()):
        nc = self.nc
        fw = {}
        for b in final_bufs:
            for (s, v) in b.w:
                fw[s] = max(fw.get(s, 0), v)
        for e in ("pe", "act", "dve", "pool"):
            if self.eng_cnt[e]:
                fw[self.eng_sem[e]] = self.eng_cnt[e]
        sems = self.sems
        ops = self.ops
        with nc.Block() as block:
            def run(engobj, lst):
                for (waits, fn, ev, inc) in lst:
                    for (s, v) in waits:
                        engobj.wait_ge(sems[s], v)
                    if fn is not None:
                        ins = fn(engobj)
                        if inc:
                            ins.then_inc(sems[ev[0]], inc)

            @block.sync
            def _(e):
                run(e, ops["sp"])
                for s, v in fw.items():
                    e.wait_ge(sems[s], v)

            @block.tensor
            def _(e):
                run(e, ops["pe"])

            @block.scalar
            def _(e):
                run(e, ops["act"])

            @block.vector
            def _(e):
                run(e, ops["dve"])

            @block.gpsimd
            def _(e):
                run(e, ops["pool"])


class Cfg:
    def __init__(self, NB=4, TL=2048, TC=256):
        self.NB, self.TL, self.TC = NB, TL, TC
        self.T = TL + TC
        self.NTOK = NB * self.T
        self.D = 1024


class Ctx:
    def __init__(self, S, cfg, io):
        self.S, self.cfg, self.io = S, cfg, io
        self.rr = 0

    def D(self, name, shape, dt=F32):
        kind = self.io.get(name, "Internal")
        return self.S.dram(name, shape, dt, kind=kind)


def alt(i):
    return "act" if i % 2 == 0 else "dve"


def evac(S, i, out_ap, in_ap, reads, writes):
    if i % 2 == 0:
        S.op("act", lambda e: e.copy(out=out_ap, in_=in_ap), reads=reads, writes=writes)
    else:
        S.op("dve", lambda e: e.tensor_copy(out=out_ap, in_=in_ap), reads=reads, writes=writes)


def phase_mod(C, c_in, cctx_in, ada_w, ada_b, modv):
    S, cfg = C.S, C.cfg
    NB = cfg.NB
    R = NB + 1
    cT = S.sb("mod_cT", [128, 8, R])
    for b in range(NB):
        S.dma(cT, cT[:, :, b:b + 1], c_in, c_in[b, :].rearrange("(k p o) -> p k o", p=128, o=1), allow_slow_non_contiguous=True)
    S.dma(cT, cT[:, :, NB:R], cctx_in, cctx_in.t.rearrange("(k p o) -> p k o", p=128, o=1), allow_slow_non_contiguous=True)
    sT = S.sb("mod_sT", [128, 8, R])
    S.op("act", lambda e: e.activation(out=sT[:], in_=cT[:], func=AF.Silu), reads=[cT], writes=[sT])
    wbufs = [S.sb("mod_w%d" % i, [128, 8, 512]) for i in range(2)]
    pss = [S.ps("mod_ps%d" % i, [R, 512]) for i in range(2)]
    bias = S.sb("mod_bias", [R, 6144])
    orow = S.sb("mod_orow", [R, 6144])
    for l in range(2):
        S.dma(bias, bias[:], ada_b, ada_b[l, :].partition_broadcast(R))
        wv = ada_w[l].rearrange("(k p) c -> p k c", p=128)
        for j in range(12):
            wb = wbufs[j % 2]
            ps = pss[j % 2]
            S.dma(wb, wb[:], ada_w, wv[:, :, j * 512:(j + 1) * 512])
            for k in range(8):
                S.op("pe", lambda e, ps=ps, wb=wb, k=k: e.matmul(ps[:], sT[:, k, :], wb[:, k, :], start=(k == 0), stop=(k == 7)),
                     reads=[sT, wb], writes=[ps], inc=(k == 7))
            S.op("dve", lambda e, ps=ps, j=j: e.tensor_tensor(out=orow[:, j * 512:(j + 1) * 512], in0=ps[:], in1=bias[:, j * 512:(j + 1) * 512], op=ALU.add),
                 reads=[ps, bias], writes=[orow])
        S.dma(modv, modv[l], orow, orow[:])


class ModTiles:
    def __init__(self, C, name, modv, layer, shift_idx, scale_idx, gvec_ap, gvec_buf):
        S, cfg = C.S, C.cfg
        R = cfg.NB + 1
        D = cfg.D
        self.G = [S.sb("%s_G%d" % (name, r), [128, D]) for r in range(R)]
        self.Sh = [S.sb("%s_S%d" % (name, r), [128, D]) for r in range(R)]
        gb = S.sb("%s_g" % name, [128, D])
        S.dma(gb, gb[:], gvec_buf, gvec_ap.partition_broadcast(128))
        for r in range(R):
            G, Sh = self.G[r], self.Sh[r]
            S.dma(G, G[:], modv, modv[layer, r, scale_idx * D:(scale_idx + 1) * D].partition_broadcast(128))
            S.dma(Sh, Sh[:], modv, modv[layer, r, shift_idx * D:(shift_idx + 1) * D].partition_broadcast(128))
            S.op("dve", lambda e, G=G: e.scalar_tensor_tensor(out=G[:], in0=G[:], scalar=1.0, in1=gb[:], op0=ALU.add, op1=ALU.mult),
                 reads=[G, gb], writes=[G])

    def row(self, cfg, tile_idx):
        tpb = cfg.T // 128
        b, tt = divmod(tile_idx, tpb)
        return cfg.NB if tt < cfg.TC // 128 else b


class NormT:
    def __init__(self, C, name, ident, want32=False):
        S = C.S
        self.C, self.name, self.ident = C, name, ident
        D = C.cfg.D
        self.xt = [S.sb("%s_xt%d" % (name, i), [128, D]) for i in range(2)]
        self.junk = S.sb("%s_junk" % name, [128, D], BF16)
        self.ss = [S.sb("%s_ss%d" % (name, i), [128, 1]) for i in range(2)]
        self.h = [S.sb("%s_h%d" % (name, i), [128, D]) for i in range(2)]
        self.pt = [S.ps("%s_pt%d" % (name, i), [128, 4, 128]) for i in range(2)]
        self.n = 0

    def tile(self, xsrc, row0, mt, r, outs):
        S = self.C.S
        D = self.C.cfg.D
        i = self.n % 2
        self.n += 1
        xt, ss, h = self.xt[i], self.ss[i], self.h[i]
        S.dma(xt, xt[:], xsrc, xsrc[row0:row0 + 128, :])
        junk = self.junk
        S.op("act", lambda e: e.activation(out=junk[:], in_=xt[:], func=AF.Square, accum_out=ss[:]), reads=[xt], writes=[junk, ss])
        S.op("act", lambda e: e.activation(out=ss[:], in_=ss[:], func=AF.Sqrt, bias=NORM_EPS, scale=1.0 / D), reads=[ss], writes=[ss])
        S.op("dve", lambda e: e.reciprocal(out=ss[:], in_=ss[:]), reads=[ss], writes=[ss])
        G, Sh = mt.G[r], mt.Sh[r]
        S.op("dve", lambda e: e.scalar_tensor_tensor(out=h[:], in0=xt[:], scalar=ss[:, 0:1], in1=G[:], op0=ALU.mult, op1=ALU.mult),
             reads=[xt, ss, G], writes=[h])
        S.op("pool", lambda e: e.tensor_tensor(out=h[:], in0=h[:], in1=Sh[:], op=ALU.add), reads=[h, Sh], writes=[h])
        ident = self.ident
        for half in range(2):
            pt = self.pt[half]
            for kk in range(4):
                k = half * 4 + kk
                S.op("pe", lambda e, pt=pt, kk=kk, k=k: e.transpose(pt[:, kk, :], h[:, k * 128:(k + 1) * 128], ident[:]),
                     reads=[h, ident], writes=[pt], inc=(kk == 3))
            for oi, (ob, ofn) in enumerate(outs):
                evac(S, half + oi, ofn(half * 4, half * 4 + 4), pt[:], [pt], [ob])


NORM_EPS = 1e-6


def load_w_bf16(C, name, wsrc_buf, wv, ncols, kch):
    S = C.S
    wb = S.sb(name, [128, kch, ncols], BF16)
    CH = max(128, min(512, 2048 // kch))
    stg = [S.sb("%s_stg%d" % (name, i), [128, kch, CH]) for i in range(2)]
    nchunk = (ncols + CH - 1) // CH
    for j in range(nchunk):
        c0 = j * CH
        c1 = min(ncols, c0 + CH)
        st = stg[j % 2]
        S.dma(st, st[:, :, 0:c1 - c0], wsrc_buf, wv[:, :, c0:c1])
        S.op("pool", lambda e, st=st, c0=c0, c1=c1: e.tensor_copy(out=wb[:, :, c0:c1], in_=st[:, :, 0:c1 - c0]), reads=[st], writes=[wb])
    return wb


def phase_normproj(C, name, xsrc, gvec_buf, gvec_ap, modv, layer, W_buf, W_ap, ncols, outT, ident):
    S, cfg = C.S, C.cfg
    mt = ModTiles(C, name + "_mt", modv, layer, 0, 1, gvec_ap, gvec_buf)
    wb = load_w_bf16(C, name + "_w", W_buf, W_ap.rearrange("(k p) c -> p k c", p=128), ncols, 8)
    nt = NormT(C, name + "_nt", ident)
    hT = [S.sb("%s_hT%d" % (name, i), [128, 8, 512], BF16) for i in range(2)]
    pss = [S.ps("%s_ps%d" % (name, i), [128, 512]) for i in range(3)]
    ost = [S.sb("%s_ost%d" % (name, i), [128, 512]) for i in range(3)]
    ntl = cfg.NTOK // 128
    nsb = (ntl + 3) // 4
    ncj = (ncols + 127) // 128
    cnt = 0
    for sb in range(nsb):
        ht = hT[sb % 2]
        nti = min(4, ntl - sb * 4)
        n = nti * 128
        for ti in range(nti):
            tile_idx = sb * 4 + ti
            r = mt.row(cfg, tile_idx)
            nt.tile(xsrc, tile_idx * 128, mt, r, [(ht, lambda k0, k1, ti=ti, ht=ht: ht[:, k0:k1, ti * 128:(ti + 1) * 128])])
        for j in range(ncj):
            c0 = j * 128
            cw = min(128, ncols - c0)
            ps = pss[cnt % 3]
            ob = ost[cnt % 3]
            for k in range(8):
                S.op("pe", lambda e, ps=ps, k=k, c0=c0, cw=cw, ht=ht, n=n: e.matmul(ps[0:cw, 0:n], wb[:, k, c0:c0 + cw], ht[:, k, 0:n], start=(k == 0), stop=(k == 7)),
                     reads=[wb, ht], writes=[ps], inc=(k == 7))
            evac(S, cnt, ob[0:cw, 0:n], ps[0:cw, 0:n], [ps], [ob])
            S.dma(outT, outT[c0:c0 + cw, sb * 512:sb * 512 + n], ob, ob[0:cw, 0:n])
            cnt += 1


def seg_blocks(cfg, blk=512):
    out = []
    for b in range(cfg.NB):
        for (s0, s1) in ((0, cfg.TC), (cfg.TC, cfg.T)):
            t = s0
            while t < s1:
                n = min(blk, s1 - t)
                out.append((b, t, n, t > s0, t + n < s1))
                t += n
    return out


def vec_cols(C, name, src_buf, src_ap_1d, nch):
    S = C.S
    t = S.sb(name, [128, nch])
    S.dma(t, t[:], src_buf, src_ap_1d.rearrange("(c p) -> p c", p=128), allow_slow_non_contiguous=True)
    return t


def phase_rwprep(C, colsT, P, A, onesblk, ident, vtok):
    S, cfg = C.S, C.cfg
    T = cfg.T
    mu = vec_cols(C, "rp_mu", P["rw_mu"], P["rw_mu"][0, :], 15)
    w0 = [vec_cols(C, "rp_w0%d" % d, P["rw_w0"], P["rw_w0"][0, d, :], 4) for d in range(2)]
    a0 = [vec_cols(C, "rp_a0%d" % d, P["rw_a0"], P["rw_a0"][0, d, :], 4) for d in range(2)]
    kkv = vec_cols(C, "rp_kkv", P["rw_kk"], P["rw_kk"][0, :], 4)
    kav = vec_cols(C, "rp_kav", P["rw_ka"], P["rw_ka"][0, :], 4)
    rkv = vec_cols(C, "rp_rkv", P["rw_rk"], P["rw_rk"][0, :], 4)
    omka = S.sb("rp_omka", [128, 4])
    S.op("dve", lambda e: e.tensor_scalar(out=omka[:], in0=kav[:], scalar1=-1.0, scalar2=1.0, op0=ALU.mult, op1=ALU.add), reads=[kav], writes=[omka])
    W2 = S.sb("rp_W2", [128, 512])
    A2 = S.sb("rp_A2", [128, 512])
    G2 = S.sb("rp_G2", [128, 512])
    for d in range(2):
        S.dma(W2, W2[d * 64:(d + 1) * 64, :], P["rw_w2"], P["rw_w2"][0, d, :, :])
        S.dma(A2, A2[d * 64:(d + 1) * 64, :], P["rw_a2"], P["rw_a2"][0, d, :, :])
    S.dma(G2, G2[:], P["rw_g2"], P["rw_g2"][0, :, :])
    NB_ = 512
    raw = [S.sb("rp_raw%d" % i, [128, NB_ + 2]) for i in range(3)]
    MX = [S.sb("rp_mx%d" % c, [128, NB_]) for c in range(15)]
    tmp = [S.sb("rp_tmp%d" % i, [128, NB_]) for i in range(2)]
    TH = S.sb("rp_th", [128, NB_])
    SG = S.sb("rp_sg", [128, NB_])
    Aa = [[S.sb("rp_a%d_%d" % (d, cc), [128, NB_]) for cc in range(4)] for d in range(2)]
    KD = [[S.sb("rp_kd%d_%d" % (d, cc), [128, NB_]) for cc in range(4)] for d in range(2)]
    KK = [S.sb("rp_kk%d" % cc, [128, NB_]) for cc in range(4)]
    ost = [S.sb("rp_ost%d" % i, [128, NB_]) for i in range(4)]
    pss = [S.ps("rp_ps%d" % i, [128, NB_]) for i in range(4)]
    vtb = [S.sb("rp_vtb%d" % i, [128, 512], BF16) for i in range(2)]
    oc = [0]
    pc = [0]

    def nps():
        pc[0] += 1
        return pss[pc[0] % 4]

    def nost():
        oc[0] += 1
        return ost[oc[0] % 4]

    def store(arr, cc, b, t0, n, buf, ap):
        S.dma(arr, arr[cc * 128:(cc + 1) * 128, b * T + t0:b * T + t0 + n], buf, ap)

    for bi, (b, t0, n, hl, hr) in enumerate(seg_blocks(cfg, NB_)):
        g0 = b * T + t0
        for c in range(15):
            rw = raw[c % 3]
            if not hl:
                S.op("pool", lambda e, rw=rw: e.memset(rw[:, 0:1], 0.0), writes=[rw])
            if not hr:
                S.op("pool", lambda e, rw=rw, n=n: e.memset(rw[:, n + 1:n + 2], 0.0), writes=[rw])
            lo = g0 - (1 if hl else 0)
            hi = g0 + n + (1 if hr else 0)
            S.dma(rw, rw[:, (0 if hl else 1):(0 if hl else 1) + hi - lo], colsT, colsT[c * 128:(c + 1) * 128, lo:hi])
            tp = tmp[c % 2]
            mx = MX[c]
            S.op("dve", lambda e, rw=rw, tp=tp, n=n: e.tensor_tensor(out=tp[:, 0:n], in0=rw[:, 0:n], in1=rw[:, 2:n + 2], op=ALU.add), reads=[rw], writes=[tp])
            S.op("dve", lambda e, rw=rw, tp=tp, n=n: e.scalar_tensor_tensor(out=tp[:, 0:n], in0=tp[:, 0:n], scalar=0.5, in1=rw[:, 1:n + 1], op0=ALU.mult, op1=ALU.subtract),
                 reads=[rw, tp], writes=[tp])
            S.op("dve", lambda e, rw=rw, tp=tp, n=n, c=c, mx=mx: e.scalar_tensor_tensor(out=mx[:, 0:n], in0=tp[:, 0:n], scalar=mu[:, c:c + 1], in1=rw[:, 1:n + 1], op0=ALU.mult, op1=ALU.add),
                 reads=[rw, tp, mu], writes=[mx])
        for cc in range(4):
            store(A["r"], cc, b, t0, n, MX[cc], MX[cc][:, 0:n])
        for j in range(n // 128):
            ps = nps()
            for cc in range(4):
                S.op("pe", lambda e, ps=ps, cc=cc, j=j: e.transpose(ps[:, cc * 128:(cc + 1) * 128], MX[8 + cc][:, j * 128:(j + 1) * 128], ident[:]),
                     reads=[MX[8 + cc], ident], writes=[ps], inc=(cc == 3))
            vb = vtb[j % 2]
            evac(S, j, vb[:], ps[:, 0:512], [ps], [vb])
            S.dma(vtok, vtok[g0 + j * 128:g0 + (j + 1) * 128, :], vb, vb[:])
        for cc in range(4):
            kk = KK[cc]
            tp = tmp[cc % 2]
            S.op("dve", lambda e, cc=cc, kk=kk, n=n: e.tensor_scalar(out=kk[:, 0:n], in0=MX[4 + cc][:, 0:n], scalar1=kkv[:, cc:cc + 1], scalar2=None, op0=ALU.mult),
                 reads=[MX[4 + cc], kkv], writes=[kk])
            S.op("pool", lambda e, kk=kk, tp=tp, n=n: e.tensor_tensor(out=tp[:, 0:n], in0=kk[:, 0:n], in1=kk[:, 0:n], op=ALU.mult), reads=[kk], writes=[tp])
            ps = nps()
            S.op("pe", lambda e, ps=ps, tp=tp, n=n: e.matmul(ps[:, 0:n], onesblk[:], tp[:, 0:n], start=True, stop=True), reads=[onesblk, tp], writes=[ps])
            S.op("act", lambda e, ps=ps, tp=tp, n=n: e.activation(out=tp[:, 0:n], in_=ps[:, 0:n], func=AF.Sqrt, bias=1e-12, scale=1.0), reads=[ps], writes=[tp])
            S.op("dve", lambda e, tp=tp, n=n: e.reciprocal(out=tp[:, 0:n], in_=tp[:, 0:n]), reads=[tp], writes=[tp])
            S.op("dve", lambda e, kk=kk, tp=tp, n=n: e.tensor_tensor(out=kk[:, 0:n], in0=kk[:, 0:n], in1=tp[:, 0:n], op=ALU.mult), reads=[kk, tp], writes=[kk])
            store(A["kk"], cc, b, t0, n, kk, kk[:, 0:n])
        S.op("act", lambda e, n=n: e.activation(out=TH[:, 0:n], in_=MX[12][:, 0:n], func=AF.Tanh), reads=[MX[12]], writes=[TH])
        for d in range(2):
            for cc in range(4):
                ps = nps()
                S.op("pe", lambda e, ps=ps, d=d, cc=cc, n=n: e.matmul(ps[:, 0:n], W2[d * 64:(d + 1) * 64, cc * 128:(cc + 1) * 128], TH[d * 64:(d + 1) * 64, 0:n], start=True, stop=True),
                     reads=[W2, TH], writes=[ps])
                ob = nost()
                S.op("act", lambda e, ps=ps, ob=ob, d=d, cc=cc, n=n: e.activation(out=ob[:, 0:n], in_=ps[:, 0:n], func=AF.Sigmoid, bias=w0[d][:, cc:cc + 1], scale=1.0),
                     reads=[ps, w0[d]], writes=[ob])
                S.op("act", lambda e, ob=ob, n=n: e.activation(out=ob[:, 0:n], in_=ob[:, 0:n], func=AF.Exp, scale=-0.6065306597126334), reads=[ob], writes=[ob])
                store(A["w%d" % d], cc, b, t0, n, ob, ob[:, 0:n])
        for d in range(2):
            for cc in range(4):
                ps = nps()
                S.op("pe", lambda e, ps=ps, d=d, cc=cc, n=n: e.matmul(ps[:, 0:n], A2[d * 64:(d + 1) * 64, cc * 128:(cc + 1) * 128], MX[13][d * 64:(d + 1) * 64, 0:n], start=True, stop=True),
                     reads=[A2, MX[13]], writes=[ps])
                aa = Aa[d][cc]
                S.op("act", lambda e, ps=ps, aa=aa, d=d, cc=cc, n=n: e.activation(out=aa[:, 0:n], in_=ps[:, 0:n], func=AF.Sigmoid, bias=a0[d][:, cc:cc + 1], scale=1.0),
                     reads=[ps, a0[d]], writes=[aa])
                ob = nost()
                S.op("pool", lambda e, ob=ob, aa=aa, cc=cc, n=n: e.tensor_tensor(out=ob[:, 0:n], in0=KK[cc][:, 0:n], in1=aa[:, 0:n], op=ALU.mult), reads=[KK[cc], aa], writes=[ob])
                store(A["b%d" % d], cc, b, t0, n, ob, ob[:, 0:n])
                kd = KD[d][cc]
                S.op("dve", lambda e, kd=kd, aa=aa, cc=cc, n=n: e.tensor_scalar(out=kd[:, 0:n], in0=aa[:, 0:n], scalar1=kav[:, cc:cc + 1], scalar2=omka[:, cc:cc + 1], op0=ALU.mult, op1=ALU.add),
                     reads=[aa, kav, omka], writes=[kd])
                S.op("dve", lambda e, kd=kd, cc=cc, n=n: e.tensor_tensor(out=kd[:, 0:n], in0=kd[:, 0:n], in1=MX[4 + cc][:, 0:n], op=ALU.mult), reads=[kd, MX[4 + cc]], writes=[kd])
                store(A["kd%d" % d], cc, b, t0, n, kd, kd[:, 0:n])
        for cc in range(4):
            tp = tmp[cc % 2]
            S.op("pool", lambda e, tp=tp, cc=cc, n=n: e.tensor_tensor(out=tp[:, 0:n], in0=KD[0][cc][:, 0:n], in1=KD[1][cc][:, 0:n], op=ALU.add), reads=[KD[0][cc], KD[1][cc]], writes=[tp])
            S.op("dve", lambda e, tp=tp, cc=cc, n=n: e.scalar_tensor_tensor(out=tp[:, 0:n], in0=tp[:, 0:n], scalar=rkv[:, cc:cc + 1], in1=MX[cc][:, 0:n], op0=ALU.mult, op1=ALU.mult),
                 reads=[tp, rkv, MX[cc]], writes=[tp])
            ps = nps()
            S.op("pe", lambda e, ps=ps, tp=tp, n=n: e.matmul(ps[:, 0:n], onesblk[:], tp[:, 0:n], start=True, stop=True), reads=[onesblk, tp], writes=[ps])
            ob = nost()
            S.op("dve", lambda e, ps=ps, ob=ob, cc=cc, n=n: e.tensor_tensor(out=ob[:, 0:n], in0=ps[:, 0:n], in1=MX[8 + cc][:, 0:n], op=ALU.mult), reads=[ps, MX[8 + cc]], writes=[ob])
            store(A["bonus"], cc, b, t0, n, ob, ob[:, 0:n])
        S.op("act", lambda e, n=n: e.activation(out=SG[:, 0:n], in_=MX[14][:, 0:n], func=AF.Sigmoid), reads=[MX[14]], writes=[SG])
        for cc in range(4):
            ps = nps()
            S.op("pe", lambda e, ps=ps, cc=cc, n=n: e.matmul(ps[:, 0:n], G2[:, cc * 128:(cc + 1) * 128], SG[:, 0:n], start=True, stop=True), reads=[G2, SG], writes=[ps])
            ob = nost()
            evac(S, cc, ob[:, 0:n], ps[:, 0:n], [ps], [ob])
            store(A["g"], cc, b, t0, n, ob, ob[:, 0:n])


SCAN_ACT = False


def cust_ap(base_ap, dims):
    pa = base_ap.ap[0]
    return bass.AP(base_ap.tensor, base_ap.offset, [[pa[0], pa[1]]] + [[st, ct] for (st, ct) in dims])


def phase_rwscan(C, A, vtok, YD, onesblk, identb, Eq):
    S, cfg = C.S, C.cfg
    NB, T, TC = cfg.NB, cfg.T, cfg.TC
    NCB = 4 * NB
    Fh = NCB * 64
    NQh = Fh // 512
    NQ = 2 * NQh
    TB = 64
    YB = 4

    def st(name):
        return [S.sb("%s%d" % (name, d), [128, Fh]) for d in range(2)]
    M, MW, TMP, TMP2, TMP3, T4 = st("sc_M"), st("sc_MW"), st("sc_TMP"), st("sc_TMP2"), st("sc_TMP3"), st("sc_T4")
    saPS = [S.ps("sc_saPS%d" % d, [128, Fh]) for d in range(2)]
    vPS = S.ps("sc_vPS", [128, Fh])
    yPS = S.ps("sc_yPS", [2 * NQ, 512])
    OPS = [S.sb("sc_OPS%d" % i, [128, 5, 2, NCB, TB]) for i in range(2)]
    VT = [S.sb("sc_VT%d" % i, [TB, 2, 2, NCB, 64], BF16) for i in range(2)]
    YS = [S.sb("sc_YS%d" % i, [2 * NQ, YB, 512]) for i in range(2)]
    for d in range(2):
        S.op("pool", lambda e, d=d: e.memset(M[d][:], 0.0), writes=[M[d]])

    def v3(buf):
        return buf[:].rearrange("p (c v) -> p c v", c=NCB)

    def tb_of(s):
        return TC - 1 - s if s < TC else T + TC - 1 - s

    def readout(sp, ops_p, col_p):
        for d, eng in ((0, "dve"), (1, "pool")):
            base = ops_p[:, 4, d, 0, col_p[d]:col_p[d] + 1]
            Ro = cust_ap(base, [(TB, NCB), (0, 64)])
            S.op(eng, lambda e, d=d, Ro=Ro: e.tensor_tensor(out=v3(T4[d]), in0=v3(M[d]), in1=Ro, op=ALU.mult), reads=[M[d], ops_p], writes=[T4[d]])
        for d in range(2):
            for q in range(NQh):
                qg = d * NQh + q
                S.op("pe", lambda e, d=d, q=q, qg=qg: e.matmul(yPS[:, :], Eq[:, qg, :], T4[d][:, q * 512:(q + 1) * 512], start=(qg == 0), stop=(qg == NQ - 1)),
                     reads=[Eq, T4[d]], writes=[yPS], inc=(qg == NQ - 1))
        ys = YS[(sp // YB) % 2]
        S.op("act", lambda e, ys=ys, sp=sp: e.copy(out=ys[:, sp % YB, :], in_=yPS[:, :]), reads=[yPS], writes=[ys])
        if sp % YB == YB - 1:
            sa_ = sp - YB + 1
            S.dma(YD, YD[0:NQ, sa_:sa_ + YB, :], ys, ys[0:NQ, :, :])
            tlo = tb_of(sp)
            S.dma(YD, YD[NQ:2 * NQ, tlo:tlo + YB, :][:, ::-1, :], ys, ys[NQ:2 * NQ, :, :])

    names = [("kk", "kk"), ("w0", "w1"), ("b0", "b1"), ("kd0", "kd1"), ("r", "r")]
    prev = None
    for blk in range(T // TB):
        s0 = blk * TB
        ops = OPS[blk % 2]
        vt = VT[blk % 2]
        lo = tb_of(s0 + TB - 1)
        tok0 = (s0, lo)
        for a_, nm in enumerate(names):
            for d in range(2):
                arr = A[nm[d]]
                av = arr.t.rearrange("(c p) (b t) -> c p b t", p=128, b=NB)
                for c in range(4):
                    S.dma(ops, ops[:, a_, d, c * NB:(c + 1) * NB, :], arr, av[c, :, :, tok0[d]:tok0[d] + TB])
        vv = vtok.t.rearrange("(b t) (c h v) -> b t c h v", b=NB, c=4, h=2)
        for d in range(2):
            for c in range(4):
                for b in range(NB):
                    S.dma(vt, vt[:, :, d, c * NB + b, :], vtok, vv[b, tok0[d]:tok0[d] + TB, c, :, :])
        for j in range(TB):
            s = s0 + j
            col = (j, tb_of(s) - lo)

            def opnd(a_, d, ops=ops, col=col):
                base = ops[:, a_, d, 0, col[d]:col[d] + 1]
                return cust_ap(base, [(TB, NCB), (0, 64)])
            for d in range(2):
                sel = identb[0:TB, col[d]:col[d] + 1].to_broadcast([TB, 64])
                for h2 in range(2):
                    vsrc = vt[:, h2, d].rearrange("t c v -> t (c v)")
                    for q in range(NQh):
                        S.op("pe", lambda e, h2=h2, q=q, sel=sel, vsrc=vsrc: e.matmul(vPS[h2 * 64:(h2 + 1) * 64, q * 512:(q + 1) * 512], sel, vsrc[:, q * 512:(q + 1) * 512], start=True, stop=True),
                             reads=[identb, vt], writes=[vPS], inc=(h2 == 1 and q == NQh - 1))
                KDo = opnd(3, d)
                S.op("dve", lambda e, d=d, KDo=KDo: e.tensor_tensor(out=v3(TMP3[d]), in0=v3(vPS), in1=KDo, op=ALU.mult), reads=[vPS, ops], writes=[TMP3[d]])
            for d in range(2):
                Wo = opnd(1, d)
                S.op("pool", lambda e, d=d, Wo=Wo: e.tensor_tensor(out=v3(MW[d]), in0=v3(M[d]), in1=Wo, op=ALU.mult), reads=[M[d], ops], writes=[MW[d]])
            for d in range(2):
                KKo = opnd(0, d)
                S.op("dve", lambda e, d=d, KKo=KKo: e.tensor_tensor(out=v3(TMP[d]), in0=v3(M[d]), in1=KKo, op=ALU.mult), reads=[M[d], ops], writes=[TMP[d]])
                for q in range(NQh):
                    S.op("pe", lambda e, d=d, q=q: e.matmul(saPS[d][:, q * 512:(q + 1) * 512], onesblk[:], TMP[d][:, q * 512:(q + 1) * 512], start=True, stop=True),
                         reads=[onesblk, TMP[d]], writes=[saPS[d]], inc=(q == NQh - 1))
            if prev is not None:
                readout(*prev)
            for d in range(2):
                Bo = opnd(2, d)
                S.op("dve", lambda e, d=d, Bo=Bo: e.tensor_tensor(out=v3(TMP2[d]), in0=v3(saPS[d]), in1=Bo, op=ALU.mult), reads=[saPS[d], ops], writes=[TMP2[d]])
            for d in range(2):
                S.op("pool", lambda e, d=d: e.tensor_tensor(out=MW[d][:], in0=MW[d][:], in1=TMP3[d][:], op=ALU.add), reads=[MW[d], TMP3[d]], writes=[MW[d]])
            for d in range(2):
                S.op("dve", lambda e, d=d: e.tensor_tensor(out=M[d][:], in0=MW[d][:], in1=TMP2[d][:], op=ALU.subtract), reads=[MW[d], TMP2[d]], writes=[M[d]])
            prev = (s, ops, col)
    readout(*prev)


def phase_rwpost(C, YD, A, P, mixT, ident):
    S, cfg = C.S, C.cfg
    NB, T = cfg.NB, cfg.T
    gnw = vec_cols(C, "rq_gnw", P["rw_gn_w"], P["rw_gn_w"][0, :], 4)
    gnb = vec_cols(C, "rq_gnb", P["rw_gn_b"], P["rw_gn_b"][0, :], 4)
    Y = [S.sb("rq_Y%d" % i, [128, 2, 4, 2, 64]) for i in range(2)]
    ysum = S.sb("rq_ysum", [128, 8, 64])
    sq = S.sb("rq_sq", [128, 8, 64])
    st1 = S.sb("rq_st1", [128, 8])
    st2 = S.sb("rq_st2", [128, 8])
    BG = [S.sb("rq_BG%d" % i, [128, 2, 4, 128]) for i in range(2)]
    pt = S.ps("rq_pt", [128, 4, 128])
    o32 = S.sb("rq_o32", [128, 4, 128])
    ob = [S.sb("rq_ob%d" % i, [128, 4, 128], BF16) for i in range(2)]
    bv = A["bonus"].t.rearrange("(c p) n -> p c n", p=128)
    gv = A["g"].t.rearrange("(c p) n -> p c n", p=128)
    mv = mixT.t[0:512, :].rearrange("(c p) n -> p c n", p=128)
    for ti in range(cfg.NTOK // 128):
        b, tt = divmod(ti, T // 128)
        t0 = tt * 128
        g0 = ti * 128
        y = Y[ti % 2]
        bg = BG[ti % 2]
        for d in range(2):
            for c in range(4):
                n0 = ((d * 4 + c) * NB + b) * 64
                q, col0 = n0 // 512, n0 % 512
                for h2 in range(2):
                    S.dma(y, y[:, d, c, h2, :], YD, YD[2 * q + h2, t0:t0 + 128, col0:col0 + 64])
        S.dma(bg, bg[:, 0], A["bonus"], bv[:, :, g0:g0 + 128])
        S.dma(bg, bg[:, 1], A["g"], gv[:, :, g0:g0 + 128])
        yf = y[:, 0].rearrange("p c h v -> p (c h) v")
        yb = y[:, 1].rearrange("p c h v -> p (c h) v")
        S.op("dve", lambda e, yf=yf, yb=yb: e.tensor_tensor(out=ysum[:], in0=yf, in1=yb, op=ALU.add), reads=[y], writes=[ysum])
        S.op("dve", lambda e: e.reduce_sum(out=st1[:], in_=ysum[:], axis=AX.X), reads=[ysum], writes=[st1])
        S.op("dve", lambda e: e.tensor_scalar(out=st1[:], in0=st1[:], scalar1=1.0 / 64, scalar2=None, op0=ALU.mult), reads=[st1], writes=[st1])
        S.op("dve", lambda e: e.tensor_tensor(out=ysum[:], in0=ysum[:], in1=st1[:].unsqueeze(2).to_broadcast([128, 8, 64]), op=ALU.subtract), reads=[ysum, st1], writes=[ysum])
        S.op("pool", lambda e: e.tensor_tensor(out=sq[:], in0=ysum[:], in1=ysum[:], op=ALU.mult), reads=[ysum], writes=[sq])
        S.op("dve", lambda e: e.reduce_sum(out=st2[:], in_=sq[:], axis=AX.X), reads=[sq], writes=[st2])
        S.op("act", lambda e: e.activation(out=st2[:], in_=st2[:], func=AF.Sqrt, bias=RW_GN_EPS, scale=1.0 / 64), reads=[st2], writes=[st2])
        S.op("dve", lambda e: e.reciprocal(out=st2[:], in_=st2[:]), reads=[st2], writes=[st2])
        S.op("dve", lambda e: e.tensor_tensor(out=ysum[:], in0=ysum[:], in1=st2[:].unsqueeze(2).to_broadcast([128, 8, 64]), op=ALU.mult), reads=[ysum, st2], writes=[ysum])
        for c in range(4):
            S.op("pe", lambda e, c=c: e.transpose(pt[:, c, :], ysum[:, 2 * c:2 * c + 2, :].rearrange("p h v -> p (h v)"), ident[:]), reads=[ysum, ident], writes=[pt], inc=(c == 3))
        for c in range(4):
            S.op("dve", lambda e, c=c: e.tensor_scalar(out=o32[:, c, :], in0=pt[:, c, :], scalar1=gnw[:, c:c + 1], scalar2=gnb[:, c:c + 1], op0=ALU.mult, op1=ALU.add),
                 reads=[pt, gnw, gnb], writes=[o32])
        S.op("pool", lambda e, bg=bg: e.tensor_tensor(out=o32[:], in0=o32[:], in1=bg[:, 0], op=ALU.add), reads=[o32, bg], writes=[o32])
        o = ob[ti % 2]
        S.op("dve", lambda e, bg=bg, o=o: e.tensor_tensor(out=o[:], in0=o32[:], in1=bg[:, 1], op=ALU.mult), reads=[o32, bg], writes=[o])
        S.dma(mixT, mv[:, :, g0:g0 + 128], o, o[:])


RW_GN_EPS = 64e-5


def phase_ssprep(C, colsT, P, xbc_tok, BCT, dtda_tok, ident):
    S, cfg = C.S, C.cfg
    T = cfg.T
    cw = S.sb("sp_cw", [128, 12, 5])
    for j in range(5):
        S.dma(cw, cw[:, :, j:j + 1], P["ssm_conv_w"], P["ssm_conv_w"][0, j, :].rearrange("(c p o) -> p c o", p=128, o=1), allow_slow_non_contiguous=True)
    cb = vec_cols(C, "sp_cb", P["ssm_conv_b"], P["ssm_conv_b"][0, :], 12)
    dtb = S.sb("sp_dtb", [64, 1])
    aneg = S.sb("sp_aneg", [64, 1])
    dbv = P["ssm_dt_bias"][0].rearrange("d (h o) -> (d h) o", o=1)
    alv = P["ssm_a_log"][0].rearrange("d (h o) -> (d h) o", o=1)
    S.dma(dtb, dtb[0:32, :], P["ssm_dt_bias"], dbv, allow_slow_non_contiguous=True)
    S.dma(dtb, dtb[32:64, :], P["ssm_dt_bias"], dbv, allow_slow_non_contiguous=True)
    S.dma(aneg, aneg[32:64, :], P["ssm_a_log"], alv, allow_slow_non_contiguous=True)
    S.op("act", lambda e: e.activation(out=aneg[32:64, :], in_=aneg[32:64, :], func=AF.Exp), reads=[aneg], writes=[aneg])
    S.op("dve", lambda e: e.tensor_scalar(out=aneg[32:64, :], in0=aneg[32:64, :], scalar1=-1.0, scalar2=None, op0=ALU.mult), reads=[aneg], writes=[aneg])
    NB_ = 512
    raw = [S.sb("sp_raw%d" % i, [128, NB_ + 4]) for i in range(3)]
    acc = [S.sb("sp_acc%d" % i, [128, NB_]) for i in range(2)]
    XC = [S.sb("sp_xc%d" % c, [128, NB_]) for c in range(12)]
    bcb = [S.sb("sp_bcb%d" % i, [128, NB_], BF16) for i in range(2)]
    DD = S.sb("sp_dd", [64, NB_])
    pss = [S.ps("sp_ps%d" % i, [128, 512]) for i in range(3)]
    pdd = S.ps("sp_pdd", [128, 64])
    tk = [S.sb("sp_tk%d" % i, [128, 1536]) for i in range(2)]
    dk = [S.sb("sp_dk%d" % i, [128, 64]) for i in range(2)]
    pc = [0]
    for (b, t0, n, hl, hr) in seg_blocks(cfg, NB_):
        g0 = b * T + t0
        for c in range(12):
            rw = raw[c % 3]
            nl = 2 if hl else 0
            nr = 2 if hr else 0
            if not hl:
                S.op("pool", lambda e, rw=rw: e.memset(rw[:, 0:2], 0.0), writes=[rw])
            if not hr:
                S.op("pool", lambda e, rw=rw, n=n: e.memset(rw[:, n + 2:n + 4], 0.0), writes=[rw])
            S.dma(rw, rw[:, 2 - nl:2 + n + nr], colsT, colsT[2944 + c * 128:2944 + (c + 1) * 128, g0 - nl:g0 + n + nr])
            ac = acc[c % 2]
            S.op("dve", lambda e, rw=rw, ac=ac, c=c, n=n: e.tensor_scalar(out=ac[:, 0:n], in0=rw[:, 0:n], scalar1=cw[:, c, 0:1], scalar2=None, op0=ALU.mult), reads=[rw, cw], writes=[ac])
            for j in range(1, 5):
                eng = "dve"
                S.op(eng, lambda e, rw=rw, ac=ac, c=c, n=n, j=j: e.scalar_tensor_tensor(out=ac[:, 0:n], in0=rw[:, j:j + n], scalar=cw[:, c, j:j + 1], in1=ac[:, 0:n], op0=ALU.mult, op1=ALU.add),
                     reads=[rw, cw, ac], writes=[ac])
            xc = XC[c]
            S.op("act", lambda e, ac=ac, xc=xc, c=c, n=n: e.activation(out=xc[:, 0:n], in_=ac[:, 0:n], func=AF.Silu, bias=cb[:, c:c + 1], scale=1.0), reads=[ac, cb], writes=[xc])
            if c >= 8:
                bb = bcb[c % 2]
                S.op("pool", lambda e, bb=bb, xc=xc, n=n: e.tensor_copy(out=bb[:, 0:n], in_=xc[:, 0:n]), reads=[xc], writes=[bb])
                S.dma(BCT, BCT[(c - 8) * 128:(c - 7) * 128, g0:g0 + n], bb, bb[:, 0:n])
        S.dma(DD, DD[0:32, 0:n], colsT, colsT[4480:4512, g0:g0 + n])
        S.dma(DD, DD[32:64, 0:n], colsT, colsT[4480:4512, g0:g0 + n])
        S.op("act", lambda e, n=n: e.activation(out=DD[:, 0:n], in_=DD[:, 0:n], func=AF.Exp, bias=dtb[:, 0:1], scale=1.0), reads=[DD, dtb], writes=[DD])
        S.op("act", lambda e, n=n: e.activation(out=DD[:, 0:n], in_=DD[:, 0:n], func=AF.Ln, bias=1.0, scale=1.0), reads=[DD], writes=[DD])
        S.op("dve", lambda e, n=n: e.tensor_scalar(out=DD[32:64, 0:n], in0=DD[32:64, 0:n], scalar1=aneg[32:64, 0:1], scalar2=None, op0=ALU.mult), reads=[DD, aneg], writes=[DD])
        for j in range(n // 128):
            tkb = tk[j % 2]
            for q in range(3):
                ps = pss[pc[0] % 3]
                pc[0] += 1
                for cc in range(4):
                    c = q * 4 + cc
                    S.op("pe", lambda e, ps=ps, cc=cc, c=c, j=j: e.transpose(ps[:, cc * 128:(cc + 1) * 128], XC[c][:, j * 128:(j + 1) * 128], ident[:]), reads=[XC[c], ident], writes=[ps], inc=(cc == 3))
                evac(S, q, tkb[:, q * 512:(q + 1) * 512], ps[:], [ps], [tkb])
            S.dma(xbc_tok, xbc_tok[g0 + j * 128:g0 + (j + 1) * 128, :], tkb, tkb[:])
            S.op("pe", lambda e, j=j: e.transpose(pdd[:, :], DD[:, j * 128:(j + 1) * 128], ident[0:64, 0:64]), reads=[DD, ident], writes=[pdd])
            dkb = dk[j % 2]
            S.op("act", lambda e, dkb=dkb: e.copy(out=dkb[:], in_=pdd[:]), reads=[pdd], writes=[dkb])
            S.dma(dtda_tok, dtda_tok[g0 + j * 128:g0 + (j + 1) * 128, :], dkb, dkb[:])


def phase_ssscan(C, xbc_tok, BCT, dtda_tok, ytmp, K):
    S, cfg = C.S, C.cfg
    NB, T, TC = cfg.NB, cfg.T, cfg.TC
    Tt, TCt = T // 128, TC // 128
    XT = [S.sb("ss_xt%d" % i, [128, 1536]) for i in range(2)]
    DT = [S.sb("ss_dt%d" % i, [128, 64]) for i in range(2)]
    BC = [S.sb("ss_bc%d" % i, [128, 4, 128], BF16) for i in range(2)]
    miscPS = S.ps("ss_miscPS", [128, 512])
    cPS = View(miscPS, miscPS[:, 0:32])
    csTPS = View(miscPS, miscPS[0:8, 32:288].rearrange("p (g l) -> p g l", g=2))
    cbPS = View(miscPS, miscPS[:, 288:416])
    segPS = [S.ps("ss_segPS%d" % i, [128, 4, 128]) for i in range(2)]
    ydPS = S.ps("ss_ydPS", [128, 1024])
    yoPS = S.ps("ss_yoPS", [128, 512])
    dsPS = S.ps("ss_dsPS", [128, 512])
    c_sb = S.sb("ss_c", [128, 16])
    dfs = S.sb("ss_dfs", [128, 16])
    cdb = S.sb("ss_cdb", [128, 16])
    dte = S.sb("ss_dte", [128, 16])
    dtdte = S.sb("ss_dtdte", [128, 16])
    negcsT = S.sb("ss_negcsT", [8, 2, 128])
    csdiag = S.sb("ss_csdiag", [8, 2, 1024])
    xdt = S.sb("ss_xdt", [128, 16, 64], BF16)
    xdd = S.sb("ss_xdd", [128, 16, 64], BF16)
    btok = S.sb("ss_btok", [128, 2, 128], BF16)
    cbt = [S.sb("ss_cbt%d" % i, [128, 128], BF16) for i in range(2)]
    Eb = [S.sb("ss_E%d" % i, [128, 4, 128], BF16) for i in range(2)]
    Gb = [S.sb("ss_G%d" % i, [128, 4, 128], BF16) for i in range(2)]
    yacc = [S.sb("ss_yacc%d" % i, [128, 1024]) for i in range(2)]
    S32 = [S.sb("ss_S32_%d" % g, [128, 512]) for g in range(2)]
    Sbf = [S.sb("ss_Sbf_%d" % g, [128, 512], BF16) for g in range(2)]
    bcv = BCT.t.rearrange("(j p) n -> p j n", p=128)
    it = 0
    for d in range(2):
        tri = K["tri%d" % d]
        mneg = K["mneg%d" % d]
        for b in range(NB):
            for g in range(2):
                S.op("pool", lambda e, g=g: e.memset(S32[g][:], 0.0), writes=[S32[g]])
                S.op("pool", lambda e, g=g: e.memset(Sbf[g][:], 0.0), writes=[Sbf[g]])
            order = list(range(Tt)) if d == 0 else (list(range(TCt - 1, -1, -1)) + list(range(Tt - 1, TCt - 1, -1)))
            for tt in order:
                g0 = b * T + tt * 128
                xt, dtt, bc = XT[it % 2], DT[it % 2], BC[it % 2]
                ya = yacc[it % 2]
                it += 1
                S.dma(xt, xt[:], xbc_tok, xbc_tok[g0:g0 + 128, :])
                S.dma(dtt, dtt[:], dtda_tok, dtda_tok[g0:g0 + 128, :])
                S.dma(bc, bc[:], BCT, bcv[:, :, g0:g0 + 128])
                da = dtt[:, 32 + d * 16:32 + d * 16 + 16]
                dtd = dtt[:, d * 16:d * 16 + 16]
                S.op("pe", lambda e, da=da, tri=tri: e.matmul(cPS[:, 0:16], tri[:], da, start=True, stop=True), reads=[tri, dtt], writes=[cPS])
                S.op("pe", lambda e, da=da: e.matmul(cPS[:, 16:32], K["ones"][:], da, start=True, stop=True), reads=[K["ones"], dtt], writes=[cPS])
                for g in range(2):
                    S.op("pe", lambda e, g=g, dtt=dtt, d=d, tri=tri: e.matmul(csTPS[:, g, :], dtt[:, 32 + d * 16 + g * 8:32 + d * 16 + g * 8 + 8], tri[:], start=True, stop=True),
                         reads=[tri, dtt], writes=[csTPS])
                S.op("act", lambda e: e.copy(out=c_sb[:], in_=cPS[:, 0:16]), reads=[cPS], writes=[c_sb])
                S.op("act", lambda e: e.activation(out=dfs[:], in_=cPS[:, 0:16], func=AF.Exp), reads=[cPS], writes=[dfs])
                S.op("act", lambda e: e.activation(out=cdb[:], in_=cPS[:, 16:32], func=AF.Exp), reads=[cPS], writes=[cdb])
                S.op("dve", lambda e: e.tensor_tensor(out=dte[:], in0=cPS[:, 16:32], in1=c_sb[:], op=ALU.subtract), reads=[cPS, c_sb], writes=[dte])
                S.op("act", lambda e: e.activation(out=dte[:], in_=dte[:], func=AF.Exp), reads=[dte], writes=[dte])
                S.op("dve", lambda e, dtd=dtd: e.tensor_tensor(out=dtdte[:], in0=dte[:], in1=dtd, op=ALU.mult), reads=[dte, dtt], writes=[dtdte])
                S.op("act", lambda e: e.mul(out=negcsT[:], in_=csTPS[:], mul=-1.0), reads=[csTPS], writes=[negcsT])
                for g in range(2):
                    S.op("dve", lambda e, g=g: e.tensor_tensor(out=csdiag[:, g, :].rearrange("p (h l) -> p h l", h=8), in0=K["blk"][:].rearrange("p (h l) -> p h l", h=8),
                                                              in1=csTPS[:, g, :].unsqueeze(1).to_broadcast([8, 8, 128]), op=ALU.mult), reads=[K["blk"], csTPS], writes=[csdiag])
                xs3 = xt[:, 0:1024].rearrange("p (h v) -> p h v", h=16)
                S.op("dve", lambda e, xs3=xs3, dtd=dtd: e.tensor_tensor(out=xdt[:], in0=xs3, in1=dtd.unsqueeze(2).to_broadcast([128, 16, 64]), op=ALU.mult), reads=[xt, dtt], writes=[xdt])
                S.op("pool", lambda e, xs3=xs3: e.tensor_tensor(out=xdd[:], in0=xs3, in1=dtdte[:].unsqueeze(2).to_broadcast([128, 16, 64]), op=ALU.mult), reads=[xt, dtdte], writes=[xdd])
                S.op("pool", lambda e, xt=xt: e.tensor_copy(out=btok[:], in_=xt[:, 1024:1280].rearrange("p (g n) -> p g n", g=2)), reads=[xt], writes=[btok])
                si = 0
                for g in range(2):
                    S.op("pe", lambda e, g=g, bc=bc: e.matmul(cbPS[:], bc[:, g, :], bc[:, 2 + g, :], start=True, stop=True), reads=[bc], writes=[cbPS])
                    cb_ = cbt[g]
                    S.op("act", lambda e, cb_=cb_: e.copy(out=cb_[:], in_=cbPS[:]), reads=[cbPS], writes=[cb_])
                    for half in range(2):
                        sp = segPS[si % 2]
                        E, G = Eb[si % 2], Gb[si % 2]
                        si += 1
                        S.op("pe", lambda e, sp=sp, g=g, half=half: e.matmul(sp[:].rearrange("p h l -> p (h l)"), K["ones8"][:], csdiag[:, g, half * 512:(half + 1) * 512], start=True, stop=False),
                             reads=[K["ones8"], csdiag], writes=[sp], inc=False)
                        S.op("pe", lambda e, sp=sp, g=g, half=half: e.matmul(sp[:].rearrange("p h l -> p (h l)"), negcsT[:, g, :], K["blk"][:, half * 512:(half + 1) * 512], start=False, stop=False),
                             reads=[negcsT, K["blk"]], writes=[sp], inc=False)
                        S.op("pe", lambda e, sp=sp, mneg=mneg: e.matmul(sp[:].rearrange("p h l -> p (h l)"), K["identb"][:], mneg[:], start=False, stop=True),
                             reads=[K["identb"], mneg], writes=[sp])
                        S.op("act", lambda e, sp=sp, E=E: e.activation(out=E[:], in_=sp[:], func=AF.Exp), reads=[sp], writes=[E])
                        S.op("dve", lambda e, E=E, G=G, cb_=cb_: e.tensor_tensor(out=G[:], in0=E[:], in1=cb_[:].unsqueeze(1).to_broadcast([128, 4, 128]), op=ALU.mult), reads=[E, cb_], writes=[G])
                        for hh in range(4):
                            h = g * 8 + half * 4 + hh
                            S.op("pe", lambda e, G=G, hh=hh, h=h: e.matmul(ydPS[:, h * 64:(h + 1) * 64], G[:, hh, :], xdt[:, h, :], start=True, stop=True), reads=[G, xdt], writes=[ydPS], inc=(hh == 3))
                    S.op("pe", lambda e, g=g, bc=bc: e.matmul(yoPS[:], bc[:, 2 + g, :], Sbf[g][:], start=True, stop=True), reads=[bc, Sbf[g]], writes=[yoPS])
                    S.op("dve", lambda e, g=g, ya=ya: e.tensor_tensor(out=ya[:, g * 512:(g + 1) * 512].rearrange("p (h v) -> p h v", h=8), in0=yoPS[:].rearrange("p (h v) -> p h v", h=8),
                                                                   in1=dfs[:, g * 8:(g + 1) * 8].unsqueeze(2).to_broadcast([128, 8, 64]), op=ALU.mult), reads=[yoPS, dfs], writes=[ya])
                    S.op("pe", lambda e, g=g: e.matmul(dsPS[:], btok[:, g, :], xdd[:, g * 8:(g + 1) * 8, :].rearrange("p h v -> p (h v)"), start=True, stop=True), reads=[btok, xdd], writes=[dsPS])
                    S.op("pool", lambda e, g=g: e.tensor_tensor(out=S32[g][:].rearrange("p (h v) -> p h v", h=8), in0=S32[g][:].rearrange("p (h v) -> p h v", h=8),
                                                               in1=cdb[:, g * 8:(g + 1) * 8].unsqueeze(2).to_broadcast([128, 8, 64]), op=ALU.mult), reads=[S32[g], cdb], writes=[S32[g]])
                    S.op("dve", lambda e, g=g: e.tensor_tensor(out=S32[g][:], in0=S32[g][:], in1=dsPS[:], op=ALU.add), reads=[S32[g], dsPS], writes=[S32[g]])
                    S.op("act", lambda e, g=g: e.copy(out=Sbf[g][:], in_=S32[g][:]), reads=[S32[g]], writes=[Sbf[g]])
                S.op("dve", lambda e, ya=ya: e.tensor_tensor(out=ya[:], in0=ya[:], in1=ydPS[:], op=ALU.add), reads=[ya, ydPS], writes=[ya])
                S.dma(ytmp[d], ytmp[d][g0:g0 + 128, :], ya, ya[:])


def phase_sspost(C, ytmp, xbc_tok, colsT, P, mixT, ident):
    S, cfg = C.S, C.cfg
    dsk = S.sb("so_dsk", [128, 16])
    S.dma(dsk, dsk[:], P["ssm_d"], P["ssm_d"][0, :].partition_broadcast(128))
    nw = S.sb("so_nw", [128, 1024])
    S.dma(nw, nw[:], P["ssm_norm_w"], P["ssm_norm_w"][0, :].partition_broadcast(128))
    Y1 = [S.sb("so_y1_%d" % i, [128, 1024]) for i in range(2)]
    Y2 = [S.sb("so_y2_%d" % i, [128, 1024]) for i in range(2)]
    XS = [S.sb("so_xs_%d" % i, [128, 1024]) for i in range(2)]
    ZT = [S.sb("so_zt_%d" % i, [128, 8, 128]) for i in range(2)]
    zs = S.sb("so_zs", [128, 1024])
    junk = S.sb("so_junk", [128, 512], BF16)
    ss = S.sb("so_ss", [128, 2])
    pz = S.ps("so_pz", [128, 1024])
    po = S.ps("so_po", [128, 8, 128])
    ob = [S.sb("so_ob%d" % i, [128, 8, 128], BF16) for i in range(2)]
    zv = colsT.t[1920:2944, :].rearrange("(c p) n -> p c n", p=128)
    mv = mixT.t[512:1536, :].rearrange("(c p) n -> p c n", p=128)
    for ti in range(cfg.NTOK // 128):
        g0 = ti * 128
        y1, y2, xs, zt = Y1[ti % 2], Y2[ti % 2], XS[ti % 2], ZT[ti % 2]
        S.dma(y1, y1[:], ytmp[0], ytmp[0][g0:g0 + 128, :])
        S.dma(y2, y2[:], ytmp[1], ytmp[1][g0:g0 + 128, :])
        S.dma(xs, xs[:], xbc_tok, xbc_tok[g0:g0 + 128, 0:1024])
        S.dma(zt, zt[:], colsT, zv[:, :, g0:g0 + 128])
        for c in range(8):
            S.op("pe", lambda e, c=c, zt=zt: e.transpose(pz[:, c * 128:(c + 1) * 128], zt[:, c, :], ident[:]), reads=[zt, ident], writes=[pz], inc=(c == 7))
        S.op("act", lambda e: e.activation(out=zs[:], in_=pz[:], func=AF.Silu), reads=[pz], writes=[zs])
        S.op("dve", lambda e, y1=y1, y2=y2: e.tensor_tensor(out=y1[:], in0=y1[:], in1=y2[:], op=ALU.add), reads=[y1, y2], writes=[y1])
        S.op("pool", lambda e, xs=xs: e.tensor_tensor(out=xs[:].rearrange("p (h v) -> p h v", h=16), in0=xs[:].rearrange("p (h v) -> p h v", h=16),
                                                    in1=dsk[:].unsqueeze(2).to_broadcast([128, 16, 64]), op=ALU.mult), reads=[xs, dsk], writes=[xs])
        S.op("dve", lambda e, y1=y1, xs=xs: e.tensor_tensor(out=y1[:], in0=y1[:], in1=xs[:], op=ALU.add), reads=[y1, xs], writes=[y1])
        S.op("dve", lambda e, y1=y1: e.tensor_tensor(out=y1[:], in0=y1[:], in1=zs[:], op=ALU.mult), reads=[y1, zs], writes=[y1])
        for gI in range(2):
            S.op("act", lambda e, y1=y1, gI=gI: e.activation(out=junk[:], in_=y1[:, gI * 512:(gI + 1) * 512], func=AF.Square, accum_out=ss[:, gI:gI + 1]), reads=[y1], writes=[junk, ss])
        S.op("act", lambda e: e.activation(out=ss[:], in_=ss[:], func=AF.Sqrt, bias=NORM_EPS, scale=1.0 / 512), reads=[ss], writes=[ss])
        S.op("dve", lambda e: e.reciprocal(out=ss[:], in_=ss[:]), reads=[ss], writes=[ss])
        S.op("dve", lambda e, y1=y1: e.tensor_tensor(out=y1[:].rearrange("p (g v) -> p g v", g=2), in0=y1[:].rearrange("p (g v) -> p g v", g=2),
                                                    in1=ss[:].unsqueeze(2).to_broadcast([128, 2, 512]), op=ALU.mult), reads=[y1, ss], writes=[y1])
        S.op("pool", lambda e, y1=y1: e.tensor_tensor(out=y1[:], in0=y1[:], in1=nw[:], op=ALU.mult), reads=[y1, nw], writes=[y1])
        for c in range(8):
            S.op("pe", lambda e, c=c, y1=y1: e.transpose(po[:, c, :], y1[:, c * 128:(c + 1) * 128], ident[:]), reads=[y1, ident], writes=[po], inc=(c == 7))
        o = ob[ti % 2]
        S.op("act", lambda e, o=o: e.copy(out=o[:, 0:4, :], in_=po[:, 0:4, :]), reads=[po], writes=[o])
        S.op("dve", lambda e, o=o: e.tensor_copy(out=o[:, 4:8, :], in_=po[:, 4:8, :]), reads=[po], writes=[o])
        S.dma(mixT, mv[:, :, g0:g0 + 128], o, o[:])


def gate_tiles(C, name, modv, layer, idx):
    S, cfg = C.S, C.cfg
    D = cfg.D
    out = []
    for r in range(cfg.NB + 1):
        t = S.sb("%s_%d" % (name, r), [128, D])
        S.dma(t, t[:], modv, modv[layer, r, idx * D:(idx + 1) * D].partition_broadcast(128))
        out.append(t)
    return out


def tile_row(cfg, tile_idx):
    tpb = cfg.T // 128
    b, tt = divmod(tile_idx, tpb)
    return cfg.NB if tt < cfg.TC // 128 else b


def lat_tiles(cfg):
    tpb = cfg.T // 128
    return [b * tpb + tt for b in range(cfg.NB) for tt in range(cfg.TC // 128, tpb)]


def phase_outproj(C, name, src, src_tokmajor, kdim, W_buf, W_ap, modv, layer, xin, xout, tiles, ident_bf):
    S, cfg = C.S, C.cfg
    kch = kdim // 128
    wb = load_w_bf16(C, name + "_w", W_buf, W_ap.rearrange("(k p) c -> p k c", p=128), 1024, kch)
    gt = gate_tiles(C, name + "_gt", modv, layer, 2)
    mt_ = [S.sb("%s_m%d" % (name, i), [128, kch, 128], BF16) for i in range(2)]
    if src_tokmajor:
        tk_ = [S.sb("%s_tk%d" % (name, i), [128, kdim], BF16) for i in range(2)]
        ptb = S.ps(name + "_ptb", [128, kch, 128], BF16)
    xt_ = [S.sb("%s_x%d" % (name, i), [128, 1024]) for i in range(2)]
    ot_ = [S.sb("%s_o%d" % (name, i), [128, 1024]) for i in range(2)]
    ps_ = [S.ps("%s_ps%d" % (name, i), [128, 1024]) for i in range(2)]
    if not src_tokmajor:
        sv = src.t.rearrange("(k p) n -> p k n", p=128)
    for i, ti in enumerate(tiles):
        g0 = ti * 128
        m, xt, ot, ps = mt_[i % 2], xt_[i % 2], ot_[i % 2], ps_[i % 2]
        if src_tokmajor:
            tk = tk_[i % 2]
            S.dma(tk, tk[:], src, src[g0:g0 + 128, :])
            for k in range(kch):
                S.op("pe", lambda e, k=k, tk=tk: e.transpose(ptb[:, k, :], tk[:, k * 128:(k + 1) * 128], ident_bf[:]), reads=[tk, ident_bf], writes=[ptb], inc=(k == kch - 1))
            evac(S, i, m[:], ptb[:], [ptb], [m])
        else:
            S.dma(m, m[:], src, sv[:, :, g0:g0 + 128])
        S.dma(xt, xt[:], xin, xin[g0:g0 + 128, :])
        for half in range(2):
            for k in range(kch):
                S.op("pe", lambda e, k=k, half=half, m=m, ps=ps: e.matmul(ps[:, half * 512:(half + 1) * 512], m[:, k, :], wb[:, k, half * 512:(half + 1) * 512], start=(k == 0), stop=(k == kch - 1)),
                     reads=[m, wb], writes=[ps], inc=(k == kch - 1))
        g = gt[tile_row(cfg, ti)]
        S.op("dve", lambda e, ps=ps, ot=ot, g=g: e.tensor_tensor(out=ot[:], in0=ps[:], in1=g[:], op=ALU.mult), reads=[ps, g], writes=[ot])
        S.op("pool", lambda e, ot=ot, xt=xt: e.tensor_tensor(out=ot[:], in0=ot[:], in1=xt[:], op=ALU.add), reads=[ot, xt], writes=[ot])
        S.dma(xout, xout[g0:g0 + 128, :], ot, ot[:])


def phase_final(C, xin, fn_buf, out):
    S, cfg = C.S, C.cfg
    D = cfg.D
    g = S.sb("fn_g", [128, D])
    S.dma(g, g[:], fn_buf, fn_buf.t.partition_broadcast(128))
    xt_ = [S.sb("fn_x%d" % i, [128, D]) for i in range(2)]
    ot_ = [S.sb("fn_o%d" % i, [128, D]) for i in range(2)]
    junk = S.sb("fn_junk", [128, D], BF16)
    ss_ = [S.sb("fn_ss%d" % i, [128, 1]) for i in range(2)]
    tpl = cfg.TL // 128
    for i, ti in enumerate(lat_tiles(cfg)):
        xt, ot, ss = xt_[i % 2], ot_[i % 2], ss_[i % 2]
        S.dma(xt, xt[:], xin, xin[ti * 128:(ti + 1) * 128, :])
        S.op("act", lambda e, xt=xt, ss=ss: e.activation(out=junk[:], in_=xt[:], func=AF.Square, accum_out=ss[:]), reads=[xt], writes=[junk, ss])
        S.op("act", lambda e, ss=ss: e.activation(out=ss[:], in_=ss[:], func=AF.Sqrt, bias=NORM_EPS, scale=1.0 / D), reads=[ss], writes=[ss])
        S.op("dve", lambda e, ss=ss: e.reciprocal(out=ss[:], in_=ss[:]), reads=[ss], writes=[ss])
        S.op("dve", lambda e, xt=xt, ot=ot, ss=ss: e.scalar_tensor_tensor(out=ot[:], in0=xt[:], scalar=ss[:, 0:1], in1=g[:], op0=ALU.mult, op1=ALU.mult), reads=[xt, ss, g], writes=[ot])
        S.dma(out, out[i * 128:(i + 1) * 128, :], ot, ot[:])


def phase_moe_cast(C, layer, EW, wbf):
    S = C.S
    st = [S.sb("mc_st%d" % i, [128, 6144]) for i in range(2)]
    ob = [S.sb("mc_ob%d" % i, [128, 6144], BF16) for i in range(2)]
    for e in range(65):
        s_, o_ = st[e % 2], ob[e % 2]
        if e < 64:
            w1, w3, w2 = EW["exp_w1"][layer, e], EW["exp_w3"][layer, e], EW["exp_w2"][layer, e]
            b1, b3, b2 = EW["exp_w1"], EW["exp_w3"], EW["exp_w2"]
        else:
            w1, w3, w2 = EW["sh_w1"][layer], EW["sh_w3"][layer], EW["sh_w2"][layer]
            b1, b3, b2 = EW["sh_w1"], EW["sh_w3"], EW["sh_w2"]
        S.dma(s_, s_[:, 0:2048].rearrange("p (k f) -> p k f", k=8), b1, w1.rearrange("(k p) f -> p k f", p=128))
        S.dma(s_, s_[:, 2048:4096].rearrange("p (k f) -> p k f", k=8), b3, w3.rearrange("(k p) f -> p k f", p=128))
        S.dma(s_, s_[:, 4096:6144].rearrange("p (j f) -> p j f", j=2), b2, w2.rearrange("(j p) f -> p j f", p=128))
        S.op("act", lambda e_, s_=s_, o_=o_: e_.copy(out=o_[:, 0:2048], in_=s_[:, 0:2048]), reads=[s_], writes=[o_])
        S.op("dve", lambda e_, s_=s_, o_=o_: e_.tensor_copy(out=o_[:, 2048:4096], in_=s_[:, 2048:4096]), reads=[s_], writes=[o_])
        S.op("pool", lambda e_, s_=s_, o_=o_: e_.tensor_copy(out=o_[:, 4096:6144], in_=s_[:, 4096:6144]), reads=[s_], writes=[o_])
        S.dma(wbf, wbf[e], o_, o_[:])


def phase_moe_route(C, layer, xin, gvec_buf, gvec_ap, modv, rw_buf, rb_buf, hT, gates, tiles, ident):
    S, cfg = C.S, C.cfg
    mt = ModTiles(C, "mr_mt", modv, layer, 3, 4, gvec_ap, gvec_buf)
    nt = NormT(C, "mr_nt", ident)
    rw = S.sb("mr_rw", [128, 8, 64])
    S.dma(rw, rw[:], rw_buf, rw_buf[layer].rearrange("(k p) e -> p k e", p=128))
    rb = S.sb("mr_rb", [128, 64])
    S.dma(rb, rb[:], rb_buf, rb_buf[layer, :].partition_broadcast(128))
    hb_ = [S.sb("mr_hb%d" % i, [128, 8, 128], BF16) for i in range(2)]
    h32_ = [S.sb("mr_h32%d" % i, [128, 8, 128]) for i in range(2)]
    lg = S.ps("mr_lg", [128, 64])
    sc = S.sb("mr_sc", [128, 64])
    sel = S.sb("mr_sel", [128, 64])
    eq = S.sb("mr_eq", [128, 64])
    m1 = S.sb("mr_m1", [128, 8])
    m2 = S.sb("mr_m2", [128, 8])
    t8 = S.sb("mr_t8", [128, 8])
    pen = S.sb("mr_pen", [128, 8])
    ws = S.sb("mr_ws", [128, 1])
    gt_ = [S.sb("mr_gt%d" % i, [128, 65]) for i in range(2)]
    for g in gt_:
        S.op("pool", lambda e, g=g: e.memset(g[:, 64:65], 1.0), writes=[g])
    hv = hT.t.rearrange("(k p) n -> p k n", p=128)

    def v3(b):
        return b[:].rearrange("p (g i) -> p g i", g=8)
    for i, ti in enumerate(tiles):
        g0 = ti * 128
        hb, h32, gt = hb_[i % 2], h32_[i % 2], gt_[i % 2]
        nt.tile(xin, g0, mt, tile_row(cfg, ti), [(hb, lambda k0, k1, hb=hb: hb[:, k0:k1, :]), (h32, lambda k0, k1, h32=h32: h32[:, k0:k1, :])])
        S.dma(hT, hv[:, :, g0:g0 + 128], hb, hb[:])
        for k in range(8):
            S.op("pe", lambda e, k=k, h32=h32: e.matmul(lg[:], h32[:, k, :], rw[:, k, :], start=(k == 0), stop=(k == 7)), reads=[h32, rw], writes=[lg], inc=(k == 7))
        S.op("act", lambda e: e.activation(out=sc[:], in_=lg[:], func=AF.Sigmoid), reads=[lg], writes=[sc])
        S.op("dve", lambda e: e.tensor_tensor(out=sel[:], in0=sc[:], in1=rb[:], op=ALU.add), reads=[sc, rb], writes=[sel])
        S.op("dve", lambda e: e.reduce_max(out=m1[:], in_=v3(sel), axis=AX.X), reads=[sel], writes=[m1])
        S.op("dve", lambda e: e.tensor_tensor(out=v3(eq), in0=v3(sel), in1=m1[:].unsqueeze(2).to_broadcast([128, 8, 8]), op=ALU.is_equal), reads=[sel, m1], writes=[eq])
        S.op("dve", lambda e: e.scalar_tensor_tensor(out=eq[:], in0=eq[:], scalar=-1e30, in1=sel[:], op0=ALU.mult, op1=ALU.add), reads=[eq, sel], writes=[eq])
        S.op("dve", lambda e: e.reduce_max(out=m2[:], in_=v3(eq), axis=AX.X), reads=[eq], writes=[m2])
        S.op("dve", lambda e: e.tensor_tensor(out=m1[:], in0=m1[:], in1=m2[:], op=ALU.add), reads=[m1, m2], writes=[m1])
        S.op("dve", lambda e: e.max(out=t8[:], in_=m1[:]), reads=[m1], writes=[t8])
        S.op("dve", lambda e: e.tensor_scalar(out=pen[:], in0=m1[:], scalar1=t8[:, 3:4], scalar2=None, op0=ALU.is_ge), reads=[m1, t8], writes=[pen])
        S.op("dve", lambda e: e.tensor_scalar(out=pen[:], in0=pen[:], scalar1=1e30, scalar2=-1e30, op0=ALU.mult, op1=ALU.add), reads=[pen], writes=[pen])
        S.op("dve", lambda e: e.tensor_tensor(out=v3(sel), in0=v3(sel), in1=pen[:].unsqueeze(2).to_broadcast([128, 8, 8]), op=ALU.add), reads=[sel, pen], writes=[sel])
        S.op("dve", lambda e: e.max(out=t8[:], in_=sel[:]), reads=[sel], writes=[t8])
        S.op("dve", lambda e: e.tensor_scalar(out=eq[:], in0=sel[:], scalar1=t8[:, 5:6], scalar2=None, op0=ALU.is_ge), reads=[sel, t8], writes=[eq])
        S.op("dve", lambda e: e.tensor_tensor(out=eq[:], in0=eq[:], in1=sc[:], op=ALU.mult), reads=[eq, sc], writes=[eq])
        S.op("dve", lambda e: e.reduce_sum(out=ws[:], in_=eq[:], axis=AX.X), reads=[eq], writes=[ws])
        S.op("dve", lambda e: e.reciprocal(out=ws[:], in_=ws[:]), reads=[ws], writes=[ws])
        S.op("dve", lambda e, gt=gt: e.tensor_scalar(out=gt[:, 0:64], in0=eq[:], scalar1=ws[:, 0:1], scalar2=ROUTED_SCALE, op0=ALU.mult, op1=ALU.mult), reads=[eq, ws], writes=[gt])
        S.dma(gates, gates[g0:g0 + 128, :], gt, gt[:])


ROUTED_SCALE = 2.5


def phase_moe_experts(C, layer, hT, gates, wbf, modv, xin, xout, tiles):
    S, cfg = C.S, C.cfg
    TSU = 8
    gtl = gate_tiles(C, "me_gt", modv, layer, 5)
    hs = S.sb("me_hs", [128, 8, TSU * 128], BF16)
    gs = S.sb("me_gs", [128, TSU, 65])
    acc = S.sb("me_acc", [128, TSU, 1024])
    wb_ = [S.sb("me_w%d" % i, [128, 6144], BF16) for i in range(3)]
    h1_ = [S.ps("me_h1_%d" % i, [128, 512]) for i in range(2)]
    h3_ = [S.ps("me_h3_%d" % i, [128, 512]) for i in range(2)]
    op_ = [S.ps("me_o%d" % i, [128, 512]) for i in range(3)]
    s1_ = [S.sb("me_s1_%d" % i, [128, 512]) for i in range(2)]
    hid_ = [S.sb("me_hid%d" % i, [128, 2, 512], BF16) for i in range(2)]
    xt_ = [S.sb("me_x%d" % i, [128, 1024]) for i in range(2)]
    hv = hT.t.rearrange("(k p) n -> p k n", p=128)
    nst = (len(tiles) + TSU - 1) // TSU
    cnt = 0
    oc = 0
    for st in range(nst):
        tl = tiles[st * TSU:(st + 1) * TSU]
        nt_ = len(tl)
        for j, ti in enumerate(tl):
            S.dma(hs, hs[:, :, j * 128:(j + 1) * 128], hT, hv[:, :, ti * 128:(ti + 1) * 128])
            S.dma(gs, gs[:, j, :], gates, gates[ti * 128:(ti + 1) * 128, :])
        S.op("pool", lambda e: e.memset(acc[:], 0.0), writes=[acc])
        items = []
        for e_ in range(65):
            wb = wb_[e_ % 3]
            for tb in range((nt_ + 3) // 4):
                items.append((e_, tb, wb))

        def H_groups(it, idx):
            e_, tb, wb = it
            ntok = min(4, nt_ - tb * 4) * 128
            hid = hid_[idx % 2]
            gl = []
            for j in range(2):
                h1, h3, s1 = h1_[j], h3_[j], s1_[j]

                def g1(j=j, h1=h1):
                    if tb == 0 and j == 0:
                        S.dma(wb, wb[:], wbf, wbf[e_])
                    for k in range(8):
                        S.op("pe", lambda e, k=k: e.matmul(h1[:, 0:ntok], wb[:, k * 256 + j * 128:k * 256 + (j + 1) * 128], hs[:, k, tb * 512:tb * 512 + ntok], start=(k == 0), stop=(k == 7)),
                             reads=[wb, hs], writes=[h1], inc=(k == 7))

                def g3(j=j, h1=h1, h3=h3, s1=s1):
                    for k in range(8):
                        S.op("pe", lambda e, k=k: e.matmul(h3[:, 0:ntok], wb[:, 2048 + k * 256 + j * 128:2048 + k * 256 + (j + 1) * 128], hs[:, k, tb * 512:tb * 512 + ntok], start=(k == 0), stop=(k == 7)),
                             reads=[wb, hs], writes=[h3], inc=(k == 7))
                    S.op("act", lambda e: e.activation(out=s1[:, 0:ntok], in_=h1[:, 0:ntok], func=AF.Silu), reads=[h1], writes=[s1])
                    S.op("dve", lambda e: e.tensor_tensor(out=hid[:, j, 0:ntok], in0=h3[:, 0:ntok], in1=s1[:, 0:ntok], op=ALU.mult), reads=[h3, s1], writes=[hid])
                gl += [g1, g3]
            return gl

        def O_groups(it, idx):
            nonlocal oc
            e_, tb, wb = it
            ntok = min(4, nt_ - tb * 4) * 128
            hid = hid_[idx % 2]
            gl = []
            for q in range(ntok // 128):
                tj = tb * 4 + q

                def go(q=q, tj=tj):
                    nonlocal oc
                    for half in range(2):
                        o = op_[oc % 3]
                        oc += 1
                        for j in range(2):
                            S.op("pe", lambda e, o=o, j=j, half=half: e.matmul(o[:], hid[:, j, q * 128:(q + 1) * 128], wb[:, 4096 + j * 1024 + half * 512:4096 + j * 1024 + (half + 1) * 512], start=(j == 0), stop=(j == 1)),
                                 reads=[hid, wb], writes=[o], inc=(j == 1))
                        S.op("dve", lambda e, o=o, half=half: e.scalar_tensor_tensor(out=acc[:, tj, half * 512:(half + 1) * 512], in0=o[:], scalar=gs[:, tj, e_:e_ + 1], in1=acc[:, tj, half * 512:(half + 1) * 512], op0=ALU.mult, op1=ALU.add),
                             reads=[o, gs, acc], writes=[acc])
                gl.append(go)
            return gl
        for g in H_groups(items[0], 0):
            g()
        for idx, it in enumerate(items):
            og = O_groups(it, idx)
            hg = H_groups(items[idx + 1], idx + 1) if idx + 1 < len(items) else []
            n = max(len(og), len(hg))
            for t in range(n):
                if t < len(hg):
                    hg[t]()
                if t < len(og):
                    og[t]()
        for j, ti in enumerate(tl):
            xt = xt_[j % 2]
            S.dma(xt, xt[:], xin, xin[ti * 128:(ti + 1) * 128, :])
            g = gtl[tile_row(cfg, ti)]
            S.op("pool", lambda e, j=j, g=g: e.tensor_tensor(out=acc[:, j, :], in0=acc[:, j, :], in1=g[:], op=ALU.mult), reads=[acc, g], writes=[acc])
            S.op("pool", lambda e, j=j, xt=xt: e.tensor_tensor(out=xt[:], in0=acc[:, j, :], in1=xt[:], op=ALU.add), reads=[acc, xt], writes=[xt])
            S.dma(xout, xout[ti * 128:(ti + 1) * 128, :], xt, xt[:])


def phase_mla_prep(C, layer, xin, gvec_buf, gvec_ap, modv, P, rope_cs, KT, QT, Vtok, ident, identb):
    S, cfg = C.S, C.cfg
    TCt = cfg.TC // 128
    tpb = cfg.T // 128
    mt = ModTiles(C, "mp_mt", modv, layer, 0, 1, gvec_ap, gvec_buf)
    nt = NormT(C, "mp_nt", ident)
    win = load_w_bf16(C, "mp_win", P["mla_w_in"], P["mla_w_in"][0].rearrange("(k p) c -> p k c", p=128), 672, 8)
    qup = load_w_bf16(C, "mp_qup", P["mla_q_up"], P["mla_q_up"][0].rearrange("(k p) c -> p k c", p=128), 1536, 3)
    kvup = load_w_bf16(C, "mp_kvup", P["mla_kv_up"], P["mla_kv_up"][0].rearrange("(k p) c -> p k c", p=128), 2048, 2)
    nb = S.sb("mp_nb", [128, 640])
    S.dma(nb, nb[:, 0:384], P["mla_q_norm"], P["mla_q_norm"][0, :].partition_broadcast(128))
    S.dma(nb, nb[:, 384:640], P["mla_kv_norm"], P["mla_kv_norm"][0, :].partition_broadcast(128))
    hb_ = [S.sb("mp_hb%d" % i, [128, 8, 128], BF16) for i in range(2)]
    A_ = S.ps("mp_A", [128, 1024])
    B_ = S.ps("mp_B", [128, 1024])
    Cp = S.ps("mp_C", [96, 16, 128], BF16)
    c_sb = S.sb("mp_c", [128, 672])
    junk = S.sb("mp_junk", [128, 384], BF16)
    ss = S.sb("mp_ss", [128, 2])
    cn = S.sb("mp_cn", [128, 640])
    cnT = S.sb("mp_cnT", [128, 5, 128], BF16)
    vt_ = [S.sb("mp_vt%d" % i, [128, 16, 64], BF16) for i in range(2)]
    Kf = S.sb("mp_Kf", [128, 16, 96], BF16)
    Qf = S.sb("mp_Qf", [128, 16, 96], BF16)
    q32 = S.sb("mp_q32", [128, 16, 96])
    cs_ = [S.sb("mp_cs%d" % i, [128, 32]) for i in range(2)]
    krr = S.sb("mp_krr", [128, 32])
    t1 = S.sb("mp_t1", [128, 16, 16])
    t2 = S.sb("mp_t2", [128, 16, 16])
    kT_ = [S.sb("mp_kT%d" % i, [96, 16, 128], BF16) for i in range(2)]
    qT_ = [S.sb("mp_qT%d" % i, [96, 16, 128], BF16) for i in range(2)]
    ktv = KT.t.rearrange("h d n -> d h n")
    qtv = QT.t.rearrange("h d n -> d h n")
    for ti in range(cfg.NTOK // 128):
        b, tt = divmod(ti, tpb)
        lat = tt >= TCt
        g0 = ti * 128
        hb, vt, kT, qT, cs = hb_[ti % 2], vt_[ti % 2], kT_[ti % 2], qT_[ti % 2], cs_[ti % 2]
        nt.tile(xin, g0, mt, tile_row(cfg, ti), [(hb, lambda k0, k1, hb=hb: hb[:, k0:k1, :])])
        for (c0, c1) in ((0, 512), (512, 672)):
            for k in range(8):
                S.op("pe", lambda e, k=k, c0=c0, c1=c1, hb=hb: e.matmul(B_[:, c0:c1], hb[:, k, :], win[:, k, c0:c1], start=(k == 0), stop=(k == 7)), reads=[hb, win], writes=[B_], inc=(k == 7))
        S.op("act", lambda e: e.copy(out=c_sb[:, 0:512], in_=B_[:, 0:512]), reads=[B_], writes=[c_sb])
        S.op("dve", lambda e: e.tensor_copy(out=c_sb[:, 512:672], in_=B_[:, 512:672]), reads=[B_], writes=[c_sb])
        S.op("act", lambda e: e.activation(out=junk[:, 0:384], in_=c_sb[:, 0:384], func=AF.Square, accum_out=ss[:, 0:1]), reads=[c_sb], writes=[junk, ss])
        S.op("act", lambda e: e.activation(out=junk[:, 0:256], in_=c_sb[:, 384:640], func=AF.Square, accum_out=ss[:, 1:2]), reads=[c_sb], writes=[junk, ss])
        S.op("act", lambda e: e.activation(out=ss[:, 0:1], in_=ss[:, 0:1], func=AF.Sqrt, bias=NORM_EPS, scale=1.0 / 384), reads=[ss], writes=[ss])
        S.op("act", lambda e: e.activation(out=ss[:, 1:2], in_=ss[:, 1:2], func=AF.Sqrt, bias=NORM_EPS, scale=1.0 / 256), reads=[ss], writes=[ss])
        S.op("dve", lambda e: e.reciprocal(out=ss[:], in_=ss[:]), reads=[ss], writes=[ss])
        S.op("dve", lambda e: e.scalar_tensor_tensor(out=cn[:, 0:384], in0=c_sb[:, 0:384], scalar=ss[:, 0:1], in1=nb[:, 0:384], op0=ALU.mult, op1=ALU.mult), reads=[c_sb, ss, nb], writes=[cn])
        S.op("dve", lambda e: e.scalar_tensor_tensor(out=cn[:, 384:640], in0=c_sb[:, 384:640], scalar=ss[:, 1:2], in1=nb[:, 384:640], op0=ALU.mult, op1=ALU.mult), reads=[c_sb, ss, nb], writes=[cn])
        Bv = B_[:, 0:640].rearrange("p (k n) -> p k n", k=5)
        for k in range(5):
            S.op("pe", lambda e, k=k, Bv=Bv: e.transpose(Bv[:, k, :], cn[:, k * 128:(k + 1) * 128], ident[:]), reads=[cn, ident], writes=[B_], inc=(k == 4))
        S.op("act", lambda e, Bv=Bv: e.copy(out=cnT[:], in_=Bv), reads=[B_], writes=[cnT])
        for ps_ in range(2):
            for blk in range(2):
                for kc in range(2):
                    S.op("pe", lambda e, ps_=ps_, blk=blk, kc=kc: e.matmul(A_[:, blk * 512:(blk + 1) * 512], cnT[:, 3 + kc, :], kvup[:, kc, ps_ * 1024 + blk * 512:ps_ * 1024 + (blk + 1) * 512], start=(kc == 0), stop=(kc == 1)),
                         reads=[cnT, kvup], writes=[A_], inc=(kc == 1))
            Av = A_[:].rearrange("p (h x) -> p h x", h=8)
            S.op("act", lambda e, ps_=ps_, Av=Av, vt=vt: e.copy(out=vt[:, ps_ * 8:(ps_ + 1) * 8, :], in_=Av[:, :, 64:128]), reads=[A_], writes=[vt])
            S.op("dve", lambda e, ps_=ps_, Av=Av: e.tensor_copy(out=Kf[:, ps_ * 8:(ps_ + 1) * 8, 0:64], in_=Av[:, :, 0:64]), reads=[A_], writes=[Kf])
        S.dma(Vtok, Vtok[g0:g0 + 128, :], vt, vt[:].rearrange("p h v -> p (h v)"))
        if lat:
            p0 = (tt - TCt) * 128
            S.dma(cs, cs[:], rope_cs, rope_cs[p0:p0 + 128, :])
            u1, u2 = c_sb[:, 640:656], c_sb[:, 656:672]
            S.op("dve", lambda e, cs=cs, u1=u1: e.tensor_tensor(out=t1[:, 0, :], in0=u1, in1=cs[:, 0:16], op=ALU.mult), reads=[c_sb, cs], writes=[t1])
            S.op("dve", lambda e, cs=cs, u2=u2: e.tensor_tensor(out=t2[:, 0, :], in0=u2, in1=cs[:, 16:32], op=ALU.mult), reads=[c_sb, cs], writes=[t2])
            S.op("dve", lambda e: e.tensor_tensor(out=krr[:, 0:16], in0=t1[:, 0, :], in1=t2[:, 0, :], op=ALU.subtract), reads=[t1, t2], writes=[krr])
            S.op("dve", lambda e, cs=cs, u1=u1: e.tensor_tensor(out=t1[:, 0, :], in0=u1, in1=cs[:, 16:32], op=ALU.mult), reads=[c_sb, cs], writes=[t1])
            S.op("dve", lambda e, cs=cs, u2=u2: e.tensor_tensor(out=t2[:, 0, :], in0=u2, in1=cs[:, 0:16], op=ALU.mult), reads=[c_sb, cs], writes=[t2])
            S.op("dve", lambda e: e.tensor_tensor(out=krr[:, 16:32], in0=t1[:, 0, :], in1=t2[:, 0, :], op=ALU.add), reads=[t1, t2], writes=[krr])
        else:
            S.op("dve", lambda e: e.tensor_copy(out=krr[:], in_=c_sb[:, 640:672]), reads=[c_sb], writes=[krr])
        S.op("dve", lambda e: e.tensor_copy(out=Kf[:, :, 64:96], in_=krr[:].unsqueeze(1).to_broadcast([128, 16, 32])), reads=[krr], writes=[Kf])
        for h in range(16):
            S.op("pe", lambda e, h=h: e.transpose(Cp[:, h, :], Kf[:, h, :], identb[:]), reads=[Kf, identb], writes=[Cp], inc=(h == 15))
        S.op("act", lambda e, kT=kT: e.copy(out=kT[:], in_=Cp[:]), reads=[Cp], writes=[kT])
        S.dma(KT, ktv[:, :, g0:g0 + 128], kT, kT[:])
        if lat:
            for ps_ in range(2):
                for (c0, c1) in ((0, 512), (512, 768)):
                    for kc in range(3):
                        S.op("pe", lambda e, ps_=ps_, c0=c0, c1=c1, kc=kc: e.matmul(A_[:, c0:c1], cnT[:, kc, :], qup[:, kc, ps_ * 768 + c0:ps_ * 768 + c1], start=(kc == 0), stop=(kc == 2)),
                             reads=[cnT, qup], writes=[A_], inc=(kc == 2))
                S.op("act", lambda e, ps_=ps_: e.copy(out=q32[:, ps_ * 8:(ps_ + 1) * 8, :], in_=A_[:, 0:768].rearrange("p (h x) -> p h x", h=8)), reads=[A_], writes=[q32])
            S.op("pool", lambda e: e.tensor_copy(out=Qf[:, :, 0:64], in_=q32[:, :, 0:64]), reads=[q32], writes=[Qf])
            U1, U2 = q32[:, :, 64:80], q32[:, :, 80:96]
            cosb = cs[:, 0:16].unsqueeze(1).to_broadcast([128, 16, 16])
            sinb = cs[:, 16:32].unsqueeze(1).to_broadcast([128, 16, 16])
            S.op("dve", lambda e, U1=U1, cosb=cosb: e.tensor_tensor(out=t1[:], in0=U1, in1=cosb, op=ALU.mult), reads=[q32, cs], writes=[t1])
            S.op("dve", lambda e, U2=U2, sinb=sinb: e.tensor_tensor(out=t2[:], in0=U2, in1=sinb, op=ALU.mult), reads=[q32, cs], writes=[t2])
            S.op("dve", lambda e: e.tensor_tensor(out=Qf[:, :, 64:80], in0=t1[:], in1=t2[:], op=ALU.subtract), reads=[t1, t2], writes=[Qf])
            S.op("dve", lambda e, U1=U1, sinb=sinb: e.tensor_tensor(out=t1[:], in0=U1, in1=sinb, op=ALU.mult), reads=[q32, cs], writes=[t1])
            S.op("dve", lambda e, U2=U2, cosb=cosb: e.tensor_tensor(out=t2[:], in0=U2, in1=cosb, op=ALU.mult), reads=[q32, cs], writes=[t2])
            S.op("dve", lambda e: e.tensor_tensor(out=Qf[:, :, 80:96], in0=t1[:], in1=t2[:], op=ALU.add), reads=[t1, t2], writes=[Qf])
            for h in range(16):
                S.op("pe", lambda e, h=h: e.transpose(Cp[:, h, :], Qf[:, h, :], identb[:]), reads=[Qf, identb], writes=[Cp], inc=(h == 15))
            S.op("act", lambda e, qT=qT: e.copy(out=qT[:], in_=Cp[:]), reads=[Cp], writes=[qT])
            S.dma(QT, qtv[:, :, g0:g0 + 128], qT, qT[:])


def phase_mla_attn(C, KT, QT, Vtok, AO):
    S, cfg = C.S, C.cfg
    NB, T, TC, TL = cfg.NB, cfg.T, cfg.TC, cfg.TL
    Tt = T // 128
    scale = (64 + 32) ** -0.5
    Ks_ = [S.sb("at_K%d" % i, [96, T], BF16) for i in range(2)]
    Qs_ = [S.sb("at_Q%d" % i, [96, TL], BF16) for i in range(2)]
    Vs_ = [S.sb("at_V%d" % i, [128, Tt, 65], BF16) for i in range(2)]
    for v in Vs_:
        S.op("pool", lambda e, v=v: e.memset(v[:, :, 64:65], 1.0), writes=[v])
    sp_ = [S.ps("at_s%d" % i, [128, 512]) for i in range(3)]
    E_ = [S.sb("at_E%d" % i, [128, 512], BF16) for i in range(3)]
    acc_ = [S.ps("at_acc%d" % i, [128, 4, 128]) for i in range(2)]
    rc = S.sb("at_rc", [128, 4, 1])
    ob_ = [S.sb("at_o%d" % i, [128, 4, 64], BF16) for i in range(2)]
    n = 0
    m = 0
    for b in range(NB):
        for h in range(16):
            Ks, Qs, Vs = Ks_[n % 2], Qs_[n % 2], Vs_[n % 2]
            n += 1
            S.dma(Ks, Ks[:], KT, KT[h, :, b * T:(b + 1) * T])
            S.dma(Qs, Qs[:], QT, QT[h, :, b * T + TC:(b + 1) * T])
            S.dma(Vs, Vs[:, :, 0:64], Vtok, Vtok[b * T:(b + 1) * T, h * 64:(h + 1) * 64].rearrange("(k p) d -> p k d", p=128))
            for qb in range(TL // 512):
                acc = acc_[qb % 2]

                def score(kt, Ks=Ks, Qs=Qs, qb=qb):
                    nonlocal m
                    sp, E = sp_[m % 3], E_[m % 3]
                    m += 1
                    S.op("pe", lambda e, sp=sp, kt=kt: e.matmul(sp[:], Ks[:, kt * 128:(kt + 1) * 128], Qs[:, qb * 512:(qb + 1) * 512], start=True, stop=True), reads=[Ks, Qs], writes=[sp])
                    return sp, E
                cur = score(0)
                for kt in range(Tt):
                    nxt = score(kt + 1) if kt + 1 < Tt else None
                    sp, E = cur
                    S.op("act", lambda e, sp=sp, E=E: e.activation(out=E[:], in_=sp[:], func=AF.Exp, scale=scale), reads=[sp], writes=[E])
                    for i in range(4):
                        S.op("pe", lambda e, acc=acc, E=E, Vs=Vs, kt=kt, i=i: e.matmul(acc[:, i, 0:65], E[:, i * 128:(i + 1) * 128], Vs[:, kt, :], start=(kt == 0 and i == 0), stop=(kt == Tt - 1), skip_group_check=True),
                             reads=[E, Vs], writes=[acc], inc=(i == 3))
                    cur = nxt
                S.op("dve", lambda e, acc=acc: e.reciprocal(out=rc[:], in_=acc[:, :, 64:65]), reads=[acc], writes=[rc])
                ob = ob_[qb % 2]
                S.op("dve", lambda e, acc=acc, ob=ob: e.tensor_tensor(out=ob[:], in0=acc[:, :, 0:64], in1=rc[:].to_broadcast([128, 4, 64]), op=ALU.mult), reads=[acc, rc], writes=[ob])
                r0 = b * T + TC + qb * 512
                S.dma(AO, AO[r0:r0 + 512, h * 64:(h + 1) * 64].rearrange("(i p) d -> p i d", p=128), ob, ob[:])


import ml_dtypes

N_CORES = 8
PARAM_SHAPES = dict(
    c=None, c_ctx=[1024], ada_w=[2, 1024, 6144], ada_b=[2, 6144], norm_mix=[2, 1024], norm_ffn=[2, 1024],
    ev_w_in=[1, 1024, 4512], ev_w_out=[1, 1536, 1024], rw_mu=[1, 1920], rw_w0=[1, 2, 512], rw_w2=[1, 2, 64, 512],
    rw_a0=[1, 2, 512], rw_a2=[1, 2, 64, 512], rw_g2=[1, 128, 512], rw_kk=[1, 512], rw_ka=[1, 512], rw_rk=[1, 512],
    rw_gn_w=[1, 512], rw_gn_b=[1, 512], ssm_conv_w=[1, 5, 1536], ssm_conv_b=[1, 1536], ssm_dt_bias=[1, 2, 16],
    ssm_a_log=[1, 2, 16], ssm_d=[1, 16], ssm_norm_w=[1, 1024], mla_w_in=[1, 1024, 672], mla_q_norm=[1, 384],
    mla_q_up=[1, 384, 1536], mla_kv_norm=[1, 256], mla_kv_up=[1, 256, 2048], mla_w_out=[1, 1024, 1024],
    router_w=[2, 1024, 64], router_bias=[2, 64], exp_w1=[2, 64, 1024, 256], exp_w3=[2, 64, 1024, 256],
    exp_w2=[2, 64, 256, 1024], sh_w1=[2, 1024, 256], sh_w3=[2, 1024, 256], sh_w2=[2, 256, 1024], final_norm=[1024])


def host_consts(cfg):
    K = {}
    bf = ml_dtypes.bfloat16
    K["ident"] = np.eye(128, dtype=np.float32)
    K["identb"] = np.eye(128).astype(bf)
    K["onesblk"] = np.kron(np.eye(2), np.ones((64, 64))).astype(np.float32)
    NQ = 8 * cfg.NB * 64 // 512
    Eq = np.zeros((128, NQ, 2 * NQ), np.float32)
    for p in range(128):
        for q in range(NQ):
            Eq[p, q, 2 * q + p // 64] = 1
    K["Eq"] = Eq
    l = np.arange(128)
    K["tri0"] = (l[:, None] <= l[None, :]).astype(np.float32)
    K["tri1"] = (l[:, None] >= l[None, :]).astype(np.float32)
    K["ones"] = np.ones((128, 128), np.float32)
    blk = np.zeros((8, 8, 128), np.float32)
    for h in range(8):
        blk[h, h, :] = 1
    K["blk"] = blk.reshape(8, 1024)
    K["ones8"] = np.ones((8, 128), np.float32)
    m0 = np.where(l[None, :] >= l[:, None], 0.0, -30000.0)
    m1 = np.where(l[None, :] <= l[:, None], 0.0, -30000.0)
    K["mneg0"] = np.tile(m0[:, None, :], (1, 4, 1)).reshape(128, 512).astype(bf)
    K["mneg1"] = np.tile(m1[:, None, :], (1, 4, 1)).reshape(128, 512).astype(bf)
    rows = cfg.TL // 64
    r_idx, c_idx = np.meshgrid(np.arange(rows), np.arange(64), indexing='ij')
    r_idx = r_idx.reshape(-1).astype(np.float32)
    c_idx = c_idx.reshape(-1).astype(np.float32)
    inv_freq = (10000.0 ** (-np.arange(0, 16, 2, dtype=np.float32) / 16)).astype(np.float32)
    ang = np.concatenate([r_idx[:, None] * inv_freq, c_idx[:, None] * inv_freq], -1).astype(np.float32)
    K["rope_cs"] = np.concatenate([np.cos(ang), np.sin(ang)], 1).astype(np.float32)
    return K


def build_program(cfg, kconst, dbg=()):
    nc = bass.Bass("TRN2", target_bir_lowering=False)
    NB, NTOK, T = cfg.NB, cfg.NTOK, cfg.T
    with ExitStack() as es:
        S = Sched(nc, es)
        io = {k: "ExternalInput" for k in PARAM_SHAPES}
        io.update(xin="ExternalInput", out="ExternalOutput")
        io.update({"K_" + k: "ExternalInput" for k in kconst})
        io.update({k: "ExternalOutput" for k in dbg})
        C = Ctx(S, cfg, io)
        P = {}
        for k, shp in PARAM_SHAPES.items():
            P[k] = C.D(k, [NB, 1024] if k == "c" else shp)
        xin = C.D("xin", [NTOK, 1024])
        out = C.D("out", [NB * cfg.TL, 1024])
        K = {}
        for k, v in kconst.items():
            dt_ = BF16 if v.dtype == ml_dtypes.bfloat16 else F32
            dd = C.D("K_" + k, list(v.shape), dt_)
            if k == "rope_cs":
                K[k] = dd
                continue
            sbt = S.sb("Ksb_" + k, list(v.shape), dt_)
            S.dma(sbt, sbt[:], dd, dd.t)
            K[k] = sbt
        ident, identb, onesblk = K["ident"], K["identb"], K["onesblk"]
        NQ = 8 * NB * 64 // 512
        modv = C.D("modv", [2, NB + 1, 6144])
        colsT = C.D("colsT", [4512, NTOK])
        A = {k: C.D("A_" + k, [512, NTOK]) for k in ["kk", "r", "bonus", "g", "w0", "w1", "b0", "b1", "kd0", "kd1"]}
        vtok = C.D("vtok", [NTOK, 512], BF16)
        YD = C.D("YD", [2 * NQ, T, 512])
        mixT = C.D("mixT", [1536, NTOK], BF16)
        xbc_tok = C.D("xbc_tok", [NTOK, 1536])
        BCT = C.D("BCT", [512, NTOK], BF16)
        dtda = C.D("dtda_tok", [NTOK, 64])
        ytmp = [C.D("ytmp%d" % d, [NTOK, 1024]) for d in range(2)]
        xr = [C.D("xr%d" % i, [NTOK, 1024]) for i in range(4)]
        hT = C.D("hT", [1024, NTOK], BF16)
        gates = C.D("gates", [NTOK, 65])
        wbf = C.D("wbf", [65, 128, 6144], BF16)
        KT = C.D("KT", [16, 96, NTOK], BF16)
        QT = C.D("QT", [16, 96, NTOK], BF16)
        Vtok = C.D("Vtok", [NTOK, 1024], BF16)
        AO = C.D("AO", [NTOK, 1024], BF16)
        EW = {k: P[k] for k in ("exp_w1", "exp_w3", "exp_w2", "sh_w1", "sh_w3", "sh_w2")}
        all_tiles = list(range(NTOK // 128))
        lt = lat_tiles(cfg)
        with S.phase():
            phase_mod(C, P["c"], P["c_ctx"], P["ada_w"], P["ada_b"], modv)
        with S.phase():
            phase_normproj(C, "np0", xin, P["norm_mix"], P["norm_mix"][0, :], modv, 0, P["ev_w_in"], P["ev_w_in"][0], 4512, colsT, ident)
        with S.phase():
            phase_rwprep(C, colsT, P, A, onesblk, ident, vtok)
        with S.phase():
            phase_rwscan(C, A, vtok, YD, onesblk, identb, K["Eq"])
        with S.phase():
            phase_rwpost(C, YD, A, P, mixT, ident)
        with S.phase():
            phase_ssprep(C, colsT, P, xbc_tok, BCT, dtda, ident)
        with S.phase():
            phase_ssscan(C, xbc_tok, BCT, dtda, ytmp, K)
        with S.phase():
            phase_sspost(C, ytmp, xbc_tok, colsT, P, mixT, ident)
        with S.phase():
            phase_outproj(C, "op0", mixT, False, 1536, P["ev_w_out"], P["ev_w_out"][0], modv, 0, xin, xr[0], all_tiles, identb)
        with S.phase():
            phase_moe_cast(C, 0, EW, wbf)
        with S.phase():
            phase_moe_route(C, 0, xr[0], P["norm_ffn"], P["norm_ffn"][0, :], modv, P["router_w"], P["router_bias"], hT, gates, all_tiles, ident)
        with S.phase():
            phase_moe_experts(C, 0, hT, gates, wbf, modv, xr[0], xr[1], all_tiles)
        with S.phase():
            phase_mla_prep(C, 1, xr[1], P["norm_mix"], P["norm_mix"][1, :], modv, P, K["rope_cs"], KT, QT, Vtok, ident, identb)
        with S.phase():
            phase_mla_attn(C, KT, QT, Vtok, AO)
        with S.phase():
            phase_outproj(C, "op1", AO, True, 1024, P["mla_w_out"], P["mla_w_out"][0], modv, 1, xr[1], xr[2], lt, identb)
        with S.phase():
            phase_moe_cast(C, 1, EW, wbf)
        with S.phase():
            phase_moe_route(C, 1, xr[2], P["norm_ffn"], P["norm_ffn"][1, :], modv, P["router_w"], P["router_bias"], hT, gates, lt, ident)
        with S.phase():
            phase_moe_experts(C, 1, hT, gates, wbf, modv, xr[2], xr[3], lt)
        with S.phase():
            phase_final(C, xr[3], P["final_norm"], out)
        S.emit(final_bufs=[out] + [b for b in S.bufs if b.space == "dram" and b.name in dbg])
        build_program.stats = (S.nins, S.nsem)
        build_program.marks = S.marks
    return nc


def kernel(**inputs):
    cfg = Cfg(4, 2048, 256)
    kconst = host_consts(cfg)
    nc = build_program(cfg, kconst)
    x = np.asarray(inputs["x"], np.float32)
    ctx = np.asarray(inputs["ctx"], np.float32)
    c = np.asarray(inputs["c"], np.float32)
    in_maps = []
    shared = {k: np.ascontiguousarray(np.asarray(inputs[k], np.float32)) for k in PARAM_SHAPES if k != "c"}
    shared.update({"K_" + k: v for k, v in kconst.items()})
    for i in range(N_CORES):
        sl = slice(i * cfg.NB, (i + 1) * cfg.NB)
        m = dict(shared)
        m["xin"] = np.ascontiguousarray(np.concatenate([ctx[sl], x[sl]], axis=1).reshape(cfg.NTOK, 1024))
        m["c"] = np.ascontiguousarray(c[sl])
        in_maps.append(m)
    res = run_bass_kernel_spmd(nc, in_maps, core_ids=list(range(N_CORES)))
    outs = [np.asarray(r["out"]).reshape(cfg.NB, cfg.TL, 1024) for r in res.results]
    return np.concatenate(outs, axis=0).astype(np.float32)
```

```python
import numpy as np
import concourse.bass as bass
import concourse.mybir as mybir
from concourse.bass_utils import run_bass_kernel_spmd
from contextlib import ExitStack

F32 = mybir.dt.float32
BF16 = mybir.dt.bfloat16
I32 = mybir.dt.int32
U32 = mybir.dt.uint32
ALU = mybir.AluOpType
AF = mybir.ActivationFunctionType
AX = mybir.AxisListType

ENGS = ("pe", "act", "dve", "pool", "sp")


class Buf:
    def __init__(self, S, name, t, space):
        self.S = S
        self.name = name
        self.t = t
        self.space = space
        self.w = []
        self.r = []
        self.dsem = None
        self.dcnt = 0

    def __getitem__(self, k):
        return self.t[k]

    def ap(self):
        return self.t.ap() if hasattr(self.t, "ap") else self.t[:]


class View:
    def __init__(self, buf, ap):
        self.buf, self.t = buf, ap

    def __getitem__(self, k):
        return self.t[k]


def _b(x):
    return x.buf if isinstance(x, View) else x


class _Phase:
    def __init__(self, S):
        self.S = S

    def __enter__(self):
        S = self.S
        self.prev = S.cur
        self.nb = len(S.bufs)
        self.prev_ds = S.phase_dsems
        S.phase_dsems = []
        self.es = ExitStack()
        self.es.__enter__()
        S.cur = self.es
        return self

    def __exit__(self, *a):
        S = self.S
        S.marks.append({e: sum(1 for o in S.ops[e] if o[1] is not None) for e in ENGS})
        S.barrier()
        S.free_dsems.extend(S.phase_dsems)
        S.phase_dsems = self.prev_ds
        S.bufs = S.bufs[:self.nb] + [b for b in S.bufs[self.nb:] if b.space == "dram"]
        S.cur = self.prev
        self.es.__exit__(*a)
        return False


class Sched:
    def __init__(self, nc, es):
        self.nc = nc
        self.es = es
        self.ops = {e: [] for e in ENGS}
        self.sems = {}
        self.eng_sem = {}
        self.eng_cnt = {e: 0 for e in ENGS}
        self.seen = {e: {} for e in ENGS}
        self.nsem = 0
        self.nins = 0
        self.cur = es
        self.bufs = []
        self.sem_val = {}
        self.free_dsems = []
        self.phase_dsems = []
        self.pending = {e: False for e in ENGS}
        self.marks = []
        for e in ("pe", "act", "dve", "pool"):
            self.eng_sem[e] = self.new_sem("prog_" + e)

    def new_sem(self, name):
        h = self.es.enter_context(self.nc.semaphore(name))
        sid = self.nsem
        self.nsem += 1
        self.sems[sid] = h
        return sid

    def sb(self, name, shape, dt=F32):
        self.uid = getattr(self, "uid", 0) + 1
        name = "%s_u%d" % (name, self.uid)
        t = self.cur.enter_context(self.nc.sbuf_tensor(name, list(shape), dt))
        b = Buf(self, name, t, "sb")
        self.bufs.append(b)
        return b

    def ps(self, name, shape, dt=F32):
        self.uid = getattr(self, "uid", 0) + 1
        name = "%s_u%d" % (name, self.uid)
        t = self.cur.enter_context(self.nc.psum_tensor(name, list(shape), dt))
        b = Buf(self, name, t, "ps")
        self.bufs.append(b)
        return b

    def dram(self, name, shape, dt=F32, kind="Internal"):
        t = self.nc.dram_tensor(name, list(shape), dt, kind=kind).ap()
        b = Buf(self, name, t, "dram")
        self.bufs.append(b)
        return b

    def alloc_dsem(self, name):
        if self.free_dsems:
            sid = self.free_dsems.pop()
        else:
            sid = self.new_sem("dma%d" % self.nsem)
            self.sem_val[sid] = 0
        self.phase_dsems.append(sid)
        return sid

    def barrier(self):
        assert not any(self.pending.values()), "un-signalled PE group at barrier"
        targets = []
        for e in ("pe", "act", "dve", "pool"):
            if self.eng_cnt[e]:
                targets.append((self.eng_sem[e], self.eng_cnt[e]))
        for sid, v in self.sem_val.items():
            if v:
                targets.append((sid, v))
        for e in ENGS:
            waits = []
            for (sid, v) in targets:
                if self.seen[e].get(sid, 0) < v:
                    self.seen[e][sid] = v
                    waits.append((sid, v))
            if waits:
                self.ops[e].append((waits, None, None, 0))
        for b in self.bufs:
            b.w = []
            b.r = []

    def phase(self):
        return _Phase(self)

    def _waits(self, eng, reads, writes):
        need = {}
        for b in reads:
            for (s, v) in b.w:
                need[s] = max(need.get(s, 0), v)
            if b.space == "ps":
                for (s, v) in b.r:
                    need[s] = max(need.get(s, 0), v)
        for b in writes:
            if not (b.space == "dram" and eng in ("sp", "pool_dma", "act_dma")):
                for (s, v) in b.w:
                    need[s] = max(need.get(s, 0), v)
            for (s, v) in b.r:
                need[s] = max(need.get(s, 0), v)
        out = []
        seen = self.seen[eng]
        for s, v in need.items():
            if seen.get(s, 0) >= v:
                continue
            seen[s] = v
            out.append((s, v))
        return out

    def _record(self, ev, reads, writes):
        for b in writes:
            b.w = [ev]
            b.r = []
        for b in reads:
            if b in writes:
                continue
            b.r = [(s, v) for (s, v) in b.r if s != ev[0]] + [ev]

    def op(self, eng, fn, reads=(), writes=(), inc=True, skip_self=False):
        reads = [_b(x) for x in reads]
        writes = [_b(x) for x in writes]
        waits = self._waits(eng, reads, writes)
        if eng == "pe" or skip_self:
            waits = [(s_, v_) for (s_, v_) in waits if s_ != self.eng_sem[eng]]
        if inc:
            self.eng_cnt[eng] += 1
            ev = (self.eng_sem[eng], self.eng_cnt[eng])
            self.pending[eng] = False
        else:
            assert eng == "pe"
            ev = (self.eng_sem[eng], self.eng_cnt[eng] + 1)
            self.pending[eng] = True
        self.ops[eng].append((waits, fn, ev, 1 if inc else 0))
        self._record(ev, reads, writes)
        self.nins += 1

    def dma(self, out_buf, out_ap, in_buf, in_ap, eng="sp", **kw):
        out_buf, in_buf = _b(out_buf), _b(in_buf)
        own = out_buf if out_buf.space != "dram" else in_buf
        if own.dsem is None:
            own.dsem = self.alloc_dsem(own.name)
            own.dcnt = self.sem_val[own.dsem]
        waits = self._waits(eng, [in_buf], [out_buf])
        same_gen = (out_buf.space != "dram" and not out_buf.r and out_buf.w and all(s_ == own.dsem for (s_, _) in out_buf.w))
        if (not same_gen) and own.dcnt > 0 and self.seen[eng].get(own.dsem, 0) < own.dcnt:
            self.seen[eng][own.dsem] = own.dcnt
            waits.append((own.dsem, own.dcnt))
        own.dcnt += 16
        self.sem_val[own.dsem] = own.dcnt
        ev = (own.dsem, own.dcnt)

        def fn(e, out_ap=out_ap, in_ap=in_ap, kw=kw):
            return e.dma_start(out=out_ap, in_=in_ap, **kw)
        self.ops[eng].append((waits, fn, ev, 16))
        if out_buf.space == "dram":
            out_buf.w = [(s, v) for (s, v) in out_buf.w if s != ev[0]] + [ev]
            out_buf.r = []
            in_buf.r = [(s, v) for (s, v) in in_buf.r if s != ev[0]] + [ev]
        else:
            self._record(ev, [in_buf], [out_buf])
        self.nins += 1

    def reset_dram(self, b):
        b.w = []
        b.r = []

    def emit(self, final_bufs=()):
        nc = self.nc
        fw = {}
        for b in final_bufs:
            for (s, v) in b.w:
                fw[s] = max(fw.get(s, 0), v)
        for e in ("pe", "act", "dve", "pool"):
            if self.eng_cnt[e]:
                fw[self.eng_sem[e]] = self.eng_cnt[e]
        sems = self.sems
        ops = self.ops
        with nc.Block() as block:
            def run(engobj, lst):
                for (waits, fn, ev, inc) in lst:
                    for (s, v) in waits:
                        engobj.wait_ge(sems[s], v)
                    if fn is not None:
                        ins = fn(engobj)
                        if inc:
                            ins.then_inc(sems[ev[0]], inc)

            @block.sync
            def _(e):
                run(e, ops["sp"])
                for s, v in fw.items():
                    e.wait_ge(sems[s], v)

            @block.tensor
            def _(e):
                run(e, ops["pe"])

            @block.scalar
            def _(e):
                run(e, ops["act"])

            @block.vector
            def _(e):
                run(e, ops["dve"])

            @block.gpsimd
            def _(e):
                run(e, ops["pool"])


class Cfg:
    def __init__(self, NB=4, TL=2048, TC=256):
        self.NB, self.TL, self.TC = NB, TL, TC
        self.T = TL + TC
        self.NTOK = NB * self.T
        self.D = 1024


class Ctx:
    def __init__(self, S, cfg, io):
        self.S, self.cfg, self.io = S, cfg, io
        self.rr = 0

    def D(self, name, shape, dt=F32):
        kind = self.io.get(name, "Internal")
        return self.S.dram(name, shape, dt, kind=kind)


def alt(i):
    return "act" if i % 2 == 0 else "dve"


def evac(S, i, out_ap, in_ap, reads, writes):
    if i % 2 == 0:
        S.op("act", lambda e: e.copy(out=out_ap, in_=in_ap), reads=reads, writes=writes)
    else:
        S.op("dve", lambda e: e.tensor_copy(out=out_ap, in_=in_ap), reads=reads, writes=writes)


def phase_mod(C, c_in, cctx_in, ada_w, ada_b, modv):
    S, cfg = C.S, C.cfg
    NB = cfg.NB
    R = NB + 1
    cT = S.sb("mod_cT", [128, 8, R])
    for b in range(NB):
        S.dma(cT, cT[:, :, b:b + 1], c_in, c_in[b, :].rearrange("(k p o) -> p k o", p=128, o=1), allow_slow_non_contiguous=True)
    S.dma(cT, cT[:, :, NB:R], cctx_in, cctx_in.t.rearrange("(k p o) -> p k o", p=128, o=1), allow_slow_non_contiguous=True)
    sT = S.sb("mod_sT", [128, 8, R])
    S.op("act", lambda e: e.activation(out=sT[:], in_=cT[:], func=AF.Silu), reads=[cT], writes=[sT])
    wbufs = [S.sb("mod_w%d" % i, [128, 8, 512]) for i in range(2)]
    pss = [S.ps("mod_ps%d" % i, [R, 512]) for i in range(2)]
    bias = S.sb("mod_bias", [R, 6144])
    orow = S.sb("mod_orow", [R, 6144])
    for l in range(2):
        S.dma(bias, bias[:], ada_b, ada_b[l, :].partition_broadcast(R))
        wv = ada_w[l].rearrange("(k p) c -> p k c", p=128)
        for j in range(12):
            wb = wbufs[j % 2]
            ps = pss[j % 2]
            S.dma(wb, wb[:], ada_w, wv[:, :, j * 512:(j + 1) * 512])
            for k in range(8):
                S.op("pe", lambda e, ps=ps, wb=wb, k=k: e.matmul(ps[:], sT[:, k, :], wb[:, k, :], start=(k == 0), stop=(k == 7)),
                     reads=[sT, wb], writes=[ps], inc=(k == 7))
            S.op("dve", lambda e, ps=ps, j=j: e.tensor_tensor(out=orow[:, j * 512:(j + 1) * 512], in0=ps[:], in1=bias[:, j * 512:(j + 1) * 512], op=ALU.add),
                 reads=[ps, bias], writes=[orow])
        S.dma(modv, modv[l], orow, orow[:])


class ModTiles:
    def __init__(self, C, name, modv, layer, shift_idx, scale_idx, gvec_ap, gvec_buf):
        S, cfg = C.S, C.cfg
        R = cfg.NB + 1
        D = cfg.D
        self.G = [S.sb("%s_G%d" % (name, r), [128, D]) for r in range(R)]
        self.Sh = [S.sb("%s_S%d" % (name, r), [128, D]) for r in range(R)]
        gb = S.sb("%s_g" % name, [128, D])
        S.dma(gb, gb[:], gvec_buf, gvec_ap.partition_broadcast(128))
        for r in range(R):
            G, Sh = self.G[r], self.Sh[r]
            S.dma(G, G[:], modv, modv[layer, r, scale_idx * D:(scale_idx + 1) * D].partition_broadcast(128))
            S.dma(Sh, Sh[:], modv, modv[layer, r, shift_idx * D:(shift_idx + 1) * D].partition_broadcast(128))
            S.op("dve", lambda e, G=G: e.scalar_tensor_tensor(out=G[:], in0=G[:], scalar=1.0, in1=gb[:], op0=ALU.add, op1=ALU.mult),
                 reads=[G, gb], writes=[G])

    def row(self, cfg, tile_idx):
        tpb = cfg.T // 128
        b, tt = divmod(tile_idx, tpb)
        return cfg.NB if tt < cfg.TC // 128 else b


class NormT:
    def __init__(self, C, name, ident, want32=False):
        S = C.S
        self.C, self.name, self.ident = C, name, ident
        D = C.cfg.D
        self.xt = [S.sb("%s_xt%d" % (name, i), [128, D]) for i in range(2)]
        self.junk = S.sb("%s_junk" % name, [128, D], BF16)
        self.ss = [S.sb("%s_ss%d" % (name, i), [128, 1]) for i in range(2)]
        self.h = [S.sb("%s_h%d" % (name, i), [128, D]) for i in range(2)]
        self.pt = [S.ps("%s_pt%d" % (name, i), [128, 4, 128]) for i in range(2)]
        self.n = 0

    def tile(self, xsrc, row0, mt, r, outs):
        S = self.C.S
        D = self.C.cfg.D
        i = self.n % 2
        self.n += 1
        xt, ss, h = self.xt[i], self.ss[i], self.h[i]
        S.dma(xt, xt[:], xsrc, xsrc[row0:row0 + 128, :])
        junk = self.junk
        S.op("act", lambda e: e.activation(out=junk[:], in_=xt[:], func=AF.Square, accum_out=ss[:]), reads=[xt], writes=[junk, ss])
        S.op("act", lambda e: e.activation(out=ss[:], in_=ss[:], func=AF.Sqrt, bias=NORM_EPS, scale=1.0 / D), reads=[ss], writes=[ss])
        S.op("dve", lambda e: e.reciprocal(out=ss[:], in_=ss[:]), reads=[ss], writes=[ss])
        G, Sh = mt.G[r], mt.Sh[r]
        S.op("dve", lambda e: e.scalar_tensor_tensor(out=h[:], in0=xt[:], scalar=ss[:, 0:1], in1=G[:], op0=ALU.mult, op1=ALU.mult),
             reads=[xt, ss, G], writes=[h])
        S.op("pool", lambda e: e.tensor_tensor(out=h[:], in0=h[:], in1=Sh[:], op=ALU.add), reads=[h, Sh], writes=[h])
        ident = self.ident
        for half in range(2):
            pt = self.pt[half]
            for kk in range(4):
                k = half * 4 + kk
                S.op("pe", lambda e, pt=pt, kk=kk, k=k: e.transpose(pt[:, kk, :], h[:, k * 128:(k + 1) * 128], ident[:]),
                     reads=[h, ident], writes=[pt], inc=(kk == 3))
            for oi, (ob, ofn) in enumerate(outs):
                evac(S, half + oi, ofn(half * 4, half * 4 + 4), pt[:], [pt], [ob])


NORM_EPS = 1e-6


def load_w_bf16(C, name, wsrc_buf, wv, ncols, kch):
    S = C.S
    wb = S.sb(name, [128, kch, ncols], BF16)
    CH = max(128, min(512, 2048 // kch))
    stg = [S.sb("%s_stg%d" % (name, i), [128, kch, CH]) for i in range(2)]
    nchunk = (ncols + CH - 1) // CH
    for j in range(nchunk):
        c0 = j * CH
        c1 = min(ncols, c0 + CH)
        st = stg[j % 2]
        S.dma(st, st[:, :, 0:c1 - c0], wsrc_buf, wv[:, :, c0:c1])
        S.op("pool", lambda e, st=st, c0=c0, c1=c1: e.tensor_copy(out=wb[:, :, c0:c1], in_=st[:, :, 0:c1 - c0]), reads=[st], writes=[wb])
    return wb


def phase_normproj(C, name, xsrc, gvec_buf, gvec_ap, modv, layer, W_buf, W_ap, ncols, outT, ident):
    S, cfg = C.S, C.cfg
    mt = ModTiles(C, name + "_mt", modv, layer, 0, 1, gvec_ap, gvec_buf)
    wb = load_w_bf16(C, name + "_w", W_buf, W_ap.rearrange("(k p) c -> p k c", p=128), ncols, 8)
    nt = NormT(C, name + "_nt", ident)
    hT = [S.sb("%s_hT%d" % (name, i), [128, 8, 512], BF16) for i in range(2)]
    pss = [S.ps("%s_ps%d" % (name, i), [128, 512]) for i in range(3)]
    ost = [S.sb("%s_ost%d" % (name, i), [128, 512]) for i in range(3)]
    ntl = cfg.NTOK // 128
    nsb = (ntl + 3) // 4
    ncj = (ncols + 127) // 128
    cnt = 0
    for sb in range(nsb):
        ht = hT[sb % 2]
        nti = min(4, ntl - sb * 4)
        n = nti * 128
        for ti in range(nti):
            tile_idx = sb * 4 + ti
            r = mt.row(cfg, tile_idx)
            nt.tile(xsrc, tile_idx * 128, mt, r, [(ht, lambda k0, k1, ti=ti, ht=ht: ht[:, k0:k1, ti * 128:(ti + 1) * 128])])
        for j in range(ncj):
            c0 = j * 128
            cw = min(128, ncols - c0)
            ps = pss[cnt % 3]
            ob = ost[cnt % 3]
            for k in range(8):
                S.op("pe", lambda e, ps=ps, k=k, c0=c0, cw=cw, ht=ht, n=n: e.matmul(ps[0:cw, 0:n], wb[:, k, c0:c0 + cw], ht[:, k, 0:n], start=(k == 0), stop=(k == 7)),
                     reads=[wb, ht], writes=[ps], inc=(k == 7))
            evac(S, cnt, ob[0:cw, 0:n], ps[0:cw, 0:n], [ps], [ob])
            S.dma(outT, outT[c0:c0 + cw, sb * 512:sb * 512 + n], ob, ob[0:cw, 0:n])
            cnt += 1


def seg_blocks(cfg, blk=512):
    out = []
    for b in range(cfg.NB):
        for (s0, s1) in ((0, cfg.TC), (cfg.TC, cfg.T)):
            t = s0
            while t < s1:
                n = min(blk, s1 - t)
                out.append((b, t, n, t > s0, t + n < s1))
                t += n
    return out


def vec_cols(C, name, src_buf, src_ap_1d, nch):
    S = C.S
    t = S.sb(name, [128, nch])
    S.dma(t, t[:], src_buf, src_ap_1d.rearrange("(c p) -> p c", p=128), allow_slow_non_contiguous=True)
    return t


def phase_rwprep(C, colsT, P, A, onesblk, ident, vtok):
    S, cfg = C.S, C.cfg
    T = cfg.T
    mu = vec_cols(C, "rp_mu", P["rw_mu"], P["rw_mu"][0, :], 15)
    w0 = [vec_cols(C, "rp_w0%d" % d, P["rw_w0"], P["rw_w0"][0, d, :], 4) for d in range(2)]
    a0 = [vec_cols(C, "rp_a0%d" % d, P["rw_a0"], P["rw_a0"][0, d, :], 4) for d in range(2)]
    kkv = vec_cols(C, "rp_kkv", P["rw_kk"], P["rw_kk"][0, :], 4)
    kav = vec_cols(C, "rp_kav", P["rw_ka"], P["rw_ka"][0, :], 4)
    rkv = vec_cols(C, "rp_rkv", P["rw_rk"], P["rw_rk"][0, :], 4)
    omka = S.sb("rp_omka", [128, 4])
    S.op("dve", lambda e: e.tensor_scalar(out=omka[:], in0=kav[:], scalar1=-1.0, scalar2=1.0, op0=ALU.mult, op1=ALU.add), reads=[kav], writes=[omka])
    W2 = S.sb("rp_W2", [128, 512])
    A2 = S.sb("rp_A2", [128, 512])
    G2 = S.sb("rp_G2", [128, 512])
    for d in range(2):
        S.dma(W2, W2[d * 64:(d + 1) * 64, :], P["rw_w2"], P["rw_w2"][0, d, :, :])
        S.dma(A2, A2[d * 64:(d + 1) * 64, :], P["rw_a2"], P["rw_a2"][0, d, :, :])
    S.dma(G2, G2[:], P["rw_g2"], P["rw_g2"][0, :, :])
    NB_ = 512
    raw = [S.sb("rp_raw%d" % i, [128, NB_ + 2]) for i in range(3)]
    MX = [S.sb("rp_mx%d" % c, [128, NB_]) for c in range(15)]
    tmp = [S.sb("rp_tmp%d" % i, [128, NB_]) for i in range(2)]
    TH = S.sb("rp_th", [128, NB_])
    SG = S.sb("rp_sg", [128, NB_])
    Aa = [[S.sb("rp_a%d_%d" % (d, cc), [128, NB_]) for cc in range(4)] for d in range(2)]
    KD = [[S.sb("rp_kd%d_%d" % (d, cc), [128, NB_]) for cc in range(4)] for d in range(2)]
    KK = [S.sb("rp_kk%d" % cc, [128, NB_]) for cc in range(4)]
    ost = [S.sb("rp_ost%d" % i, [128, NB_]) for i in range(4)]
    pss = [S.ps("rp_ps%d" % i, [128, NB_]) for i in range(4)]
    vtb = [S.sb("rp_vtb%d" % i, [128, 512], BF16) for i in range(2)]
    oc = [0]
    pc = [0]

    def nps():
        pc[0] += 1
        return pss[pc[0] % 4]

    def nost():
        oc[0] += 1
        return ost[oc[0] % 4]

    def store(arr, cc, b, t0, n, buf, ap):
        S.dma(arr, arr[cc * 128:(cc + 1) * 128, b * T + t0:b * T + t0 + n], buf, ap)

    for bi, (b, t0, n, hl, hr) in enumerate(seg_blocks(cfg, NB_)):
        g0 = b * T + t0
        for c in range(15):
            rw = raw[c % 3]
            if not hl:
                S.op("pool", lambda e, rw=rw: e.memset(rw[:, 0:1], 0.0), writes=[rw])
            if not hr:
                S.op("pool", lambda e, rw=rw, n=n: e.memset(rw[:, n + 1:n + 2], 0.0), writes=[rw])
            lo = g0 - (1 if hl else 0)
            hi = g0 + n + (1 if hr else 0)
            S.dma(rw, rw[:, (0 if hl else 1):(0 if hl else 1) + hi - lo], colsT, colsT[c * 128:(c + 1) * 128, lo:hi])
            tp = tmp[c % 2]
            mx = MX[c]
            S.op("dve", lambda e, rw=rw, tp=tp, n=n: e.tensor_tensor(out=tp[:, 0:n], in0=rw[:, 0:n], in1=rw[:, 2:n + 2], op=ALU.add), reads=[rw], writes=[tp])
            S.op("dve", lambda e, rw=rw, tp=tp, n=n: e.scalar_tensor_tensor(out=tp[:, 0:n], in0=tp[:, 0:n], scalar=0.5, in1=rw[:, 1:n + 1], op0=ALU.mult, op1=ALU.subtract),
                 reads=[rw, tp], writes=[tp])
            S.op("dve", lambda e, rw=rw, tp=tp, n=n, c=c, mx=mx: e.scalar_tensor_tensor(out=mx[:, 0:n], in0=tp[:, 0:n], scalar=mu[:, c:c + 1], in1=rw[:, 1:n + 1], op0=ALU.mult, op1=ALU.add),
                 reads=[rw, tp, mu], writes=[mx])
        for cc in range(4):
            store(A["r"], cc, b, t0, n, MX[cc], MX[cc][:, 0:n])
        for j in range(n // 128):
            ps = nps()
            for cc in range(4):
                S.op("pe", lambda e, ps=ps, cc=cc, j=j: e.transpose(ps[:, cc * 128:(cc + 1) * 128], MX[8 + cc][:, j * 128:(j + 1) * 128], ident[:]),
                     reads=[MX[8 + cc], ident], writes=[ps], inc=(cc == 3))
            vb = vtb[j % 2]
            evac(S, j, vb[:], ps[:, 0:512], [ps], [vb])
            S.dma(vtok, vtok[g0 + j * 128:g0 + (j + 1) * 128, :], vb, vb[:])
        for cc in range(4):
            kk = KK[cc]
            tp = tmp[cc % 2]
            S.op("dve", lambda e, cc=cc, kk=kk, n=n: e.tensor_scalar(out=kk[:, 0:n], in0=MX[4 + cc][:, 0:n], scalar1=kkv[:, cc:cc + 1], scalar2=None, op0=ALU.mult),
                 reads=[MX[4 + cc], kkv], writes=[kk])
            S.op("pool", lambda e, kk=kk, tp=tp, n=n: e.tensor_tensor(out=tp[:, 0:n], in0=kk[:, 0:n], in1=kk[:, 0:n], op=ALU.mult), reads=[kk], writes=[tp])
            ps = nps()
            S.op("pe", lambda e, ps=ps, tp=tp, n=n: e.matmul(ps[:, 0:n], onesblk[:], tp[:, 0:n], start=True, stop=True), reads=[onesblk, tp], writes=[ps])
            S.op("act", lambda e, ps=ps, tp=tp, n=n: e.activation(out=tp[:, 0:n], in_=ps[:, 0:n], func=AF.Sqrt, bias=1e-12, scale=1.0), reads=[ps], writes=[tp])
            S.op("dve", lambda e, tp=tp, n=n: e.reciprocal(out=tp[:, 0:n], in_=tp[:, 0:n]), reads=[tp], writes=[tp])
            S.op("dve", lambda e, kk=kk, tp=tp, n=n: e.tensor_tensor(out=kk[:, 0:n], in0=kk[:, 0:n], in1=tp[:, 0:n], op=ALU.mult), reads=[kk, tp], writes=[kk])
            store(A["kk"], cc, b, t0, n, kk, kk[:, 0:n])
        S.op("act", lambda e, n=n: e.activation(out=TH[:, 0:n], in_=MX[12][:, 0:n], func=AF.Tanh), reads=[MX[12]], writes=[TH])
        for d in range(2):
            for cc in range(4):
                ps = nps()
                S.op("pe", lambda e, ps=ps, d=d, cc=cc, n=n: e.matmul(ps[:, 0:n], W2[d * 64:(d + 1) * 64, cc * 128:(cc + 1) * 128], TH[d * 64:(d + 1) * 64, 0:n], start=True, stop=True),
                     reads=[W2, TH], writes=[ps])
                ob = nost()
                S.op("act", lambda e, ps=ps, ob=ob, d=d, cc=cc, n=n: e.activation(out=ob[:, 0:n], in_=ps[:, 0:n], func=AF.Sigmoid, bias=w0[d][:, cc:cc + 1], scale=1.0),
                     reads=[ps, w0[d]], writes=[ob])
                S.op("act", lambda e, ob=ob, n=n: e.activation(out=ob[:, 0:n], in_=ob[:, 0:n], func=AF.Exp, scale=-0.6065306597126334), reads=[ob], writes=[ob])
                store(A["w%d" % d], cc, b, t0, n, ob, ob[:, 0:n])
        for d in range(2):
            for cc in range(4):
                ps = nps()
                S.op("pe", lambda e, ps=ps, d=d, cc=cc, n=n: e.matmul(ps[:, 0:n], A2[d * 64:(d + 1) * 64, cc * 128:(cc + 1) * 128], MX[13][d * 64:(d + 1) * 64, 0:n], start=True, stop=True),
                     reads=[A2, MX[13]], writes=[ps])
                aa = Aa[d][cc]
                S.op("act", lambda e, ps=ps, aa=aa, d=d, cc=cc, n=n: e.activation(out=aa[:, 0:n], in_=ps[:, 0:n], func=AF.Sigmoid, bias=a0[d][:, cc:cc + 1], scale=1.0),
                     reads=[ps, a0[d]], writes=[aa])
                ob = nost()
                S.op("pool", lambda e, ob=ob, aa=aa, cc=cc, n=n: e.tensor_tensor(out=ob[:, 0:n], in0=KK[cc][:, 0:n], in1=aa[:, 0:n], op=ALU.mult), reads=[KK[cc], aa], writes=[ob])
                store(A["b%d" % d], cc, b, t0, n, ob, ob[:, 0:n])
                kd = KD[d][cc]
                S.op("dve", lambda e, kd=kd, aa=aa, cc=cc, n=n: e.tensor_scalar(out=kd[:, 0:n], in0=aa[:, 0:n], scalar1=kav[:, cc:cc + 1], scalar2=omka[:, cc:cc + 1], op0=ALU.mult, op1=ALU.add),
                     reads=[aa, kav, omka], writes=[kd])
                S.op("dve", lambda e, kd=kd, cc=cc, n=n: e.tensor_tensor(out=kd[:, 0:n], in0=kd[:, 0:n], in1=MX[4 + cc][:, 0:n], op=ALU.mult), reads=[kd, MX[4 + cc]], writes=[kd])
                store(A["kd%d" % d], cc, b, t0, n, kd, kd[:, 0:n])
        for cc in range(4):
            tp = tmp[cc % 2]
            S.op("pool", lambda e, tp=tp, cc=cc, n=n: e.tensor_tensor(out=tp[:, 0:n], in0=KD[0][cc][:, 0:n], in1=KD[1][cc][:, 0:n], op=ALU.add), reads=[KD[0][cc], KD[1][cc]], writes=[tp])
            S.op("dve", lambda e, tp=tp, cc=cc, n=n: e.scalar_tensor_tensor(out=tp[:, 0:n], in0=tp[:, 0:n], scalar=rkv[:, cc:cc + 1], in1=MX[cc][:, 0:n], op0=ALU.mult, op1=ALU.mult),
                 reads=[tp, rkv, MX[cc]], writes=[tp])
            ps = nps()
            S.op("pe", lambda e, ps=ps, tp=tp, n=n: e.matmul(ps[:, 0:n], onesblk[:], tp[:, 0:n], start=True, stop=True), reads=[onesblk, tp], writes=[ps])
            ob = nost()
            S.op("dve", lambda e, ps=ps, ob=ob, cc=cc, n=n: e.tensor_tensor(out=ob[:, 0:n], in0=ps[:, 0:n], in1=MX[8 + cc][:, 0:n], op=ALU.mult), reads=[ps, MX[8 + cc]], writes=[ob])
            store(A["bonus"], cc, b, t0, n, ob, ob[:, 0:n])
        S.op("act", lambda e, n=n: e.activation(out=SG[:, 0:n], in_=MX[14][:, 0:n], func=AF.Sigmoid), reads=[MX[14]], writes=[SG])
        for cc in range(4):
            ps = nps()
            S.op("pe", lambda e, ps=ps, cc=cc, n=n: e.matmul(ps[:, 0:n], G2[:, cc * 128:(cc + 1) * 128], SG[:, 0:n], start=True, stop=True), reads=[G2, SG], writes=[ps])
            ob = nost()
            evac(S, cc, ob[:, 0:n], ps[:, 0:n], [ps], [ob])
            store(A["g"], cc, b, t0, n, ob, ob[:, 0:n])


SCAN_ACT = False


def cust_ap(base_ap, dims):
    pa = base_ap.ap[0]
    return bass.AP(base_ap.tensor, base_ap.offset, [[pa[0], pa[1]]] + [[st, ct] for (st, ct) in dims])


def phase_rwscan(C, A, vtok, YD, onesblk, identb, Eq, bg=None, bg_every=5):
    S, cfg = C.S, C.cfg
    NB, T, TC = cfg.NB, cfg.T, cfg.TC
    NCB = 4 * NB
    Fh = NCB * 64
    NQh = Fh // 512
    NQ = 2 * NQh
    TB = 64
    YB = 4

    def st(name):
        return [S.sb("%s%d" % (name, d), [128, Fh]) for d in range(2)]
    M, MW, TMP, TMP2, TMP3, T4 = st("sc_M"), st("sc_MW"), st("sc_TMP"), st("sc_TMP2"), st("sc_TMP3"), st("sc_T4")
    saPS = [S.ps("sc_saPS%d" % d, [128, Fh]) for d in range(2)]
    vPS = S.ps("sc_vPS", [128, Fh])
    yPS = S.ps("sc_yPS", [2 * NQ, 512])
    OPS = [S.sb("sc_OPS%d" % i, [128, 5, 2, NCB, TB]) for i in range(2)]
    VT = [S.sb("sc_VT%d" % i, [TB, 2, 2, NCB, 64], BF16) for i in range(2)]
    YS = [S.sb("sc_YS%d" % i, [2 * NQ, YB, 512]) for i in range(2)]
    for d in range(2):
        S.op("pool", lambda e, d=d: e.memset(M[d][:], 0.0), writes=[M[d]])

    def v3(buf):
        return buf[:].rearrange("p (c v) -> p c v", c=NCB)

    def tb_of(s):
        return TC - 1 - s if s < TC else T + TC - 1 - s

    def readout(sp, ops_p, col_p):
        for d, eng in ((0, "dve"), (1, "pool")):
            base = ops_p[:, 4, d, 0, col_p[d]:col_p[d] + 1]
            Ro = cust_ap(base, [(TB, NCB), (0, 64)])
            S.op(eng, lambda e, d=d, Ro=Ro: e.tensor_tensor(out=v3(T4[d]), in0=v3(M[d]), in1=Ro, op=ALU.mult), reads=[M[d], ops_p], writes=[T4[d]])
        for d in range(2):
            for q in range(NQh):
                qg = d * NQh + q
                S.op("pe", lambda e, d=d, q=q, qg=qg: e.matmul(yPS[:, :], Eq[:, qg, :], T4[d][:, q * 512:(q + 1) * 512], start=(qg == 0), stop=(qg == NQ - 1)),
                     reads=[Eq, T4[d]], writes=[yPS], inc=(qg == NQ - 1))
        ys = YS[(sp // YB) % 2]
        S.op("act", lambda e, ys=ys, sp=sp: e.copy(out=ys[:, sp % YB, :], in_=yPS[:, :]), reads=[yPS], writes=[ys])
        if sp % YB == YB - 1:
            sa_ = sp - YB + 1
            S.dma(YD, YD[0:NQ, sa_:sa_ + YB, :], ys, ys[0:NQ, :, :])
            tlo = tb_of(sp)
            S.dma(YD, YD[NQ:2 * NQ, tlo:tlo + YB, :][:, ::-1, :], ys, ys[NQ:2 * NQ, :, :])

    names = [("kk", "kk"), ("w0", "w1"), ("b0", "b1"), ("kd0", "kd1"), ("r", "r")]
    prev = None
    for blk in range(T // TB):
        s0 = blk * TB
        ops = OPS[blk % 2]
        vt = VT[blk % 2]
        lo = tb_of(s0 + TB - 1)
        tok0 = (s0, lo)
        for a_, nm in enumerate(names):
            for d in range(2):
                arr = A[nm[d]]
                av = arr.t.rearrange("(c p) (b t) -> c p b t", p=128, b=NB)
                for c in range(4):
                    S.dma(ops, ops[:, a_, d, c * NB:(c + 1) * NB, :], arr, av[c, :, :, tok0[d]:tok0[d] + TB])
        vv = vtok.t.rearrange("(b t) (c h v) -> b t c h v", b=NB, c=4, h=2)
        for d in range(2):
            for c in range(4):
                for b in range(NB):
                    S.dma(vt, vt[:, :, d, c * NB + b, :], vtok, vv[b, tok0[d]:tok0[d] + TB, c, :, :])
        for j in range(TB):
            s = s0 + j
            col = (j, tb_of(s) - lo)

            def opnd(a_, d, ops=ops, col=col):
                base = ops[:, a_, d, 0, col[d]:col[d] + 1]
                return cust_ap(base, [(TB, NCB), (0, 64)])
            for d in range(2):
                sel = identb[0:TB, col[d]:col[d] + 1].to_broadcast([TB, 64])
                for h2 in range(2):
                    vsrc = vt[:, h2, d].rearrange("t c v -> t (c v)")
                    for q in range(NQh):
                        S.op("pe", lambda e, h2=h2, q=q, sel=sel, vsrc=vsrc: e.matmul(vPS[h2 * 64:(h2 + 1) * 64, q * 512:(q + 1) * 512], sel, vsrc[:, q * 512:(q + 1) * 512], start=True, stop=True),
                             reads=[identb, vt], writes=[vPS], inc=(h2 == 1 and q == NQh - 1))
                KDo = opnd(3, d)
                S.op("dve", lambda e, d=d, KDo=KDo: e.tensor_tensor(out=v3(TMP3[d]), in0=v3(vPS), in1=KDo, op=ALU.mult), reads=[vPS, ops], writes=[TMP3[d]])
            for d in range(2):
                Wo = opnd(1, d)
                S.op("pool", lambda e, d=d, Wo=Wo: e.tensor_tensor(out=v3(MW[d]), in0=v3(M[d]), in1=Wo, op=ALU.mult), reads=[M[d], ops], writes=[MW[d]])
            for d in range(2):
                KKo = opnd(0, d)
                S.op("dve", lambda e, d=d, KKo=KKo: e.tensor_tensor(out=v3(TMP[d]), in0=v3(M[d]), in1=KKo, op=ALU.mult), reads=[M[d], ops], writes=[TMP[d]])
                for q in range(NQh):
                    S.op("pe", lambda e, d=d, q=q: e.matmul(saPS[d][:, q * 512:(q + 1) * 512], onesblk[:], TMP[d][:, q * 512:(q + 1) * 512], start=True, stop=True),
                         reads=[onesblk, TMP[d]], writes=[saPS[d]], inc=(q == NQh - 1))
            if prev is not None:
                readout(*prev)
            for d in range(2):
                Bo = opnd(2, d)
                S.op("dve", lambda e, d=d, Bo=Bo: e.tensor_tensor(out=v3(TMP2[d]), in0=v3(saPS[d]), in1=Bo, op=ALU.mult), reads=[saPS[d], ops], writes=[TMP2[d]])
            for d in range(2):
                S.op("pool", lambda e, d=d: e.tensor_tensor(out=MW[d][:], in0=MW[d][:], in1=TMP3[d][:], op=ALU.add), reads=[MW[d], TMP3[d]], writes=[MW[d]])
            for d in range(2):
                S.op("dve", lambda e, d=d: e.tensor_tensor(out=M[d][:], in0=MW[d][:], in1=TMP2[d][:], op=ALU.subtract), reads=[MW[d], TMP2[d]], writes=[M[d]])
            prev = (s, ops, col)
            if bg is not None and s % bg_every == 0:
                next(bg, None)
    readout(*prev)
    if bg is not None:
        for _ in bg:
            pass


def phase_rwpost(C, YD, A, P, mixT, ident):
    S, cfg = C.S, C.cfg
    NB, T = cfg.NB, cfg.T
    gnw = vec_cols(C, "rq_gnw", P["rw_gn_w"], P["rw_gn_w"][0, :], 4)
    gnb = vec_cols(C, "rq_gnb", P["rw_gn_b"], P["rw_gn_b"][0, :], 4)
    Y = [S.sb("rq_Y%d" % i, [128, 2, 4, 2, 64]) for i in range(2)]
    ysum = S.sb("rq_ysum", [128, 8, 64])
    sq = S.sb("rq_sq", [128, 8, 64])
    st1 = S.sb("rq_st1", [128, 8])
    st2 = S.sb("rq_st2", [128, 8])
    BG = [S.sb("rq_BG%d" % i, [128, 2, 4, 128]) for i in range(2)]
    pt = S.ps("rq_pt", [128, 4, 128])
    o32 = S.sb("rq_o32", [128, 4, 128])
    ob = [S.sb("rq_ob%d" % i, [128, 4, 128], BF16) for i in range(2)]
    bv = A["bonus"].t.rearrange("(c p) n -> p c n", p=128)
    gv = A["g"].t.rearrange("(c p) n -> p c n", p=128)
    mv = mixT.t[0:512, :].rearrange("(c p) n -> p c n", p=128)
    for ti in range(cfg.NTOK // 128):
        b, tt = divmod(ti, T // 128)
        t0 = tt * 128
        g0 = ti * 128
        y = Y[ti % 2]
        bg = BG[ti % 2]
        for d in range(2):
            for c in range(4):
                n0 = ((d * 4 + c) * NB + b) * 64
                q, col0 = n0 // 512, n0 % 512
                for h2 in range(2):
                    S.dma(y, y[:, d, c, h2, :], YD, YD[2 * q + h2, t0:t0 + 128, col0:col0 + 64])
        S.dma(bg, bg[:, 0], A["bonus"], bv[:, :, g0:g0 + 128])
        S.dma(bg, bg[:, 1], A["g"], gv[:, :, g0:g0 + 128])
        yf = y[:, 0].rearrange("p c h v -> p (c h) v")
        yb = y[:, 1].rearrange("p c h v -> p (c h) v")
        S.op("dve", lambda e, yf=yf, yb=yb: e.tensor_tensor(out=ysum[:], in0=yf, in1=yb, op=ALU.add), reads=[y], writes=[ysum])
        S.op("dve", lambda e: e.reduce_sum(out=st1[:], in_=ysum[:], axis=AX.X), reads=[ysum], writes=[st1])
        S.op("dve", lambda e: e.tensor_scalar(out=st1[:], in0=st1[:], scalar1=1.0 / 64, scalar2=None, op0=ALU.mult), reads=[st1], writes=[st1])
        S.op("dve", lambda e: e.tensor_tensor(out=ysum[:], in0=ysum[:], in1=st1[:].unsqueeze(2).to_broadcast([128, 8, 64]), op=ALU.subtract), reads=[ysum, st1], writes=[ysum])
        S.op("pool", lambda e: e.tensor_tensor(out=sq[:], in0=ysum[:], in1=ysum[:], op=ALU.mult), reads=[ysum], writes=[sq])
        S.op("dve", lambda e: e.reduce_sum(out=st2[:], in_=sq[:], axis=AX.X), reads=[sq], writes=[st2])
        S.op("act", lambda e: e.activation(out=st2[:], in_=st2[:], func=AF.Sqrt, bias=RW_GN_EPS, scale=1.0 / 64), reads=[st2], writes=[st2])
        S.op("dve", lambda e: e.reciprocal(out=st2[:], in_=st2[:]), reads=[st2], writes=[st2])
        S.op("dve", lambda e: e.tensor_tensor(out=ysum[:], in0=ysum[:], in1=st2[:].unsqueeze(2).to_broadcast([128, 8, 64]), op=ALU.mult), reads=[ysum, st2], writes=[ysum])
        for c in range(4):
            S.op("pe", lambda e, c=c: e.transpose(pt[:, c, :], ysum[:, 2 * c:2 * c + 2, :].rearrange("p h v -> p (h v)"), ident[:]), reads=[ysum, ident], writes=[pt], inc=(c == 3))
        for c in range(4):
            S.op("dve", lambda e, c=c: e.tensor_scalar(out=o32[:, c, :], in0=pt[:, c, :], scalar1=gnw[:, c:c + 1], scalar2=gnb[:, c:c + 1], op0=ALU.mult, op1=ALU.add),
                 reads=[pt, gnw, gnb], writes=[o32])
        S.op("pool", lambda e, bg=bg: e.tensor_tensor(out=o32[:], in0=o32[:], in1=bg[:, 0], op=ALU.add), reads=[o32, bg], writes=[o32])
        o = ob[ti % 2]
        S.op("dve", lambda e, bg=bg, o=o: e.tensor_tensor(out=o[:], in0=o32[:], in1=bg[:, 1], op=ALU.mult), reads=[o32, bg], writes=[o])
        S.dma(mixT, mv[:, :, g0:g0 + 128], o, o[:])


RW_GN_EPS = 64e-5


def phase_ssprep(C, colsT, P, xbc_tok, BCT, dtda_tok, ident):
    S, cfg = C.S, C.cfg
    T = cfg.T
    cw = S.sb("sp_cw", [128, 12, 5])
    for j in range(5):
        S.dma(cw, cw[:, :, j:j + 1], P["ssm_conv_w"], P["ssm_conv_w"][0, j, :].rearrange("(c p o) -> p c o", p=128, o=1), allow_slow_non_contiguous=True)
    cb = vec_cols(C, "sp_cb", P["ssm_conv_b"], P["ssm_conv_b"][0, :], 12)
    dtb = S.sb("sp_dtb", [64, 1])
    aneg = S.sb("sp_aneg", [64, 1])
    dbv = P["ssm_dt_bias"][0].rearrange("d (h o) -> (d h) o", o=1)
    alv = P["ssm_a_log"][0].rearrange("d (h o) -> (d h) o", o=1)
    S.dma(dtb, dtb[0:32, :], P["ssm_dt_bias"], dbv, allow_slow_non_contiguous=True)
    S.dma(dtb, dtb[32:64, :], P["ssm_dt_bias"], dbv, allow_slow_non_contiguous=True)
    S.dma(aneg, aneg[32:64, :], P["ssm_a_log"], alv, allow_slow_non_contiguous=True)
    S.op("act", lambda e: e.activation(out=aneg[32:64, :], in_=aneg[32:64, :], func=AF.Exp), reads=[aneg], writes=[aneg])
    S.op("dve", lambda e: e.tensor_scalar(out=aneg[32:64, :], in0=aneg[32:64, :], scalar1=-1.0, scalar2=None, op0=ALU.mult), reads=[aneg], writes=[aneg])
    NB_ = 512
    raw = [S.sb("sp_raw%d" % i, [128, NB_ + 4]) for i in range(3)]
    acc = [S.sb("sp_acc%d" % i, [128, NB_]) for i in range(2)]
    XC = [S.sb("sp_xc%d" % c, [128, NB_]) for c in range(12)]
    bcb = [S.sb("sp_bcb%d" % i, [128, NB_], BF16) for i in range(2)]
    DD = S.sb("sp_dd", [64, NB_])
    pss = [S.ps("sp_ps%d" % i, [128, 512]) for i in range(3)]
    pdd = S.ps("sp_pdd", [128, 64])
    tk = [S.sb("sp_tk%d" % i, [128, 1536]) for i in range(2)]
    dk = [S.sb("sp_dk%d" % i, [128, 64]) for i in range(2)]
    pc = [0]
    for (b, t0, n, hl, hr) in seg_blocks(cfg, NB_):
        g0 = b * T + t0
        for c in range(12):
            rw = raw[c % 3]
            nl = 2 if hl else 0
            nr = 2 if hr else 0
            if not hl:
                S.op("pool", lambda e, rw=rw: e.memset(rw[:, 0:2], 0.0), writes=[rw])
            if not hr:
                S.op("pool", lambda e, rw=rw, n=n: e.memset(rw[:, n + 2:n + 4], 0.0), writes=[rw])
            S.dma(rw, rw[:, 2 - nl:2 + n + nr], colsT, colsT[2944 + c * 128:2944 + (c + 1) * 128, g0 - nl:g0 + n + nr])
            ac = acc[c % 2]
            S.op("dve", lambda e, rw=rw, ac=ac, c=c, n=n: e.tensor_scalar(out=ac[:, 0:n], in0=rw[:, 0:n], scalar1=cw[:, c, 0:1], scalar2=None, op0=ALU.mult), reads=[rw, cw], writes=[ac])
            for j in range(1, 5):
                eng = "dve"
                S.op(eng, lambda e, rw=rw, ac=ac, c=c, n=n, j=j: e.scalar_tensor_tensor(out=ac[:, 0:n], in0=rw[:, j:j + n], scalar=cw[:, c, j:j + 1], in1=ac[:, 0:n], op0=ALU.mult, op1=ALU.add),
                     reads=[rw, cw, ac], writes=[ac])
            xc = XC[c]
            S.op("act", lambda e, ac=ac, xc=xc, c=c, n=n: e.activation(out=xc[:, 0:n], in_=ac[:, 0:n], func=AF.Silu, bias=cb[:, c:c + 1], scale=1.0), reads=[ac, cb], writes=[xc])
            if c >= 8:
                bb = bcb[c % 2]
                S.op("pool", lambda e, bb=bb, xc=xc, n=n: e.tensor_copy(out=bb[:, 0:n], in_=xc[:, 0:n]), reads=[xc], writes=[bb])
                S.dma(BCT, BCT[(c - 8) * 128:(c - 7) * 128, g0:g0 + n], bb, bb[:, 0:n])
        S.dma(DD, DD[0:32, 0:n], colsT, colsT[4480:4512, g0:g0 + n])
        S.dma(DD, DD[32:64, 0:n], colsT, colsT[4480:4512, g0:g0 + n])
        S.op("act", lambda e, n=n: e.activation(out=DD[:, 0:n], in_=DD[:, 0:n], func=AF.Exp, bias=dtb[:, 0:1], scale=1.0), reads=[DD, dtb], writes=[DD])
        S.op("act", lambda e, n=n: e.activation(out=DD[:, 0:n], in_=DD[:, 0:n], func=AF.Ln, bias=1.0, scale=1.0), reads=[DD], writes=[DD])
        S.op("dve", lambda e, n=n: e.tensor_scalar(out=DD[32:64, 0:n], in0=DD[32:64, 0:n], scalar1=aneg[32:64, 0:1], scalar2=None, op0=ALU.mult), reads=[DD, aneg], writes=[DD])
        for j in range(n // 128):
            tkb = tk[j % 2]
            for q in range(3):
                ps = pss[pc[0] % 3]
                pc[0] += 1
                for cc in range(4):
                    c = q * 4 + cc
                    S.op("pe", lambda e, ps=ps, cc=cc, c=c, j=j: e.transpose(ps[:, cc * 128:(cc + 1) * 128], XC[c][:, j * 128:(j + 1) * 128], ident[:]), reads=[XC[c], ident], writes=[ps], inc=(cc == 3))
                evac(S, q, tkb[:, q * 512:(q + 1) * 512], ps[:], [ps], [tkb])
            S.dma(xbc_tok, xbc_tok[g0 + j * 128:g0 + (j + 1) * 128, :], tkb, tkb[:])
            S.op("pe", lambda e, j=j: e.transpose(pdd[:, :], DD[:, j * 128:(j + 1) * 128], ident[0:64, 0:64]), reads=[DD, ident], writes=[pdd])
            dkb = dk[j % 2]
            S.op("act", lambda e, dkb=dkb: e.copy(out=dkb[:], in_=pdd[:]), reads=[pdd], writes=[dkb])
            S.dma(dtda_tok, dtda_tok[g0 + j * 128:g0 + (j + 1) * 128, :], dkb, dkb[:])


def phase_ssscan(C, xbc_tok, BCT, dtda_tok, ytmp, K):
    S, cfg = C.S, C.cfg
    NB, T, TC = cfg.NB, cfg.T, cfg.TC
    Tt, TCt = T // 128, TC // 128
    XT = [S.sb("ss_xt%d" % i, [128, 1536]) for i in range(2)]
    DT = [S.sb("ss_dt%d" % i, [128, 64]) for i in range(2)]
    BC = [S.sb("ss_bc%d" % i, [128, 4, 128], BF16) for i in range(2)]
    miscPS = S.ps("ss_miscPS", [128, 512])
    cPS = View(miscPS, miscPS[:, 0:32])
    csTPS = View(miscPS, miscPS[0:8, 32:288].rearrange("p (g l) -> p g l", g=2))
    cbPS = View(miscPS, miscPS[:, 288:416])
    segPS = [S.ps("ss_segPS%d" % i, [128, 4, 128]) for i in range(2)]
    ydPS = S.ps("ss_ydPS", [128, 1024])
    yoPS = S.ps("ss_yoPS", [128, 512])
    dsPS = S.ps("ss_dsPS", [128, 512])
    c_sb = S.sb("ss_c", [128, 16])
    dfs = S.sb("ss_dfs", [128, 16])
    cdb = S.sb("ss_cdb", [128, 16])
    dte = S.sb("ss_dte", [128, 16])
    dtdte = S.sb("ss_dtdte", [128, 16])
    negcsT = S.sb("ss_negcsT", [8, 2, 128])
    csdiag = S.sb("ss_csdiag", [8, 2, 1024])
    xdt = S.sb("ss_xdt", [128, 16, 64], BF16)
    xdd = S.sb("ss_xdd", [128, 16, 64], BF16)
    btok = S.sb("ss_btok", [128, 2, 128], BF16)
    cbt = [S.sb("ss_cbt%d" % i, [128, 128], BF16) for i in range(2)]
    Eb = [S.sb("ss_E%d" % i, [128, 4, 128], BF16) for i in range(2)]
    Gb = [S.sb("ss_G%d" % i, [128, 4, 128], BF16) for i in range(2)]
    yacc = [S.sb("ss_yacc%d" % i, [128, 1024]) for i in range(2)]
    S32 = [S.sb("ss_S32_%d" % g, [128, 512]) for g in range(2)]
    Sbf = [S.sb("ss_Sbf_%d" % g, [128, 512], BF16) for g in range(2)]
    bcv = BCT.t.rearrange("(j p) n -> p j n", p=128)
    it = 0
    for d in range(2):
        tri = K["tri%d" % d]
        mneg = K["mneg%d" % d]
        for b in range(NB):
            for g in range(2):
                S.op("pool", lambda e, g=g: e.memset(S32[g][:], 0.0), writes=[S32[g]])
                S.op("pool", lambda e, g=g: e.memset(Sbf[g][:], 0.0), writes=[Sbf[g]])
            order = list(range(Tt)) if d == 0 else (list(range(TCt - 1, -1, -1)) + list(range(Tt - 1, TCt - 1, -1)))
            for tt in order:
                g0 = b * T + tt * 128
                xt, dtt, bc = XT[it % 2], DT[it % 2], BC[it % 2]
                ya = yacc[it % 2]
                it += 1
                S.dma(xt, xt[:], xbc_tok, xbc_tok[g0:g0 + 128, :])
                S.dma(dtt, dtt[:], dtda_tok, dtda_tok[g0:g0 + 128, :])
                S.dma(bc, bc[:], BCT, bcv[:, :, g0:g0 + 128])
                da = dtt[:, 32 + d * 16:32 + d * 16 + 16]
                dtd = dtt[:, d * 16:d * 16 + 16]
                S.op("pe", lambda e, da=da, tri=tri: e.matmul(cPS[:, 0:16], tri[:], da, start=True, stop=True), reads=[tri, dtt], writes=[cPS])
                S.op("pe", lambda e, da=da: e.matmul(cPS[:, 16:32], K["ones"][:], da, start=True, stop=True), reads=[K["ones"], dtt], writes=[cPS])
                for g in range(2):
                    S.op("pe", lambda e, g=g, dtt=dtt, d=d, tri=tri: e.matmul(csTPS[:, g, :], dtt[:, 32 + d * 16 + g * 8:32 + d * 16 + g * 8 + 8], tri[:], start=True, stop=True),
                         reads=[tri, dtt], writes=[csTPS])
                S.op("act", lambda e: e.copy(out=c_sb[:], in_=cPS[:, 0:16]), reads=[cPS], writes=[c_sb])
                S.op("act", lambda e: e.activation(out=dfs[:], in_=cPS[:, 0:16], func=AF.Exp), reads=[cPS], writes=[dfs])
                S.op("act", lambda e: e.activation(out=cdb[:], in_=cPS[:, 16:32], func=AF.Exp), reads=[cPS], writes=[cdb])
                S.op("dve", lambda e: e.tensor_tensor(out=dte[:], in0=cPS[:, 16:32], in1=c_sb[:], op=ALU.subtract), reads=[cPS, c_sb], writes=[dte])
                S.op("act", lambda e: e.activation(out=dte[:], in_=dte[:], func=AF.Exp), reads=[dte], writes=[dte])
                S.op("dve", lambda e, dtd=dtd: e.tensor_tensor(out=dtdte[:], in0=dte[:], in1=dtd, op=ALU.mult), reads=[dte, dtt], writes=[dtdte])
                S.op("act", lambda e: e.mul(out=negcsT[:], in_=csTPS[:], mul=-1.0), reads=[csTPS], writes=[negcsT])
                for g in range(2):
                    S.op("dve", lambda e, g=g: e.tensor_tensor(out=csdiag[:, g, :].rearrange("p (h l) -> p h l", h=8), in0=K["blk"][:].rearrange("p (h l) -> p h l", h=8),
                                                              in1=csTPS[:, g, :].unsqueeze(1).to_broadcast([8, 8, 128]), op=ALU.mult), reads=[K["blk"], csTPS], writes=[csdiag])
                xs3 = xt[:, 0:1024].rearrange("p (h v) -> p h v", h=16)
                S.op("dve", lambda e, xs3=xs3, dtd=dtd: e.tensor_tensor(out=xdt[:], in0=xs3, in1=dtd.unsqueeze(2).to_broadcast([128, 16, 64]), op=ALU.mult), reads=[xt, dtt], writes=[xdt])
                S.op("pool", lambda e, xs3=xs3: e.tensor_tensor(out=xdd[:], in0=xs3, in1=dtdte[:].unsqueeze(2).to_broadcast([128, 16, 64]), op=ALU.mult), reads=[xt, dtdte], writes=[xdd])
                S.op("pool", lambda e, xt=xt: e.tensor_copy(out=btok[:], in_=xt[:, 1024:1280].rearrange("p (g n) -> p g n", g=2)), reads=[xt], writes=[btok])
                si = 0
                for g in range(2):
                    S.op("pe", lambda e, g=g, bc=bc: e.matmul(cbPS[:], bc[:, g, :], bc[:, 2 + g, :], start=True, stop=True), reads=[bc], writes=[cbPS])
                    cb_ = cbt[g]
                    S.op("act", lambda e, cb_=cb_: e.copy(out=cb_[:], in_=cbPS[:]), reads=[cbPS], writes=[cb_])
                    for half in range(2):
                        sp = segPS[si % 2]
                        E, G = Eb[si % 2], Gb[si % 2]
                        si += 1
                        S.op("pe", lambda e, sp=sp, g=g, half=half: e.matmul(sp[:].rearrange("p h l -> p (h l)"), K["ones8"][:], csdiag[:, g, half * 512:(half + 1) * 512], start=True, stop=False),
                             reads=[K["ones8"], csdiag], writes=[sp], inc=False)
                        S.op("pe", lambda e, sp=sp, g=g, half=half: e.matmul(sp[:].rearrange("p h l -> p (h l)"), negcsT[:, g, :], K["blk"][:, half * 512:(half + 1) * 512], start=False, stop=False),
                             reads=[negcsT, K["blk"]], writes=[sp], inc=False)
                        S.op("pe", lambda e, sp=sp, mneg=mneg: e.matmul(sp[:].rearrange("p h l -> p (h l)"), K["identb"][:], mneg[:], start=False, stop=True),
                             reads=[K["identb"], mneg], writes=[sp])
                        S.op("act", lambda e, sp=sp, E=E: e.activation(out=E[:], in_=sp[:], func=AF.Exp), reads=[sp], writes=[E])
                        S.op("dve", lambda e, E=E, G=G, cb_=cb_: e.tensor_tensor(out=G[:], in0=E[:], in1=cb_[:].unsqueeze(1).to_broadcast([128, 4, 128]), op=ALU.mult), reads=[E, cb_], writes=[G])
                        for hh in range(4):
                            h = g * 8 + half * 4 + hh
                            S.op("pe", lambda e, G=G, hh=hh, h=h: e.matmul(ydPS[:, h * 64:(h + 1) * 64], G[:, hh, :], xdt[:, h, :], start=True, stop=True), reads=[G, xdt], writes=[ydPS], inc=(hh == 3))
                    S.op("pe", lambda e, g=g, bc=bc: e.matmul(yoPS[:], bc[:, 2 + g, :], Sbf[g][:], start=True, stop=True), reads=[bc, Sbf[g]], writes=[yoPS])
                    S.op("dve", lambda e, g=g, ya=ya: e.tensor_tensor(out=ya[:, g * 512:(g + 1) * 512].rearrange("p (h v) -> p h v", h=8), in0=yoPS[:].rearrange("p (h v) -> p h v", h=8),
                                                                   in1=dfs[:, g * 8:(g + 1) * 8].unsqueeze(2).to_broadcast([128, 8, 64]), op=ALU.mult), reads=[yoPS, dfs], writes=[ya])
                    S.op("pe", lambda e, g=g: e.matmul(dsPS[:], btok[:, g, :], xdd[:, g * 8:(g + 1) * 8, :].rearrange("p h v -> p (h v)"), start=True, stop=True), reads=[btok, xdd], writes=[dsPS])
                    S.op("pool", lambda e, g=g: e.tensor_tensor(out=S32[g][:].rearrange("p (h v) -> p h v", h=8), in0=S32[g][:].rearrange("p (h v) -> p h v", h=8),
                                                               in1=cdb[:, g * 8:(g + 1) * 8].unsqueeze(2).to_broadcast([128, 8, 64]), op=ALU.mult), reads=[S32[g], cdb], writes=[S32[g]])
                    S.op("dve", lambda e, g=g: e.tensor_tensor(out=S32[g][:], in0=S32[g][:], in1=dsPS[:], op=ALU.add), reads=[S32[g], dsPS], writes=[S32[g]])
                    S.op("act", lambda e, g=g: e.copy(out=Sbf[g][:], in_=S32[g][:]), reads=[S32[g]], writes=[Sbf[g]])
                S.op("dve", lambda e, ya=ya: e.tensor_tensor(out=ya[:], in0=ya[:], in1=ydPS[:], op=ALU.add), reads=[ya, ydPS], writes=[ya])
                S.dma(ytmp[d], ytmp[d][g0:g0 + 128, :], ya, ya[:])


def phase_sspost(C, ytmp, xbc_tok, colsT, P, mixT, ident):
    S, cfg = C.S, C.cfg
    dsk = S.sb("so_dsk", [128, 16])
    S.dma(dsk, dsk[:], P["ssm_d"], P["ssm_d"][0, :].partition_broadcast(128))
    nw = S.sb("so_nw", [128, 1024])
    S.dma(nw, nw[:], P["ssm_norm_w"], P["ssm_norm_w"][0, :].partition_broadcast(128))
    Y1 = [S.sb("so_y1_%d" % i, [128, 1024]) for i in range(2)]
    Y2 = [S.sb("so_y2_%d" % i, [128, 1024]) for i in range(2)]
    XS = [S.sb("so_xs_%d" % i, [128, 1024]) for i in range(2)]
    ZT = [S.sb("so_zt_%d" % i, [128, 8, 128]) for i in range(2)]
    zs = S.sb("so_zs", [128, 1024])
    junk = S.sb("so_junk", [128, 512], BF16)
    ss = S.sb("so_ss", [128, 2])
    pz = S.ps("so_pz", [128, 1024])
    po = S.ps("so_po", [128, 8, 128])
    ob = [S.sb("so_ob%d" % i, [128, 8, 128], BF16) for i in range(2)]
    zv = colsT.t[1920:2944, :].rearrange("(c p) n -> p c n", p=128)
    mv = mixT.t[512:1536, :].rearrange("(c p) n -> p c n", p=128)
    for ti in range(cfg.NTOK // 128):
        g0 = ti * 128
        y1, y2, xs, zt = Y1[ti % 2], Y2[ti % 2], XS[ti % 2], ZT[ti % 2]
        S.dma(y1, y1[:], ytmp[0], ytmp[0][g0:g0 + 128, :])
        S.dma(y2, y2[:], ytmp[1], ytmp[1][g0:g0 + 128, :])
        S.dma(xs, xs[:], xbc_tok, xbc_tok[g0:g0 + 128, 0:1024])
        S.dma(zt, zt[:], colsT, zv[:, :, g0:g0 + 128])
        for c in range(8):
            S.op("pe", lambda e, c=c, zt=zt: e.transpose(pz[:, c * 128:(c + 1) * 128], zt[:, c, :], ident[:]), reads=[zt, ident], writes=[pz], inc=(c == 7))
        S.op("act", lambda e: e.activation(out=zs[:], in_=pz[:], func=AF.Silu), reads=[pz], writes=[zs])
        S.op("dve", lambda e, y1=y1, y2=y2: e.tensor_tensor(out=y1[:], in0=y1[:], in1=y2[:], op=ALU.add), reads=[y1, y2], writes=[y1])
        S.op("pool", lambda e, xs=xs: e.tensor_tensor(out=xs[:].rearrange("p (h v) -> p h v", h=16), in0=xs[:].rearrange("p (h v) -> p h v", h=16),
                                                    in1=dsk[:].unsqueeze(2).to_broadcast([128, 16, 64]), op=ALU.mult), reads=[xs, dsk], writes=[xs])
        S.op("dve", lambda e, y1=y1, xs=xs: e.tensor_tensor(out=y1[:], in0=y1[:], in1=xs[:], op=ALU.add), reads=[y1, xs], writes=[y1])
        S.op("dve", lambda e, y1=y1: e.tensor_tensor(out=y1[:], in0=y1[:], in1=zs[:], op=ALU.mult), reads=[y1, zs], writes=[y1])
        for gI in range(2):
            S.op("act", lambda e, y1=y1, gI=gI: e.activation(out=junk[:], in_=y1[:, gI * 512:(gI + 1) * 512], func=AF.Square, accum_out=ss[:, gI:gI + 1]), reads=[y1], writes=[junk, ss])
        S.op("act", lambda e: e.activation(out=ss[:], in_=ss[:], func=AF.Sqrt, bias=NORM_EPS, scale=1.0 / 512), reads=[ss], writes=[ss])
        S.op("dve", lambda e: e.reciprocal(out=ss[:], in_=ss[:]), reads=[ss], writes=[ss])
        S.op("dve", lambda e, y1=y1: e.tensor_tensor(out=y1[:].rearrange("p (g v) -> p g v", g=2), in0=y1[:].rearrange("p (g v) -> p g v", g=2),
                                                    in1=ss[:].unsqueeze(2).to_broadcast([128, 2, 512]), op=ALU.mult), reads=[y1, ss], writes=[y1])
        S.op("pool", lambda e, y1=y1: e.tensor_tensor(out=y1[:], in0=y1[:], in1=nw[:], op=ALU.mult), reads=[y1, nw], writes=[y1])
        for c in range(8):
            S.op("pe", lambda e, c=c, y1=y1: e.transpose(po[:, c, :], y1[:, c * 128:(c + 1) * 128], ident[:]), reads=[y1, ident], writes=[po], inc=(c == 7))
        o = ob[ti % 2]
        S.op("act", lambda e, o=o: e.copy(out=o[:, 0:4, :], in_=po[:, 0:4, :]), reads=[po], writes=[o])
        S.op("dve", lambda e, o=o: e.tensor_copy(out=o[:, 4:8, :], in_=po[:, 4:8, :]), reads=[po], writes=[o])
        S.dma(mixT, mv[:, :, g0:g0 + 128], o, o[:])


def gate_tiles(C, name, modv, layer, idx):
    S, cfg = C.S, C.cfg
    D = cfg.D
    out = []
    for r in range(cfg.NB + 1):
        t = S.sb("%s_%d" % (name, r), [128, D])
        S.dma(t, t[:], modv, modv[layer, r, idx * D:(idx + 1) * D].partition_broadcast(128))
        out.append(t)
    return out


def tile_row(cfg, tile_idx):
    tpb = cfg.T // 128
    b, tt = divmod(tile_idx, tpb)
    return cfg.NB if tt < cfg.TC // 128 else b


def lat_tiles(cfg):
    tpb = cfg.T // 128
    return [b * tpb + tt for b in range(cfg.NB) for tt in range(cfg.TC // 128, tpb)]


def phase_outproj(C, name, src, src_tokmajor, kdim, W_buf, W_ap, modv, layer, xin, xout, tiles, ident_bf):
    S, cfg = C.S, C.cfg
    kch = kdim // 128
    wb = load_w_bf16(C, name + "_w", W_buf, W_ap.rearrange("(k p) c -> p k c", p=128), 1024, kch)
    gt = gate_tiles(C, name + "_gt", modv, layer, 2)
    mt_ = [S.sb("%s_m%d" % (name, i), [128, kch, 128], BF16) for i in range(2)]
    if src_tokmajor:
        tk_ = [S.sb("%s_tk%d" % (name, i), [128, kdim], BF16) for i in range(2)]
        ptb = S.ps(name + "_ptb", [128, kch, 128], BF16)
    xt_ = [S.sb("%s_x%d" % (name, i), [128, 1024]) for i in range(2)]
    ot_ = [S.sb("%s_o%d" % (name, i), [128, 1024]) for i in range(2)]
    ps_ = [S.ps("%s_ps%d" % (name, i), [128, 1024]) for i in range(2)]
    if not src_tokmajor:
        sv = src.t.rearrange("(k p) n -> p k n", p=128)
    for i, ti in enumerate(tiles):
        g0 = ti * 128
        m, xt, ot, ps = mt_[i % 2], xt_[i % 2], ot_[i % 2], ps_[i % 2]
        if src_tokmajor:
            tk = tk_[i % 2]
            S.dma(tk, tk[:], src, src[g0:g0 + 128, :])
            for k in range(kch):
                S.op("pe", lambda e, k=k, tk=tk: e.transpose(ptb[:, k, :], tk[:, k * 128:(k + 1) * 128], ident_bf[:]), reads=[tk, ident_bf], writes=[ptb], inc=(k == kch - 1))
            evac(S, i, m[:], ptb[:], [ptb], [m])
        else:
            S.dma(m, m[:], src, sv[:, :, g0:g0 + 128])
        S.dma(xt, xt[:], xin, xin[g0:g0 + 128, :])
        for half in range(2):
            for k in range(kch):
                S.op("pe", lambda e, k=k, half=half, m=m, ps=ps: e.matmul(ps[:, half * 512:(half + 1) * 512], m[:, k, :], wb[:, k, half * 512:(half + 1) * 512], start=(k == 0), stop=(k == kch - 1)),
                     reads=[m, wb], writes=[ps], inc=(k == kch - 1))
        g = gt[tile_row(cfg, ti)]
        S.op("dve", lambda e, ps=ps, ot=ot, g=g: e.tensor_tensor(out=ot[:], in0=ps[:], in1=g[:], op=ALU.mult), reads=[ps, g], writes=[ot])
        S.op("pool", lambda e, ot=ot, xt=xt: e.tensor_tensor(out=ot[:], in0=ot[:], in1=xt[:], op=ALU.add), reads=[ot, xt], writes=[ot])
        S.dma(xout, xout[g0:g0 + 128, :], ot, ot[:])


def phase_final(C, xin, fn_buf, out):
    S, cfg = C.S, C.cfg
    D = cfg.D
    g = S.sb("fn_g", [128, D])
    S.dma(g, g[:], fn_buf, fn_buf.t.partition_broadcast(128))
    xt_ = [S.sb("fn_x%d" % i, [128, D]) for i in range(2)]
    ot_ = [S.sb("fn_o%d" % i, [128, D]) for i in range(2)]
    junk = S.sb("fn_junk", [128, D], BF16)
    ss_ = [S.sb("fn_ss%d" % i, [128, 1]) for i in range(2)]
    tpl = cfg.TL // 128
    for i, ti in enumerate(lat_tiles(cfg)):
        xt, ot, ss = xt_[i % 2], ot_[i % 2], ss_[i % 2]
        S.dma(xt, xt[:], xin, xin[ti * 128:(ti + 1) * 128, :])
        S.op("act", lambda e, xt=xt, ss=ss: e.activation(out=junk[:], in_=xt[:], func=AF.Square, accum_out=ss[:]), reads=[xt], writes=[junk, ss])
        S.op("act", lambda e, ss=ss: e.activation(out=ss[:], in_=ss[:], func=AF.Sqrt, bias=NORM_EPS, scale=1.0 / D), reads=[ss], writes=[ss])
        S.op("dve", lambda e, ss=ss: e.reciprocal(out=ss[:], in_=ss[:]), reads=[ss], writes=[ss])
        S.op("dve", lambda e, xt=xt, ot=ot, ss=ss: e.scalar_tensor_tensor(out=ot[:], in0=xt[:], scalar=ss[:, 0:1], in1=g[:], op0=ALU.mult, op1=ALU.mult), reads=[xt, ss, g], writes=[ot])
        S.dma(out, out[i * 128:(i + 1) * 128, :], ot, ot[:])


def phase_moe_cast(C, layer, EW, wbf):
    S = C.S
    st = [S.sb("mc_st%d" % i, [128, 6144]) for i in range(2)]
    ob = [S.sb("mc_ob%d" % i, [128, 6144], BF16) for i in range(2)]
    for e in range(65):
        s_, o_ = st[e % 2], ob[e % 2]
        if e < 64:
            w1, w3, w2 = EW["exp_w1"][layer, e], EW["exp_w3"][layer, e], EW["exp_w2"][layer, e]
            b1, b3, b2 = EW["exp_w1"], EW["exp_w3"], EW["exp_w2"]
        else:
            w1, w3, w2 = EW["sh_w1"][layer], EW["sh_w3"][layer], EW["sh_w2"][layer]
            b1, b3, b2 = EW["sh_w1"], EW["sh_w3"], EW["sh_w2"]
        S.dma(s_, s_[:, 0:2048].rearrange("p (k f) -> p k f", k=8), b1, w1.rearrange("(k p) f -> p k f", p=128))
        S.dma(s_, s_[:, 2048:4096].rearrange("p (k f) -> p k f", k=8), b3, w3.rearrange("(k p) f -> p k f", p=128))
        S.dma(s_, s_[:, 4096:6144].rearrange("p (j f) -> p j f", j=2), b2, w2.rearrange("(j p) f -> p j f", p=128))
        S.op("act", lambda e_, s_=s_, o_=o_: e_.copy(out=o_[:, 0:2048], in_=s_[:, 0:2048]), reads=[s_], writes=[o_])
        S.op("dve", lambda e_, s_=s_, o_=o_: e_.tensor_copy(out=o_[:, 2048:4096], in_=s_[:, 2048:4096]), reads=[s_], writes=[o_])
        S.op("pool", lambda e_, s_=s_, o_=o_: e_.tensor_copy(out=o_[:, 4096:6144], in_=s_[:, 4096:6144]), reads=[s_], writes=[o_])
        S.dma(wbf, wbf[e], o_, o_[:])


def moe_cast_bg(C, layer, EW, wbf, bufs):
    S = C.S
    st, ob = bufs
    n = 0
    for e in range(65):
        if e < 64:
            srcs = [(EW["exp_w1"], EW["exp_w1"][layer, e], 8), (EW["exp_w3"], EW["exp_w3"][layer, e], 8), (EW["exp_w2"], EW["exp_w2"][layer, e], 2)]
        else:
            srcs = [(EW["sh_w1"], EW["sh_w1"][layer], 8), (EW["sh_w3"], EW["sh_w3"][layer], 8), (EW["sh_w2"], EW["sh_w2"][layer], 2)]
        for pi, (buf, ap, kk_) in enumerate(srcs):
            s_, o_ = st[n % 2], ob[n % 2]
            n += 1
            S.dma(s_, s_[:].rearrange("p (k f) -> p k f", k=kk_), buf, ap.rearrange("(k p) f -> p k f", p=128))
            S.op("act", lambda e_, s_=s_, o_=o_: e_.copy(out=o_[:], in_=s_[:]), reads=[s_], writes=[o_])
            S.dma(wbf, wbf[e][:, pi * 2048:(pi + 1) * 2048], o_, o_[:])
            yield


def phase_moe_route(C, layer, xin, gvec_buf, gvec_ap, modv, rw_buf, rb_buf, hT, gates, tiles, ident):
    S, cfg = C.S, C.cfg
    mt = ModTiles(C, "mr_mt", modv, layer, 3, 4, gvec_ap, gvec_buf)
    nt = NormT(C, "mr_nt", ident)
    rw = S.sb("mr_rw", [128, 8, 64])
    S.dma(rw, rw[:], rw_buf, rw_buf[layer].rearrange("(k p) e -> p k e", p=128))
    rb = S.sb("mr_rb", [128, 64])
    S.dma(rb, rb[:], rb_buf, rb_buf[layer, :].partition_broadcast(128))
    hb_ = [S.sb("mr_hb%d" % i, [128, 8, 128], BF16) for i in range(2)]
    h32_ = [S.sb("mr_h32%d" % i, [128, 8, 128]) for i in range(2)]
    lg = S.ps("mr_lg", [128, 64])
    sc = S.sb("mr_sc", [128, 64])
    sel = S.sb("mr_sel", [128, 64])
    eq = S.sb("mr_eq", [128, 64])
    m1 = S.sb("mr_m1", [128, 8])
    m2 = S.sb("mr_m2", [128, 8])
    t8 = S.sb("mr_t8", [128, 8])
    pen = S.sb("mr_pen", [128, 8])
    ws = S.sb("mr_ws", [128, 1])
    gt_ = [S.sb("mr_gt%d" % i, [128, 65]) for i in range(2)]
    for g in gt_:
        S.op("pool", lambda e, g=g: e.memset(g[:, 64:65], 1.0), writes=[g])
    hv = hT.t.rearrange("(k p) n -> p k n", p=128)

    def v3(b):
        return b[:].rearrange("p (g i) -> p g i", g=8)
    for i, ti in enumerate(tiles):
        g0 = ti * 128
        hb, h32, gt = hb_[i % 2], h32_[i % 2], gt_[i % 2]
        nt.tile(xin, g0, mt, tile_row(cfg, ti), [(hb, lambda k0, k1, hb=hb: hb[:, k0:k1, :]), (h32, lambda k0, k1, h32=h32: h32[:, k0:k1, :])])
        S.dma(hT, hv[:, :, g0:g0 + 128], hb, hb[:])
        for k in range(8):
            S.op("pe", lambda e, k=k, h32=h32: e.matmul(lg[:], h32[:, k, :], rw[:, k, :], start=(k == 0), stop=(k == 7)), reads=[h32, rw], writes=[lg], inc=(k == 7))
        S.op("act", lambda e: e.activation(out=sc[:], in_=lg[:], func=AF.Sigmoid), reads=[lg], writes=[sc])
        S.op("dve", lambda e: e.tensor_tensor(out=sel[:], in0=sc[:], in1=rb[:], op=ALU.add), reads=[sc, rb], writes=[sel])
        S.op("dve", lambda e: e.reduce_max(out=m1[:], in_=v3(sel), axis=AX.X), reads=[sel], writes=[m1])
        S.op("dve", lambda e: e.tensor_tensor(out=v3(eq), in0=v3(sel), in1=m1[:].unsqueeze(2).to_broadcast([128, 8, 8]), op=ALU.is_equal), reads=[sel, m1], writes=[eq])
        S.op("dve", lambda e: e.scalar_tensor_tensor(out=eq[:], in0=eq[:], scalar=-1e30, in1=sel[:], op0=ALU.mult, op1=ALU.add), reads=[eq, sel], writes=[eq])
        S.op("dve", lambda e: e.reduce_max(out=m2[:], in_=v3(eq), axis=AX.X), reads=[eq], writes=[m2])
        S.op("dve", lambda e: e.tensor_tensor(out=m1[:], in0=m1[:], in1=m2[:], op=ALU.add), reads=[m1, m2], writes=[m1])
        S.op("dve", lambda e: e.max(out=t8[:], in_=m1[:]), reads=[m1], writes=[t8])
        S.op("dve", lambda e: e.tensor_scalar(out=pen[:], in0=m1[:], scalar1=t8[:, 3:4], scalar2=None, op0=ALU.is_ge), reads=[m1, t8], writes=[pen])
        S.op("dve", lambda e: e.tensor_scalar(out=pen[:], in0=pen[:], scalar1=1e30, scalar2=-1e30, op0=ALU.mult, op1=ALU.add), reads=[pen], writes=[pen])
        S.op("dve", lambda e: e.tensor_tensor(out=v3(sel), in0=v3(sel), in1=pen[:].unsqueeze(2).to_broadcast([128, 8, 8]), op=ALU.add), reads=[sel, pen], writes=[sel])
        S.op("dve", lambda e: e.max(out=t8[:], in_=sel[:]), reads=[sel], writes=[t8])
        S.op("dve", lambda e: e.tensor_scalar(out=eq[:], in0=sel[:], scalar1=t8[:, 5:6], scalar2=None, op0=ALU.is_ge), reads=[sel, t8], writes=[eq])
        S.op("dve", lambda e: e.tensor_tensor(out=eq[:], in0=eq[:], in1=sc[:], op=ALU.mult), reads=[eq, sc], writes=[eq])
        S.op("dve", lambda e: e.reduce_sum(out=ws[:], in_=eq[:], axis=AX.X), reads=[eq], writes=[ws])
        S.op("dve", lambda e: e.reciprocal(out=ws[:], in_=ws[:]), reads=[ws], writes=[ws])
        S.op("dve", lambda e, gt=gt: e.tensor_scalar(out=gt[:, 0:64], in0=eq[:], scalar1=ws[:, 0:1], scalar2=ROUTED_SCALE, op0=ALU.mult, op1=ALU.mult), reads=[eq, ws], writes=[gt])
        S.dma(gates, gates[g0:g0 + 128, :], gt, gt[:])


ROUTED_SCALE = 2.5


def phase_moe_experts(C, layer, hT, gates, wbf, modv, xin, xout, tiles):
    S, cfg = C.S, C.cfg
    TSU = 8
    gtl = gate_tiles(C, "me_gt", modv, layer, 5)
    hs = S.sb("me_hs", [128, 8, TSU * 128], BF16)
    gs = S.sb("me_gs", [128, TSU, 65])
    acc = S.sb("me_acc", [128, TSU, 1024])
    wb_ = [S.sb("me_w%d" % i, [128, 6144], BF16) for i in range(3)]
    h1_ = [S.ps("me_h1_%d" % i, [128, 512]) for i in range(2)]
    h3_ = [S.ps("me_h3_%d" % i, [128, 512]) for i in range(2)]
    op_ = [S.ps("me_o%d" % i, [128, 512]) for i in range(3)]
    s1_ = [S.sb("me_s1_%d" % i, [128, 512]) for i in range(2)]
    hid_ = [S.sb("me_hid%d" % i, [128, 2, 512], BF16) for i in range(2)]
    xt_ = [S.sb("me_x%d" % i, [128, 1024]) for i in range(2)]
    hv = hT.t.rearrange("(k p) n -> p k n", p=128)
    nst = (len(tiles) + TSU - 1) // TSU
    cnt = 0
    oc = 0
    for st in range(nst):
        tl = tiles[st * TSU:(st + 1) * TSU]
        nt_ = len(tl)
        for j, ti in enumerate(tl):
            S.dma(hs, hs[:, :, j * 128:(j + 1) * 128], hT, hv[:, :, ti * 128:(ti + 1) * 128])
            S.dma(gs, gs[:, j, :], gates, gates[ti * 128:(ti + 1) * 128, :])
        S.op("pool", lambda e: e.memset(acc[:], 0.0), writes=[acc])
        items = []
        for e_ in range(65):
            wb = wb_[e_ % 3]
            for tb in range((nt_ + 3) // 4):
                items.append((e_, tb, wb))

        def H_groups(it, idx):
            e_, tb, wb = it
            ntok = min(4, nt_ - tb * 4) * 128
            hid = hid_[idx % 2]
            gl = []
            for j in range(2):
                h1, h3, s1 = h1_[j], h3_[j], s1_[j]

                def g1(j=j, h1=h1):
                    if tb == 0 and j == 0:
                        S.dma(wb, wb[:], wbf, wbf[e_])
                    for k in range(8):
                        S.op("pe", lambda e, k=k: e.matmul(h1[:, 0:ntok], wb[:, k * 256 + j * 128:k * 256 + (j + 1) * 128], hs[:, k, tb * 512:tb * 512 + ntok], start=(k == 0), stop=(k == 7)),
                             reads=[wb, hs], writes=[h1], inc=(k == 7))

                def g3(j=j, h1=h1, h3=h3, s1=s1):
                    for k in range(8):
                        S.op("pe", lambda e, k=k: e.matmul(h3[:, 0:ntok], wb[:, 2048 + k * 256 + j * 128:2048 + k * 256 + (j + 1) * 128], hs[:, k, tb * 512:tb * 512 + ntok], start=(k == 0), stop=(k == 7)),
                             reads=[wb, hs], writes=[h3], inc=(k == 7))
                    S.op("act", lambda e: e.activation(out=s1[:, 0:ntok], in_=h1[:, 0:ntok], func=AF.Silu), reads=[h1], writes=[s1])
                    S.op("dve", lambda e: e.tensor_tensor(out=hid[:, j, 0:ntok], in0=h3[:, 0:ntok], in1=s1[:, 0:ntok], op=ALU.mult), reads=[h3, s1], writes=[hid])
                gl += [g1, g3]
            return gl

        def O_groups(it, idx):
            nonlocal oc
            e_, tb, wb = it
            ntok = min(4, nt_ - tb * 4) * 128
            hid = hid_[idx % 2]
            gl = []
            for q in range(ntok // 128):
                tj = tb * 4 + q

                def go(q=q, tj=tj):
                    nonlocal oc
                    for half in range(2):
                        o = op_[oc % 3]
                        oc += 1
                        for j in range(2):
                            S.op("pe", lambda e, o=o, j=j, half=half: e.matmul(o[:], hid[:, j, q * 128:(q + 1) * 128], wb[:, 4096 + j * 1024 + half * 512:4096 + j * 1024 + (half + 1) * 512], start=(j == 0), stop=(j == 1)),
                                 reads=[hid, wb], writes=[o], inc=(j == 1))
                        S.op("dve", lambda e, o=o, half=half: e.scalar_tensor_tensor(out=acc[:, tj, half * 512:(half + 1) * 512], in0=o[:], scalar=gs[:, tj, e_:e_ + 1], in1=acc[:, tj, half * 512:(half + 1) * 512], op0=ALU.mult, op1=ALU.add),
                             reads=[o, gs, acc], writes=[acc])
                gl.append(go)
            return gl
        for g in H_groups(items[0], 0):
            g()
        for idx, it in enumerate(items):
            og = O_groups(it, idx)
            hg = H_groups(items[idx + 1], idx + 1) if idx + 1 < len(items) else []
            n = max(len(og), len(hg))
            for t in range(n):
                if t < len(hg):
                    hg[t]()
                if t < len(og):
                    og[t]()
        for j, ti in enumerate(tl):
            xt = xt_[j % 2]
            S.dma(xt, xt[:], xin, xin[ti * 128:(ti + 1) * 128, :])
            g = gtl[tile_row(cfg, ti)]
            S.op("pool", lambda e, j=j, g=g: e.tensor_tensor(out=acc[:, j, :], in0=acc[:, j, :], in1=g[:], op=ALU.mult), reads=[acc, g], writes=[acc])
            S.op("pool", lambda e, j=j, xt=xt: e.tensor_tensor(out=xt[:], in0=acc[:, j, :], in1=xt[:], op=ALU.add), reads=[acc, xt], writes=[xt])
            S.dma(xout, xout[ti * 128:(ti + 1) * 128, :], xt, xt[:])


def phase_mla_prep(C, layer, xin, gvec_buf, gvec_ap, modv, P, rope_cs, KT, QT, Vtok, ident, identb):
    S, cfg = C.S, C.cfg
    TCt = cfg.TC // 128
    tpb = cfg.T // 128
    mt = ModTiles(C, "mp_mt", modv, layer, 0, 1, gvec_ap, gvec_buf)
    nt = NormT(C, "mp_nt", ident)
    win = load_w_bf16(C, "mp_win", P["mla_w_in"], P["mla_w_in"][0].rearrange("(k p) c -> p k c", p=128), 672, 8)
    qup = load_w_bf16(C, "mp_qup", P["mla_q_up"], P["mla_q_up"][0].rearrange("(k p) c -> p k c", p=128), 1536, 3)
    kvup = load_w_bf16(C, "mp_kvup", P["mla_kv_up"], P["mla_kv_up"][0].rearrange("(k p) c -> p k c", p=128), 2048, 2)
    nb = S.sb("mp_nb", [128, 640])
    S.dma(nb, nb[:, 0:384], P["mla_q_norm"], P["mla_q_norm"][0, :].partition_broadcast(128))
    S.dma(nb, nb[:, 384:640], P["mla_kv_norm"], P["mla_kv_norm"][0, :].partition_broadcast(128))
    hb_ = [S.sb("mp_hb%d" % i, [128, 8, 128], BF16) for i in range(2)]
    A_ = S.ps("mp_A", [128, 1024])
    B_ = S.ps("mp_B", [128, 1024])
    Cp = S.ps("mp_C", [96, 16, 128], BF16)
    c_sb = S.sb("mp_c", [128, 672])
    junk = S.sb("mp_junk", [128, 384], BF16)
    ss = S.sb("mp_ss", [128, 2])
    cn = S.sb("mp_cn", [128, 640])
    cnT = S.sb("mp_cnT", [128, 5, 128], BF16)
    vt_ = [S.sb("mp_vt%d" % i, [128, 16, 64], BF16) for i in range(2)]
    Kf = S.sb("mp_Kf", [128, 16, 96], BF16)
    Qf = S.sb("mp_Qf", [128, 16, 96], BF16)
    q32 = S.sb("mp_q32", [128, 16, 96])
    cs_ = [S.sb("mp_cs%d" % i, [128, 32]) for i in range(2)]
    krr = S.sb("mp_krr", [128, 32])
    t1 = S.sb("mp_t1", [128, 16, 16])
    t2 = S.sb("mp_t2", [128, 16, 16])
    kT_ = [S.sb("mp_kT%d" % i, [96, 16, 128], BF16) for i in range(2)]
    qT_ = [S.sb("mp_qT%d" % i, [96, 16, 128], BF16) for i in range(2)]
    ktv = KT.t.rearrange("h d n -> d h n")
    qtv = QT.t.rearrange("h d n -> d h n")
    for ti in range(cfg.NTOK // 128):
        b, tt = divmod(ti, tpb)
        lat = tt >= TCt
        g0 = ti * 128
        hb, vt, kT, qT, cs = hb_[ti % 2], vt_[ti % 2], kT_[ti % 2], qT_[ti % 2], cs_[ti % 2]
        nt.tile(xin, g0, mt, tile_row(cfg, ti), [(hb, lambda k0, k1, hb=hb: hb[:, k0:k1, :])])
        for (c0, c1) in ((0, 512), (512, 672)):
            for k in range(8):
                S.op("pe", lambda e, k=k, c0=c0, c1=c1, hb=hb: e.matmul(B_[:, c0:c1], hb[:, k, :], win[:, k, c0:c1], start=(k == 0), stop=(k == 7)), reads=[hb, win], writes=[B_], inc=(k == 7))
        S.op("act", lambda e: e.copy(out=c_sb[:, 0:512], in_=B_[:, 0:512]), reads=[B_], writes=[c_sb])
        S.op("dve", lambda e: e.tensor_copy(out=c_sb[:, 512:672], in_=B_[:, 512:672]), reads=[B_], writes=[c_sb])
        S.op("act", lambda e: e.activation(out=junk[:, 0:384], in_=c_sb[:, 0:384], func=AF.Square, accum_out=ss[:, 0:1]), reads=[c_sb], writes=[junk, ss])
        S.op("act", lambda e: e.activation(out=junk[:, 0:256], in_=c_sb[:, 384:640], func=AF.Square, accum_out=ss[:, 1:2]), reads=[c_sb], writes=[junk, ss])
        S.op("act", lambda e: e.activation(out=ss[:, 0:1], in_=ss[:, 0:1], func=AF.Sqrt, bias=NORM_EPS, scale=1.0 / 384), reads=[ss], writes=[ss])
        S.op("act", lambda e: e.activation(out=ss[:, 1:2], in_=ss[:, 1:2], func=AF.Sqrt, bias=NORM_EPS, scale=1.0 / 256), reads=[ss], writes=[ss])
        S.op("dve", lambda e: e.reciprocal(out=ss[:], in_=ss[:]), reads=[ss], writes=[ss])
        S.op("dve", lambda e: e.scalar_tensor_tensor(out=cn[:, 0:384], in0=c_sb[:, 0:384], scalar=ss[:, 0:1], in1=nb[:, 0:384], op0=ALU.mult, op1=ALU.mult), reads=[c_sb, ss, nb], writes=[cn])
        S.op("dve", lambda e: e.scalar_tensor_tensor(out=cn[:, 384:640], in0=c_sb[:, 384:640], scalar=ss[:, 1:2], in1=nb[:, 384:640], op0=ALU.mult, op1=ALU.mult), reads=[c_sb, ss, nb], writes=[cn])
        Bv = B_[:, 0:640].rearrange("p (k n) -> p k n", k=5)
        for k in range(5):
            S.op("pe", lambda e, k=k, Bv=Bv: e.transpose(Bv[:, k, :], cn[:, k * 128:(k + 1) * 128], ident[:]), reads=[cn, ident], writes=[B_], inc=(k == 4))
        S.op("act", lambda e, Bv=Bv: e.copy(out=cnT[:], in_=Bv), reads=[B_], writes=[cnT])
        for ps_ in range(2):
            for blk in range(2):
                for kc in range(2):
                    S.op("pe", lambda e, ps_=ps_, blk=blk, kc=kc: e.matmul(A_[:, blk * 512:(blk + 1) * 512], cnT[:, 3 + kc, :], kvup[:, kc, ps_ * 1024 + blk * 512:ps_ * 1024 + (blk + 1) * 512], start=(kc == 0), stop=(kc == 1)),
                         reads=[cnT, kvup], writes=[A_], inc=(kc == 1))
            Av = A_[:].rearrange("p (h x) -> p h x", h=8)
            S.op("act", lambda e, ps_=ps_, Av=Av, vt=vt: e.copy(out=vt[:, ps_ * 8:(ps_ + 1) * 8, :], in_=Av[:, :, 64:128]), reads=[A_], writes=[vt])
            S.op("dve", lambda e, ps_=ps_, Av=Av: e.tensor_copy(out=Kf[:, ps_ * 8:(ps_ + 1) * 8, 0:64], in_=Av[:, :, 0:64]), reads=[A_], writes=[Kf])
        S.dma(Vtok, Vtok[g0:g0 + 128, :], vt, vt[:].rearrange("p h v -> p (h v)"))
        if lat:
            p0 = (tt - TCt) * 128
            S.dma(cs, cs[:], rope_cs, rope_cs[p0:p0 + 128, :])
            u1, u2 = c_sb[:, 640:656], c_sb[:, 656:672]
            S.op("dve", lambda e, cs=cs, u1=u1: e.tensor_tensor(out=t1[:, 0, :], in0=u1, in1=cs[:, 0:16], op=ALU.mult), reads=[c_sb, cs], writes=[t1])
            S.op("dve", lambda e, cs=cs, u2=u2: e.tensor_tensor(out=t2[:, 0, :], in0=u2, in1=cs[:, 16:32], op=ALU.mult), reads=[c_sb, cs], writes=[t2])
            S.op("dve", lambda e: e.tensor_tensor(out=krr[:, 0:16], in0=t1[:, 0, :], in1=t2[:, 0, :], op=ALU.subtract), reads=[t1, t2], writes=[krr])
            S.op("dve", lambda e, cs=cs, u1=u1: e.tensor_tensor(out=t1[:, 0, :], in0=u1, in1=cs[:, 16:32], op=ALU.mult), reads=[c_sb, cs], writes=[t1])
            S.op("dve", lambda e, cs=cs, u2=u2: e.tensor_tensor(out=t2[:, 0, :], in0=u2, in1=cs[:, 0:16], op=ALU.mult), reads=[c_sb, cs], writes=[t2])
            S.op("dve", lambda e: e.tensor_tensor(out=krr[:, 16:32], in0=t1[:, 0, :], in1=t2[:, 0, :], op=ALU.add), reads=[t1, t2], writes=[krr])
        else:
            S.op("dve", lambda e: e.tensor_copy(out=krr[:], in_=c_sb[:, 640:672]), reads=[c_sb], writes=[krr])
        S.op("dve", lambda e: e.tensor_copy(out=Kf[:, :, 64:96], in_=krr[:].unsqueeze(1).to_broadcast([128, 16, 32])), reads=[krr], writes=[Kf])
        for h in range(16):
            S.op("pe", lambda e, h=h: e.transpose(Cp[:, h, :], Kf[:, h, :], identb[:]), reads=[Kf, identb], writes=[Cp], inc=(h == 15))
        S.op("act", lambda e, kT=kT: e.copy(out=kT[:], in_=Cp[:]), reads=[Cp], writes=[kT])
        S.dma(KT, ktv[:, :, g0:g0 + 128], kT, kT[:])
        if lat:
            for ps_ in range(2):
                for (c0, c1) in ((0, 512), (512, 768)):
                    for kc in range(3):
                        S.op("pe", lambda e, ps_=ps_, c0=c0, c1=c1, kc=kc: e.matmul(A_[:, c0:c1], cnT[:, kc, :], qup[:, kc, ps_ * 768 + c0:ps_ * 768 + c1], start=(kc == 0), stop=(kc == 2)),
                             reads=[cnT, qup], writes=[A_], inc=(kc == 2))
                S.op("act", lambda e, ps_=ps_: e.copy(out=q32[:, ps_ * 8:(ps_ + 1) * 8, :], in_=A_[:, 0:768].rearrange("p (h x) -> p h x", h=8)), reads=[A_], writes=[q32])
            S.op("pool", lambda e: e.tensor_copy(out=Qf[:, :, 0:64], in_=q32[:, :, 0:64]), reads=[q32], writes=[Qf])
            U1, U2 = q32[:, :, 64:80], q32[:, :, 80:96]
            cosb = cs[:, 0:16].unsqueeze(1).to_broadcast([128, 16, 16])
            sinb = cs[:, 16:32].unsqueeze(1).to_broadcast([128, 16, 16])
            S.op("dve", lambda e, U1=U1, cosb=cosb: e.tensor_tensor(out=t1[:], in0=U1, in1=cosb, op=ALU.mult), reads=[q32, cs], writes=[t1])
            S.op("dve", lambda e, U2=U2, sinb=sinb: e.tensor_tensor(out=t2[:], in0=U2, in1=sinb, op=ALU.mult), reads=[q32, cs], writes=[t2])
            S.op("dve", lambda e: e.tensor_tensor(out=Qf[:, :, 64:80], in0=t1[:], in1=t2[:], op=ALU.subtract), reads=[t1, t2], writes=[Qf])
            S.op("dve", lambda e, U1=U1, sinb=sinb: e.tensor_tensor(out=t1[:], in0=U1, in1=sinb, op=ALU.mult), reads=[q32, cs], writes=[t1])
            S.op("dve", lambda e, U2=U2, cosb=cosb: e.tensor_tensor(out=t2[:], in0=U2, in1=cosb, op=ALU.mult), reads=[q32, cs], writes=[t2])
            S.op("dve", lambda e: e.tensor_tensor(out=Qf[:, :, 80:96], in0=t1[:], in1=t2[:], op=ALU.add), reads=[t1, t2], writes=[Qf])
            for h in range(16):
                S.op("pe", lambda e, h=h: e.transpose(Cp[:, h, :], Qf[:, h, :], identb[:]), reads=[Qf, identb], writes=[Cp], inc=(h == 15))
            S.op("act", lambda e, qT=qT: e.copy(out=qT[:], in_=Cp[:]), reads=[Cp], writes=[qT])
            S.dma(QT, qtv[:, :, g0:g0 + 128], qT, qT[:])


def phase_mla_attn(C, KT, QT, Vtok, AO):
    S, cfg = C.S, C.cfg
    NB, T, TC, TL = cfg.NB, cfg.T, cfg.TC, cfg.TL
    Tt = T // 128
    scale = (64 + 32) ** -0.5
    Ks_ = [S.sb("at_K%d" % i, [96, T], BF16) for i in range(2)]
    Qs_ = [S.sb("at_Q%d" % i, [96, TL], BF16) for i in range(2)]
    Vs_ = [S.sb("at_V%d" % i, [128, Tt, 65], BF16) for i in range(2)]
    for v in Vs_:
        S.op("pool", lambda e, v=v: e.memset(v[:, :, 64:65], 1.0), writes=[v])
    sp_ = [S.ps("at_s%d" % i, [128, 512]) for i in range(3)]
    E_ = [S.sb("at_E%d" % i, [128, 512], BF16) for i in range(3)]
    acc_ = [S.ps("at_acc%d" % i, [128, 4, 128]) for i in range(2)]
    rc = S.sb("at_rc", [128, 4, 1])
    ob_ = [S.sb("at_o%d" % i, [128, 4, 64], BF16) for i in range(2)]
    n = 0
    m = 0
    for b in range(NB):
        for h in range(16):
            Ks, Qs, Vs = Ks_[n % 2], Qs_[n % 2], Vs_[n % 2]
            n += 1
            S.dma(Ks, Ks[:], KT, KT[h, :, b * T:(b + 1) * T])
            S.dma(Qs, Qs[:], QT, QT[h, :, b * T + TC:(b + 1) * T])
            S.dma(Vs, Vs[:, :, 0:64], Vtok, Vtok[b * T:(b + 1) * T, h * 64:(h + 1) * 64].rearrange("(k p) d -> p k d", p=128))
            for qb in range(TL // 512):
                acc = acc_[qb % 2]

                def score(kt, Ks=Ks, Qs=Qs, qb=qb):
                    nonlocal m
                    sp, E = sp_[m % 3], E_[m % 3]
                    m += 1
                    S.op("pe", lambda e, sp=sp, kt=kt: e.matmul(sp[:], Ks[:, kt * 128:(kt + 1) * 128], Qs[:, qb * 512:(qb + 1) * 512], start=True, stop=True), reads=[Ks, Qs], writes=[sp])
                    return sp, E
                cur = score(0)
                for kt in range(Tt):
                    nxt = score(kt + 1) if kt + 1 < Tt else None
                    sp, E = cur
                    S.op("act", lambda e, sp=sp, E=E: e.activation(out=E[:], in_=sp[:], func=AF.Exp, scale=scale), reads=[sp], writes=[E])
                    for i in range(4):
                        S.op("pe", lambda e, acc=acc, E=E, Vs=Vs, kt=kt, i=i: e.matmul(acc[:, i, 0:65], E[:, i * 128:(i + 1) * 128], Vs[:, kt, :], start=(kt == 0 and i == 0), stop=(kt == Tt - 1), skip_group_check=True),
                             reads=[E, Vs], writes=[acc], inc=(i == 3))
                    cur = nxt
                S.op("dve", lambda e, acc=acc: e.reciprocal(out=rc[:], in_=acc[:, :, 64:65]), reads=[acc], writes=[rc])
                ob = ob_[qb % 2]
                S.op("dve", lambda e, acc=acc, ob=ob: e.tensor_tensor(out=ob[:], in0=acc[:, :, 0:64], in1=rc[:].to_broadcast([128, 4, 64]), op=ALU.mult), reads=[acc, rc], writes=[ob])
                r0 = b * T + TC + qb * 512
                S.dma(AO, AO[r0:r0 + 512, h * 64:(h + 1) * 64].rearrange("(i p) d -> p i d", p=128), ob, ob[:])


import ml_dtypes

N_CORES = 8
PARAM_SHAPES = dict(
    c=None, c_ctx=[1024], ada_w=[2, 1024, 6144], ada_b=[2, 6144], norm_mix=[2, 1024], norm_ffn=[2, 1024],
    ev_w_in=[1, 1024, 4512], ev_w_out=[1, 1536, 1024], rw_mu=[1, 1920], rw_w0=[1, 2, 512], rw_w2=[1, 2, 64, 512],
    rw_a0=[1, 2, 512], rw_a2=[1, 2, 64, 512], rw_g2=[1, 128, 512], rw_kk=[1, 512], rw_ka=[1, 512], rw_rk=[1, 512],
    rw_gn_w=[1, 512], rw_gn_b=[1, 512], ssm_conv_w=[1, 5, 1536], ssm_conv_b=[1, 1536], ssm_dt_bias=[1, 2, 16],
    ssm_a_log=[1, 2, 16], ssm_d=[1, 16], ssm_norm_w=[1, 1024], mla_w_in=[1, 1024, 672], mla_q_norm=[1, 384],
    mla_q_up=[1, 384, 1536], mla_kv_norm=[1, 256], mla_kv_up=[1, 256, 2048], mla_w_out=[1, 1024, 1024],
    router_w=[2, 1024, 64], router_bias=[2, 64], exp_w1=[2, 64, 1024, 256], exp_w3=[2, 64, 1024, 256],
    exp_w2=[2, 64, 256, 1024], sh_w1=[2, 1024, 256], sh_w3=[2, 1024, 256], sh_w2=[2, 256, 1024], final_norm=[1024])


def host_consts(cfg):
    K = {}
    bf = ml_dtypes.bfloat16
    K["ident"] = np.eye(128, dtype=np.float32)
    K["identb"] = np.eye(128).astype(bf)
    K["onesblk"] = np.kron(np.eye(2), np.ones((64, 64))).astype(np.float32)
    NQ = 8 * cfg.NB * 64 // 512
    Eq = np.zeros((128, NQ, 2 * NQ), np.float32)
    for p in range(128):
        for q in range(NQ):
            Eq[p, q, 2 * q + p // 64] = 1
    K["Eq"] = Eq
    l = np.arange(128)
    K["tri0"] = (l[:, None] <= l[None, :]).astype(np.float32)
    K["tri1"] = (l[:, None] >= l[None, :]).astype(np.float32)
    K["ones"] = np.ones((128, 128), np.float32)
    blk = np.zeros((8, 8, 128), np.float32)
    for h in range(8):
        blk[h, h, :] = 1
    K["blk"] = blk.reshape(8, 1024)
    K["ones8"] = np.ones((8, 128), np.float32)
    m0 = np.where(l[None, :] >= l[:, None], 0.0, -30000.0)
    m1 = np.where(l[None, :] <= l[:, None], 0.0, -30000.0)
    K["mneg0"] = np.tile(m0[:, None, :], (1, 4, 1)).reshape(128, 512).astype(bf)
    K["mneg1"] = np.tile(m1[:, None, :], (1, 4, 1)).reshape(128, 512).astype(bf)
    rows = cfg.TL // 64
    r_idx, c_idx = np.meshgrid(np.arange(rows), np.arange(64), indexing='ij')
    r_idx = r_idx.reshape(-1).astype(np.float32)
    c_idx = c_idx.reshape(-1).astype(np.float32)
    inv_freq = (10000.0 ** (-np.arange(0, 16, 2, dtype=np.float32) / 16)).astype(np.float32)
    ang = np.concatenate([r_idx[:, None] * inv_freq, c_idx[:, None] * inv_freq], -1).astype(np.float32)
    K["rope_cs"] = np.concatenate([np.cos(ang), np.sin(ang)], 1).astype(np.float32)
    return K


def build_program(cfg, kconst, dbg=()):
    nc = bass.Bass("TRN2", target_bir_lowering=False)
    NB, NTOK, T = cfg.NB, cfg.NTOK, cfg.T
    with ExitStack() as es:
        S = Sched(nc, es)
        io = {k: "ExternalInput" for k in PARAM_SHAPES}
        io.update(xin="ExternalInput", out="ExternalOutput")
        io.update({"K_" + k: "ExternalInput" for k in kconst})
        io.update({k: "ExternalOutput" for k in dbg})
        C = Ctx(S, cfg, io)
        P = {}
        for k, shp in PARAM_SHAPES.items():
            P[k] = C.D(k, [NB, 1024] if k == "c" else shp)
        xin = C.D("xin", [NTOK, 1024])
        out = C.D("out", [NB * cfg.TL, 1024])
        K = {}
        for k, v in kconst.items():
            dt_ = BF16 if v.dtype == ml_dtypes.bfloat16 else F32
            dd = C.D("K_" + k, list(v.shape), dt_)
            if k == "rope_cs":
                K[k] = dd
                continue
            sbt = S.sb("Ksb_" + k, list(v.shape), dt_)
            S.dma(sbt, sbt[:], dd, dd.t)
            K[k] = sbt
        ident, identb, onesblk = K["ident"], K["identb"], K["onesblk"]
        NQ = 8 * NB * 64 // 512
        modv = C.D("modv", [2, NB + 1, 6144])
        colsT = C.D("colsT", [4512, NTOK])
        A = {k: C.D("A_" + k, [512, NTOK]) for k in ["kk", "r", "bonus", "g", "w0", "w1", "b0", "b1", "kd0", "kd1"]}
        vtok = C.D("vtok", [NTOK, 512], BF16)
        YD = C.D("YD", [2 * NQ, T, 512])
        mixT = C.D("mixT", [1536, NTOK], BF16)
        xbc_tok = C.D("xbc_tok", [NTOK, 1536])
        BCT = C.D("BCT", [512, NTOK], BF16)
        dtda = C.D("dtda_tok", [NTOK, 64])
        ytmp = [C.D("ytmp%d" % d, [NTOK, 1024]) for d in range(2)]
        xr = [C.D("xr%d" % i, [NTOK, 1024]) for i in range(4)]
        hT = C.D("hT", [1024, NTOK], BF16)
        gates = C.D("gates", [NTOK, 65])
        wbfs = [C.D("wbf%d" % l, [65, 128, 6144], BF16) for l in range(2)]
        KT = C.D("KT", [16, 96, NTOK], BF16)
        QT = C.D("QT", [16, 96, NTOK], BF16)
        Vtok = C.D("Vtok", [NTOK, 1024], BF16)
        AO = C.D("AO", [NTOK, 1024], BF16)
        EW = {k: P[k] for k in ("exp_w1", "exp_w3", "exp_w2", "sh_w1", "sh_w3", "sh_w2")}
        all_tiles = list(range(NTOK // 128))
        lt = lat_tiles(cfg)
        with S.phase():
            phase_mod(C, P["c"], P["c_ctx"], P["ada_w"], P["ada_b"], modv)
        with S.phase():
            phase_normproj(C, "np0", xin, P["norm_mix"], P["norm_mix"][0, :], modv, 0, P["ev_w_in"], P["ev_w_in"][0], 4512, colsT, ident)
        with S.phase():
            phase_rwprep(C, colsT, P, A, onesblk, ident, vtok)
        with S.phase():
            def bg_all():
                bufs = ([S.sb("mcb_st%d" % i, [128, 2048]) for i in range(2)], [S.sb("mcb_ob%d" % i, [128, 2048], BF16) for i in range(2)])
                for l in range(2):
                    yield from moe_cast_bg(C, l, EW, wbfs[l], bufs)
            phase_rwscan(C, A, vtok, YD, onesblk, identb, K["Eq"], bg=bg_all())
        with S.phase():
            phase_rwpost(C, YD, A, P, mixT, ident)
        with S.phase():
            phase_ssprep(C, colsT, P, xbc_tok, BCT, dtda, ident)
        with S.phase():
            phase_ssscan(C, xbc_tok, BCT, dtda, ytmp, K)
        with S.phase():
            phase_sspost(C, ytmp, xbc_tok, colsT, P, mixT, ident)
        with S.phase():
            phase_outproj(C, "op0", mixT, False, 1536, P["ev_w_out"], P["ev_w_out"][0], modv, 0, xin, xr[0], all_tiles, identb)
        with S.phase():
            phase_moe_route(C, 0, xr[0], P["norm_ffn"], P["norm_ffn"][0, :], modv, P["router_w"], P["router_bias"], hT, gates, all_tiles, ident)
        with S.phase():
            phase_moe_experts(C, 0, hT, gates, wbfs[0], modv, xr[0], xr[1], all_tiles)
        with S.phase():
            phase_mla_prep(C, 1, xr[1], P["norm_mix"], P["norm_mix"][1, :], modv, P, K["rope_cs"], KT, QT, Vtok, ident, identb)
        with S.phase():
            phase_mla_attn(C, KT, QT, Vtok, AO)
        with S.phase():
            phase_outproj(C, "op1", AO, True, 1024, P["mla_w_out"], P["mla_w_out"][0], modv, 1, xr[1], xr[2], lt, identb)
        with S.phase():
            phase_moe_route(C, 1, xr[2], P["norm_ffn"], P["norm_ffn"][1, :], modv, P["router_w"], P["router_bias"], hT, gates, lt, ident)
        with S.phase():
            phase_moe_experts(C, 1, hT, gates, wbfs[1], modv, xr[2], xr[3], lt)
        with S.phase():
            phase_final(C, xr[3], P["final_norm"], out)
        S.emit(final_bufs=[out] + [b for b in S.bufs if b.space == "dram" and b.name in dbg])
        build_program.stats = (S.nins, S.nsem)
        build_program.marks = S.marks
    return nc


def kernel(**inputs):
    cfg = Cfg(4, 2048, 256)
    kconst = host_consts(cfg)
    nc = build_program(cfg, kconst)
    x = np.asarray(inputs["x"], np.float32)
    ctx = np.asarray(inputs["ctx"], np.float32)
    c = np.asarray(inputs["c"], np.float32)
    in_maps = []
    shared = {k: np.ascontiguousarray(np.asarray(inputs[k], np.float32)) for k in PARAM_SHAPES if k != "c"}
    shared.update({"K_" + k: v for k, v in kconst.items()})
    for i in range(N_CORES):
        sl = slice(i * cfg.NB, (i + 1) * cfg.NB)
        m = dict(shared)
        m["xin"] = np.ascontiguousarray(np.concatenate([ctx[sl], x[sl]], axis=1).reshape(cfg.NTOK, 1024))
        m["c"] = np.ascontiguousarray(c[sl])
        in_maps.append(m)
    res = run_bass_kernel_spmd(nc, in_maps, core_ids=list(range(N_CORES)))
    outs = [np.asarray(r["out"]).reshape(cfg.NB, cfg.TL, 1024) for r in res.results]
    return np.concatenate(outs, axis=0).astype(np.float32)
```
